# Optimizing a Trainium2 kernel written in Bass

```python
import math
import jax, jax.numpy as jnp
from jax import lax
import numpy as np

D_MODEL = 2048
BATCH = 8
SEQ = 2048
DEPTH = 2

A_HEADS = 8
A_HEAD_DIM = 64
A_WIDTH = A_HEADS * A_HEAD_DIM
A_PATTERNS = ((128, 1), (512, 4), (2048, 16))
REL_BUCKETS = 32
REL_MAX_DIST = 1024
B_HEADS = 8
B_NOPE = 64
B_ROPE = 32
B_V = 64
B_Q_LORA = 512
B_KV_LORA = 256
B_QBLOCK = 128
ROPE_THETA = 10000.0
C_CH = 512
C_KERNEL = 31
D_HEADS = 8
D_HEAD_DIM = 64
D_INNER = D_HEADS * D_HEAD_DIM
D_STATE = 128
D_GROUPS = 2
D_CONV = 5
D_CHUNK = 128
N_BRANCH = 4
BRANCH_W = 512
IN_SPLITS = (A_WIDTH, A_WIDTH, A_WIDTH,
             B_Q_LORA, B_KV_LORA, B_ROPE,
             2 * C_CH,
             D_INNER, D_INNER, D_GROUPS * D_STATE, D_GROUPS * D_STATE, 2 * D_HEADS)
IN_COLS = sum(IN_SPLITS)
N_GROUPS = 4
EXP_PER_GROUP = 8
N_EXPERTS = N_GROUPS * EXP_PER_GROUP
TOP_K = 2
D_FF = 512
MOE_BLOCK = 128
ALPHA = (2 * DEPTH) ** 0.25
BETA = (8 * DEPTH) ** -0.25
EPS = 1e-5
NEG_INF = -1e30

kernel_name = 'hybrid_gated_bidir_encoder'


def layernorm(x, g, b):
    xf = x.astype(jnp.float32)
    mu = jnp.mean(xf, axis=-1, keepdims=True)
    var = jnp.mean(jnp.square(xf - mu), axis=-1, keepdims=True)
    return ((xf - mu) * lax.rsqrt(var + EPS) * g + b).astype(x.dtype)


def rmsnorm(x, g):
    xf = x.astype(jnp.float32)
    return (xf * lax.rsqrt(jnp.mean(jnp.square(xf), axis=-1, keepdims=True) + EPS) * g).astype(x.dtype)


def depthwise_conv(x, w, b):
    width = w.shape[0]
    y = lax.conv_general_dilated(x, w[:, None, :].astype(x.dtype), (1,), ((width // 2, width // 2),),
                                 dimension_numbers=('NWC', 'WIO', 'NWC'), feature_group_count=x.shape[-1])
    return y + b


def t5_bucket(rel):
    half = REL_BUCKETS // 2
    max_exact = half // 2
    n = np.abs(rel)
    large = max_exact + (np.log(np.maximum(n, 1) / max_exact) / np.log(REL_MAX_DIST / max_exact)
                         * (half - max_exact)).astype(np.int32)
    large = np.minimum(large, half - 1)
    return (rel > 0).astype(np.int32) * half + np.where(n < max_exact, n, large)


def dilated_window_attention(q, k, v, rel_bias, band, dilation):
    Bsz, S, H, Dh = q.shape
    L = S // dilation
    nb = -(-L // band)
    Lp = nb * band

    def to_sub(t):
        return t.reshape(Bsz, L, dilation, H, Dh).transpose(0, 2, 1, 3, 4).reshape(Bsz * dilation, L, H, Dh)

    def banded(t):
        tp = jnp.pad(t, ((0, 0), (band, Lp - L + band), (0, 0), (0, 0))).reshape(-1, nb + 2, band, H, Dh)
        return jnp.concatenate([tp[:, :-2], tp[:, 1:-1], tp[:, 2:]], axis=2)

    qs, ks, vs = to_sub(q), to_sub(k), to_sub(v)
    qb = jnp.pad(qs, ((0, 0), (0, Lp - L), (0, 0), (0, 0))).reshape(-1, nb, band, H, Dh)
    kb, vb = banded(ks), banded(vs)
    qi = np.arange(band)[:, None]
    ki = np.arange(3 * band)[None, :]
    rel = ki - band - qi
    bias = jnp.transpose(rel_bias[t5_bucket(rel * dilation)], (2, 0, 1)).astype(jnp.float32)
    kpos = np.arange(nb)[:, None] * band + ki - band
    valid = (np.abs(rel) <= band)[None] & ((kpos >= 0) & (kpos < L))[:, None, :]
    s = jnp.einsum('znqhd,znkhd->znhqk', qb, kb).astype(jnp.float32) * (Dh ** -0.5) + bias
    s = jnp.where(valid[None, :, None], s, NEG_INF)
    m = jnp.max(s, axis=-1, keepdims=True)
    p = jnp.exp(s - m)
    den = jnp.sum(p, axis=-1)
    o = jnp.einsum('znhqk,znkhd->znqhd', p, vb) / jnp.swapaxes(den, 2, 3)[..., None]
    lse = jnp.swapaxes(m[..., 0] + jnp.log(den), 2, 3)
    o = o.reshape(Bsz, dilation, Lp, H, Dh)[:, :, :L].transpose(0, 2, 1, 3, 4).reshape(Bsz, S, H, Dh)
    lse = lse.reshape(Bsz, dilation, Lp, H)[:, :, :L].transpose(0, 2, 1, 3).reshape(Bsz, S, H)
    return o, lse


def dilated_mixture_attention(q, k, v, rel_bias):
    outs, lses = [], []
    for window, dilation in A_PATTERNS:
        o, lse = dilated_window_attention(q, k, v, rel_bias, window // (2 * dilation), dilation)
        outs.append(o)
        lses.append(lse)
    w = jax.nn.softmax(jnp.stack(lses, axis=0), axis=0)
    return sum(w[i][..., None] * outs[i] for i in range(len(outs)))


def rope(t, cos, sin):
    half = t.shape[-1] // 2
    t1, t2 = t[..., :half], t[..., half:]
    return jnp.concatenate([t1 * cos - t2 * sin, t2 * cos + t1 * sin], axis=-1).astype(t.dtype)


def mla(cq, ckv, kr, g_cq, w_uq, g_ckv, w_ukv):
    Bsz, S, _ = cq.shape
    inv_freq = ROPE_THETA ** (-jnp.arange(0, B_ROPE, 2, dtype=jnp.float32) / B_ROPE)
    ang = jnp.arange(S, dtype=jnp.float32)[:, None] * inv_freq[None]
    cos, sin = jnp.cos(ang), jnp.sin(ang)
    q = (rmsnorm(cq, g_cq) @ w_uq).reshape(Bsz, S, B_HEADS, B_NOPE + B_ROPE)
    q_nope = q[..., :B_NOPE]
    q_rope = rope(q[..., B_NOPE:], cos[:, None], sin[:, None])
    kv = (rmsnorm(ckv, g_ckv) @ w_ukv).reshape(Bsz, S, B_HEADS, B_NOPE + B_V)
    k_nope, v = kv[..., :B_NOPE], kv[..., B_NOPE:]
    k_rope = rope(kr, cos, sin)
    scale = (B_NOPE + B_ROPE) ** -0.5
    nqb = S // B_QBLOCK

    def to_blocks(t):
        return jnp.moveaxis(t.reshape(Bsz, nqb, B_QBLOCK, *t.shape[2:]), 1, 0)

    def attend(blk):
        qn, qr = blk
        s = jnp.einsum('bqhd,bkhd->bhqk', qn, k_nope) + jnp.einsum('bqhd,bkd->bhqk', qr, k_rope)
        p = jax.nn.softmax(s.astype(jnp.float32) * scale, axis=-1)
        return jnp.einsum('bhqk,bkhd->bqhd', p, v)

    o = lax.map(attend, (to_blocks(q_nope), to_blocks(q_rope)))
    return jnp.moveaxis(o, 0, 1).reshape(Bsz, S, B_HEADS * B_V)


def conformer_conv(u, w_dw, b_dw, ln_g, ln_b):
    a, gate = jnp.split(u, 2, axis=-1)
    h = a * jax.nn.sigmoid(gate)
    h = depthwise_conv(h, w_dw, b_dw)
    return jax.nn.silu(layernorm(h, ln_g, ln_b))


def ssd_scan(x, dt, A, Bm, Cm):
    Bsz, S, H, P = x.shape
    Q, G, N = D_CHUNK, D_GROUPS, D_STATE
    Hg = H // G
    nc = S // Q
    xc = x.reshape(Bsz, nc, Q, G, Hg, P)
    dtc = dt.reshape(Bsz, nc, Q, G, Hg)
    Bc = Bm.reshape(Bsz, nc, Q, G, N)
    Cc = Cm.reshape(Bsz, nc, Q, G, N)
    cs = jnp.cumsum(dtc * A.reshape(G, Hg), axis=2)
    csq = jnp.moveaxis(cs, 2, -1)
    tril = np.tril(np.ones((Q, Q), dtype=bool))
    decay = jnp.exp(jnp.where(tril, csq[..., :, None] - csq[..., None, :], -jnp.inf))
    cb = jnp.einsum('bcign,bcjgn->bcgij', Cc, Bc)
    xdt = xc * dtc[..., None]
    y_intra = jnp.einsum('bcghij,bcjghp->bcighp', cb[:, :, :, None] * decay, xdt)
    decay_end = jnp.exp(cs[:, :, -1:] - cs)
    states = jnp.einsum('bcjgn,bcjghp->bcghnp', Bc, xdt * decay_end[..., None])
    chunk_decay = jnp.exp(cs[:, :, -1])

    def step(h, inp):
        dec, st = inp
        return dec[..., None, None] * h + st, h

    h0 = jnp.zeros((Bsz, G, Hg, N, P), dtype=states.dtype)
    _, h_prev = lax.scan(step, h0, (jnp.moveaxis(chunk_decay, 1, 0), jnp.moveaxis(states, 1, 0)))
    h_prev = jnp.moveaxis(h_prev, 0, 1)
    y_inter = jnp.einsum('bcign,bcghnp->bcighp', Cc, h_prev) * jnp.exp(cs)[..., None]
    return (y_intra + y_inter).reshape(Bsz, S, H, P)


def ssd_mixer(z, xs, bm, cm, dt_raw, w_conv, b_conv, a_log_f, a_log_b, dt_bias_f, dt_bias_b, d_skip, g_norm):
    Bsz, S, _ = xs.shape
    xbc = jax.nn.silu(depthwise_conv(jnp.concatenate([xs, bm, cm], axis=-1), w_conv, b_conv))
    xs, bm, cm = jnp.split(xbc, [D_INNER, D_INNER + D_GROUPS * D_STATE], axis=-1)
    x = xs.reshape(Bsz, S, D_HEADS, D_HEAD_DIM)
    bm = bm.reshape(Bsz, S, D_GROUPS, D_STATE)
    cm = cm.reshape(Bsz, S, D_GROUPS, D_STATE)
    dtf = dt_raw.astype(jnp.float32)
    dt_f = jax.nn.softplus(dtf[..., :D_HEADS] + dt_bias_f)
    dt_b = jax.nn.softplus(dtf[..., D_HEADS:] + dt_bias_b)
    flip = lambda t: t[:, ::-1]
    y_f = ssd_scan(x, dt_f, -jnp.exp(a_log_f.astype(jnp.float32)), bm, cm)
    y_b = flip(ssd_scan(flip(x), flip(dt_b), -jnp.exp(a_log_b.astype(jnp.float32)), flip(bm), flip(cm)))
    y = (y_f + y_b + x * d_skip[:, None]).reshape(Bsz, S, D_INNER)
    return rmsnorm(y * jax.nn.silu(z), g_norm)


def hybrid_mixer(h, w_in, rel_bias, g_cq, w_uq, g_ckv, w_ukv, w_dw_c, b_dw_c, ln_c_g, ln_c_b,
                 w_conv_d, b_conv_d, a_log_f, a_log_b, dt_bias_f, dt_bias_b, d_skip, g_norm_d,
                 w_br, w_gate, b_gate, w_out):
    Bsz, S, _ = h.shape
    cuts = np.cumsum(IN_SPLITS)[:-1].tolist()
    qa, ka, va, cq, ckv, kr, glu, z, xs, bm, cm, dt = jnp.split(h @ w_in, cuts, axis=-1)
    heads = lambda t: t.reshape(Bsz, S, A_HEADS, A_HEAD_DIM)
    y_a = dilated_mixture_attention(heads(qa), heads(ka), heads(va), rel_bias).reshape(Bsz, S, A_WIDTH)
    y_b = mla(cq, ckv, kr, g_cq, w_uq, g_ckv, w_ukv)
    y_c = conformer_conv(glu, w_dw_c, b_dw_c, ln_c_g, ln_c_b)
    y_d = ssd_mixer(z, xs, bm, cm, dt, w_conv_d, b_conv_d, a_log_f, a_log_b, dt_bias_f, dt_bias_b,
                    d_skip, g_norm_d)
    branches = (y_a, y_b, y_c, y_d)
    merged = sum(jax.nn.sigmoid(h @ w_gate[i] + b_gate[i]) * (branches[i] @ w_br[i]) for i in range(N_BRANCH))
    return merged @ w_out


def hier_moe(h, w_rg, b_rg, w_re, b_re, w_e_gate, w_e_up, w_e_down):
    Bsz, S, D = h.shape
    T = Bsz * S
    A_ROWS = T * TOP_K
    xt = h.reshape(T, D)
    g_prob = jax.nn.softmax((xt @ w_rg + b_rg).astype(jnp.float32), axis=-1)
    g_w, g_idx = lax.top_k(g_prob, 1)
    e_logits = (xt @ w_re + b_re).astype(jnp.float32).reshape(T, N_GROUPS, EXP_PER_GROUP)
    e_in = jnp.take_along_axis(e_logits, g_idx[:, :, None], axis=1)[:, 0]
    e_w, e_loc = lax.top_k(jax.nn.softmax(e_in, axis=-1), TOP_K)
    wts = g_w * e_w / jnp.sum(e_w, axis=-1, keepdims=True)
    expert = g_idx * EXP_PER_GROUP + e_loc
    flat_e = expert.reshape(-1)
    flat_tok = jnp.repeat(jnp.arange(T, dtype=jnp.int32), TOP_K)
    order = jnp.argsort(flat_e)
    se, stok, sw = flat_e[order], flat_tok[order], wts.reshape(-1)[order]
    counts = jnp.bincount(flat_e, length=N_EXPERTS)
    padded = (counts + MOE_BLOCK - 1) // MOE_BLOCK * MOE_BLOCK
    starts = jnp.cumsum(counts) - counts
    pends = jnp.cumsum(padded)
    pstarts = pends - padded
    dest = pstarts[se] + (jnp.arange(A_ROWS, dtype=jnp.int32) - starts[se])
    n_rows = A_ROWS + N_EXPERTS * MOE_BLOCK
    n_blocks = n_rows // MOE_BLOCK
    row_tok = jnp.full((n_rows,), T, dtype=jnp.int32).at[dest].set(stok)
    blk_exp = jnp.minimum(jnp.searchsorted(pends, jnp.arange(n_blocks, dtype=jnp.int32) * MOE_BLOCK,
                                           side='right'), N_EXPERTS - 1)
    xrows = jnp.concatenate([xt, jnp.zeros((1, D), xt.dtype)], axis=0)[row_tok].reshape(n_blocks, MOE_BLOCK, D)

    def expert_block(args):
        xb, e = args
        return (jax.nn.silu(xb @ w_e_gate[e]) * (xb @ w_e_up[e])) @ w_e_down[e]

    yrows = lax.map(expert_block, (xrows, blk_exp)).reshape(n_rows, D)
    y = jax.ops.segment_sum(yrows[dest] * sw[:, None].astype(yrows.dtype), stok, num_segments=T)
    return y.reshape(Bsz, S, D)


def setup_inputs(seed: int = 0) -> dict:
    key = jax.random.key(seed)
    ks = iter(jax.random.split(key, 48))
    L, D = DEPTH, D_MODEL
    nrm = lambda shape, scale: jax.random.normal(next(ks), shape, jnp.float32) * scale
    gain = lambda shape: 1.0 + nrm(shape, 0.02)

    def dt_bias(shape):
        dt = jnp.exp(jax.random.uniform(next(ks), shape, jnp.float32, math.log(1e-3), math.log(1e-1)))
        return dt + jnp.log(-jnp.expm1(-dt))

    return {
        'x': nrm((BATCH, SEQ, D), 1.0),
        'ln_in_g': gain((D,)),
        'ln_in_b': nrm((D,), 0.02),
        'rel_bias': nrm((REL_BUCKETS, A_HEADS), 0.2),
        'w_in': nrm((L, D, IN_COLS), D ** -0.5),
        'g_cq': gain((L, B_Q_LORA)),
        'w_uq': nrm((L, B_Q_LORA, B_HEADS * (B_NOPE + B_ROPE)), B_Q_LORA ** -0.5),
        'g_ckv': gain((L, B_KV_LORA)),
        'w_ukv': nrm((L, B_KV_LORA, B_HEADS * (B_NOPE + B_V)), B_KV_LORA ** -0.5),
        'w_dw_c': nrm((L, C_KERNEL, C_CH), C_KERNEL ** -0.5),
        'b_dw_c': nrm((L, C_CH), 0.02),
        'ln_c_g': gain((L, C_CH)),
        'ln_c_b': nrm((L, C_CH), 0.02),
        'w_conv_d': nrm((L, D_CONV, D_INNER + 2 * D_GROUPS * D_STATE), D_CONV ** -0.5),
        'b_conv_d': nrm((L, D_INNER + 2 * D_GROUPS * D_STATE), 0.02),
        'a_log_f': jnp.log(jax.random.uniform(next(ks), (L, D_HEADS), jnp.float32, 1.0, 16.0)),
        'a_log_b': jnp.log(jax.random.uniform(next(ks), (L, D_HEADS), jnp.float32, 1.0, 16.0)),
        'dt_bias_f': dt_bias((L, D_HEADS)),
        'dt_bias_b': dt_bias((L, D_HEADS)),
        'd_skip': gain((L, D_HEADS)),
        'g_norm_d': gain((L, D_INNER)),
        'w_br': nrm((L, N_BRANCH, BRANCH_W, D), BETA * BRANCH_W ** -0.5),
        'w_gate': nrm((L, N_BRANCH, D, D), D ** -0.5),
        'b_gate': nrm((L, N_BRANCH, D), 0.02),
        'w_out': nrm((L, D, D), BETA * D ** -0.5),
        'ln1_g': gain((L, D)),
        'ln1_b': nrm((L, D), 0.02),
        'w_rg': nrm((L, D, N_GROUPS), D ** -0.5),
        'b_rg': nrm((L, N_GROUPS), 0.01),
        'w_re': nrm((L, D, N_EXPERTS), D ** -0.5),
        'b_re': nrm((L, N_EXPERTS), 0.01),
        'w_e_gate': nrm((L, N_EXPERTS, D, D_FF), D ** -0.5),
        'w_e_up': nrm((L, N_EXPERTS, D, D_FF), D ** -0.5),
        'w_e_down': nrm((L, N_EXPERTS, D_FF, D), BETA * D_FF ** -0.5),
        'ln2_g': gain((L, D)),
        'ln2_b': nrm((L, D), 0.02),
    }


def reference(x, ln_in_g, ln_in_b, rel_bias, w_in, g_cq, w_uq, g_ckv, w_ukv, w_dw_c, b_dw_c, ln_c_g, ln_c_b,
              w_conv_d, b_conv_d, a_log_f, a_log_b, dt_bias_f, dt_bias_b, d_skip, g_norm_d, w_br, w_gate, b_gate,
              w_out, ln1_g, ln1_b, w_rg, b_rg, w_re, b_re, w_e_gate, w_e_up, w_e_down, ln2_g, ln2_b):
    h = layernorm(x, ln_in_g, ln_in_b)
    for l in range(DEPTH):
        mix = hybrid_mixer(h, w_in[l], rel_bias, g_cq[l], w_uq[l], g_ckv[l], w_ukv[l], w_dw_c[l], b_dw_c[l],
                           ln_c_g[l], ln_c_b[l], w_conv_d[l], b_conv_d[l], a_log_f[l], a_log_b[l],
                           dt_bias_f[l], dt_bias_b[l], d_skip[l], g_norm_d[l], w_br[l], w_gate[l], b_gate[l],
                           w_out[l])
        h = layernorm(ALPHA * h + mix, ln1_g[l], ln1_b[l])
        moe = hier_moe(h, w_rg[l], b_rg[l], w_re[l], b_re[l], w_e_gate[l], w_e_up[l], w_e_down[l])
        h = layernorm(ALPHA * h + moe, ln2_g[l], ln2_b[l])
    return h
```

```python
import math
from contextlib import ExitStack

import numpy as np
import ml_dtypes
import concourse.bass as bass
import concourse.mybir as mybir
from concourse.bass_utils import run_bass_kernel_spmd

F32 = mybir.dt.float32
BF16 = mybir.dt.bfloat16
I32 = mybir.dt.int32
AF = mybir.ActivationFunctionType
ALU = mybir.AluOpType
AX = mybir.AxisListType

S = 2048
D = 2048
NT = 16
NL = 2
ALPHA = 4.0 ** 0.25
EPS = 1e-5
R_E = 1535
RVLEN = 3072
CAP = 256
NEXP = 32
ENGS = ("pe", "act", "dve", "pool", "sp")


class KB:
    def __init__(self, nc, n_dma_sems=10):
        self.nc = nc
        self.ops = {e: [] for e in ENGS}
        self.sem = {}
        self.cnt = {e: 0 for e in ENGS}
        self.seen = {e: {} for e in ENGS}
        self.lastw = {}
        self.readers = {}
        self.n_dma_sems = n_dma_sems
        self.dma_sems = {}
        self.dma_rr = {e: 0 for e in ENGS}
        self.pstack = None
        self.out_events = []
        self.n_ins = 0

    def open(self, stack):
        nc = self.nc
        for e in ENGS:
            self.sem[e] = stack.enter_context(nc.semaphore("s_" + e))
        for q in ("sp", "act", "pool"):
            lst = []
            for i in range(3 if q == "pool" else self.n_dma_sems):
                key = "d_%s_%d" % (q, i)
                self.sem[key] = stack.enter_context(nc.semaphore(key))
                lst.append([key, 0])
            self.dma_sems[q] = lst
        self.gstack = stack

    def _uniq(self, name):
        self.uid = getattr(self, "uid", 0) + 1
        return "%s_u%d" % (name, self.uid)

    def gsb(self, name, shape, dt):
        return self.gstack.enter_context(self.nc.sbuf_tensor(self._uniq(name), list(shape), dt))

    def sb(self, name, shape, dt):
        return self.pstack.enter_context(self.nc.sbuf_tensor(self._uniq(name), list(shape), dt))

    def ps(self, name, shape, dt=F32):
        return self.pstack.enter_context(self.nc.psum_tensor(self._uniq(name), list(shape), dt))

    def _deps(self, reads, writes):
        evs = []
        for t in reads:
            ev = self.lastw.get(t)
            if ev is not None:
                evs.append(ev)
        for t in writes:
            ev = self.lastw.get(t)
            if ev is not None:
                evs.append(ev)
            evs.extend(self.readers.get(t, ()))
        return evs

    def _waits(self, eng, evs):
        seen = self.seen[eng]
        need = {}
        for (k, v) in evs:
            if seen.get(k, 0) >= v:
                continue
            if need.get(k, 0) < v:
                need[k] = v
        for k, v in need.items():
            seen[k] = v
        return list(need.items())

    def _commit(self, ev, reads, writes):
        for t in writes:
            self.lastw[t] = ev
            self.readers[t] = []
        for t in reads:
            if t in writes:
                continue
            self.readers.setdefault(t, []).append(ev)

    def op(self, eng, name, reads=(), writes=(), **kw):
        evs = self._deps(reads, writes)
        waits = self._waits(eng, evs)
        self.cnt[eng] += 1
        ev = (eng, self.cnt[eng])
        self.ops[eng].append((waits, name, kw, (eng, 1)))
        self._commit(ev, reads, writes)
        return ev

    def op_noinc(self, eng, name, reads=(), writes=(), **kw):
        evs = self._deps(reads, writes)
        waits = self._waits(eng, evs)
        self.ops[eng].append((waits, name, kw, None))

    def mm(self, out, lhsT, rhs, start, stop, reads=(), writes=(), last=None):
        if last is None:
            last = stop
        f = self.op if last else self.op_noinc
        return f("pe", "matmul", reads=reads, writes=writes, out=out, lhsT=lhsT, rhs=rhs, start=start, stop=stop)

    def dma(self, q, out, in_, reads=(), writes=(), is_out=False, name="dma_start", **kw):
        evs = self._deps(reads, writes)
        lst = self.dma_sems[q]
        i = self.dma_rr[q] % len(lst)
        self.dma_rr[q] += 1
        key, c = lst[i]
        if c > 0:
            evs.append((key, c))
        waits = self._waits(q, evs)
        lst[i][1] = c + 16
        ev = (key, c + 16)
        kw = dict(kw)
        kw["out"] = out
        kw["in_"] = in_
        self.ops[q].append((waits, name, kw, (key, 16)))
        self._commit(ev, reads, writes)
        if is_out:
            self.out_events.append(ev)
        return ev

    def barrier(self):
        evs = []
        for q, lst in self.dma_sems.items():
            for key, c in lst:
                if c > 0:
                    evs.append((key, c))
        for e in ENGS:
            if e != "sp" and self.cnt[e] > 0:
                evs.append((e, self.cnt[e]))
        waits = self._waits("sp", evs)
        self.cnt["sp"] += 1
        self.ops["sp"].append((waits, "sem_inc", dict(sem=self.sem["sp"], val=1), None))
        mark = ("sp", self.cnt["sp"])
        for e in ENGS:
            if e == "sp":
                continue
            w = self._waits(e, [mark])
            self.ops[e].append((w, None, None, None))
            for (k, v) in evs:
                if self.seen[e].get(k, 0) < v:
                    self.seen[e][k] = v
        self.lastw = {}
        self.readers = {}

    def emit(self):
        nc = self.nc
        sem = self.sem
        ops = self.ops
        with nc.Block() as block:
            def run(engname):
                def body(e):
                    for waits, name, kw, inc in ops[engname]:
                        for k, v in waits:
                            e.wait_ge(sem[k], v)
                        if name is None:
                            continue
                        ins = getattr(e, name)(**kw)
                        self.n_ins += 1
                        if inc is not None:
                            ins.then_inc(sem[inc[0]], inc[1])
                return body
            block.tensor(run("pe"))
            block.scalar(run("act"))
            block.vector(run("dve"))
            block.gpsimd(run("pool"))
            block.sync(run("sp"))
        self.ops = {e: [] for e in ENGS}

    def phase_begin(self):
        self.pstack = ExitStack()
        self.pstack.__enter__()

    def phase_end(self):
        self.barrier()
        self.emit()
        self.pstack.close()
        self.pstack = None


class Ring:
    def __init__(self, kb, name, n, shape, dt, psum=False):
        self.tiles = []
        self.names = []
        for i in range(n):
            nm = "%s%d" % (name, i)
            t = kb.ps(nm, shape, dt) if psum else kb.sb(nm, shape, dt)
            self.tiles.append(t)
            self.names.append(nm)
        self.i = 0

    @classmethod
    def wrap(cls, tiles, names):
        r = cls.__new__(cls)
        r.tiles = list(tiles)
        r.names = list(names)
        r.i = 0
        return r

    def next(self):
        j = self.i % len(self.tiles)
        self.i += 1
        return self.tiles[j], self.names[j]


def _t5_bucket(rel):
    half = 16
    max_exact = 8
    n = np.abs(rel)
    large = max_exact + (np.log(np.maximum(n, 1) / max_exact) / np.log(1024 / max_exact) * (half - max_exact)).astype(np.int32)
    large = np.minimum(large, half - 1)
    return (rel > 0).astype(np.int32) * half + np.where(n < max_exact, n, large)


def _host_consts():
    c = {}
    i = np.arange(RVLEN)
    r = R_E - i
    a = np.abs(r)
    mult = (a <= 64).astype(np.float32) + ((r % 4 == 0) & (a <= 256)).astype(np.float32) + ((r % 16 == 0) & (a <= 1024)).astype(np.float32)
    bk = _t5_bucket(r)
    mohr = np.zeros((32, RVLEN), np.float32)
    mohr[bk, i] = mult
    c["mohr"] = mohr
    p = np.arange(128)[:, None]
    f = np.arange(128)[None, :]
    cst = np.zeros((128, 6, 128), np.float32)
    cst[:, 0] = (p == f)
    cst[:, 1] = (p + f == 127)
    cst[:, 2] = (p <= f)
    cst[:, 3] = (p >= f)
    cst[:, 4] = (p < f)
    cst[:, 5] = 1.0
    c["cst"] = cst
    inv_freq = (np.float32(10000.0) ** (-np.arange(0, 32, 2, dtype=np.float32) / np.float32(32))).astype(np.float32)
    ang = (np.arange(S, dtype=np.float32)[:, None] * inv_freq[None]).astype(np.float32)
    cos = np.cos(ang).astype(np.float32).T
    sin = np.sin(ang).astype(np.float32).T
    rope = np.zeros((2, 96, S), np.float32)
    rope[0, 64:80] = cos
    rope[0, 80:96] = cos
    rope[1, 64:80] = -sin
    rope[1, 80:96] = sin
    c["rope"] = rope
    m = np.ones((16, S), np.float32)
    m[:, ::128] = 0.0
    c["scanmask"] = m
    rowc = np.zeros((1, 64), np.float32)
    rowc[0, 0:32] = np.arange(32) * CAP
    c["rowc"] = rowc
    return c


def _host_layout(inp):
    f = np.float32
    o = {}
    w_in = inp["w_in"]
    cuts = np.cumsum((512, 512, 512, 512, 256, 32, 1024, 512, 512, 256, 256, 16))
    st = np.concatenate([[0], cuts[:-1]])
    seg = {n: (int(a), int(b)) for n, a, b in zip(("qa", "ka", "va", "cq", "ckv", "kr", "glu", "z", "xs", "bm", "cm", "dt"), st, cuts)}
    cols = []
    def rng(n, a=0, b=None):
        s0, s1 = seg[n]
        b = (s1 - s0) if b is None else b
        return list(range(s0 + a, s0 + b))
    cols += rng("qa") + rng("ka") + rng("cq") + rng("ckv")
    kr0 = seg["kr"][0]
    cols += rng("kr") + [kr0 + (r + 16) % 32 for r in range(32)] + [-1] * 64
    cols += rng("glu", 0, 512) + rng("glu", 512, 1024) + rng("xs") + rng("bm") + rng("cm")
    cols = np.array(cols)
    assert cols.size == 31 * 128
    w_in_t = np.zeros((NL, 31, 128, 16, 128), f)
    w_dt_tm = np.zeros((NL, 128, 16, 16), f)
    w_in_tm = np.zeros((NL, 2, 128, 16, 512), f)
    for l in range(NL):
        wc = np.where(cols[None, :] >= 0, w_in[l][:, np.maximum(cols, 0)], 0.0).astype(f)
        w_in_t[l] = wc.reshape(16, 128, 31, 128).transpose(2, 1, 0, 3)
        w_dt_tm[l] = w_in[l][:, seg["dt"][0]:seg["dt"][1]].reshape(16, 128, 16).transpose(1, 0, 2)
        for b, n in enumerate(("va", "z")):
            s0, s1 = seg[n]
            w_in_tm[l, b] = w_in[l][:, s0:s1].reshape(16, 128, 512).transpose(1, 0, 2)
    o["w_in_t"] = w_in_t
    o["w_in_tm"] = w_in_tm
    o["w_dt_tm"] = w_dt_tm
    w_uq = inp["w_uq"]
    pc = np.array([96 * h + 64 + (r + 16) % 32 for h in range(8) for r in range(32)])
    w_uq_t = np.concatenate([w_uq, w_uq[:, :, pc]], axis=2)
    o["w_uq_t"] = np.ascontiguousarray(w_uq_t.reshape(NL, 4, 128, 1024).transpose(0, 2, 1, 3))
    w_ukv = inp["w_ukv"]
    kc_ = np.array([128 * h + j for h in range(8) for j in range(64)])
    vc_ = kc_ + 64
    w_ukv_r = np.concatenate([w_ukv[:, :, kc_], w_ukv[:, :, vc_]], axis=2)
    o["w_ukv_t"] = np.ascontiguousarray(w_ukv_r.reshape(NL, 2, 128, 1024).transpose(0, 2, 1, 3))
    o["w_gate_t"] = np.ascontiguousarray(inp["w_gate"].reshape(NL, 4, 16, 128, 16, 128).transpose(0, 1, 4, 3, 2, 5))
    o["w_br_t"] = np.ascontiguousarray(inp["w_br"].reshape(NL, 4, 4, 128, 16, 128).transpose(0, 1, 4, 3, 2, 5))
    o["w_out_t"] = np.ascontiguousarray(inp["w_out"].reshape(NL, 16, 128, 4, 512).transpose(0, 3, 2, 1, 4))
    w_r = np.concatenate([inp["w_rg"], inp["w_re"]], axis=2)
    o["w_r_t"] = np.ascontiguousarray(w_r.reshape(NL, 16, 128, 36).transpose(0, 2, 1, 3))
    o["w_e_gate"] = inp["w_e_gate"]
    o["w_e_up"] = inp["w_e_up"]
    o["w_e_down"] = inp["w_e_down"]
    colp = np.zeros((NL, 128, 256), f)
    for l in range(NL):
        c0 = 0
        colp[l, :, 0:64] = inp["b_gate"][l].reshape(4, 16, 128).transpose(2, 0, 1).reshape(128, 64)
        colp[l, :, 64:68] = inp["g_cq"][l].reshape(4, 128).T
        colp[l, :, 68:70] = inp["g_ckv"][l].reshape(2, 128).T
        colp[l, :, 70:74] = inp["b_dw_c"][l].reshape(4, 128).T
        colp[l, :, 74:78] = inp["ln_c_g"][l].reshape(4, 128).T
        colp[l, :, 78:82] = inp["ln_c_b"][l].reshape(4, 128).T
        colp[l, :, 82:206] = inp["w_dw_c"][l].reshape(31, 4, 128).transpose(2, 1, 0).reshape(128, 124)
        colp[l, :, 206:246] = inp["w_conv_d"][l].reshape(5, 8, 128).transpose(2, 1, 0).reshape(128, 40)
        colp[l, :, 246:254] = inp["b_conv_d"][l].reshape(8, 128).T
    o["colp"] = colp
    rowp = np.zeros((NL, 1, 8832), f)
    for l in range(NL):
        rowp[l, 0, 0:2048] = inp["ln1_g"][l]
        rowp[l, 0, 2048:4096] = inp["ln1_b"][l]
        rowp[l, 0, 4096:6144] = inp["ln2_g"][l]
        rowp[l, 0, 6144:8192] = inp["ln2_b"][l]
        rowp[l, 0, 8192:8704] = inp["g_norm_d"][l]
        rowp[l, 0, 8704:8708] = inp["b_rg"][l]
        rowp[l, 0, 8708:8740] = inp["b_re"][l]
        rowp[l, 0, 8740:8748] = inp["d_skip"][l]
        rowp[l, 0, 8748:8756] = inp["dt_bias_f"][l]
        rowp[l, 0, 8756:8764] = inp["dt_bias_b"][l]
        rowp[l, 0, 8764:8772] = inp["a_log_f"][l]
        rowp[l, 0, 8772:8780] = inp["a_log_b"][l]
    o["rowp"] = rowp
    o["rowg"] = np.concatenate([inp["ln_in_g"], inp["ln_in_b"]]).reshape(1, 4096).astype(f)
    o["rel_bias"] = inp["rel_bias"].astype(f)
    o.update(_host_consts())
    return o


IN_SHAPES = {
    "x": ([S, D], F32),
    "w_in_t": ([NL, 31, 128, 16, 128], F32),
    "w_dt_tm": ([NL, 128, 16, 16], F32),
    "w_in_tm": ([NL, 2, 128, 16, 512], F32),
    "w_uq_t": ([NL, 128, 4, 1024], F32),
    "w_ukv_t": ([NL, 128, 2, 1024], F32),
    "w_gate_t": ([NL, 4, 16, 128, 16, 128], F32),
    "w_br_t": ([NL, 4, 16, 128, 4, 128], F32),
    "w_out_t": ([NL, 4, 128, 16, 512], F32),
    "w_r_t": ([NL, 128, 16, 36], F32),
    "w_e_gate": ([NL, 32, 2048, 512], F32),
    "w_e_up": ([NL, 32, 2048, 512], F32),
    "w_e_down": ([NL, 32, 512, 2048], F32),
    "colp": ([NL, 128, 256], F32),
    "rowp": ([NL, 1, 8832], F32),
    "rowg": ([1, 4096], F32),
    "rel_bias": ([32, 8], F32),
    "mohr": ([32, RVLEN], F32),
    "cst": ([128, 6, 128], F32),
    "rope": ([2, 96, S], F32),
    "scanmask": ([16, S], F32),
    "rowc": ([1, 64], F32),
}


CH = dict(qa=0, ka=4, cq=8, ckv=12, krx=14, glua=15, glug=19, xs=23, bm=27, cm=29)


def build(debug=False, stop_after=None, small_moe=False):
    nc = bass.Bass("TRN2", target_bir_lowering=False)
    scr = "ExternalOutput" if debug else "Internal"
    din = {}
    hnd = {}
    for n, (shp, dt) in IN_SHAPES.items():
        if small_moe and n.startswith("w_e_"):
            shp = [shp[0], 1] + list(shp[2:])
        hnd[n] = nc.dram_tensor(n, shp, dt, kind="ExternalInput")
        din[n] = hnd[n].ap()
    out_d = nc.dram_tensor("out", [S, D], F32, kind="ExternalOutput").ap()

    def scratch(name, shape, dt):
        h = nc.dram_tensor(name, shape, dt, kind=scr)
        hnd[name] = h
        return h.ap()

    HTM = scratch("HTM", [S, D], F32)
    HFM = scratch("HFM", [128, 16, S], BF16)
    PROJ = scratch("PROJ", [31, 128, S], BF16)
    DTR = scratch("DTR", [S, 16], F32)
    MG = scratch("MG", [128, 16, S], BF16)
    UD = scratch("UD", [S, D], F32)
    SLD = scratch("SLD", [S, 2], I32)
    WTD = scratch("WTD", [S, 2], F32)
    VATM = scratch("VATM", [S, 512], BF16)
    ZSTM = scratch("ZSTM", [S, 512], BF16)
    YBR = scratch("YBR", [4, 128, 4, S], BF16)
    RV = scratch("RV", [8, RVLEN], F32)
    H1B = scratch("H1B", [S, D], BF16)
    LGT = scratch("LGT", [S, 36], F32)
    XROWS = scratch("XROWS", [NEXP * CAP, D], BF16)
    YROWS = scratch("YROWS", [NEXP * CAP, D], F32)

    kb = KB(nc)
    done = [False]

    def finish_check(name):
        if stop_after == name:
            done[0] = True
        return done[0]

    with ExitStack() as gstack:
        kb.open(gstack)
        cstf = kb.gsb("cstf", [128, 6, 128], F32)
        cstb = kb.gsb("cstb", [128, 6, 128], BF16)
        ident_f = cstf[:, 0, :]
        ident_b = cstb[:, 0, :]
        ones_f = cstf[:, 5, :]
        ones_b = cstb[:, 5, :]

        def ln_stats(src, tag, tg):
            st = kb._ln_st
            kb.op("dve", "bn_stats", reads=[tag], writes=["lnst0"], out=st["stats"][:, 0, :], in_=src[:, 0:512])
            kb.op("dve", "bn_stats", reads=[tag], writes=["lnst1"], out=st["stats"][:, 1, :], in_=src[:, 512:1024])
            kb.op("dve", "bn_stats", reads=[tag], writes=["lnst2"], out=st["stats"][:, 2, :], in_=src[:, 1024:1536])
            kb.op("dve", "bn_stats", reads=[tag], writes=["lnst3"], out=st["stats"][:, 3, :], in_=src[:, 1536:2048])
            kb.op("dve", "bn_aggr", reads=["lnst0", "lnst1", "lnst2", "lnst3"], writes=["lnmv"], out=st["mv"][:, :], in_=st["stats"][:, :, :])
            kb.op("dve", "tensor_scalar_add", reads=["lnmv"], writes=["lnve"], out=st["ve"][:, :], in0=st["mv"][:, 1:2], scalar1=EPS)
            kb.op("act", "activation", reads=["lnve"], writes=["lnsd"], out=st["sd"][:, :], in_=st["ve"][:, :], func=AF.Sqrt)
            kb.op("dve", "reciprocal", reads=["lnsd"], writes=[tg + "rs"], out=st[tg + "rs"][:, :], in_=st["sd"][:, :])
            kb.op("dve", "tensor_scalar", reads=["lnmv", tg + "rs"], writes=[tg + "nm"], out=st[tg + "nm"][:, :], in0=st["mv"][:, 0:1],
                  scalar1=st[tg + "rs"][:, 0:1], scalar2=-1.0, op0=ALU.mult, op1=ALU.mult)

        def ln_alloc():
            st = {}
            st["stats"] = kb.sb("ln_stats", [128, 4, 6], F32)
            st["mv"] = kb.sb("ln_mv", [128, 2], F32)
            st["ve"] = kb.sb("ln_ve", [128, 1], F32)
            st["sd"] = kb.sb("ln_sd", [128, 1], F32)
            for tg in ("a", "b"):
                st[tg + "rs"] = kb.sb("ln_rs" + tg, [128, 1], F32)
                st[tg + "nm"] = kb.sb("ln_nm" + tg, [128, 1], F32)
            kb._ln_st = st
            kb._ln_i = 0

        def ln_apply(src, stag, dst, dtag, gbc, bbc, gtag):
            tg = "ab"[kb._ln_i % 2]
            kb._ln_i += 1
            st = kb._ln_st
            ln_stats(src, stag, tg)
            kb.op("act", "activation", reads=[stag, tg + "rs", tg + "nm"], writes=[dtag], out=dst, in_=src, func=AF.Identity,
                  scale=st[tg + "rs"][:, 0:1], bias=st[tg + "nm"][:, 0:1])
            kb.op("dve", "tensor_tensor", reads=[dtag, gtag], writes=[dtag], out=dst, in0=dst, in1=gbc, op=ALU.mult)
            kb.op("pool", "tensor_tensor", reads=[dtag, gtag], writes=[dtag], out=dst, in0=dst, in1=bbc, op=ALU.add)

        def emit_h_tile(t, hT, htag, rings, write_out):
            if write_out:
                kb.dma("sp", out=out_d[t * 128:(t + 1) * 128, :], in_=hT, reads=[htag], is_out=True)
                return
            kb.dma("sp", out=HTM[t * 128:(t + 1) * 128, :], in_=hT, reads=[htag], writes=[("HTM", t)])
            hb, hbt = rings["hb"].next()
            kb.op("act", "activation", reads=[htag], writes=[hbt], out=hb[:, :], in_=hT, func=AF.Copy)
            fm, fmt = rings["fm"].next()
            for g in range(4):
                pt, ptt = rings["pt"].next()
                for j in range(4):
                    c = g * 4 + j
                    kb.op("pe", "transpose", reads=[hbt], writes=[ptt], out=pt[:, j * 128:(j + 1) * 128], in_=hb[:, c * 128:(c + 1) * 128], identity=ident_b)
                eng = "dve" if g % 2 == 0 else "act"
                if eng == "dve":
                    kb.op("dve", "tensor_copy", reads=[ptt], writes=[fmt], out=fm[:, g * 4:(g + 1) * 4, :], in_=pt[:, :].rearrange("p (a b) -> p a b", a=4))
                else:
                    kb.op("act", "activation", reads=[ptt], writes=[fmt], out=fm[:, g * 4:(g + 1) * 4, :], in_=pt[:, :].rearrange("p (a b) -> p a b", a=4), func=AF.Copy)
            kb.dma("sp", out=HFM[:, :, t * 128:(t + 1) * 128], in_=fm[:, :, :], reads=[fmt], writes=[("HFM", t)])

        kb.phase_begin()
        kb.dma("sp", out=cstf[:, :, :], in_=din["cst"], writes=["cstf"])
        kb.op("dve", "tensor_copy", reads=["cstf"], writes=["cstb"], out=cstb[:, :, :], in_=cstf[:, :, :])
        rb = kb.sb("rb", [32, 8], F32)
        eb = kb.sb("eb", [32, 8], F32)
        mo = kb.sb("mo", [32, RVLEN], F32)
        rv = kb.sb("rv", [8, RVLEN], F32)
        kb.dma("sp", out=rb[:, :], in_=din["rel_bias"], writes=["rb"])
        kb.dma("sp", out=mo[:, :], in_=din["mohr"], writes=["mo"])
        kb.op("act", "activation", reads=["rb"], writes=["eb"], out=eb[:, :], in_=rb[:, :], func=AF.Exp)
        pr = Ring(kb, "ipr", 2, [128, 512], F32, psum=True)
        for j in range(RVLEN // 512):
            p, ptg = pr.next()
            kb.mm(p[0:8, :], eb[:, :], mo[:, j * 512:(j + 1) * 512], True, True, reads=["eb", "mo"], writes=[ptg])
            kb.op("dve", "tensor_copy", reads=[ptg], writes=["rv"], out=rv[:, j * 512:(j + 1) * 512], in_=p[0:8, :])
        kb.dma("sp", out=RV, in_=rv[:, :], reads=["rv"], writes=["RV"])
        zt = kb.sb("zt", [128, 2048], BF16)
        kb.op("pool", "memset", writes=["zt"], ap=zt[:, :], constant=0.0)
        for i in range(NEXP * CAP // 128):
            kb.dma("sp", out=XROWS[i * 128:(i + 1) * 128, :], in_=zt[:, :], reads=["zt"], writes=[("XR0", i)])
        kb.phase_end()

        kb.phase_begin()
        ln_alloc()
        gbc = kb.sb("gbc", [128, 2048], F32)
        bbc = kb.sb("bbc", [128, 2048], F32)
        kb.dma("sp", out=gbc[:, :], in_=din["rowg"][0:1, 0:2048].partition_broadcast(128), writes=["gb"])
        kb.dma("sp", out=bbc[:, :], in_=din["rowg"][0:1, 2048:4096].partition_broadcast(128), writes=["gb"])
        xr = Ring(kb, "xin", 2, [128, 2048], F32)
        hr = Ring(kb, "hout", 2, [128, 2048], F32)
        rings = dict(hb=Ring(kb, "hb", 2, [128, 2048], BF16), fm=Ring(kb, "fm", 2, [128, 16, 128], BF16),
                     pt=Ring(kb, "ptb", 4, [128, 512], BF16, psum=True))
        for t in range(NT):
            xt, xtag = xr.next()
            kb.dma("sp", out=xt[:, :], in_=din["x"][t * 128:(t + 1) * 128, :], writes=[xtag])
            ht, htag = hr.next()
            ln_apply(xt[:, :], xtag, ht[:, :], htag, gbc[:, :], bbc[:, :], "gb")
            emit_h_tile(t, ht[:, :], htag, rings, False)
        kb.phase_end()
        if finish_check("ln_in"):
            return nc, kb

        for l in range(NL):
            build_layer(nc, kb, din, hnd, out_d, l, finish_check, dict(
                HTM=HTM, HFM=HFM, PROJ=PROJ, DTR=DTR, VATM=VATM, ZSTM=ZSTM, YBR=YBR, RV=RV, MG=MG, UD=UD, SLD=SLD, WTD=WTD, H1B=H1B, LGT=LGT, XROWS=XROWS, YROWS=YROWS),
                dict(cstf=cstf, cstb=cstb), ln_alloc, ln_apply, emit_h_tile)
            if done[0]:
                return nc, kb
    return nc, kb


def attention(kb, nheads, kT_ap, qT_ap, v_ap, ones64, kcs_for, e_info, scale, yst, prefix, reads_k, reads_q, reads_v, pre_head=None, srr=None):
    if srr is None:
        srr = Ring(kb, prefix + "S", 3, [128, 512], F32, psum=True)
    accr = Ring(kb, prefix + "acc", 2, [128, 1024], F32, psum=True)
    ptr = Ring(kb, prefix + "pt", 3, [128, 512], BF16)
    pmr = Ring(kb, prefix + "pm", 3, [128, 512], BF16) if e_info is not None else None
    rden = kb.sb(prefix + "rden", [128, 512], F32)
    units = []
    for h in range(nheads):
        for qb in range(4):
            kcs = kcs_for(qb)
            for i, kc in enumerate(kcs):
                units.append((h, qb, kc, i == 0, i == len(kcs) - 1))
    state = {}
    cur_acc = {}
    seen_heads = set()

    def stage1(u):
        h, qb, kc, first, last = u
        if pre_head is not None and h not in seen_heads:
            seen_heads.add(h)
            pre_head(h)
        sp_, spt = srr.next()
        kb.mm(sp_[:, :], kT_ap(h, kc), qT_ap(h, qb), True, True, reads=[reads_k(h), reads_q(h)], writes=[spt])
        pt_, ptt = ptr.next()
        kb.op("act", "activation", reads=[spt], writes=[ptt], out=pt_[:, :], in_=sp_[:, :], func=AF.Exp, scale=scale)
        if e_info is not None:
            eap, etok = e_info(h, qb, kc)
            pm_, pmt = pmr.next()
            kb.op("dve", "tensor_tensor", reads=[ptt, etok], writes=[pmt], out=pm_[:, :], in0=pt_[:, :], in1=eap, op=ALU.mult)
            state[u] = (pm_, pmt)
        else:
            state[u] = (pt_, ptt)

    def stage2(u):
        h, qb, kc, first, last = u
        pm_, pmt = state.pop(u)
        if first:
            cur_acc[0] = accr.next()
        acc, acct = cur_acc[0]
        po = (h % 2) * 64
        kb.mm(acc[po:po + 64, 0:512], v_ap(h, kc), pm_[:, :], first, last, reads=[reads_v(h), pmt], writes=[acct + "n"])
        kb.mm(acc[po:po + 64, 512:1024], ones64, pm_[:, :], first, last, reads=["cstb", pmt], writes=[acct + "d"])
        if last:
            kb.op("dve", "reciprocal", reads=[acct + "d"], writes=[prefix + "rden"], out=rden[po:po + 64, :], in_=acc[po:po + 64, 512:1024])
            kb.op("dve", "tensor_tensor", reads=[acct + "n", prefix + "rden"], writes=[("yst", h, qb)],
                  out=yst[po:po + 64, h // 2, qb * 512:(qb + 1) * 512], in0=acc[po:po + 64, 0:512], in1=rden[po:po + 64, :], op=ALU.mult)

    LAG = 2
    for idx in range(len(units) + LAG):
        if idx < len(units):
            stage1(units[idx])
        if idx - LAG >= 0:
            stage2(units[idx - LAG])


def moe_experts(kb, l, din, XROWS, YROWS, ident_b, nexp):
    wgr = Ring(kb, "weg", 2, [128, 16, 512], BF16)
    wur = Ring(kb, "weu", 2, [128, 16, 512], BF16)
    wdr = Ring(kb, "wed", 2, [128, 4, 2048], BF16)
    xrr = Ring(kb, "xrE", 4, [128, 2048], BF16)
    xTr = Ring(kb, "xTE", 2, [128, 16, 256], BF16)
    aTr = Ring(kb, "aTE", 2, [128, 4, 256], BF16)
    sgr = Ring(kb, "sgE", 2, [128, 256], F32)
    yrr = Ring(kb, "yrE", 2, [128, 2048], F32)
    ptxr = Ring(kb, "ptx", 2, [128, 512], BF16, psum=True)
    pgur = Ring(kb, "pgu", 3, [128, 512], F32, psum=True)
    pyr = Ring(kb, "pyE", 3, [128, 512], F32, psum=True)
    ke = 0
    for e in range(nexp):
        wg, wgt = wgr.next()
        wu, wut = wur.next()
        wd, wdt = wdr.next()
        kb.dma("pool", out=wg[:, :, :], in_=din["w_e_gate"][l, e].rearrange("(kc p) n -> p kc n", p=128), writes=[wgt])
        kb.dma("pool", out=wu[:, :, :], in_=din["w_e_up"][l, e].rearrange("(kc p) n -> p kc n", p=128), writes=[wut])
        for nb in range(4):
            kb.dma("pool", out=wd[:, :, nb * 512:(nb + 1) * 512], in_=din["w_e_down"][l, e].rearrange("(kc p) n -> p kc n", p=128)[:, :, nb * 512:(nb + 1) * 512], writes=[wdt])
        xT, xTt = xTr.next()
        for s2 in range(2):
            xr, xrt = xrr.next()
            r0 = e * CAP + s2 * 128
            kb.dma("sp", out=xr[:, :], in_=XROWS[r0:r0 + 128, :], writes=[xrt])
            for g in range(4):
                pt, ptt = ptxr.next()
                for j in range(4):
                    c = g * 4 + j
                    kb.op("pe", "transpose", reads=[xrt, "cstb"], writes=[ptt], out=pt[:, j * 128:(j + 1) * 128], in_=xr[:, c * 128:(c + 1) * 128], identity=ident_b)
                ke += 1
                if ke % 2 == 0:
                    kb.op("dve", "tensor_copy", reads=[ptt], writes=[(xTt, s2, g)], out=xT[:, g * 4:(g + 1) * 4, s2 * 128:(s2 + 1) * 128], in_=pt[:, :].rearrange("p (a b) -> p a b", a=4))
                else:
                    kb.op("act", "activation", reads=[ptt], writes=[(xTt, s2, g)], out=xT[:, g * 4:(g + 1) * 4, s2 * 128:(s2 + 1) * 128], in_=pt[:, :].rearrange("p (a b) -> p a b", a=4), func=AF.Copy)
        xdeps = [(xTt, s2, g) for s2 in range(2) for g in range(4)]
        aT, aTt = aTr.next()
        for fcn in range(4):
            bank, bkt = pgur.next()
            for kc in range(16):
                kb.mm(bank[:, 0:256], wg[:, kc, fcn * 128:(fcn + 1) * 128], xT[:, kc, :], kc == 0, kc == 15, reads=[wgt] + xdeps, writes=[bkt])
            for kc in range(16):
                kb.mm(bank[:, 256:512], wu[:, kc, fcn * 128:(fcn + 1) * 128], xT[:, kc, :], kc == 0, kc == 15, reads=[wut] + xdeps, writes=[bkt])
            sg, sgt = sgr.next()
            kb.op("act", "activation", reads=[bkt], writes=[sgt], out=sg[:, :], in_=bank[:, 0:256], func=AF.Silu)
            kb.op("dve", "tensor_tensor", reads=[sgt, bkt], writes=[(aTt, fcn)], out=aT[:, fcn, :], in0=bank[:, 256:512], in1=sg[:, :], op=ALU.mult)
        for s2 in range(2):
            yr, yrt = yrr.next()
            for nb in range(4):
                py, pyt = pyr.next()
                for kc in range(4):
                    kb.mm(py[:, :], aT[:, kc, s2 * 128:(s2 + 1) * 128], wd[:, kc, nb * 512:(nb + 1) * 512], kc == 0, kc == 3, reads=[wdt] + [(aTt, f) for f in range(4)], writes=[pyt])
                ke += 1
                if ke % 2 == 0:
                    kb.op("dve", "tensor_copy", reads=[pyt], writes=[(yrt, nb)], out=yr[:, nb * 512:(nb + 1) * 512], in_=py[:, :])
                else:
                    kb.op("act", "activation", reads=[pyt], writes=[(yrt, nb)], out=yr[:, nb * 512:(nb + 1) * 512], in_=py[:, :], func=AF.Copy)
            r0 = e * CAP + s2 * 128
            kb.dma("sp", out=YROWS[r0:r0 + 128, :], in_=yr[:, :], reads=[(yrt, nb) for nb in range(4)], writes=[("YR", e, s2)])


def build_layer(nc, kb, din, hnd, out_d, l, finish_check, SC, CST, ln_alloc, ln_apply, emit_h_tile):
    HTM, HFM, PROJ, DTR, VATM, ZSTM, YBR, RV, MG, H1B, LGT, XROWS, YROWS = [SC[k] for k in
        ("HTM", "HFM", "PROJ", "DTR", "VATM", "ZSTM", "YBR", "RV", "MG", "H1B", "LGT", "XROWS", "YROWS")]
    cstf, cstb = CST["cstf"], CST["cstb"]
    ident_b = cstb[:, 0, :]
    ident_f = cstf[:, 0, :]
    J_b = cstb[:, 1, :]
    U_f = cstf[:, 2, :]
    Lo_f = cstf[:, 3, :]
    ones_b = cstb[:, 5, :]
    ones_f = cstf[:, 5, :]

    kb.phase_begin()
    hfm = kb.sb("hfm", [128, 16, S], BF16)
    for tb in range(4):
        kb.dma("sp", out=hfm[:, :, tb * 512:(tb + 1) * 512], in_=HFM[:, :, tb * 512:(tb + 1) * 512], writes=[("hfm", tb)])
    wr = Ring(kb, "wpin", 4, [128, 16, 128], BF16)
    pr = Ring(kb, "pp", 6, [128, 512], F32, psum=True)
    sr = Ring(kb, "pstage", 3, [128, S], BF16)
    k = 0
    for c in range(31):
        w, wt = wr.next()
        kb.dma("pool", out=w[:, :, :], in_=din["w_in_t"][l, c], writes=[wt])
        stg, stt = sr.next()
        for tb in range(4):
            p, pt = pr.next()
            for kc in range(16):
                kb.mm(p[:, :], w[:, kc, :], hfm[:, kc, tb * 512:(tb + 1) * 512], kc == 0, kc == 15, reads=[wt, ("hfm", tb)], writes=[pt])
            k += 1
            if k % 2 == 0:
                kb.op("act", "activation", reads=[pt], writes=[(stt, tb)], out=stg[:, tb * 512:(tb + 1) * 512], in_=p[:, :], func=AF.Copy)
            else:
                kb.op("dve", "tensor_copy", reads=[pt], writes=[(stt, tb)], out=stg[:, tb * 512:(tb + 1) * 512], in_=p[:, :])
        kb.dma("sp", out=PROJ[c], in_=stg[:, :], reads=[(stt, tb) for tb in range(4)], writes=[("PROJ", c)])
    wtm = Ring(kb, "wtm", 2, [128, 16, 512], BF16)
    tmst = kb.sb("tmstage", [128, 16, 512], BF16)
    for b in range(2):
        w, wt = wtm.next()
        kb.dma("pool", out=w[:, :, :], in_=din["w_in_tm"][l, b], writes=[wt])
        for t in range(NT):
            p, pt = pr.next()
            for kc in range(16):
                kb.mm(p[:, :], hfm[:, kc, t * 128:(t + 1) * 128], w[:, kc, :], kc == 0, kc == 15, reads=[wt, ("hfm", t // 4)], writes=[pt])
            if b == 1:
                kb.op("act", "activation", reads=[pt], writes=[("tmst", t)], out=tmst[:, t, :], in_=p[:, :], func=AF.Silu)
            elif t % 2 == 0:
                kb.op("act", "activation", reads=[pt], writes=[("tmst", t)], out=tmst[:, t, :], in_=p[:, :], func=AF.Copy)
            else:
                kb.op("dve", "tensor_copy", reads=[pt], writes=[("tmst", t)], out=tmst[:, t, :], in_=p[:, :])
        dst = VATM if b == 0 else ZSTM
        kb.dma("sp", out=dst.rearrange("(t p) n -> p t n", p=128), in_=tmst[:, :, :], reads=[("tmst", t) for t in range(NT)], writes=["TMD%d" % b])
    wdt = kb.sb("wdt", [128, 16, 16], BF16)
    dtst = kb.sb("dtst", [128, 16, 16], F32)
    kb.dma("pool", out=wdt[:, :, :], in_=din["w_dt_tm"][l], writes=["wdt"])
    for t in range(NT):
        p, pt = pr.next()
        for kc in range(16):
            kb.mm(p[:, 0:16], hfm[:, kc, t * 128:(t + 1) * 128], wdt[:, kc, :], kc == 0, kc == 15, reads=["wdt", ("hfm", t // 4)], writes=[pt])
        kb.op("dve", "tensor_copy", reads=[pt], writes=[("dtst", t)], out=dtst[:, t, :], in_=p[:, 0:16])
    kb.dma("sp", out=DTR.rearrange("(t p) n -> p t n", p=128), in_=dtst[:, :, :], reads=[("dtst", t) for t in range(NT)], writes=["DTR"])
    kb.phase_end()
    if finish_check("P%d" % l):
        return

    kb.phase_begin()
    qT = kb.sb("qT", [128, 4, S], BF16)
    kT = kb.sb("kT", [128, 4, S], BF16)
    vtm = kb.sb("vtm", [128, 16, 512], BF16)
    yst = kb.sb("yst", [128, 4, S], BF16)
    for c in range(4):
        kb.dma("sp", out=qT[:, c, :], in_=PROJ[CH["qa"] + c], writes=[("qT", c)])
        kb.dma("sp", out=kT[:, c, :], in_=PROJ[CH["ka"] + c], writes=[("kT", c)])
    kb.dma("sp", out=vtm[:, :, :], in_=VATM.rearrange("(t p) n -> p t n", p=128), writes=["vtm"])
    DEL = [d for d in range(-1536, 1921, 128) if -1151 <= d <= 1535]
    Et = kb.sb("Et", [128, 2, len(DEL), 512], BF16)
    hkr = Ring(kb, "hk", 3, [128, 512], BF16)
    pj = kb.ps("pj", [128, 512], F32)

    def build_E(h):
        for di, d in enumerate(DEL):
            hk, hkt = hkr.next()
            base = 1408 - d
            kb.dma("pool", out=hk[:, :], in_=bass.AP(hnd["RV"], h * RVLEN + base, [[1, 128], [1, 512]]), writes=[hkt])
            kb.mm(pj[:, :], J_b, hk[:, :], True, True, reads=[hkt, "cstb"], writes=["pj"])
            if di % 2 == 0:
                kb.op("act", "activation", reads=["pj"], writes=[("E", h % 2, di)], out=Et[:, h % 2, di, :], in_=pj[:, :], func=AF.Copy)
            else:
                kb.op("dve", "tensor_copy", reads=["pj"], writes=[("E", h % 2, di)], out=Et[:, h % 2, di, :], in_=pj[:, :])

    def pre_head(h):
        if h == 0:
            build_E(0)
        if h + 1 < 8:
            build_E(h + 1)

    def e_info(h, qb, kc):
        di = DEL.index(128 * kc - 512 * qb)
        return Et[:, h % 2, di, :], ("E", h % 2, di)

    attention(kb, 8,
              kT_ap=lambda h, kc: kT[(h % 2) * 64:(h % 2) * 64 + 64, h // 2, kc * 128:(kc + 1) * 128],
              qT_ap=lambda h, qb: qT[(h % 2) * 64:(h % 2) * 64 + 64, h // 2, qb * 512:(qb + 1) * 512],
              v_ap=lambda h, kc: vtm[:, kc, h * 64:(h + 1) * 64],
              ones64=ones_b[:, 0:64],
              kcs_for=lambda qb: [kc for kc in range(16) if -1151 <= 128 * kc - 512 * qb <= 1535],
              e_info=e_info, scale=0.125, yst=yst, prefix="A",
              reads_k=lambda h: ("kT", h // 2), reads_q=lambda h: ("qT", h // 2), reads_v=lambda h: "vtm", pre_head=pre_head)
    kb.dma("sp", out=YBR[0], in_=yst[:, :, :], reads=[("yst", h, qb) for h in range(8) for qb in range(4)], writes=[("YBR", 0)])
    kb.phase_end()
    if finish_check("A%d" % l):
        return

    kb.phase_begin()
    cp = kb.sb("cp", [128, 256], F32)
    kb.dma("sp", out=cp[:, :], in_=din["colp"][l], writes=["cp"])
    wuq = kb.sb("wuq", [128, 4, 1024], BF16)
    wukv = kb.sb("wukv", [128, 2, 1024], BF16)
    kb.dma("pool", out=wuq[:, :, :], in_=din["w_uq_t"][l], writes=["wuq"])
    kb.dma("pool", out=wukv[:, :, :], in_=din["w_ukv_t"][l], writes=["wukv"])
    cc = kb.sb("cc", [96, S], F32)
    ss = kb.sb("ss", [96, S], F32)
    kb.dma("sp", out=cc[64:96, :], in_=din["rope"][0, 64:96, :], writes=["cc"])
    kb.dma("sp", out=ss[64:96, :], in_=din["rope"][1, 64:96, :], writes=["ss"])
    krA = kb.sb("krA", [96, S], BF16)
    krB = kb.sb("krB", [96, S], BF16)
    kb.dma("sp", out=krA[64:96, :], in_=PROJ[CH["krx"], 0:32, :], writes=["krA"])
    kb.dma("sp", out=krB[64:96, :], in_=PROJ[CH["krx"], 32:64, :], writes=["krB"])
    QT = kb.sb("QT", [96, 8, S], BF16)
    KT = kb.sb("KT", [96, 8, S], BF16)
    vtm = kb.sb("vtmB", [128, 16, 512], BF16)
    yst = kb.sb("ystB", [128, 4, S], BF16)
    cqr = Ring(kb, "cqb", 2, [128, 4, 512], BF16)
    ckr = Ring(kb, "ckb", 2, [128, 2, 512], BF16)
    sqb = kb.sb("sqb", [128, 4, 512], BF16)
    cqn = kb.sb("cqn", [128, 4, 512], BF16)
    ckvn = kb.sb("ckvn", [128, 2, 512], BF16)
    rt = kb.sb("rt", [128, 512], F32)
    t1 = kb.sb("t1", [96, 512], F32)
    t2 = kb.sb("t2", [96, 512], F32)
    krope = kb.sb("krope", [96, 512], BF16)
    pss = kb.ps("pss", [128, 512], F32)
    srrB = Ring(kb, "BS", 3, [128, 512], F32, psum=True)
    pqr = Ring.wrap(srrB.tiles[0:2], srrB.names[0:2])
    pq2r = Ring.wrap(srrB.tiles[2:3], srrB.names[2:3])

    def rms_block(src, stag, nch, gcol, dst, dtag, inv_n):
        kb.op("act", "activation", reads=[stag], writes=["sqb"], out=sqb[:, 0:nch, :], in_=src[:, 0:nch, :], func=AF.Square)
        for c in range(nch):
            kb.mm(pss[:, :], ones_b, sqb[:, c, :], c == 0, c == nch - 1, reads=["sqb", "cstb"], writes=["pss"])
        kb.op("dve", "tensor_scalar", reads=["pss"], writes=["rt"], out=rt[:, :], in0=pss[:, :], scalar1=inv_n, scalar2=EPS, op0=ALU.mult, op1=ALU.add)
        kb.op("act", "activation", reads=["rt"], writes=["rt"], out=rt[:, :], in_=rt[:, :], func=AF.Sqrt)
        kb.op("dve", "reciprocal", reads=["rt"], writes=["rt"], out=rt[:, :], in_=rt[:, :])
        for c in range(nch):
            kb.op("dve", "scalar_tensor_tensor", reads=[stag, "rt", "cp"], writes=[dtag], out=dst[:, c, :], in0=src[:, c, :],
                  scalar=cp[:, gcol + c:gcol + c + 1], in1=rt[:, :], op0=ALU.mult, op1=ALU.mult)

    for tb in range(4):
        tbs = slice(tb * 512, (tb + 1) * 512)
        cqb, cqt = cqr.next()
        ckb, ckt = ckr.next()
        for c in range(4):
            kb.dma("sp", out=cqb[:, c, :], in_=PROJ[CH["cq"] + c, :, tbs], writes=[cqt])
        for c in range(2):
            kb.dma("sp", out=ckb[:, c, :], in_=PROJ[CH["ckv"] + c, :, tbs], writes=[ckt])
        rms_block(cqb, cqt, 4, 64, cqn, "cqn", 1.0 / 512)
        for h in range(8):
            pq, pqt = pqr.next()
            pq2, pq2t = pq2r.next()
            for kc in range(4):
                kb.mm(pq[0:96, :], wuq[:, kc, 96 * h:96 * h + 96], cqn[:, kc, :], kc == 0, kc == 3, reads=["wuq", "cqn"], writes=[pqt])
            for kc in range(4):
                kb.mm(pq2[64:96, :], wuq[:, kc, 768 + 32 * h:768 + 32 * h + 32], cqn[:, kc, :], kc == 0, kc == 3, reads=["wuq", "cqn"], writes=[pq2t])
            kb.op("act", "activation", reads=[pqt], writes=[("QT", h, tb)], out=QT[0:64, h, tbs], in_=pq[0:64, :], func=AF.Copy)
            kb.op("dve", "tensor_tensor", reads=[pqt, "cc"], writes=["t1"], out=t1[64:96, :], in0=pq[64:96, :], in1=cc[64:96, tbs], op=ALU.mult)
            kb.op("dve", "tensor_tensor", reads=[pq2t, "ss"], writes=["t2"], out=t2[64:96, :], in0=pq2[64:96, :], in1=ss[64:96, tbs], op=ALU.mult)
            kb.op("pool", "tensor_tensor", reads=["t1", "t2"], writes=[("QT", h, tb)], out=QT[64:96, h, tbs], in0=t1[64:96, :], in1=t2[64:96, :], op=ALU.add)
        rms_block(ckb, ckt, 2, 68, ckvn, "ckvn", 1.0 / 256)
        for h in range(8):
            pq, pqt = pqr.next()
            for kc in range(2):
                kb.mm(pq[0:64, :], wukv[:, kc, 64 * h:64 * h + 64], ckvn[:, kc, :], kc == 0, kc == 1, reads=["wukv", "ckvn"], writes=[pqt])
            if h % 2 == 0:
                kb.op("act", "activation", reads=[pqt], writes=[("KT", h, tb)], out=KT[0:64, h, tbs], in_=pq[0:64, :], func=AF.Copy)
            else:
                kb.op("dve", "tensor_copy", reads=[pqt], writes=[("KT", h, tb)], out=KT[0:64, h, tbs], in_=pq[0:64, :])
        kb.op("dve", "tensor_tensor", reads=["krA", "cc"], writes=["t1"], out=t1[64:96, :], in0=krA[64:96, tbs], in1=cc[64:96, tbs], op=ALU.mult)
        kb.op("dve", "tensor_tensor", reads=["krB", "ss"], writes=["t2"], out=t2[64:96, :], in0=krB[64:96, tbs], in1=ss[64:96, tbs], op=ALU.mult)
        kb.op("pool", "tensor_tensor", reads=["t1", "t2"], writes=["krope"], out=krope[64:96, :], in0=t1[64:96, :], in1=t2[64:96, :], op=ALU.add)
        for h in range(8):
            kb.op("pool", "tensor_copy", reads=["krope"], writes=[("KT", h, tb)], out=KT[64:96, h, tbs], in_=krope[64:96, :])
        for tt in range(4):
            pq, pqt = pqr.next()
            for kc in range(2):
                kb.mm(pq[:, :], ckvn[:, kc, tt * 128:(tt + 1) * 128], wukv[:, kc, 512:1024], kc == 0, kc == 1, reads=["wukv", "ckvn"], writes=[pqt])
            if tt % 2 == 0:
                kb.op("act", "activation", reads=[pqt], writes=[("vtm", tb)], out=vtm[:, tb * 4 + tt, :], in_=pq[:, :], func=AF.Copy)
            else:
                kb.op("dve", "tensor_copy", reads=[pqt], writes=[("vtm", tb)], out=vtm[:, tb * 4 + tt, :], in_=pq[:, :])

    attention(kb, 8,
              kT_ap=lambda h, kc: KT[0:96, h, kc * 128:(kc + 1) * 128],
              qT_ap=lambda h, qb: QT[0:96, h, qb * 512:(qb + 1) * 512],
              v_ap=lambda h, kc: vtm[:, kc, h * 64:(h + 1) * 64],
              ones64=ones_b[:, 0:64],
              kcs_for=lambda qb: list(range(16)),
              e_info=None, scale=96.0 ** -0.5, yst=yst, prefix="B",
              reads_k=lambda h: ("KTall", h), reads_q=lambda h: ("QTall", h), reads_v=lambda h: "vtmall",
              pre_head=lambda h: [kb.op("pool", "engine_nop", reads=[("KT", h, tb) for tb in range(4)] + [("QT", h, tb) for tb in range(4)] + [("vtm", tb) for tb in range(4)],
                                        writes=[("KTall", h), ("QTall", h), "vtmall"])], srr=srrB)
    kb.dma("sp", out=YBR[1], in_=yst[:, :, :], reads=[("yst", h, qb) for h in range(8) for qb in range(4)], writes=[("YBR", 1)])
    kb.phase_end()
    if finish_check("B%d" % l):
        return

    kb.phase_begin()
    cp = kb.sb("cpC", [128, 256], F32)
    kb.dma("sp", out=cp[:, :], in_=din["colp"][l], writes=["cp"])
    hg = kb.sb("hg", [128, 4, S + 30], BF16)
    kb.op("pool", "memset", writes=["hgpad"], ap=hg[:, :, 0:15], constant=0.0)
    kb.op("pool", "memset", writes=["hgpad"], ap=hg[:, :, S + 15:S + 30], constant=0.0)
    gar = Ring(kb, "ga", 2, [128, S], BF16)
    ggr = Ring(kb, "gg", 2, [128, S], BF16)
    for c in range(4):
        ga, gat = gar.next()
        gg, ggt = ggr.next()
        kb.dma("sp", out=ga[:, :], in_=PROJ[CH["glua"] + c], writes=[gat])
        kb.dma("sp", out=gg[:, :], in_=PROJ[CH["glug"] + c], writes=[ggt])
        kb.op("act", "activation", reads=[ggt], writes=[ggt], out=gg[:, :], in_=gg[:, :], func=AF.Sigmoid)
        kb.op("dve", "tensor_tensor", reads=[gat, ggt, "hgpad"], writes=[("hg", c)], out=hg[:, c, 15:15 + S], in0=ga[:, :], in1=gg[:, :], op=ALU.mult)
    dg = kb.sb("dg", [128, 4, 31, 128], BF16)
    for c in range(4):
        for j in range(31):
            eng = "dve" if (c * 31 + j) % 2 == 0 else "pool"
            kb.op(eng, "tensor_scalar", reads=["cp", "cstb"], writes=[("dg", c)], out=dg[:, c, j, :], in0=ident_b,
                  scalar1=cp[:, 82 + c * 31 + j:83 + c * 31 + j], scalar2=None, op0=ALU.mult)
    yc = kb.sb("yc", [128, 4, 512], F32)
    ysq = kb.sb("ysq", [128, 4, 512], F32)
    mt = kb.sb("mt", [128, 512], F32)
    m2 = kb.sb("m2", [128, 512], F32)
    vt = kb.sb("vt", [128, 512], F32)
    tmr = Ring(kb, "tmpC", 2, [128, 512], F32)
    yo = kb.sb("yoC", [128, 4, S], BF16)
    pcr = Ring(kb, "pc", 3, [128, 512], F32, psum=True)
    psm = kb.ps("psm", [128, 512], F32)
    psq = kb.ps("psq", [128, 512], F32)
    for tb in range(4):
        tbs = slice(tb * 512, (tb + 1) * 512)
        for c in range(4):
            pc, pct = pcr.next()
            for j in range(31):
                kb.mm(pc[:, :], dg[:, c, j, :], hg[:, c, tb * 512 + j:tb * 512 + j + 512], j == 0, j == 30, reads=[("dg", c), ("hg", c)], writes=[pct])
            kb.op("act", "activation", reads=[pct, "cp"], writes=[("yc", c)], out=yc[:, c, :], in_=pc[:, :], func=AF.Identity, bias=cp[:, 70 + c:71 + c], scale=1.0)
            kb.op("act", "activation", reads=[("yc", c)], writes=[("ysq", c)], out=ysq[:, c, :], in_=yc[:, c, :], func=AF.Square)
        for c in range(4):
            kb.mm(psm[:, :], ones_f, yc[:, c, :], c == 0, c == 3, reads=[("yc", c), "cstf"], writes=["psm"])
        for c in range(4):
            kb.mm(psq[:, :], ones_f, ysq[:, c, :], c == 0, c == 3, reads=[("ysq", c), "cstf"], writes=["psq"])
        kb.op("dve", "tensor_scalar", reads=["psm"], writes=["mt"], out=mt[:, :], in0=psm[:, :], scalar1=1.0 / 512, scalar2=None, op0=ALU.mult)
        kb.op("dve", "tensor_tensor", reads=["mt"], writes=["m2"], out=m2[:, :], in0=mt[:, :], in1=mt[:, :], op=ALU.mult)
        kb.op("dve", "scalar_tensor_tensor", reads=["psq", "m2"], writes=["vt"], out=vt[:, :], in0=psq[:, :], scalar=1.0 / 512, in1=m2[:, :], op0=ALU.mult, op1=ALU.subtract)
        kb.op("dve", "tensor_scalar", reads=["vt"], writes=["vt"], out=vt[:, :], in0=vt[:, :], scalar1=EPS, scalar2=None, op0=ALU.add)
        kb.op("act", "activation", reads=["vt"], writes=["vt"], out=vt[:, :], in_=vt[:, :], func=AF.Sqrt)
        kb.op("dve", "reciprocal", reads=["vt"], writes=["vt"], out=vt[:, :], in_=vt[:, :])
        for c in range(4):
            tm, tmt = tmr.next()
            kb.op("dve", "tensor_tensor", reads=[("yc", c), "mt"], writes=[tmt], out=tm[:, :], in0=yc[:, c, :], in1=mt[:, :], op=ALU.subtract)
            kb.op("pool", "tensor_tensor", reads=[tmt, "vt"], writes=[tmt], out=tm[:, :], in0=tm[:, :], in1=vt[:, :], op=ALU.mult)
            kb.op("act", "activation", reads=[tmt, "cp"], writes=[("yoC", c, tb)], out=yo[:, c, tbs], in_=tm[:, :], func=AF.Silu,
                  scale=cp[:, 74 + c:75 + c], bias=cp[:, 78 + c:79 + c])
    kb.dma("sp", out=YBR[2], in_=yo[:, :, :], reads=[("yoC", c, tb) for c in range(4) for tb in range(4)], writes=[("YBR", 2)])
    kb.phase_end()
    if finish_check("C%d" % l):
        return

    kb.phase_begin()
    cp = kb.sb("cpD", [128, 256], F32)
    kb.dma("sp", out=cp[:, :], in_=din["colp"][l], writes=["cp"])
    rp = kb.sb("rpD", [128, 640], F32)
    kb.dma("sp", out=rp[:, :], in_=din["rowp"][l, 0:1, 8192:8832].partition_broadcast(128), writes=["rp"])
    gnd = rp[:, 0:512]
    dsk = rp[:, 548:556]
    pbig = kb.ps("pbig", [128, 1024], F32)
    pA = kb.ps("pA", [128, 512], F32)
    pB = kb.ps("pB", [128, 512], F32)
    pC = kb.ps("pC", [128, 512], F32)
    pD = kb.ps("pD", [128, 512], F32)
    pTr = Ring(kb, "pT", 2, [128, 512], BF16, psum=True)
    convr = Ring.wrap([pA, pB, pC], ["pA", "pB", "pC"])
    dgd = kb.sb("dgd", [128, 8, 5, 128], BF16)
    for c in range(8):
        for j in range(5):
            eng = "dve" if (c * 5 + j) % 2 == 0 else "pool"
            kb.op(eng, "tensor_scalar", reads=["cp", "cstb"], writes=[("dgd", c)], out=dgd[:, c, j, :], in0=ident_b,
                  scalar1=cp[:, 206 + c * 5 + j:207 + c * 5 + j], scalar2=None, op0=ALU.mult)
    xbc = kb.sb("xbc", [128, 8, S], BF16)
    xinr = Ring(kb, "xinD", 2, [128, S + 4], BF16)
    for i in range(2):
        kb.op("pool", "memset", writes=[xinr.names[i] + "pad"], ap=xinr.tiles[i][:, 0:2], constant=0.0)
        kb.op("pool", "memset", writes=[xinr.names[i] + "pad"], ap=xinr.tiles[i][:, S + 2:S + 4], constant=0.0)
    for c in range(8):
        xi, xit = xinr.next()
        kb.dma("sp", out=xi[:, 2:2 + S], in_=PROJ[CH["xs"] + c], reads=[xit + "pad"], writes=[xit])
        for tb in range(4):
            pc, pct = convr.next()
            for j in range(5):
                kb.mm(pc[:, :], dgd[:, c, j, :], xi[:, tb * 512 + j:tb * 512 + j + 512], j == 0, j == 4, reads=[("dgd", c), xit, xit + "pad"], writes=[pct])
            kb.op("act", "activation", reads=[pct, "cp"], writes=[("xbc", c)], out=xbc[:, c, tb * 512:(tb + 1) * 512], in_=pc[:, :], func=AF.Silu,
                  bias=cp[:, 246 + c:247 + c], scale=1.0)
    xtm = kb.sb("xtm", [128, 16, 512], BF16)
    btm = kb.sb("btm", [128, 16, 256], BF16)
    for t in range(NT):
        pt, ptt = pTr.next()
        for c in range(4):
            kb.op("pe", "transpose", reads=[("xbc", c), "cstb"], writes=[ptt], out=pt[:, c * 128:(c + 1) * 128], in_=xbc[:, c, t * 128:(t + 1) * 128], identity=ident_b)
        kb.op("dve", "tensor_copy", reads=[ptt], writes=[("xtm", t)], out=xtm[:, t, :], in_=pt[:, :])
        pt, ptt = pTr.next()
        for g in range(2):
            kb.op("pe", "transpose", reads=[("xbc", 4 + g), "cstb"], writes=[ptt], out=pt[:, g * 128:(g + 1) * 128], in_=xbc[:, 4 + g, t * 128:(t + 1) * 128], identity=ident_b)
        kb.op("act", "activation", reads=[ptt], writes=[("btm", t)], out=btm[:, t, :], in_=pt[:, 0:256], func=AF.Copy)
    def small(name):
        return kb.sb(name, [128, 16, 16], F32)
    dtr, dtv, av, Ecs, Tot, negE, wend, expE, dec = [small(n) for n in ("dtr", "dtv", "av", "Ecs", "Tot", "negE", "wend", "expE", "dec")]
    negA = kb.sb("negA", [128, 16], F32)
    kb.dma("sp", out=dtr[:, :, :], in_=DTR.rearrange("(t p) n -> p t n", p=128), writes=["dtr"])
    kb.op("dve", "tensor_tensor", reads=["dtr", "rp"], writes=["dtv"], out=dtv[:, :, :], in0=dtr[:, :, :],
          in1=rp[:, 556:572].unsqueeze(1).to_broadcast([128, 16, 16]), op=ALU.add)
    kb.op("act", "activation", reads=["dtv"], writes=["dtv"], out=dtv[:, :, :], in_=dtv[:, :, :], func=AF.Exp)
    kb.op("act", "activation", reads=["dtv"], writes=["dtv"], out=dtv[:, :, :], in_=dtv[:, :, :], func=AF.Ln, bias=1.0, scale=1.0)
    kb.op("act", "activation", reads=["rp"], writes=["negA"], out=negA[:, :], in_=rp[:, 572:588], func=AF.Exp)
    kb.op("dve", "tensor_scalar", reads=["negA"], writes=["negA"], out=negA[:, :], in0=negA[:, :], scalar1=-1.0, scalar2=None, op0=ALU.mult)
    kb.op("dve", "tensor_tensor", reads=["dtv", "negA"], writes=["av"], out=av[:, :, :], in0=dtv[:, :, :],
          in1=negA[:, :].unsqueeze(1).to_broadcast([128, 16, 16]), op=ALU.mult)
    for t in range(NT):
        kb.mm(pD[:, t * 16:t * 16 + 8], U_f, av[:, t, 0:8], True, True, reads=["av", "cstf"], writes=["pD"], last=False)
        kb.mm(pD[:, t * 16 + 8:t * 16 + 16], Lo_f, av[:, t, 8:16], True, True, reads=["av", "cstf"], writes=["pD"], last=False)
        kb.mm(pD[:, 256 + t * 16:256 + t * 16 + 16], ones_f, av[:, t, :], True, True, reads=["av", "cstf"], writes=["pD"], last=(t == NT - 1))
    kb.op("dve", "tensor_copy", reads=["pD"], writes=["Ecs"], out=Ecs[:, :, :], in_=pD[:, 0:256].rearrange("p (a b) -> p a b", b=16))
    kb.op("dve", "tensor_copy", reads=["pD"], writes=["Tot"], out=Tot[:, :, :], in_=pD[:, 256:512].rearrange("p (a b) -> p a b", b=16))
    kb.op("dve", "tensor_scalar", reads=["Ecs"], writes=["negE"], out=negE[:, :, :], in0=Ecs[:, :, :], scalar1=-1.0, scalar2=None, op0=ALU.mult)
    kb.op("dve", "tensor_tensor", reads=["Tot", "Ecs"], writes=["wend"], out=wend[:, :, :], in0=Tot[:, :, :], in1=Ecs[:, :, :], op=ALU.subtract)
    kb.op("act", "activation", reads=["wend"], writes=["wend"], out=wend[:, :, :], in_=wend[:, :, :], func=AF.Exp)
    kb.op("dve", "tensor_tensor", reads=["wend", "dtv"], writes=["wend"], out=wend[:, :, :], in0=wend[:, :, :], in1=dtv[:, :, :], op=ALU.mult)
    kb.op("act", "activation", reads=["Ecs"], writes=["expE"], out=expE[:, :, :], in_=Ecs[:, :, :], func=AF.Exp)
    kb.op("act", "activation", reads=["Tot"], writes=["dec"], out=dec[:, :, :], in_=Tot[:, :, :], func=AF.Exp)

    ytm = kb.sb("ytm", [128, 16, 512], F32)
    cbb = kb.sb("cbb", [128, 16, 2, 128], BF16)
    cbf = kb.sb("cbf", [128, 2, 128], BF16)
    Dt = kb.sb("Dt", [128, 8, 128], F32)
    ex = kb.sb("ex", [128, 8, 128], BF16)
    Mtr = Ring(kb, "Mt", 2, [128, 8, 128], BF16)
    xdr = Ring(kb, "xdt", 2, [128, 8, 64], BF16)
    xwr = Ring(kb, "xw", 2, [128, 8, 64], BF16)
    tmpa = kb.sb("tmpDa", [128, 512], F32)
    tmpb = kb.sb("tmpDb", [128, 512], F32)
    Hs = [kb.sb("Hs%d" % d, [128, 512], F32) for d in range(2)]
    Hb = [kb.sb("Hb%d" % d, [128, 512], BF16) for d in range(2)]
    for d in range(2):
        kb.op("pool", "memset", writes=["Hs%d" % d], ap=Hs[d][:, :], constant=0.0)
        kb.op("pool", "memset", writes=["Hb%d" % d], ap=Hb[d][:, :], constant=0.0)
    units = [(0, c) for c in range(16)] + [(1, c) for c in range(15, -1, -1)]
    st = {}

    def d_stage1(u):
        d, c = u
        cs = slice(c * 128, (c + 1) * 128)
        if d == 0:
            for g in range(2):
                kb.mm(pA[:, g * 128:(g + 1) * 128], xbc[:, 4 + g, cs], xbc[:, 6 + g, cs], True, True, reads=[("xbc", 4 + g), ("xbc", 6 + g)], writes=["pA"], last=(g == 1))
            kb.op("dve", "tensor_tensor", reads=["pA", "cstf"], writes=["cbf"], out=cbf[:, :, :], in0=pA[:, 0:256].rearrange("p (g i) -> p g i", g=2),
                  in1=U_f.unsqueeze(1).to_broadcast([128, 2, 128]), op=ALU.mult)
            kb.op("dve", "tensor_tensor", reads=["pA", "cstf"], writes=[("cbb", c)], out=cbb[:, c, :, :], in0=pA[:, 0:256].rearrange("p (g i) -> p g i", g=2),
                  in1=Lo_f.unsqueeze(1).to_broadcast([128, 2, 128]), op=ALU.mult)
        tri = U_f if d == 0 else Lo_f
        for h in range(8):
            hd = d * 8 + h
            kb.mm(pbig[:, h * 128:(h + 1) * 128], av[:, c, hd:hd + 1].to_broadcast([128, 128]), tri, True, True, reads=["av", "cstf"], writes=["pbig"], last=(h == 7))
        kb.op("dve", "tensor_tensor", reads=["pbig", "negE"], writes=["Dt"], out=Dt[:, :, :], in0=pbig[:, :].rearrange("p (h i) -> p h i", h=8),
              in1=negE[:, c, d * 8:d * 8 + 8].unsqueeze(2).to_broadcast([128, 8, 128]), op=ALU.add)
        kb.op("act", "activation", reads=["Dt"], writes=["ex"], out=ex[:, :, :], in_=Dt[:, :, :], func=AF.Exp)
        Mt, Mtt = Mtr.next()
        for g in range(2):
            cbx = cbf[:, g:g + 1, :] if d == 0 else cbb[:, c, g:g + 1, :]
            kb.op("dve", "scalar_tensor_tensor", reads=["ex", "cbf" if d == 0 else ("cbb", c)], writes=[Mtt], out=Mt[:, 4 * g:4 * g + 4, :], in0=ex[:, 4 * g:4 * g + 4, :],
                  scalar=1.0, in1=cbx.to_broadcast([128, 4, 128]), op0=ALU.min, op1=ALU.mult)
        xd, xdt_ = xdr.next()
        xw, xwt = xwr.next()
        x3 = xtm[:, c, :].rearrange("p (h e) -> p h e", h=8)
        kb.op("pool", "tensor_tensor", reads=[("xtm", c), "dtv"], writes=[xdt_], out=xd[:, :, :], in0=x3,
              in1=dtv[:, c, d * 8:d * 8 + 8].unsqueeze(2).to_broadcast([128, 8, 64]), op=ALU.mult)
        kb.op("pool", "tensor_tensor", reads=[("xtm", c), "wend"], writes=[xwt], out=xw[:, :, :], in0=x3,
              in1=wend[:, c, d * 8:d * 8 + 8].unsqueeze(2).to_broadcast([128, 8, 64]), op=ALU.mult)
        st[u] = (Mt, Mtt, xd, xdt_, xw, xwt)

    def d_stage2(u):
        d, c = u
        cs = slice(c * 128, (c + 1) * 128)
        Mt, Mtt, xd, xdt_, xw, xwt = st.pop(u)
        for h in range(8):
            kb.mm(pB[:, h * 64:(h + 1) * 64], Mt[:, h, :], xd[:, h, :], True, True, reads=[Mtt, xdt_], writes=["pB"], last=(h == 7))
        for g in range(2):
            kb.mm(pD[:, g * 256:(g + 1) * 256], xbc[:, 6 + g, cs], Hb[d][:, g * 256:(g + 1) * 256], True, True, reads=[("xbc", 6 + g), "Hb%d" % d], writes=["pD"], last=(g == 1))
        for g in range(2):
            kb.mm(pC[:, g * 256:(g + 1) * 256], btm[:, c, g * 128:(g + 1) * 128], xw[:, 4 * g:4 * g + 4, :].rearrange("p h e -> p (h e)"), True, True,
                  reads=[("btm", c), xwt], writes=["pC"], last=(g == 1))
        kb.op("dve", "tensor_tensor", reads=["pD", "expE"], writes=["tmpDa"], out=tmpa[:, :].rearrange("p (h e) -> p h e", h=8), in0=pD[:, :].rearrange("p (h e) -> p h e", h=8),
              in1=expE[:, c, d * 8:d * 8 + 8].unsqueeze(2).to_broadcast([128, 8, 64]), op=ALU.mult)
        if d == 0:
            kb.op("dve", "tensor_tensor", reads=["pB", "tmpDa"], writes=[("ytm", c)], out=ytm[:, c, :], in0=pB[:, :], in1=tmpa[:, :], op=ALU.add)
        else:
            kb.op("dve", "tensor_tensor", reads=["pB", "tmpDa"], writes=["tmpDb"], out=tmpb[:, :], in0=pB[:, :], in1=tmpa[:, :], op=ALU.add)
            kb.op("pool", "tensor_tensor", reads=["tmpDb", ("ytm", c)], writes=[("ytm", c)], out=ytm[:, c, :], in0=ytm[:, c, :], in1=tmpb[:, :], op=ALU.add)
        hn = "Hs%d" % d
        kb.op("dve", "tensor_tensor", reads=[hn, "dec"], writes=[hn], out=Hs[d][:, :].rearrange("p (h e) -> p h e", h=8), in0=Hs[d][:, :].rearrange("p (h e) -> p h e", h=8),
              in1=dec[:, c, d * 8:d * 8 + 8].unsqueeze(2).to_broadcast([128, 8, 64]), op=ALU.mult)
        kb.op("dve", "tensor_tensor", reads=[hn, "pC"], writes=[hn], out=Hs[d][:, :], in0=pC[:, :], in1=Hs[d][:, :], op=ALU.add)
        kb.op("act", "activation", reads=[hn], writes=["Hb%d" % d], out=Hb[d][:, :], in_=Hs[d][:, :], func=AF.Copy)

    for idx in range(len(units) + 1):
        if idx < len(units):
            d_stage1(units[idx])
        if idx >= 1:
            d_stage2(units[idx - 1])

    zsr = Ring(kb, "zs", 2, [128, 512], BF16)
    yz = kb.sb("yz", [128, 512], F32)
    junk = kb.sb("junkD", [128, 512], F32)
    yoD = kb.sb("yoD", [128, 512], BF16)
    ssq = kb.sb("ssq", [128, 1], F32)
    ydr = Ring(kb, "ydT", 2, [128, 4, 128], BF16)
    for c in range(NT):
        zs, zst = zsr.next()
        kb.dma("sp", out=zs[:, :], in_=ZSTM[c * 128:(c + 1) * 128, :], writes=[zst])
        kb.op("pool", "tensor_tensor", reads=[("xtm", c), "rp"], writes=["tmpDb"], out=tmpb[:, :].rearrange("p (h e) -> p h e", h=8), in0=xtm[:, c, :].rearrange("p (h e) -> p h e", h=8),
              in1=dsk.unsqueeze(2).to_broadcast([128, 8, 64]), op=ALU.mult)
        kb.op("pool", "tensor_tensor", reads=["tmpDb", ("ytm", c)], writes=[("ytm", c)], out=ytm[:, c, :], in0=ytm[:, c, :], in1=tmpb[:, :], op=ALU.add)
        kb.op("dve", "tensor_tensor", reads=[("ytm", c), zst], writes=["yz"], out=yz[:, :], in0=ytm[:, c, :], in1=zs[:, :], op=ALU.mult)
        kb.op("act", "activation", reads=["yz"], writes=["junkD", "ssq"], out=junk[:, :], in_=yz[:, :], func=AF.Square, accum_out=ssq[:, 0:1])
        kb.op("dve", "tensor_scalar", reads=["ssq"], writes=["ssq"], out=ssq[:, :], in0=ssq[:, :], scalar1=1.0 / 512, scalar2=EPS, op0=ALU.mult, op1=ALU.add)
        kb.op("act", "activation", reads=["ssq"], writes=["ssq"], out=ssq[:, :], in_=ssq[:, :], func=AF.Sqrt)
        kb.op("dve", "reciprocal", reads=["ssq"], writes=["ssq"], out=ssq[:, :], in_=ssq[:, :])
        kb.op("dve", "scalar_tensor_tensor", reads=["yz", "ssq", "rp"], writes=["yoD"], out=yoD[:, :], in0=yz[:, :], scalar=ssq[:, 0:1], in1=gnd, op0=ALU.mult, op1=ALU.mult)
        pt, ptt = pTr.next()
        for j in range(4):
            kb.op("pe", "transpose", reads=["yoD", "cstb"], writes=[ptt], out=pt[:, j * 128:(j + 1) * 128], in_=yoD[:, j * 128:(j + 1) * 128], identity=ident_b)
        yd, ydt = ydr.next()
        kb.op("act", "activation", reads=[ptt], writes=[ydt], out=yd[:, :, :], in_=pt[:, :].rearrange("p (a b) -> p a b", a=4), func=AF.Copy)
        kb.dma("sp", out=YBR[3, :, :, c * 128:(c + 1) * 128], in_=yd[:, :, :], reads=[ydt], writes=[("YBR3", c)])
    kb.phase_end()
    if finish_check("D%d" % l):
        return

    kb.phase_begin()
    cp = kb.sb("cpM", [128, 256], F32)
    kb.dma("sp", out=cp[:, :], in_=din["colp"][l], writes=["cp"])
    hfb = kb.sb("hfbM", [128, 16, 1024], BF16)
    ybm = kb.sb("ybm", [128, 4, 4, 1024], BF16)
    mg = kb.sb("mgM", [128, 16, 1024], BF16)
    wgr = Ring(kb, "wg", 6, [128, 16, 128], BF16)
    wbr = Ring(kb, "wb", 6, [128, 4, 128], BF16)
    gsr = Ring(kb, "gs", 3, [128, 512], BF16)
    maccr = Ring(kb, "macc", 2, [128, 512], F32)
    tmr = Ring(kb, "tmpM", 3, [128, 512], F32)
    pgr = Ring(kb, "pg", 4, [128, 512], F32, psum=True)
    pbr = Ring(kb, "pbm", 4, [128, 512], F32, psum=True)
    for sbk in range(2):
        sbs = slice(sbk * 1024, (sbk + 1) * 1024)
        kb.dma("sp", out=hfb[:, :, :], in_=HFM[:, :, sbs], writes=["hfb"])
        for i in range(4):
            kb.dma("sp", out=ybm[:, i, :, :], in_=YBR[i, :, :, sbs], writes=[("ybm", i)])
        for fc in range(16):
            macs = [maccr.next() for _ in range(2)]
            for i in range(4):
                wg, wgt = wgr.next()
                wb, wbt = wbr.next()
                kb.dma("pool", out=wg[:, :, :], in_=din["w_gate_t"][l, i, fc], writes=[wgt])
                kb.dma("pool", out=wb[:, :, :], in_=din["w_br_t"][l, i, fc], writes=[wbt])
                for hf in range(2):
                    hs = slice(hf * 512, (hf + 1) * 512)
                    pg, pgt = pgr.next()
                    pb, pbt = pbr.next()
                    for kc in range(16):
                        kb.mm(pg[:, :], wg[:, kc, :], hfb[:, kc, hs], kc == 0, kc == 15, reads=[wgt, "hfb"], writes=[pgt])
                    for kc in range(4):
                        kb.mm(pb[:, :], wb[:, kc, :], ybm[:, i, kc, hs], kc == 0, kc == 3, reads=[wbt, ("ybm", i)], writes=[pbt])
                    gs, gst = gsr.next()
                    kb.op("act", "activation", reads=[pgt, "cp"], writes=[gst], out=gs[:, :], in_=pg[:, :], func=AF.Sigmoid, bias=cp[:, i * 16 + fc:i * 16 + fc + 1], scale=1.0)
                    mac, mact = macs[hf]
                    if i == 0:
                        kb.op("dve", "tensor_tensor", reads=[gst, pbt], writes=[mact], out=mac[:, :], in0=pb[:, :], in1=gs[:, :], op=ALU.mult)
                    else:
                        tm, tmt = tmr.next()
                        kb.op("dve", "tensor_tensor", reads=[gst, pbt], writes=[tmt], out=tm[:, :], in0=pb[:, :], in1=gs[:, :], op=ALU.mult)
                        if i < 3:
                            kb.op("pool", "tensor_tensor", reads=[tmt, mact], writes=[mact], out=mac[:, :], in0=mac[:, :], in1=tm[:, :], op=ALU.add)
                        else:
                            kb.op("pool", "tensor_tensor", reads=[tmt, mact], writes=[("mg", fc)], out=mg[:, fc, hs], in0=mac[:, :], in1=tm[:, :], op=ALU.add)
        kb.dma("sp", out=MG[:, :, sbs], in_=mg[:, :, :], reads=[("mg", fc) for fc in range(16)], writes=[("MG", sbk)])
    kb.phase_end()
    if finish_check("M1%d" % l):
        return

    UD = SC["UD"]
    kb.phase_begin()
    mga = kb.sb("mga", [128, 16, S], BF16)
    for tb in range(4):
        kb.dma("sp", out=mga[:, :, tb * 512:(tb + 1) * 512], in_=MG[:, :, tb * 512:(tb + 1) * 512], writes=[("mga", tb)])
    wor = Ring(kb, "wo", 2, [128, 16, 512], BF16)
    hsr = Ring(kb, "hsl", 3, [128, 512], F32)
    usr = Ring(kb, "usl", 3, [128, 512], F32)
    por = Ring(kb, "po", 4, [128, 512], F32, psum=True)
    for nb in range(4):
        nbs = slice(nb * 512, (nb + 1) * 512)
        wo, wot = wor.next()
        kb.dma("pool", out=wo[:, :, :], in_=din["w_out_t"][l, nb], writes=[wot])
        for t in range(NT):
            hs_, hst = hsr.next()
            kb.dma("sp", out=hs_[:, :], in_=HTM[t * 128:(t + 1) * 128, nbs], writes=[hst])
            po, pot = por.next()
            for kc in range(16):
                kb.mm(po[:, :], mga[:, kc, t * 128:(t + 1) * 128], wo[:, kc, :], kc == 0, kc == 15, reads=[wot, ("mga", t // 4)], writes=[pot])
            us, ust = usr.next()
            kb.op("dve", "scalar_tensor_tensor", reads=[hst, pot], writes=[ust], out=us[:, :], in0=hs_[:, :], scalar=ALPHA, in1=po[:, :], op0=ALU.mult, op1=ALU.add)
            kb.dma("sp", out=UD[t * 128:(t + 1) * 128, nbs], in_=us[:, :], reads=[ust], writes=[("UD", t, nb)])
    kb.phase_end()
    if finish_check("M2%d" % l):
        return

    kb.phase_begin()
    ln_alloc()
    g1 = kb.sb("g1", [128, 2048], F32)
    b1 = kb.sb("b1", [128, 2048], F32)
    kb.dma("sp", out=g1[:, :], in_=din["rowp"][l, 0:1, 0:2048].partition_broadcast(128), writes=["gb1"])
    kb.dma("sp", out=b1[:, :], in_=din["rowp"][l, 0:1, 2048:4096].partition_broadcast(128), writes=["gb1"])
    wrt = kb.sb("wrt", [128, 16, 36], F32)
    kb.dma("sp", out=wrt[:, :, :], in_=din["w_r_t"][l], writes=["wrt"])
    brt = kb.sb("brt", [128, 36], F32)
    kb.dma("sp", out=brt[:, :], in_=din["rowp"][l, 0:1, 8704:8740].partition_broadcast(128), writes=["brt"])
    ur = Ring(kb, "uM", 2, [128, 2048], F32)
    h1r = Ring(kb, "h1M", 2, [128, 2048], F32)
    hbr = Ring(kb, "hbM", 2, [128, 2048], BF16)
    h1fm = kb.sb("h1fm", [128, 16, 128], F32)
    lgr = Ring(kb, "lgs", 2, [128, 36], F32)
    ptfr = Ring(kb, "ptf", 3, [128, 512], F32, psum=True)
    plg = kb.ps("plg", [128, 512], F32)
    for t in range(NT):
        u, ut = ur.next()
        kb.dma("sp", out=u[:, :], in_=UD[t * 128:(t + 1) * 128, :], writes=[ut])
        h1, h1t = h1r.next()
        ln_apply(u[:, :], ut, h1[:, :], h1t, g1[:, :], b1[:, :], "gb1")
        kb.dma("sp", out=HTM[t * 128:(t + 1) * 128, :], in_=h1[:, :], reads=[h1t], writes=[("HTM", t)])
        hb, hbt = hbr.next()
        kb.op("act", "activation", reads=[h1t], writes=[hbt], out=hb[:, :], in_=h1[:, :], func=AF.Copy)
        kb.dma("sp", out=H1B[t * 128:(t + 1) * 128, :], in_=hb[:, :], reads=[hbt], writes=[("H1B", t)])
        for g in range(4):
            pt, ptt = ptfr.next()
            for j in range(4):
                c = g * 4 + j
                kb.op("pe", "transpose", reads=[h1t, "cstf"], writes=[ptt], out=pt[:, j * 128:(j + 1) * 128], in_=h1[:, c * 128:(c + 1) * 128], identity=ident_f)
            if g % 2 == 0:
                kb.op("dve", "tensor_copy", reads=[ptt], writes=[("h1fm", g)], out=h1fm[:, g * 4:(g + 1) * 4, :], in_=pt[:, :].rearrange("p (a b) -> p a b", a=4))
            else:
                kb.op("act", "activation", reads=[ptt], writes=[("h1fm", g)], out=h1fm[:, g * 4:(g + 1) * 4, :], in_=pt[:, :].rearrange("p (a b) -> p a b", a=4), func=AF.Copy)
        for kc in range(16):
            kb.mm(plg[:, 0:36], h1fm[:, kc, :], wrt[:, kc, :], kc == 0, kc == 15, reads=[("h1fm", kc // 4), "wrt"], writes=["plg"])
        lg_, lgt = lgr.next()
        kb.op("dve", "tensor_tensor", reads=["plg", "brt"], writes=[lgt], out=lg_[:, :], in0=plg[:, 0:36], in1=brt[:, :], op=ALU.add)
        kb.dma("sp", out=LGT[t * 128:(t + 1) * 128, :], in_=lg_[:, :], reads=[lgt], writes=[("LGT", t)])
    kb.phase_end()
    if finish_check("M3%d" % l):
        return

    SLD, WTD = SC["SLD"], SC["WTD"]
    BIG = 1.0e9
    kb.phase_begin()
    lg = kb.sb("lgE", [128, 16, 36], F32)
    kb.dma("sp", out=lg[:, :, :], in_=LGT.rearrange("(t p) n -> p t n", p=128), writes=["lg"])
    sbase = kb.sb("sbase", [128, 32], F32)
    kb.dma("sp", out=sbase[:, :], in_=din["rowc"][0:1, 0:32].partition_broadcast(128), writes=["sbase"])
    def t2(name, n):
        return kb.sb(name, [128, 16, n], F32)
    gmax = kb.sb("gmax", [128, 16], F32)
    gsum = kb.sb("gsum", [128, 16], F32)
    gw = kb.sb("gw", [128, 16], F32)
    gd, goh, pen = t2("gd", 4), t2("goh", 4), t2("pen", 4)
    elm, oh1, elm2, oh2, tmpP, tmpQ = [t2(n, 32) for n in ("elm", "oh1", "elm2", "oh2", "tmpP", "tmpQ")]
    m1 = kb.sb("m1", [128, 16], F32)
    m2_ = kb.sb("m2E", [128, 16], F32)
    w1 = kb.sb("w1", [128, 16], F32)
    wts = kb.sb("wts", [128, 16, 2], F32)
    s12 = kb.sb("s12", [128, 16, 2], F32)
    sli = kb.sb("sli", [128, 16, 2], I32)
    Cb = kb.sb("Cb", [128, 16, 32], BF16)
    gl = lg[:, :, 0:4]
    el = lg[:, :, 4:36]
    def bc(ap2, n):
        return ap2.unsqueeze(2).to_broadcast([128, 16, n])
    kb.op("dve", "tensor_reduce", reads=["lg"], writes=["gmax"], out=gmax[:, :], in_=gl, axis=AX.X, op=ALU.max)
    kb.op("dve", "tensor_tensor", reads=["lg", "gmax"], writes=["gd"], out=gd[:, :, :], in0=gl, in1=bc(gmax[:, :], 4), op=ALU.subtract)
    kb.op("act", "activation", reads=["gd"], writes=["gd"], out=gd[:, :, :], in_=gd[:, :, :], func=AF.Exp)
    kb.op("dve", "tensor_reduce", reads=["gd"], writes=["gsum"], out=gsum[:, :], in_=gd[:, :, :], axis=AX.X, op=ALU.add)
    kb.op("dve", "reciprocal", reads=["gsum"], writes=["gw"], out=gw[:, :], in_=gsum[:, :])
    kb.op("dve", "tensor_tensor", reads=["lg", "gmax"], writes=["goh"], out=goh[:, :, :], in0=gl, in1=bc(gmax[:, :], 4), op=ALU.is_equal)
    kb.op("dve", "tensor_scalar", reads=["goh"], writes=["pen"], out=pen[:, :, :], in0=goh[:, :, :], scalar1=BIG, scalar2=-BIG, op0=ALU.mult, op1=ALU.add)
    kb.op("dve", "tensor_tensor", reads=["lg", "pen"], writes=["elm"], out=elm[:, :, :].rearrange("p t (g e) -> p t g e", g=4),
          in0=el.rearrange("p t (g e) -> p t g e", g=4), in1=pen[:, :, :].unsqueeze(3).to_broadcast([128, 16, 4, 8]), op=ALU.add)
    kb.op("dve", "tensor_reduce", reads=["elm"], writes=["m1"], out=m1[:, :], in_=elm[:, :, :], axis=AX.X, op=ALU.max)
    kb.op("dve", "tensor_tensor", reads=["elm", "m1"], writes=["oh1"], out=oh1[:, :, :], in0=elm[:, :, :], in1=bc(m1[:, :], 32), op=ALU.is_equal)
    kb.op("dve", "scalar_tensor_tensor", reads=["oh1", "elm"], writes=["elm2"], out=elm2[:, :, :], in0=oh1[:, :, :], scalar=-BIG, in1=elm[:, :, :], op0=ALU.mult, op1=ALU.add)
    kb.op("dve", "tensor_reduce", reads=["elm2"], writes=["m2"], out=m2_[:, :], in_=elm2[:, :, :], axis=AX.X, op=ALU.max)
    kb.op("dve", "tensor_tensor", reads=["elm2", "m2"], writes=["oh2"], out=oh2[:, :, :], in0=elm2[:, :, :], in1=bc(m2_[:, :], 32), op=ALU.is_equal)
    kb.op("dve", "tensor_tensor", reads=["m1", "m2"], writes=["w1"], out=w1[:, :], in0=m1[:, :], in1=m2_[:, :], op=ALU.subtract)
    kb.op("act", "activation", reads=["w1"], writes=["w1"], out=w1[:, :], in_=w1[:, :], func=AF.Sigmoid)
    kb.op("dve", "tensor_tensor", reads=["w1", "gw"], writes=["wts0"], out=wts[:, :, 0], in0=w1[:, :], in1=gw[:, :], op=ALU.mult)
    kb.op("dve", "tensor_tensor", reads=["wts0", "gw"], writes=["wts1"], out=wts[:, :, 1], in0=gw[:, :], in1=wts[:, :, 0], op=ALU.subtract)
    kb.op("dve", "tensor_tensor", reads=["oh1", "oh2"], writes=["Cb"], out=Cb[:, :, :], in0=oh1[:, :, :], in1=oh2[:, :, :], op=ALU.add)
    ppre = kb.ps("ppre", [128, 512], F32)
    strict_b = cstb[:, 4, :]
    for t in range(NT):
        for tp in range(t):
            kb.mm(ppre[:, t * 32:(t + 1) * 32], ones_b, Cb[:, tp, :], tp == 0, False, reads=["Cb", "cstb"], writes=["ppre"], last=False)
        kb.mm(ppre[:, t * 32:(t + 1) * 32], strict_b, Cb[:, t, :], t == 0, True, reads=["Cb", "cstb"], writes=["ppre"], last=(t == NT - 1))
    kb.op("dve", "tensor_tensor", reads=["ppre", "sbase"], writes=["tmpP"], out=tmpP[:, :, :], in0=ppre[:, :].rearrange("p (t e) -> p t e", t=16),
          in1=sbase[:, :].unsqueeze(1).to_broadcast([128, 16, 32]), op=ALU.add)
    kb.op("dve", "tensor_tensor", reads=["tmpP", "oh1"], writes=["tmpQ"], out=tmpQ[:, :, :], in0=tmpP[:, :, :], in1=oh1[:, :, :], op=ALU.mult)
    kb.op("dve", "tensor_reduce", reads=["tmpQ"], writes=["s120"], out=s12[:, :, 0], in_=tmpQ[:, :, :], axis=AX.X, op=ALU.add)
    kb.op("dve", "tensor_tensor", reads=["tmpP", "oh2", "s120"], writes=["tmpQ"], out=tmpQ[:, :, :], in0=tmpP[:, :, :], in1=oh2[:, :, :], op=ALU.mult)
    kb.op("dve", "tensor_reduce", reads=["tmpQ"], writes=["s121"], out=s12[:, :, 1], in_=tmpQ[:, :, :], axis=AX.X, op=ALU.add)
    kb.op("dve", "tensor_copy", reads=["s120", "s121"], writes=["sli"], out=sli[:, :, :], in_=s12[:, :, :])
    kb.dma("sp", out=SLD.rearrange("(t p) n -> p t n", p=128), in_=sli[:, :, :], reads=["sli"], writes=["SLD"])
    kb.dma("sp", out=WTD.rearrange("(t p) n -> p t n", p=128), in_=wts[:, :, :], reads=["wts0", "wts1"], writes=["WTD"])
    hbr = Ring(kb, "h1bE", 3, [128, 2048], BF16)
    for t in range(NT):
        hb, hbt = hbr.next()
        kb.dma("sp", out=hb[:, :], in_=H1B[t * 128:(t + 1) * 128, :], writes=[hbt])
        for k2 in range(2):
            kb.dma("pool", out=XROWS, in_=hb[:, :], reads=[hbt, "sli"], writes=[("XR", t, k2)], name="indirect_dma_start",
                   out_offset=bass.IndirectOffsetOnAxis(ap=sli[:, t, k2:k2 + 1], axis=0), in_offset=None)
    kb.phase_end()
    if finish_check("E1%d" % l):
        return

    kb.phase_begin()
    moe_experts(kb, l, din, XROWS, YROWS, ident_b, NEXP)
    kb.phase_end()
    if finish_check("E3%d" % l):
        return

    kb.phase_begin()
    ln_alloc()
    g2 = kb.sb("g2", [128, 2048], F32)
    b2 = kb.sb("b2", [128, 2048], F32)
    kb.dma("sp", out=g2[:, :], in_=din["rowp"][l, 0:1, 4096:6144].partition_broadcast(128), writes=["gb2"])
    kb.dma("sp", out=b2[:, :], in_=din["rowp"][l, 0:1, 6144:8192].partition_broadcast(128), writes=["gb2"])
    sli = kb.sb("sliF", [128, 16, 2], I32)
    wts = kb.sb("wtsF", [128, 16, 2], F32)
    kb.dma("sp", out=sli[:, :, :], in_=SLD.rearrange("(t p) n -> p t n", p=128), writes=["sli"])
    kb.dma("sp", out=wts[:, :, :], in_=WTD.rearrange("(t p) n -> p t n", p=128), writes=["wts"])
    gr1 = Ring(kb, "gy1", 2, [128, 2048], F32)
    gr2 = Ring(kb, "gy2", 2, [128, 2048], F32)
    hTr = Ring(kb, "hTE", 2, [128, 2048], F32)
    h2r = Ring(kb, "h2E", 2, [128, 2048], F32)
    rings = dict(hb=Ring(kb, "hbE4", 2, [128, 2048], BF16), fm=Ring(kb, "fmE4", 2, [128, 16, 128], BF16),
                 pt=Ring(kb, "ptE4", 4, [128, 512], BF16, psum=True))
    for t in range(NT):
        ga, gat = gr1.next()
        gb_, gbt = gr2.next()
        kb.dma("pool", out=ga[:, :], in_=YROWS, reads=["sli"], writes=[gat], name="indirect_dma_start", out_offset=None,
               in_offset=bass.IndirectOffsetOnAxis(ap=sli[:, t, 0:1], axis=0))
        kb.dma("pool", out=gb_[:, :], in_=YROWS, reads=["sli"], writes=[gbt], name="indirect_dma_start", out_offset=None,
               in_offset=bass.IndirectOffsetOnAxis(ap=sli[:, t, 1:2], axis=0))
        hT, hTt = hTr.next()
        kb.dma("sp", out=hT[:, :], in_=HTM[t * 128:(t + 1) * 128, :], writes=[hTt])
        kb.op("act", "activation", reads=[hTt], writes=[hTt], out=hT[:, :], in_=hT[:, :], func=AF.Copy, scale=ALPHA)
        kb.op("dve", "scalar_tensor_tensor", reads=[gat, hTt, "wts"], writes=[hTt], out=hT[:, :], in0=ga[:, :], scalar=wts[:, t, 0:1], in1=hT[:, :], op0=ALU.mult, op1=ALU.add)
        kb.op("dve", "scalar_tensor_tensor", reads=[gbt, hTt, "wts"], writes=[hTt], out=hT[:, :], in0=gb_[:, :], scalar=wts[:, t, 1:2], in1=hT[:, :], op0=ALU.mult, op1=ALU.add)
        h2, h2t = h2r.next()
        ln_apply(hT[:, :], hTt, h2[:, :], h2t, g2[:, :], b2[:, :], "gb2")
        emit_h_tile(t, h2[:, :], h2t, rings, l == NL - 1)
    kb.phase_end()
    if finish_check("E4%d" % l):
        return


_CACHE = {}


def kernel(**inputs):
    inp = {k: np.asarray(v) for k, v in inputs.items()}
    lay = _host_layout(inp)
    if "nc" not in _CACHE:
        _CACHE["nc"] = build()[0]
    nc = _CACHE["nc"]
    x = inp["x"].astype(np.float32)
    in_maps = []
    for b in range(x.shape[0]):
        m = dict(lay)
        m["x"] = np.ascontiguousarray(x[b])
        in_maps.append(m)
    res = run_bass_kernel_spmd(nc, in_maps, core_ids=list(range(x.shape[0])))
    out = np.stack([np.asarray(r["out"], dtype=np.float32) for r in res.results], axis=0)
    return out
```

```python
import math
from contextlib import ExitStack

import numpy as np
import ml_dtypes
import concourse.bass as bass
import concourse.mybir as mybir
from concourse.bass_utils import run_bass_kernel_spmd

F32 = mybir.dt.float32
BF16 = mybir.dt.bfloat16
I32 = mybir.dt.int32
AF = mybir.ActivationFunctionType
ALU = mybir.AluOpType
AX = mybir.AxisListType

S = 2048
D = 2048
NT = 16
NL = 2
ALPHA = 4.0 ** 0.25
EPS = 1e-5
R_E = 1535
RVLEN = 3072
CAP = 256
NEXP = 32
ENGS = ("pe", "act", "dve", "pool", "sp")


class KB:
    def __init__(self, nc, n_dma_sems=10):
        self.nc = nc
        self.ops = {e: [] for e in ENGS}
        self.sem = {}
        self.cnt = {e: 0 for e in ENGS}
        self.seen = {e: {} for e in ENGS}
        self.lastw = {}
        self.readers = {}
        self.n_dma_sems = n_dma_sems
        self.dma_sems = {}
        self.dma_rr = {e: 0 for e in ENGS}
        self.pstack = None
        self.out_events = []
        self.n_ins = 0

    def open(self, stack):
        nc = self.nc
        for e in ENGS:
            self.sem[e] = stack.enter_context(nc.semaphore("s_" + e))
        for q in ("sp", "act", "pool"):
            lst = []
            for i in range(6 if q == "pool" else self.n_dma_sems):
                key = "d_%s_%d" % (q, i)
                self.sem[key] = stack.enter_context(nc.semaphore(key))
                lst.append([key, 0])
            self.dma_sems[q] = lst
        self.gstack = stack

    def _uniq(self, name):
        self.uid = getattr(self, "uid", 0) + 1
        return "%s_u%d" % (name, self.uid)

    def gsb(self, name, shape, dt):
        return self.gstack.enter_context(self.nc.sbuf_tensor(self._uniq(name), list(shape), dt))

    def sb(self, name, shape, dt):
        return self.pstack.enter_context(self.nc.sbuf_tensor(self._uniq(name), list(shape), dt))

    def ps(self, name, shape, dt=F32):
        return self.pstack.enter_context(self.nc.psum_tensor(self._uniq(name), list(shape), dt))

    def _deps(self, reads, writes):
        evs = []
        for t in reads:
            ev = self.lastw.get(t)
            if ev is not None:
                evs.append(ev)
        for t in writes:
            ev = self.lastw.get(t)
            if ev is not None:
                evs.append(ev)
            evs.extend(self.readers.get(t, ()))
        return evs

    def _waits(self, eng, evs):
        seen = self.seen[eng]
        need = {}
        for (k, v) in evs:
            if seen.get(k, 0) >= v:
                continue
            if need.get(k, 0) < v:
                need[k] = v
        for k, v in need.items():
            seen[k] = v
        return list(need.items())

    def _commit(self, ev, reads, writes):
        for t in writes:
            self.lastw[t] = ev
            self.readers[t] = []
        for t in reads:
            if t in writes:
                continue
            self.readers.setdefault(t, []).append(ev)

    def op(self, eng, name, reads=(), writes=(), **kw):
        evs = self._deps(reads, writes)
        waits = self._waits(eng, evs)
        self.cnt[eng] += 1
        ev = (eng, self.cnt[eng])
        self.ops[eng].append((waits, name, kw, (eng, 1)))
        self._commit(ev, reads, writes)
        return ev

    def op_noinc(self, eng, name, reads=(), writes=(), **kw):
        evs = self._deps(reads, writes)
        waits = self._waits(eng, evs)
        self.ops[eng].append((waits, name, kw, None))

    def mm(self, out, lhsT, rhs, start, stop, reads=(), writes=(), last=None):
        if last is None:
            last = stop
        f = self.op if last else self.op_noinc
        return f("pe", "matmul", reads=reads, writes=writes, out=out, lhsT=lhsT, rhs=rhs, start=start, stop=stop)

    def dma(self, q, out, in_, reads=(), writes=(), is_out=False, name="dma_start", **kw):
        evs = self._deps(reads, writes)
        lst = self.dma_sems[q]
        i = self.dma_rr[q] % len(lst)
        self.dma_rr[q] += 1
        key, c = lst[i]
        if c > 0:
            evs.append((key, c))
        waits = self._waits(q, evs)
        lst[i][1] = c + 16
        ev = (key, c + 16)
        kw = dict(kw)
        kw["out"] = out
        kw["in_"] = in_
        self.ops[q].append((waits, name, kw, (key, 16)))
        self._commit(ev, reads, writes)
        if is_out:
            self.out_events.append(ev)
        return ev

    def barrier(self):
        evs = []
        for q, lst in self.dma_sems.items():
            for key, c in lst:
                if c > 0:
                    evs.append((key, c))
        for e in ENGS:
            if e != "sp" and self.cnt[e] > 0:
                evs.append((e, self.cnt[e]))
        waits = self._waits("sp", evs)
        self.cnt["sp"] += 1
        self.ops["sp"].append((waits, "sem_inc", dict(sem=self.sem["sp"], val=1), None))
        mark = ("sp", self.cnt["sp"])
        for e in ENGS:
            if e == "sp":
                continue
            w = self._waits(e, [mark])
            self.ops[e].append((w, None, None, None))
            for (k, v) in evs:
                if self.seen[e].get(k, 0) < v:
                    self.seen[e][k] = v
        self.lastw = {}
        self.readers = {}

    def emit(self):
        nc = self.nc
        sem = self.sem
        ops = self.ops
        with nc.named_scope("ph%02d" % getattr(self, "phase_no", 0)), nc.Block() as block:
            def run(engname):
                def body(e):
                    for waits, name, kw, inc in ops[engname]:
                        for k, v in waits:
                            e.wait_ge(sem[k], v)
                        if name is None:
                            continue
                        ins = getattr(e, name)(**kw)
                        self.n_ins += 1
                        if inc is not None:
                            ins.then_inc(sem[inc[0]], inc[1])
                return body
            block.tensor(run("pe"))
            block.scalar(run("act"))
            block.vector(run("dve"))
            block.gpsimd(run("pool"))
            block.sync(run("sp"))
        self.ops = {e: [] for e in ENGS}

    def phase_begin(self):
        self.phase_no = getattr(self, "phase_no", -1) + 1
        self.pstack = ExitStack()
        self.pstack.__enter__()

    def phase_end(self):
        self.barrier()
        self.emit()
        self.pstack.close()
        self.pstack = None


class Ring:
    def __init__(self, kb, name, n, shape, dt, psum=False):
        self.tiles = []
        self.names = []
        for i in range(n):
            nm = "%s%d" % (name, i)
            t = kb.ps(nm, shape, dt) if psum else kb.sb(nm, shape, dt)
            self.tiles.append(t)
            self.names.append(nm)
        self.i = 0

    @classmethod
    def wrap(cls, tiles, names):
        r = cls.__new__(cls)
        r.tiles = list(tiles)
        r.names = list(names)
        r.i = 0
        return r

    def next(self):
        j = self.i % len(self.tiles)
        self.i += 1
        return self.tiles[j], self.names[j]


def _t5_bucket(rel):
    half = 16
    max_exact = 8
    n = np.abs(rel)
    large = max_exact + (np.log(np.maximum(n, 1) / max_exact) / np.log(1024 / max_exact) * (half - max_exact)).astype(np.int32)
    large = np.minimum(large, half - 1)
    return (rel > 0).astype(np.int32) * half + np.where(n < max_exact, n, large)


def _host_consts():
    c = {}
    i = np.arange(RVLEN)
    r = R_E - i
    a = np.abs(r)
    mult = (a <= 64).astype(np.float32) + ((r % 4 == 0) & (a <= 256)).astype(np.float32) + ((r % 16 == 0) & (a <= 1024)).astype(np.float32)
    bk = _t5_bucket(r)
    mohr = np.zeros((32, RVLEN), np.float32)
    mohr[bk, i] = mult
    c["mohr"] = mohr
    p = np.arange(128)[:, None]
    f = np.arange(128)[None, :]
    cst = np.zeros((128, 6, 128), np.float32)
    cst[:, 0] = (p == f)
    cst[:, 1] = (p + f == 127)
    cst[:, 2] = (p <= f)
    cst[:, 3] = (p >= f)
    cst[:, 4] = (p < f)
    cst[:, 5] = 1.0
    c["cst"] = cst
    inv_freq = (np.float32(10000.0) ** (-np.arange(0, 32, 2, dtype=np.float32) / np.float32(32))).astype(np.float32)
    ang = (np.arange(S, dtype=np.float32)[:, None] * inv_freq[None]).astype(np.float32)
    cos = np.cos(ang).astype(np.float32).T
    sin = np.sin(ang).astype(np.float32).T
    rope = np.zeros((2, 96, S), np.float32)
    rope[0, 64:80] = cos
    rope[0, 80:96] = cos
    rope[1, 64:80] = -sin
    rope[1, 80:96] = sin
    c["rope"] = rope
    m = np.ones((16, S), np.float32)
    m[:, ::128] = 0.0
    c["scanmask"] = m
    rowc = np.zeros((1, 64), np.float32)
    rowc[0, 0:32] = np.arange(32) * CAP
    c["rowc"] = rowc
    return c


def _host_layout(inp):
    f = np.float32
    o = {}
    w_in = inp["w_in"]
    cuts = np.cumsum((512, 512, 512, 512, 256, 32, 1024, 512, 512, 256, 256, 16))
    st = np.concatenate([[0], cuts[:-1]])
    seg = {n: (int(a), int(b)) for n, a, b in zip(("qa", "ka", "va", "cq", "ckv", "kr", "glu", "z", "xs", "bm", "cm", "dt"), st, cuts)}
    cols = []
    def rng(n, a=0, b=None):
        s0, s1 = seg[n]
        b = (s1 - s0) if b is None else b
        return list(range(s0 + a, s0 + b))
    cols += rng("qa") + rng("ka") + rng("cq") + rng("ckv")
    kr0 = seg["kr"][0]
    cols += rng("kr") + [kr0 + (r + 16) % 32 for r in range(32)] + [-1] * 64
    cols += rng("glu", 0, 512) + rng("glu", 512, 1024) + rng("xs") + rng("bm") + rng("cm")
    cols = np.array(cols)
    assert cols.size == 31 * 128
    w_in_t = np.zeros((NL, 31, 128, 16, 128), f)
    w_dt_tm = np.zeros((NL, 128, 16, 16), f)
    w_in_tm = np.zeros((NL, 2, 128, 16, 512), f)
    for l in range(NL):
        wc = np.where(cols[None, :] >= 0, w_in[l][:, np.maximum(cols, 0)], 0.0).astype(f)
        w_in_t[l] = wc.reshape(16, 128, 31, 128).transpose(2, 1, 0, 3)
        w_dt_tm[l] = w_in[l][:, seg["dt"][0]:seg["dt"][1]].reshape(16, 128, 16).transpose(1, 0, 2)
        for b, n in enumerate(("va", "z")):
            s0, s1 = seg[n]
            w_in_tm[l, b] = w_in[l][:, s0:s1].reshape(16, 128, 512).transpose(1, 0, 2)
    o["w_in_t"] = w_in_t
    o["w_in_tm"] = w_in_tm
    o["w_dt_tm"] = w_dt_tm
    w_uq = inp["w_uq"]
    pc = np.array([96 * h + 64 + (r + 16) % 32 for h in range(8) for r in range(32)])
    w_uq_t = np.concatenate([w_uq, w_uq[:, :, pc]], axis=2)
    o["w_uq_t"] = np.ascontiguousarray(w_uq_t.reshape(NL, 4, 128, 1024).transpose(0, 2, 1, 3))
    w_ukv = inp["w_ukv"]
    kc_ = np.array([128 * h + j for h in range(8) for j in range(64)])
    vc_ = kc_ + 64
    w_ukv_r = np.concatenate([w_ukv[:, :, kc_], w_ukv[:, :, vc_]], axis=2)
    o["w_ukv_t"] = np.ascontiguousarray(w_ukv_r.reshape(NL, 2, 128, 1024).transpose(0, 2, 1, 3))
    o["w_gate_t"] = np.ascontiguousarray(inp["w_gate"].reshape(NL, 4, 16, 128, 16, 128).transpose(0, 1, 4, 3, 2, 5))
    o["w_br_t"] = np.ascontiguousarray(inp["w_br"].reshape(NL, 4, 4, 128, 16, 128).transpose(0, 1, 4, 3, 2, 5))
    o["w_out_t"] = np.ascontiguousarray(inp["w_out"].reshape(NL, 16, 128, 4, 512).transpose(0, 3, 2, 1, 4))
    w_r = np.concatenate([inp["w_rg"], inp["w_re"]], axis=2)
    o["w_r_t"] = np.ascontiguousarray(w_r.reshape(NL, 16, 128, 36).transpose(0, 2, 1, 3))
    o["w_e_gate"] = inp["w_e_gate"]
    o["w_e_up"] = inp["w_e_up"]
    o["w_e_down"] = inp["w_e_down"]
    colp = np.zeros((NL, 128, 256), f)
    for l in range(NL):
        c0 = 0
        colp[l, :, 0:64] = inp["b_gate"][l].reshape(4, 16, 128).transpose(2, 0, 1).reshape(128, 64)
        colp[l, :, 64:68] = inp["g_cq"][l].reshape(4, 128).T
        colp[l, :, 68:70] = inp["g_ckv"][l].reshape(2, 128).T
        colp[l, :, 70:74] = inp["b_dw_c"][l].reshape(4, 128).T
        colp[l, :, 74:78] = inp["ln_c_g"][l].reshape(4, 128).T
        colp[l, :, 78:82] = inp["ln_c_b"][l].reshape(4, 128).T
        colp[l, :, 82:206] = inp["w_dw_c"][l].reshape(31, 4, 128).transpose(2, 1, 0).reshape(128, 124)
        colp[l, :, 206:246] = inp["w_conv_d"][l].reshape(5, 8, 128).transpose(2, 1, 0).reshape(128, 40)
        colp[l, :, 246:254] = inp["b_conv_d"][l].reshape(8, 128).T
    o["colp"] = colp
    rowp = np.zeros((NL, 1, 8832), f)
    for l in range(NL):
        rowp[l, 0, 0:2048] = inp["ln1_g"][l]
        rowp[l, 0, 2048:4096] = inp["ln1_b"][l]
        rowp[l, 0, 4096:6144] = inp["ln2_g"][l]
        rowp[l, 0, 6144:8192] = inp["ln2_b"][l]
        rowp[l, 0, 8192:8704] = inp["g_norm_d"][l]
        rowp[l, 0, 8704:8708] = inp["b_rg"][l]
        rowp[l, 0, 8708:8740] = inp["b_re"][l]
        rowp[l, 0, 8740:8748] = inp["d_skip"][l]
        rowp[l, 0, 8748:8756] = inp["dt_bias_f"][l]
        rowp[l, 0, 8756:8764] = inp["dt_bias_b"][l]
        rowp[l, 0, 8764:8772] = inp["a_log_f"][l]
        rowp[l, 0, 8772:8780] = inp["a_log_b"][l]
    o["rowp"] = rowp
    o["rowg"] = np.concatenate([inp["ln_in_g"], inp["ln_in_b"]]).reshape(1, 4096).astype(f)
    o["rel_bias"] = inp["rel_bias"].astype(f)
    o.update(_host_consts())
    return o


IN_SHAPES = {
    "x": ([S, D], F32),
    "w_in_t": ([NL, 31, 128, 16, 128], F32),
    "w_dt_tm": ([NL, 128, 16, 16], F32),
    "w_in_tm": ([NL, 2, 128, 16, 512], F32),
    "w_uq_t": ([NL, 128, 4, 1024], F32),
    "w_ukv_t": ([NL, 128, 2, 1024], F32),
    "w_gate_t": ([NL, 4, 16, 128, 16, 128], F32),
    "w_br_t": ([NL, 4, 16, 128, 4, 128], F32),
    "w_out_t": ([NL, 4, 128, 16, 512], F32),
    "w_r_t": ([NL, 128, 16, 36], F32),
    "w_e_gate": ([NL, 32, 2048, 512], F32),
    "w_e_up": ([NL, 32, 2048, 512], F32),
    "w_e_down": ([NL, 32, 512, 2048], F32),
    "colp": ([NL, 128, 256], F32),
    "rowp": ([NL, 1, 8832], F32),
    "rowg": ([1, 4096], F32),
    "rel_bias": ([32, 8], F32),
    "mohr": ([32, RVLEN], F32),
    "cst": ([128, 6, 128], F32),
    "rope": ([2, 96, S], F32),
    "scanmask": ([16, S], F32),
    "rowc": ([1, 64], F32),
}


CH = dict(qa=0, ka=4, cq=8, ckv=12, krx=14, glua=15, glug=19, xs=23, bm=27, cm=29)


def build(debug=False, stop_after=None, small_moe=False):
    nc = bass.Bass("TRN2", target_bir_lowering=False)
    scr = "ExternalOutput" if debug else "Internal"
    din = {}
    hnd = {}
    for n, (shp, dt) in IN_SHAPES.items():
        if small_moe and n.startswith("w_e_"):
            shp = [shp[0], 1] + list(shp[2:])
        hnd[n] = nc.dram_tensor(n, shp, dt, kind="ExternalInput")
        din[n] = hnd[n].ap()
    out_d = nc.dram_tensor("out", [S, D], F32, kind="ExternalOutput").ap()

    def scratch(name, shape, dt):
        h = nc.dram_tensor(name, shape, dt, kind=scr)
        hnd[name] = h
        return h.ap()

    HTM = scratch("HTM", [S, D], F32)
    HFM = scratch("HFM", [128, 16, S], BF16)
    PROJ = scratch("PROJ", [31, 128, S], BF16)
    DTR = scratch("DTR", [S, 16], F32)
    MG = scratch("MG", [128, 16, S], BF16)
    UD = scratch("UD", [S, D], F32)
    SLD = scratch("SLD", [S, 2], I32)
    WTD = scratch("WTD", [S, 2], F32)
    VATM = scratch("VATM", [S, 512], BF16)
    ZSTM = scratch("ZSTM", [S, 512], BF16)
    YBR = scratch("YBR", [4, 128, 4, S], BF16)
    RV = scratch("RV", [8, RVLEN], F32)
    H1B = scratch("H1B", [S, D], BF16)
    LGT = scratch("LGT", [S, 36], F32)
    XROWS = scratch("XROWS", [NEXP * CAP, D], BF16)
    YROWS = scratch("YROWS", [NEXP * CAP, D], F32)

    kb = KB(nc)
    done = [False]

    def finish_check(name):
        if stop_after == name:
            done[0] = True
        return done[0]

    with ExitStack() as gstack:
        kb.open(gstack)
        cstf = kb.gsb("cstf", [128, 6, 128], F32)
        cstb = kb.gsb("cstb", [128, 6, 128], BF16)
        ident_f = cstf[:, 0, :]
        ident_b = cstb[:, 0, :]
        ones_f = cstf[:, 5, :]
        ones_b = cstb[:, 5, :]

        def ln_stats(src, tag):
            tg = "ab"[kb._ln_i % 2]
            kb._ln_i += 1
            st = kb._ln_st
            for q in range(4):
                kb.op("dve", "bn_stats", reads=[tag], writes=[tg + "lnst%d" % q], out=st[tg + "stats"][:, q, :], in_=src[:, q * 512:(q + 1) * 512])
            kb.op("dve", "bn_aggr", reads=[tg + "lnst%d" % q for q in range(4)], writes=[tg + "lnmv"], out=st[tg + "mv"][:, :], in_=st[tg + "stats"][:, :, :])
            kb.op("dve", "tensor_scalar_add", reads=[tg + "lnmv"], writes=[tg + "lnve"], out=st[tg + "ve"][:, :], in0=st[tg + "mv"][:, 1:2], scalar1=EPS)
            kb.op("pool", "tensor_tensor", reads=[tg + "lnve", "lnmhalf"], writes=[tg + "rs"], out=st[tg + "rs"][:, :], in0=st[tg + "ve"][:, :], in1=st["mhalf"][:, :], op=ALU.pow)
            return tg

        def ln_alloc():
            st = {}
            for tg in ("a", "b"):
                st[tg + "stats"] = kb.sb("ln_stats" + tg, [128, 4, 6], F32)
                st[tg + "mv"] = kb.sb("ln_mv" + tg, [128, 2], F32)
                st[tg + "ve"] = kb.sb("ln_ve" + tg, [128, 1], F32)
                st[tg + "rs"] = kb.sb("ln_rs" + tg, [128, 1], F32)
            st["mhalf"] = kb.sb("ln_mhalf", [128, 1], F32)
            kb.op("pool", "memset", writes=["lnmhalf"], ap=st["mhalf"][:, :], constant=-0.5)
            kb._ln_st = st
            kb._ln_i = 0

        def ln_apply(src, stag, dst, dtag, gbc, bbc, gtag, tg=None):
            if tg is None:
                tg = ln_stats(src, stag)
            st = kb._ln_st
            kb.op("dve", "scalar_tensor_tensor", reads=[stag, tg + "lnmv", gtag], writes=[dtag], out=dst, in0=src, scalar=st[tg + "mv"][:, 0:1], in1=gbc,
                  op0=ALU.subtract, op1=ALU.mult)
            kb.op("dve", "scalar_tensor_tensor", reads=[dtag, tg + "rs", gtag], writes=[dtag], out=dst, in0=dst, scalar=st[tg + "rs"][:, 0:1], in1=bbc,
                  op0=ALU.mult, op1=ALU.add)

        kb.ln_stats = ln_stats

        def emit_h_tile(t, hT, htag, rings, write_out):
            if write_out:
                kb.dma("sp", out=out_d[t * 128:(t + 1) * 128, :], in_=hT, reads=[htag], is_out=True)
                return
            kb.dma("sp", out=HTM[t * 128:(t + 1) * 128, :], in_=hT, reads=[htag], writes=[("HTM", t)])
            hb, hbt = rings["hb"].next()
            kb.op("act", "activation", reads=[htag], writes=[hbt], out=hb[:, :], in_=hT, func=AF.Copy)
            fm, fmt = rings["fm"].next()
            for g in range(4):
                pt, ptt = rings["pt"].next()
                for j in range(4):
                    c = g * 4 + j
                    kb.op("pe", "transpose", reads=[hbt], writes=[ptt], out=pt[:, j * 128:(j + 1) * 128], in_=hb[:, c * 128:(c + 1) * 128], identity=ident_b)
                eng = "dve" if g % 2 == 0 else "act"
                if eng == "dve":
                    kb.op("dve", "tensor_copy", reads=[ptt], writes=[fmt], out=fm[:, g * 4:(g + 1) * 4, :], in_=pt[:, :].rearrange("p (a b) -> p a b", a=4))
                else:
                    kb.op("act", "activation", reads=[ptt], writes=[fmt], out=fm[:, g * 4:(g + 1) * 4, :], in_=pt[:, :].rearrange("p (a b) -> p a b", a=4), func=AF.Copy)
            kb.dma("sp", out=HFM[:, :, t * 128:(t + 1) * 128], in_=fm[:, :, :], reads=[fmt], writes=[("HFM", t)])

        kb.phase_begin()
        kb.dma("sp", out=cstf[:, :, :], in_=din["cst"], writes=["cstf"])
        kb.op("dve", "tensor_copy", reads=["cstf"], writes=["cstb"], out=cstb[:, :, :], in_=cstf[:, :, :])
        rb = kb.sb("rb", [32, 8], F32)
        eb = kb.sb("eb", [32, 8], F32)
        mo = kb.sb("mo", [32, RVLEN], F32)
        rv = kb.sb("rv", [8, RVLEN], F32)
        kb.dma("sp", out=rb[:, :], in_=din["rel_bias"], writes=["rb"])
        kb.dma("sp", out=mo[:, :], in_=din["mohr"], writes=["mo"])
        kb.op("act", "activation", reads=["rb"], writes=["eb"], out=eb[:, :], in_=rb[:, :], func=AF.Exp)
        pr = Ring(kb, "ipr", 2, [128, 512], F32, psum=True)
        for j in range(RVLEN // 512):
            p, ptg = pr.next()
            kb.mm(p[0:8, :], eb[:, :], mo[:, j * 512:(j + 1) * 512], True, True, reads=["eb", "mo"], writes=[ptg])
            kb.op("dve", "tensor_copy", reads=[ptg], writes=["rv"], out=rv[:, j * 512:(j + 1) * 512], in_=p[0:8, :])
        kb.dma("sp", out=RV, in_=rv[:, :], reads=["rv"], writes=["RV"])
        zt = kb.sb("zt", [128, 2048], BF16)
        kb.op("pool", "memset", writes=["zt"], ap=zt[:, :], constant=0.0)
        for i in range(NEXP * CAP // 128):
            kb.dma("sp", out=XROWS[i * 128:(i + 1) * 128, :], in_=zt[:, :], reads=["zt"], writes=[("XR0", i)])
        kb.phase_end()

        kb.phase_begin()
        ln_alloc()
        gbc = kb.sb("gbc", [128, 2048], F32)
        bbc = kb.sb("bbc", [128, 2048], F32)
        kb.dma("sp", out=gbc[:, :], in_=din["rowg"][0:1, 0:2048].partition_broadcast(128), writes=["gb"])
        kb.dma("sp", out=bbc[:, :], in_=din["rowg"][0:1, 2048:4096].partition_broadcast(128), writes=["gb"])
        xr = Ring(kb, "xin", 4, [128, 2048], F32)
        hr = Ring(kb, "hout", 2, [128, 2048], F32)
        rings = dict(hb=Ring(kb, "hb", 2, [128, 2048], BF16), fm=Ring(kb, "fm", 2, [128, 16, 128], BF16),
                     pt=Ring(kb, "ptb", 4, [128, 512], BF16, psum=True))
        loaded = {}
        stat = {}
        for i in range(-2, NT):
            tl = i + 2
            if tl < NT:
                xt, xtag = xr.next()
                kb.dma("sp", out=xt[:, :], in_=din["x"][tl * 128:(tl + 1) * 128, :], writes=[xtag])
                loaded[tl] = (xt, xtag)
            ta = i + 1
            if 0 <= ta < NT:
                xt, xtag = loaded[ta]
                stat[ta] = ln_stats(xt[:, :], xtag)
            if i >= 0:
                pxt, pxtag = loaded.pop(i)
                ht, htag = hr.next()
                ln_apply(pxt[:, :], pxtag, ht[:, :], htag, gbc[:, :], bbc[:, :], "gb", tg=stat.pop(i))
                emit_h_tile(i, ht[:, :], htag, rings, False)
        kb.phase_end()
        if finish_check("ln_in"):
            return nc, kb

        for l in range(NL):
            build_layer(nc, kb, din, hnd, out_d, l, finish_check, dict(
                HTM=HTM, HFM=HFM, PROJ=PROJ, DTR=DTR, VATM=VATM, ZSTM=ZSTM, YBR=YBR, RV=RV, MG=MG, UD=UD, SLD=SLD, WTD=WTD, H1B=H1B, LGT=LGT, XROWS=XROWS, YROWS=YROWS),
                dict(cstf=cstf, cstb=cstb), ln_alloc, ln_apply, emit_h_tile)
            if done[0]:
                return nc, kb
    return nc, kb


def attention(kb, nheads, kT_ap, qT_ap, v_ap, kcs_for, e_info, scale, yst, prefix, reads_k, reads_q, reads_v, pre_head=None, srr=None, per_unit=None):
    if srr is None:
        srr = Ring(kb, prefix + "S", 4, [128, 512], F32, psum=True)
    accr = Ring(kb, prefix + "acc", 3, [128, 512], F32, psum=True)
    ptr = Ring(kb, prefix + "pt", 4, [128, 512], BF16)
    pmr = Ring(kb, prefix + "pm", 4, [128, 512], BF16) if e_info is not None else None
    rden = kb.sb(prefix + "rden", [128, 512], F32)
    units = []
    for h in range(nheads):
        for qb in range(4):
            kcs = kcs_for(qb)
            for i, kc in enumerate(kcs):
                units.append((h, qb, kc, i == 0, i == len(kcs) - 1))
    state = {}
    cur_acc = {}
    seen_heads = set()

    def stage1(u):
        h, qb, kc, first, last = u
        if pre_head is not None and h not in seen_heads:
            seen_heads.add(h)
            pre_head(h)
        sp_, spt = srr.next()
        kb.mm(sp_[:, :], kT_ap(h, kc), qT_ap(h, qb), True, True, reads=[reads_k(h), reads_q(h)], writes=[spt])
        pt_, ptt = ptr.next()
        kb.op("act", "activation", reads=[spt], writes=[ptt], out=pt_[:, :], in_=sp_[:, :], func=AF.Exp, scale=scale)
        if e_info is not None:
            eap, etok = e_info(h, qb, kc)
            pm_, pmt = pmr.next()
            kb.op("dve", "tensor_tensor", reads=[ptt, etok], writes=[pmt], out=pm_[:, :], in0=pt_[:, :], in1=eap, op=ALU.mult)
            state[u] = (pm_, pmt)
        else:
            state[u] = (pt_, ptt)

    def stage2(u):
        h, qb, kc, first, last = u
        pm_, pmt = state.pop(u)
        if first:
            cur_acc[0] = accr.next()
        acc, acct = cur_acc[0]
        kb.mm(acc[:, :], v_ap(h, kc), pm_[:, :], first, last, reads=[reads_v(h), pmt], writes=[acct])
        if last:
            po = (h % 2) * 64
            do = 64 - po
            kb.op("dve", "reciprocal", reads=[acct], writes=[prefix + "rden"], out=rden[do:do + 64, :], in_=acc[do:do + 64, :])
            kb.op("dve", "tensor_tensor", reads=[acct, prefix + "rden"], writes=[("yst", h, qb)],
                  out=yst[po:po + 64, h // 2, qb * 512:(qb + 1) * 512], in0=acc[po:po + 64, :], in1=rden[do:do + 64, :], op=ALU.mult)

    LAG = 3
    for idx in range(len(units) + LAG):
        if idx < len(units):
            stage1(units[idx])
            if per_unit is not None:
                per_unit(idx)
        if idx - LAG >= 0:
            stage2(units[idx - LAG])


def build_vaug(kb, vaug, vsrc, reads, wtok):
    kb.op("pool", "memset", writes=[wtok + "ones"], ap=vaug[:, :, :, :], constant=1.0)
    v5 = vsrc.rearrange("p k (hp two e) -> p k hp two e", two=2, e=64)
    a5 = vaug.rearrange("p k (hp two) e -> p k hp two e", two=2)
    kb.op("dve", "tensor_copy", reads=reads + [wtok + "ones"], writes=[wtok + "e"], out=a5[:, :, :, 0, 0:64], in_=v5[:, :, :, 0, :])
    kb.op("pool", "tensor_copy", reads=reads + [wtok + "ones"], writes=[wtok + "o"], out=a5[:, :, :, 1, 64:128], in_=v5[:, :, :, 1, :])
    kb.op("pool", "engine_nop", reads=[wtok + "e", wtok + "o"], writes=[wtok])


def moe_experts(kb, l, din, XROWS, YROWS, ident_b, nexp):
    wgr = Ring(kb, "weg", 2, [128, 16, 512], BF16)
    wur = Ring(kb, "weu", 2, [128, 16, 512], BF16)
    wdr = Ring(kb, "wed", 2, [128, 4, 2048], BF16)
    xrr = Ring(kb, "xrE", 4, [128, 2048], BF16)
    xTr = Ring(kb, "xTE", 2, [128, 16, 256], BF16)
    aTr = Ring(kb, "aTE", 2, [128, 4, 256], BF16)
    sgr = Ring(kb, "sgE", 2, [128, 256], F32)
    yrr = Ring(kb, "yrE", 2, [128, 2048], F32)
    ptxr = Ring(kb, "ptx", 2, [128, 512], BF16, psum=True)
    pgur = Ring(kb, "pgu", 3, [128, 512], F32, psum=True)
    pyr = Ring(kb, "pyE", 3, [128, 512], F32, psum=True)
    ke = 0
    for e in range(nexp):
        wg, wgt = wgr.next()
        wu, wut = wur.next()
        wd, wdt = wdr.next()
        kb.dma("pool", out=wg[:, :, :], in_=din["w_e_gate"][l, e].rearrange("(kc p) n -> p kc n", p=128), writes=[wgt])
        kb.dma("pool", out=wu[:, :, :], in_=din["w_e_up"][l, e].rearrange("(kc p) n -> p kc n", p=128), writes=[wut])
        for nb in range(4):
            kb.dma("pool", out=wd[:, :, nb * 512:(nb + 1) * 512], in_=din["w_e_down"][l, e].rearrange("(kc p) n -> p kc n", p=128)[:, :, nb * 512:(nb + 1) * 512], writes=[wdt])
        xT, xTt = xTr.next()
        for s2 in range(2):
            xr, xrt = xrr.next()
            r0 = e * CAP + s2 * 128
            kb.dma("sp", out=xr[:, :], in_=XROWS[r0:r0 + 128, :], writes=[xrt])
            for g in range(4):
                pt, ptt = ptxr.next()
                for j in range(4):
                    c = g * 4 + j
                    kb.op("pe", "transpose", reads=[xrt, "cstb"], writes=[ptt], out=pt[:, j * 128:(j + 1) * 128], in_=xr[:, c * 128:(c + 1) * 128], identity=ident_b)
                ke += 1
                if ke % 2 == 0:
                    kb.op("dve", "tensor_copy", reads=[ptt], writes=[(xTt, s2, g)], out=xT[:, g * 4:(g + 1) * 4, s2 * 128:(s2 + 1) * 128], in_=pt[:, :].rearrange("p (a b) -> p a b", a=4))
                else:
                    kb.op("act", "activation", reads=[ptt], writes=[(xTt, s2, g)], out=xT[:, g * 4:(g + 1) * 4, s2 * 128:(s2 + 1) * 128], in_=pt[:, :].rearrange("p (a b) -> p a b", a=4), func=AF.Copy)
        xdeps = [(xTt, s2, g) for s2 in range(2) for g in range(4)]
        aT, aTt = aTr.next()
        for fcn in range(4):
            bank, bkt = pgur.next()
            for kc in range(16):
                kb.mm(bank[:, 0:256], wg[:, kc, fcn * 128:(fcn + 1) * 128], xT[:, kc, :], kc == 0, kc == 15, reads=[wgt] + xdeps, writes=[bkt])
            for kc in range(16):
                kb.mm(bank[:, 256:512], wu[:, kc, fcn * 128:(fcn + 1) * 128], xT[:, kc, :], kc == 0, kc == 15, reads=[wut] + xdeps, writes=[bkt])
            sg, sgt = sgr.next()
            kb.op("act", "activation", reads=[bkt], writes=[sgt], out=sg[:, :], in_=bank[:, 0:256], func=AF.Silu)
            kb.op("dve", "tensor_tensor", reads=[sgt, bkt], writes=[(aTt, fcn)], out=aT[:, fcn, :], in0=bank[:, 256:512], in1=sg[:, :], op=ALU.mult)
        for s2 in range(2):
            yr, yrt = yrr.next()
            for nb in range(4):
                py, pyt = pyr.next()
                for kc in range(4):
                    kb.mm(py[:, :], aT[:, kc, s2 * 128:(s2 + 1) * 128], wd[:, kc, nb * 512:(nb + 1) * 512], kc == 0, kc == 3, reads=[wdt] + [(aTt, f) for f in range(4)], writes=[pyt])
                ke += 1
                if ke % 2 == 0:
                    kb.op("dve", "tensor_copy", reads=[pyt], writes=[(yrt, nb)], out=yr[:, nb * 512:(nb + 1) * 512], in_=py[:, :])
                else:
                    kb.op("act", "activation", reads=[pyt], writes=[(yrt, nb)], out=yr[:, nb * 512:(nb + 1) * 512], in_=py[:, :], func=AF.Copy)
            r0 = e * CAP + s2 * 128
            kb.dma("sp", out=YROWS[r0:r0 + 128, :], in_=yr[:, :], reads=[(yrt, nb) for nb in range(4)], writes=[("YR", e, s2)])


def build_layer(nc, kb, din, hnd, out_d, l, finish_check, SC, CST, ln_alloc, ln_apply, emit_h_tile):
    HTM, HFM, PROJ, DTR, VATM, ZSTM, YBR, RV, MG, H1B, LGT, XROWS, YROWS = [SC[k] for k in
        ("HTM", "HFM", "PROJ", "DTR", "VATM", "ZSTM", "YBR", "RV", "MG", "H1B", "LGT", "XROWS", "YROWS")]
    cstf, cstb = CST["cstf"], CST["cstb"]
    ident_b = cstb[:, 0, :]
    ident_f = cstf[:, 0, :]
    J_b = cstb[:, 1, :]
    U_f = cstf[:, 2, :]
    Lo_f = cstf[:, 3, :]
    ones_b = cstb[:, 5, :]
    ones_f = cstf[:, 5, :]

    kb.phase_begin()
    hfm = kb.sb("hfm", [128, 16, S], BF16)
    for tb in range(4):
        kb.dma("sp", out=hfm[:, :, tb * 512:(tb + 1) * 512], in_=HFM[:, :, tb * 512:(tb + 1) * 512], writes=[("hfm", tb)])
    wr = Ring(kb, "wpin", 4, [128, 16, 128], BF16)
    pr = Ring(kb, "pp", 6, [128, 512], F32, psum=True)
    sr = Ring(kb, "pstage", 3, [128, S], BF16)
    k = 0
    for c in range(31):
        w, wt = wr.next()
        kb.dma("pool", out=w[:, :, :], in_=din["w_in_t"][l, c], writes=[wt])
        stg, stt = sr.next()
        for tb in range(4):
            p, pt = pr.next()
            for kc in range(16):
                kb.mm(p[:, :], w[:, kc, :], hfm[:, kc, tb * 512:(tb + 1) * 512], kc == 0, kc == 15, reads=[wt, ("hfm", tb)], writes=[pt])
            k += 1
            if k % 2 == 0:
                kb.op("act", "activation", reads=[pt], writes=[(stt, tb)], out=stg[:, tb * 512:(tb + 1) * 512], in_=p[:, :], func=AF.Copy)
            else:
                kb.op("dve", "tensor_copy", reads=[pt], writes=[(stt, tb)], out=stg[:, tb * 512:(tb + 1) * 512], in_=p[:, :])
        kb.dma("sp", out=PROJ[c], in_=stg[:, :], reads=[(stt, tb) for tb in range(4)], writes=[("PROJ", c)])
    wtm = Ring(kb, "wtm", 2, [128, 16, 512], BF16)
    tmst = kb.sb("tmstage", [128, 16, 512], BF16)
    for b in range(2):
        w, wt = wtm.next()
        kb.dma("pool", out=w[:, :, :], in_=din["w_in_tm"][l, b], writes=[wt])
        for t in range(NT):
            p, pt = pr.next()
            for kc in range(16):
                kb.mm(p[:, :], hfm[:, kc, t * 128:(t + 1) * 128], w[:, kc, :], kc == 0, kc == 15, reads=[wt, ("hfm", t // 4)], writes=[pt])
            if b == 1:
                kb.op("act", "activation", reads=[pt], writes=[("tmst", t)], out=tmst[:, t, :], in_=p[:, :], func=AF.Silu)
            elif t % 2 == 0:
                kb.op("act", "activation", reads=[pt], writes=[("tmst", t)], out=tmst[:, t, :], in_=p[:, :], func=AF.Copy)
            else:
                kb.op("dve", "tensor_copy", reads=[pt], writes=[("tmst", t)], out=tmst[:, t, :], in_=p[:, :])
        dst = VATM if b == 0 else ZSTM
        kb.dma("sp", out=dst.rearrange("(t p) n -> p t n", p=128), in_=tmst[:, :, :], reads=[("tmst", t) for t in range(NT)], writes=["TMD%d" % b])
    wdt = kb.sb("wdt", [128, 16, 16], BF16)
    dtst = kb.sb("dtst", [128, 16, 16], F32)
    kb.dma("pool", out=wdt[:, :, :], in_=din["w_dt_tm"][l], writes=["wdt"])
    for t in range(NT):
        p, pt = pr.next()
        for kc in range(16):
            kb.mm(p[:, 0:16], hfm[:, kc, t * 128:(t + 1) * 128], wdt[:, kc, :], kc == 0, kc == 15, reads=["wdt", ("hfm", t // 4)], writes=[pt])
        kb.op("dve", "tensor_copy", reads=[pt], writes=[("dtst", t)], out=dtst[:, t, :], in_=p[:, 0:16])
    kb.dma("sp", out=DTR.rearrange("(t p) n -> p t n", p=128), in_=dtst[:, :, :], reads=[("dtst", t) for t in range(NT)], writes=["DTR"])
    kb.phase_end()
    if finish_check("P%d" % l):
        return

    kb.phase_begin()
    qT = kb.sb("qT", [128, 4, S], BF16)
    kT = kb.sb("kT", [128, 4, S], BF16)
    vtm = kb.sb("vtm", [128, 16, 512], BF16)
    yst = kb.sb("yst", [128, 4, S], BF16)
    for c in range(4):
        kb.dma("sp", out=qT[:, c, :], in_=PROJ[CH["qa"] + c], writes=[("qT", c)])
        kb.dma("sp", out=kT[:, c, :], in_=PROJ[CH["ka"] + c], writes=[("kT", c)])
    kb.dma("sp", out=vtm[:, :, :], in_=VATM.rearrange("(t p) n -> p t n", p=128), writes=["vtm"])
    vaug = kb.sb("vaugA", [128, 16, 8, 128], BF16)
    build_vaug(kb, vaug[:, :, :, :], vtm[:, :, :], ["vtm"], "vaug")
    DEL = [d for d in range(-1536, 1921, 128) if -1151 <= d <= 1535]
    Et = kb.sb("Et", [128, 2, len(DEL), 512], BF16)
    hkr = Ring(kb, "hk", 4, [128, 512], BF16)
    pj = kb.ps("pj", [128, 512], F32)

    def E_dma(h, di):
        hk, hkt = hkr.next()
        base = 1408 - DEL[di]
        kb.dma("pool", out=hk[:, :], in_=bass.AP(hnd["RV"], h * RVLEN + base, [[1, 128], [1, 512]]), writes=[hkt])
        return (h, di, hk, hkt)

    def E_mm(h, di, hk, hkt):
        kb.mm(pj[:, :], J_b, hk[:, :], True, True, reads=[hkt, "cstb"], writes=["pj"])
        if di % 2 == 0:
            kb.op("act", "activation", reads=["pj"], writes=[("E", h % 2, di)], out=Et[:, h % 2, di, :], in_=pj[:, :], func=AF.Copy)
        else:
            kb.op("dve", "tensor_copy", reads=["pj"], writes=[("E", h % 2, di)], out=Et[:, h % 2, di, :], in_=pj[:, :])

    pendingE = []
    inflight = []

    def pre_head(h):
        if h == 0:
            q = []
            for di in range(len(DEL)):
                q.append(E_dma(0, di))
                if len(q) > 2:
                    E_mm(*q.pop(0))
            while q:
                E_mm(*q.pop(0))
        while inflight:
            E_mm(*inflight.pop(0))
        while pendingE:
            E_mm(*E_dma(*pendingE.pop(0)))
        if h + 1 < 8:
            pendingE.extend((h + 1, di) for di in range(len(DEL)))

    def per_unit(i):
        if i % 2 == 1:
            if len(inflight) >= 2 or (inflight and not pendingE):
                E_mm(*inflight.pop(0))
            if pendingE:
                inflight.append(E_dma(*pendingE.pop(0)))

    def e_info(h, qb, kc):
        di = DEL.index(128 * kc - 512 * qb)
        return Et[:, h % 2, di, :], ("E", h % 2, di)

    attention(kb, 8,
              kT_ap=lambda h, kc: kT[(h % 2) * 64:(h % 2) * 64 + 64, h // 2, kc * 128:(kc + 1) * 128],
              qT_ap=lambda h, qb: qT[(h % 2) * 64:(h % 2) * 64 + 64, h // 2, qb * 512:(qb + 1) * 512],
              v_ap=lambda h, kc: vaug[:, kc, h, :],
              kcs_for=lambda qb: [kc for kc in range(16) if -1151 <= 128 * kc - 512 * qb <= 1535],
              e_info=e_info, scale=0.125, yst=yst, prefix="A",
              reads_k=lambda h: ("kT", h // 2), reads_q=lambda h: ("qT", h // 2), reads_v=lambda h: "vaug", pre_head=pre_head, per_unit=per_unit)
    kb.dma("sp", out=YBR[0], in_=yst[:, :, :], reads=[("yst", h, qb) for h in range(8) for qb in range(4)], writes=[("YBR", 0)])
    kb.phase_end()
    if finish_check("A%d" % l):
        return

    kb.phase_begin()
    cp = kb.sb("cp", [128, 256], F32)
    kb.dma("sp", out=cp[:, :], in_=din["colp"][l], writes=["cp"])
    wuq = kb.sb("wuq", [128, 4, 1024], BF16)
    wukv = kb.sb("wukv", [128, 2, 1024], BF16)
    kb.dma("pool", out=wuq[:, :, :], in_=din["w_uq_t"][l], writes=["wuq"])
    kb.dma("pool", out=wukv[:, :, :], in_=din["w_ukv_t"][l], writes=["wukv"])
    ccr = Ring(kb, "cc", 2, [96, 512], F32)
    ssr = Ring(kb, "ss", 2, [96, 512], F32)
    krAr = Ring(kb, "krA", 2, [96, 512], BF16)
    krBr = Ring(kb, "krB", 2, [96, 512], BF16)
    QT = kb.sb("QT", [96, 8, S], BF16)
    KT = kb.sb("KT", [96, 8, S], BF16)
    vaug = kb.sb("vaugB", [128, 16, 8, 128], BF16)
    vtm = kb.sb("vtmB", [128, 16, 512], BF16)
    yst = kb.sb("ystB", [128, 4, S], BF16)
    cqr = Ring(kb, "cqb", 2, [128, 4, 512], BF16)
    ckr = Ring(kb, "ckb", 2, [128, 2, 512], BF16)
    sqb = kb.sb("sqb", [128, 4, 512], BF16)
    cqn = kb.sb("cqn", [128, 4, 512], BF16)
    ckvn = kb.sb("ckvn", [128, 2, 512], BF16)
    rt = kb.sb("rt", [128, 512], F32)
    t1 = kb.sb("t1", [96, 512], F32)
    t2 = kb.sb("t2", [96, 512], F32)
    krope = kb.sb("krope", [96, 512], BF16)
    pss = kb.ps("pss", [128, 512], F32)
    srrB = Ring(kb, "BS", 4, [128, 512], F32, psum=True)
    pqr = Ring.wrap(srrB.tiles[0:2], srrB.names[0:2])
    pq2r = Ring.wrap(srrB.tiles[2:3], srrB.names[2:3])

    def rms_block(src, stag, nch, gcol, dst, dtag, inv_n):
        kb.op("act", "activation", reads=[stag], writes=["sqb"], out=sqb[:, 0:nch, :], in_=src[:, 0:nch, :], func=AF.Square)
        for c in range(nch):
            kb.mm(pss[:, :], ones_b, sqb[:, c, :], c == 0, c == nch - 1, reads=["sqb", "cstb"], writes=["pss"])
        kb.op("dve", "tensor_scalar", reads=["pss"], writes=["rt"], out=rt[:, :], in0=pss[:, :], scalar1=inv_n, scalar2=EPS, op0=ALU.mult, op1=ALU.add)
        kb.op("act", "activation", reads=["rt"], writes=["rt"], out=rt[:, :], in_=rt[:, :], func=AF.Sqrt)
        kb.op("dve", "reciprocal", reads=["rt"], writes=["rt"], out=rt[:, :], in_=rt[:, :])
        for c in range(nch):
            kb.op("dve", "scalar_tensor_tensor", reads=[stag, "rt", "cp"], writes=[dtag], out=dst[:, c, :], in0=src[:, c, :],
                  scalar=cp[:, gcol + c:gcol + c + 1], in1=rt[:, :], op0=ALU.mult, op1=ALU.mult)

    for tb in range(4):
        tbs = slice(tb * 512, (tb + 1) * 512)
        cqb, cqt = cqr.next()
        ckb, ckt = ckr.next()
        cc, cct = ccr.next()
        ss, sst = ssr.next()
        krA, krAt = krAr.next()
        krB, krBt = krBr.next()
        kb.dma("sp", out=cc[64:96, :], in_=din["rope"][0, 64:96, tbs], writes=[cct])
        kb.dma("sp", out=ss[64:96, :], in_=din["rope"][1, 64:96, tbs], writes=[sst])
        kb.dma("sp", out=krA[64:96, :], in_=PROJ[CH["krx"], 0:32, tbs], writes=[krAt])
        kb.dma("sp", out=krB[64:96, :], in_=PROJ[CH["krx"], 32:64, tbs], writes=[krBt])
        for c in range(4):
            kb.dma("sp", out=cqb[:, c, :], in_=PROJ[CH["cq"] + c, :, tbs], writes=[cqt])
        for c in range(2):
            kb.dma("sp", out=ckb[:, c, :], in_=PROJ[CH["ckv"] + c, :, tbs], writes=[ckt])
        rms_block(cqb, cqt, 4, 64, cqn, "cqn", 1.0 / 512)
        for h in range(8):
            pq, pqt = pqr.next()
            pq2, pq2t = pq2r.next()
            for kc in range(4):
                kb.mm(pq[0:96, :], wuq[:, kc, 96 * h:96 * h + 96], cqn[:, kc, :], kc == 0, kc == 3, reads=["wuq", "cqn"], writes=[pqt])
            for kc in range(4):
                kb.mm(pq2[64:96, :], wuq[:, kc, 768 + 32 * h:768 + 32 * h + 32], cqn[:, kc, :], kc == 0, kc == 3, reads=["wuq", "cqn"], writes=[pq2t])
            kb.op("act", "activation", reads=[pqt], writes=[("QT", h, tb)], out=QT[0:64, h, tbs], in_=pq[0:64, :], func=AF.Copy)
            kb.op("dve", "tensor_tensor", reads=[pqt, cct], writes=["t1"], out=t1[64:96, :], in0=pq[64:96, :], in1=cc[64:96, :], op=ALU.mult)
            kb.op("dve", "tensor_tensor", reads=[pq2t, sst], writes=["t2"], out=t2[64:96, :], in0=pq2[64:96, :], in1=ss[64:96, :], op=ALU.mult)
            kb.op("pool", "tensor_tensor", reads=["t1", "t2"], writes=[("QT", h, tb)], out=QT[64:96, h, tbs], in0=t1[64:96, :], in1=t2[64:96, :], op=ALU.add)
        rms_block(ckb, ckt, 2, 68, ckvn, "ckvn", 1.0 / 256)
        for h in range(8):
            pq, pqt = pqr.next()
            for kc in range(2):
                kb.mm(pq[0:64, :], wukv[:, kc, 64 * h:64 * h + 64], ckvn[:, kc, :], kc == 0, kc == 1, reads=["wukv", "ckvn"], writes=[pqt])
            if h % 2 == 0:
                kb.op("act", "activation", reads=[pqt], writes=[("KT", h, tb)], out=KT[0:64, h, tbs], in_=pq[0:64, :], func=AF.Copy)
            else:
                kb.op("dve", "tensor_copy", reads=[pqt], writes=[("KT", h, tb)], out=KT[0:64, h, tbs], in_=pq[0:64, :])
        kb.op("dve", "tensor_tensor", reads=[krAt, cct], writes=["t1"], out=t1[64:96, :], in0=krA[64:96, :], in1=cc[64:96, :], op=ALU.mult)
        kb.op("dve", "tensor_tensor", reads=[krBt, sst], writes=["t2"], out=t2[64:96, :], in0=krB[64:96, :], in1=ss[64:96, :], op=ALU.mult)
        kb.op("pool", "tensor_tensor", reads=["t1", "t2"], writes=["krope"], out=krope[64:96, :], in0=t1[64:96, :], in1=t2[64:96, :], op=ALU.add)
        for h in range(8):
            kb.op("pool", "tensor_copy", reads=["krope"], writes=[("KT", h, tb)], out=KT[64:96, h, tbs], in_=krope[64:96, :])
        for tt in range(4):
            pq, pqt = pqr.next()
            for kc in range(2):
                kb.mm(pq[:, :], ckvn[:, kc, tt * 128:(tt + 1) * 128], wukv[:, kc, 512:1024], kc == 0, kc == 1, reads=["wukv", "ckvn"], writes=[pqt])
            if tt % 2 == 0:
                kb.op("act", "activation", reads=[pqt], writes=[("vtm", tb, tt)], out=vtm[:, tb * 4 + tt, :], in_=pq[:, :], func=AF.Copy)
            else:
                kb.op("dve", "tensor_copy", reads=[pqt], writes=[("vtm", tb, tt)], out=vtm[:, tb * 4 + tt, :], in_=pq[:, :])

    build_vaug(kb, vaug[:, :, :, :], vtm[:, :, :], [("vtm", tb, tt) for tb in range(4) for tt in range(4)], "vaugB")
    attention(kb, 8,
              kT_ap=lambda h, kc: KT[0:96, h, kc * 128:(kc + 1) * 128],
              qT_ap=lambda h, qb: QT[0:96, h, qb * 512:(qb + 1) * 512],
              v_ap=lambda h, kc: vaug[:, kc, h, :],
              kcs_for=lambda qb: list(range(16)),
              e_info=None, scale=96.0 ** -0.5, yst=yst, prefix="B",
              reads_k=lambda h: ("KTall", h), reads_q=lambda h: ("QTall", h), reads_v=lambda h: "vtmall",
              pre_head=lambda h: [kb.op("pool", "engine_nop", reads=[("KT", h, tb) for tb in range(4)] + [("QT", h, tb) for tb in range(4)] + ["vaugB"],
                                        writes=[("KTall", h), ("QTall", h), "vtmall"])], srr=srrB)
    kb.dma("sp", out=YBR[1], in_=yst[:, :, :], reads=[("yst", h, qb) for h in range(8) for qb in range(4)], writes=[("YBR", 1)])
    kb.phase_end()
    if finish_check("B%d" % l):
        return

    kb.phase_begin()
    cp = kb.sb("cpC", [128, 256], F32)
    kb.dma("sp", out=cp[:, :], in_=din["colp"][l], writes=["cp"])
    hg = kb.sb("hg", [128, 4, S + 30], BF16)
    kb.op("pool", "memset", writes=["hgpad"], ap=hg[:, :, 0:15], constant=0.0)
    kb.op("pool", "memset", writes=["hgpad"], ap=hg[:, :, S + 15:S + 30], constant=0.0)
    gar = Ring(kb, "ga", 2, [128, S], BF16)
    ggr = Ring(kb, "gg", 2, [128, S], BF16)
    for c in range(4):
        ga, gat = gar.next()
        gg, ggt = ggr.next()
        kb.dma("sp", out=ga[:, :], in_=PROJ[CH["glua"] + c], writes=[gat])
        kb.dma("sp", out=gg[:, :], in_=PROJ[CH["glug"] + c], writes=[ggt])
        kb.op("act", "activation", reads=[ggt], writes=[ggt], out=gg[:, :], in_=gg[:, :], func=AF.Sigmoid)
        kb.op("dve", "tensor_tensor", reads=[gat, ggt, "hgpad"], writes=[("hg", c)], out=hg[:, c, 15:15 + S], in0=ga[:, :], in1=gg[:, :], op=ALU.mult)
    dg = kb.sb("dg", [128, 4, 31, 128], BF16)
    for c in range(4):
        for j in range(31):
            eng = "dve" if (c * 31 + j) % 2 == 0 else "pool"
            kb.op(eng, "tensor_scalar", reads=["cp", "cstb"], writes=[("dg", c)], out=dg[:, c, j, :], in0=ident_b,
                  scalar1=cp[:, 82 + c * 31 + j:83 + c * 31 + j], scalar2=None, op0=ALU.mult)
    yc = kb.sb("yc", [128, 4, 512], F32)
    ysq = kb.sb("ysq", [128, 4, 512], F32)
    mt = kb.sb("mt", [128, 512], F32)
    m2 = kb.sb("m2", [128, 512], F32)
    vt = kb.sb("vt", [128, 512], F32)
    tmr = Ring(kb, "tmpC", 2, [128, 512], F32)
    yo = kb.sb("yoC", [128, 4, S], BF16)
    pcr = Ring(kb, "pc", 3, [128, 512], F32, psum=True)
    psm = kb.ps("psm", [128, 512], F32)
    psq = kb.ps("psq", [128, 512], F32)
    for tb in range(4):
        tbs = slice(tb * 512, (tb + 1) * 512)
        for c in range(4):
            pc, pct = pcr.next()
            for j in range(31):
                kb.mm(pc[:, :], dg[:, c, j, :], hg[:, c, tb * 512 + j:tb * 512 + j + 512], j == 0, j == 30, reads=[("dg", c), ("hg", c)], writes=[pct])
            kb.op("act", "activation", reads=[pct, "cp"], writes=[("yc", c)], out=yc[:, c, :], in_=pc[:, :], func=AF.Identity, bias=cp[:, 70 + c:71 + c], scale=1.0)
            kb.op("act", "activation", reads=[("yc", c)], writes=[("ysq", c)], out=ysq[:, c, :], in_=yc[:, c, :], func=AF.Square)
        for c in range(4):
            kb.mm(psm[:, :], ones_f, yc[:, c, :], c == 0, c == 3, reads=[("yc", c), "cstf"], writes=["psm"])
        for c in range(4):
            kb.mm(psq[:, :], ones_f, ysq[:, c, :], c == 0, c == 3, reads=[("ysq", c), "cstf"], writes=["psq"])
        kb.op("dve", "tensor_scalar", reads=["psm"], writes=["mt"], out=mt[:, :], in0=psm[:, :], scalar1=1.0 / 512, scalar2=None, op0=ALU.mult)
        kb.op("dve", "tensor_tensor", reads=["mt"], writes=["m2"], out=m2[:, :], in0=mt[:, :], in1=mt[:, :], op=ALU.mult)
        kb.op("dve", "scalar_tensor_tensor", reads=["psq", "m2"], writes=["vt"], out=vt[:, :], in0=psq[:, :], scalar=1.0 / 512, in1=m2[:, :], op0=ALU.mult, op1=ALU.subtract)
        kb.op("dve", "tensor_scalar", reads=["vt"], writes=["vt"], out=vt[:, :], in0=vt[:, :], scalar1=EPS, scalar2=None, op0=ALU.add)
        kb.op("act", "activation", reads=["vt"], writes=["vt"], out=vt[:, :], in_=vt[:, :], func=AF.Sqrt)
        kb.op("dve", "reciprocal", reads=["vt"], writes=["vt"], out=vt[:, :], in_=vt[:, :])
        for c in range(4):
            tm, tmt = tmr.next()
            kb.op("dve", "tensor_tensor", reads=[("yc", c), "mt"], writes=[tmt], out=tm[:, :], in0=yc[:, c, :], in1=mt[:, :], op=ALU.subtract)
            kb.op("pool", "tensor_tensor", reads=[tmt, "vt"], writes=[tmt], out=tm[:, :], in0=tm[:, :], in1=vt[:, :], op=ALU.mult)
            kb.op("act", "activation", reads=[tmt, "cp"], writes=[("yoC", c, tb)], out=yo[:, c, tbs], in_=tm[:, :], func=AF.Silu,
                  scale=cp[:, 74 + c:75 + c], bias=cp[:, 78 + c:79 + c])
    kb.dma("sp", out=YBR[2], in_=yo[:, :, :], reads=[("yoC", c, tb) for c in range(4) for tb in range(4)], writes=[("YBR", 2)])
    kb.phase_end()
    if finish_check("C%d" % l):
        return

    kb.phase_begin()
    cp = kb.sb("cpD", [128, 256], F32)
    kb.dma("sp", out=cp[:, :], in_=din["colp"][l], writes=["cp"])
    rp = kb.sb("rpD", [128, 640], F32)
    kb.dma("sp", out=rp[:, :], in_=din["rowp"][l, 0:1, 8192:8832].partition_broadcast(128), writes=["rp"])
    gnd = rp[:, 0:512]
    dsk = rp[:, 548:556]
    pbig = kb.ps("pbig", [128, 1024], F32)
    pA = kb.ps("pA", [128, 512], F32)
    pB = kb.ps("pB", [128, 512], F32)
    pC = kb.ps("pC", [128, 512], F32)
    pD = kb.ps("pD", [128, 512], F32)
    pTr = Ring(kb, "pT", 2, [128, 512], BF16, psum=True)
    convr = Ring.wrap([pA, pB, pC], ["pA", "pB", "pC"])
    dgd = kb.sb("dgd", [128, 8, 5, 128], BF16)
    for c in range(8):
        for j in range(5):
            eng = "dve" if (c * 5 + j) % 2 == 0 else "pool"
            kb.op(eng, "tensor_scalar", reads=["cp", "cstb"], writes=[("dgd", c)], out=dgd[:, c, j, :], in0=ident_b,
                  scalar1=cp[:, 206 + c * 5 + j:207 + c * 5 + j], scalar2=None, op0=ALU.mult)
    xbc = kb.sb("xbc", [128, 8, S], BF16)
    xinr = Ring(kb, "xinD", 2, [128, S + 4], BF16)
    for i in range(2):
        kb.op("pool", "memset", writes=[xinr.names[i] + "pad"], ap=xinr.tiles[i][:, 0:2], constant=0.0)
        kb.op("pool", "memset", writes=[xinr.names[i] + "pad"], ap=xinr.tiles[i][:, S + 2:S + 4], constant=0.0)
    for c in range(8):
        xi, xit = xinr.next()
        kb.dma("sp", out=xi[:, 2:2 + S], in_=PROJ[CH["xs"] + c], reads=[xit + "pad"], writes=[xit])
        for tb in range(4):
            pc, pct = convr.next()
            for j in range(5):
                kb.mm(pc[:, :], dgd[:, c, j, :], xi[:, tb * 512 + j:tb * 512 + j + 512], j == 0, j == 4, reads=[("dgd", c), xit, xit + "pad"], writes=[pct])
            kb.op("act", "activation", reads=[pct, "cp"], writes=[("xbc", c)], out=xbc[:, c, tb * 512:(tb + 1) * 512], in_=pc[:, :], func=AF.Silu,
                  bias=cp[:, 246 + c:247 + c], scale=1.0)
    xtm = kb.sb("xtm", [128, 16, 512], BF16)
    btm = kb.sb("btm", [128, 16, 256], BF16)
    for t in range(NT):
        pt, ptt = pTr.next()
        for c in range(4):
            kb.op("pe", "transpose", reads=[("xbc", c), "cstb"], writes=[ptt], out=pt[:, c * 128:(c + 1) * 128], in_=xbc[:, c, t * 128:(t + 1) * 128], identity=ident_b)
        kb.op("dve", "tensor_copy", reads=[ptt], writes=[("xtm", t)], out=xtm[:, t, :], in_=pt[:, :])
        pt, ptt = pTr.next()
        for g in range(2):
            kb.op("pe", "transpose", reads=[("xbc", 4 + g), "cstb"], writes=[ptt], out=pt[:, g * 128:(g + 1) * 128], in_=xbc[:, 4 + g, t * 128:(t + 1) * 128], identity=ident_b)
        kb.op("act", "activation", reads=[ptt], writes=[("btm", t)], out=btm[:, t, :], in_=pt[:, 0:256], func=AF.Copy)
    def small(name):
        return kb.sb(name, [128, 16, 16], F32)
    dtr, dtv, av, Ecs, Tot, negE, wend, expE, dec = [small(n) for n in ("dtr", "dtv", "av", "Ecs", "Tot", "negE", "wend", "expE", "dec")]
    negA = kb.sb("negA", [128, 16], F32)
    kb.dma("sp", out=dtr[:, :, :], in_=DTR.rearrange("(t p) n -> p t n", p=128), writes=["dtr"])
    kb.op("dve", "tensor_tensor", reads=["dtr", "rp"], writes=["dtv"], out=dtv[:, :, :], in0=dtr[:, :, :],
          in1=rp[:, 556:572].unsqueeze(1).to_broadcast([128, 16, 16]), op=ALU.add)
    kb.op("act", "activation", reads=["dtv"], writes=["dtv"], out=dtv[:, :, :], in_=dtv[:, :, :], func=AF.Exp)
    kb.op("act", "activation", reads=["dtv"], writes=["dtv"], out=dtv[:, :, :], in_=dtv[:, :, :], func=AF.Ln, bias=1.0, scale=1.0)
    kb.op("act", "activation", reads=["rp"], writes=["negA"], out=negA[:, :], in_=rp[:, 572:588], func=AF.Exp)
    kb.op("dve", "tensor_scalar", reads=["negA"], writes=["negA"], out=negA[:, :], in0=negA[:, :], scalar1=-1.0, scalar2=None, op0=ALU.mult)
    kb.op("dve", "tensor_tensor", reads=["dtv", "negA"], writes=["av"], out=av[:, :, :], in0=dtv[:, :, :],
          in1=negA[:, :].unsqueeze(1).to_broadcast([128, 16, 16]), op=ALU.mult)
    for t in range(NT):
        kb.mm(pD[:, t * 16:t * 16 + 8], U_f, av[:, t, 0:8], True, True, reads=["av", "cstf"], writes=["pD"], last=False)
        kb.mm(pD[:, t * 16 + 8:t * 16 + 16], Lo_f, av[:, t, 8:16], True, True, reads=["av", "cstf"], writes=["pD"], last=False)
        kb.mm(pD[:, 256 + t * 16:256 + t * 16 + 16], ones_f, av[:, t, :], True, True, reads=["av", "cstf"], writes=["pD"], last=(t == NT - 1))
    kb.op("dve", "tensor_copy", reads=["pD"], writes=["Ecs"], out=Ecs[:, :, :], in_=pD[:, 0:256].rearrange("p (a b) -> p a b", b=16))
    kb.op("dve", "tensor_copy", reads=["pD"], writes=["Tot"], out=Tot[:, :, :], in_=pD[:, 256:512].rearrange("p (a b) -> p a b", b=16))
    kb.op("dve", "tensor_scalar", reads=["Ecs"], writes=["negE"], out=negE[:, :, :], in0=Ecs[:, :, :], scalar1=-1.0, scalar2=None, op0=ALU.mult)
    kb.op("dve", "tensor_tensor", reads=["Tot", "Ecs"], writes=["wend"], out=wend[:, :, :], in0=Tot[:, :, :], in1=Ecs[:, :, :], op=ALU.subtract)
    kb.op("act", "activation", reads=["wend"], writes=["wend"], out=wend[:, :, :], in_=wend[:, :, :], func=AF.Exp)
    kb.op("dve", "tensor_tensor", reads=["wend", "dtv"], writes=["wend"], out=wend[:, :, :], in0=wend[:, :, :], in1=dtv[:, :, :], op=ALU.mult)
    kb.op("act", "activation", reads=["Ecs"], writes=["expE"], out=expE[:, :, :], in_=Ecs[:, :, :], func=AF.Exp)
    kb.op("act", "activation", reads=["Tot"], writes=["dec"], out=dec[:, :, :], in_=Tot[:, :, :], func=AF.Exp)

    ytm = kb.sb("ytm", [128, 16, 512], F32)
    cbb = kb.sb("cbb", [128, 16, 2, 128], BF16)
    cbf = kb.sb("cbf", [128, 2, 128], BF16)
    Dt = kb.sb("Dt", [128, 8, 128], F32)
    ex = kb.sb("ex", [128, 8, 128], BF16)
    Mtr = Ring(kb, "Mt", 2, [128, 8, 128], BF16)
    xdr = Ring(kb, "xdt", 2, [128, 8, 64], BF16)
    xwr = Ring(kb, "xw", 2, [128, 8, 64], BF16)
    tmpa = kb.sb("tmpDa", [128, 512], F32)
    tmpb = kb.sb("tmpDb", [128, 512], F32)
    Hs = [kb.sb("Hs%d" % d, [128, 512], F32) for d in range(2)]
    Hb = [kb.sb("Hb%d" % d, [128, 512], BF16) for d in range(2)]
    for d in range(2):
        kb.op("pool", "memset", writes=["Hs%d" % d], ap=Hs[d][:, :], constant=0.0)
        kb.op("pool", "memset", writes=["Hb%d" % d], ap=Hb[d][:, :], constant=0.0)
    units = [(0, c) for c in range(16)] + [(1, c) for c in range(15, -1, -1)]
    st = {}

    def d_stage1(u):
        d, c = u
        cs = slice(c * 128, (c + 1) * 128)
        if d == 0:
            for g in range(2):
                kb.mm(pA[:, g * 128:(g + 1) * 128], xbc[:, 4 + g, cs], xbc[:, 6 + g, cs], True, True, reads=[("xbc", 4 + g), ("xbc", 6 + g)], writes=["pA"], last=(g == 1))
            kb.op("dve", "tensor_tensor", reads=["pA", "cstf"], writes=["cbf"], out=cbf[:, :, :], in0=pA[:, 0:256].rearrange("p (g i) -> p g i", g=2),
                  in1=U_f.unsqueeze(1).to_broadcast([128, 2, 128]), op=ALU.mult)
            kb.op("dve", "tensor_tensor", reads=["pA", "cstf"], writes=[("cbb", c)], out=cbb[:, c, :, :], in0=pA[:, 0:256].rearrange("p (g i) -> p g i", g=2),
                  in1=Lo_f.unsqueeze(1).to_broadcast([128, 2, 128]), op=ALU.mult)
        tri = U_f if d == 0 else Lo_f
        for h in range(8):
            hd = d * 8 + h
            kb.mm(pbig[:, h * 128:(h + 1) * 128], av[:, c, hd:hd + 1].to_broadcast([128, 128]), tri, True, True, reads=["av", "cstf"], writes=["pbig"], last=(h == 7))
        kb.op("dve", "tensor_tensor", reads=["pbig", "negE"], writes=["Dt"], out=Dt[:, :, :], in0=pbig[:, :].rearrange("p (h i) -> p h i", h=8),
              in1=negE[:, c, d * 8:d * 8 + 8].unsqueeze(2).to_broadcast([128, 8, 128]), op=ALU.add)
        kb.op("act", "activation", reads=["Dt"], writes=["ex"], out=ex[:, :, :], in_=Dt[:, :, :], func=AF.Exp)
        Mt, Mtt = Mtr.next()
        for g in range(2):
            cbx = cbf[:, g:g + 1, :] if d == 0 else cbb[:, c, g:g + 1, :]
            kb.op("dve", "scalar_tensor_tensor", reads=["ex", "cbf" if d == 0 else ("cbb", c)], writes=[Mtt], out=Mt[:, 4 * g:4 * g + 4, :], in0=ex[:, 4 * g:4 * g + 4, :],
                  scalar=1.0, in1=cbx.to_broadcast([128, 4, 128]), op0=ALU.min, op1=ALU.mult)
        xd, xdt_ = xdr.next()
        xw, xwt = xwr.next()
        x3 = xtm[:, c, :].rearrange("p (h e) -> p h e", h=8)
        kb.op("pool", "tensor_tensor", reads=[("xtm", c), "dtv"], writes=[xdt_], out=xd[:, :, :], in0=x3,
              in1=dtv[:, c, d * 8:d * 8 + 8].unsqueeze(2).to_broadcast([128, 8, 64]), op=ALU.mult)
        kb.op("pool", "tensor_tensor", reads=[("xtm", c), "wend"], writes=[xwt], out=xw[:, :, :], in0=x3,
              in1=wend[:, c, d * 8:d * 8 + 8].unsqueeze(2).to_broadcast([128, 8, 64]), op=ALU.mult)
        st[u] = (Mt, Mtt, xd, xdt_, xw, xwt)

    def d_stage2(u):
        d, c = u
        cs = slice(c * 128, (c + 1) * 128)
        Mt, Mtt, xd, xdt_, xw, xwt = st.pop(u)
        for h in range(8):
            kb.mm(pB[:, h * 64:(h + 1) * 64], Mt[:, h, :], xd[:, h, :], True, True, reads=[Mtt, xdt_], writes=["pB"], last=(h == 7))
        for g in range(2):
            kb.mm(pD[:, g * 256:(g + 1) * 256], xbc[:, 6 + g, cs], Hb[d][:, g * 256:(g + 1) * 256], True, True, reads=[("xbc", 6 + g), "Hb%d" % d], writes=["pD"], last=(g == 1))
        for g in range(2):
            kb.mm(pC[:, g * 256:(g + 1) * 256], btm[:, c, g * 128:(g + 1) * 128], xw[:, 4 * g:4 * g + 4, :].rearrange("p h e -> p (h e)"), True, True,
                  reads=[("btm", c), xwt], writes=["pC"], last=(g == 1))
        kb.op("dve", "tensor_tensor", reads=["pD", "expE"], writes=["tmpDa"], out=tmpa[:, :].rearrange("p (h e) -> p h e", h=8), in0=pD[:, :].rearrange("p (h e) -> p h e", h=8),
              in1=expE[:, c, d * 8:d * 8 + 8].unsqueeze(2).to_broadcast([128, 8, 64]), op=ALU.mult)
        if d == 0:
            kb.op("dve", "tensor_tensor", reads=["pB", "tmpDa"], writes=[("ytm", c)], out=ytm[:, c, :], in0=pB[:, :], in1=tmpa[:, :], op=ALU.add)
        else:
            kb.op("dve", "tensor_tensor", reads=["pB", "tmpDa"], writes=["tmpDb"], out=tmpb[:, :], in0=pB[:, :], in1=tmpa[:, :], op=ALU.add)
            kb.op("pool", "tensor_tensor", reads=["tmpDb", ("ytm", c)], writes=[("ytm", c)], out=ytm[:, c, :], in0=ytm[:, c, :], in1=tmpb[:, :], op=ALU.add)
        hn = "Hs%d" % d
        kb.op("dve", "tensor_tensor", reads=[hn, "dec"], writes=[hn], out=Hs[d][:, :].rearrange("p (h e) -> p h e", h=8), in0=Hs[d][:, :].rearrange("p (h e) -> p h e", h=8),
              in1=dec[:, c, d * 8:d * 8 + 8].unsqueeze(2).to_broadcast([128, 8, 64]), op=ALU.mult)
        kb.op("dve", "tensor_tensor", reads=[hn, "pC"], writes=[hn], out=Hs[d][:, :], in0=pC[:, :], in1=Hs[d][:, :], op=ALU.add)
        kb.op("act", "activation", reads=[hn], writes=["Hb%d" % d], out=Hb[d][:, :], in_=Hs[d][:, :], func=AF.Copy)

    for idx in range(len(units) + 1):
        if idx < len(units):
            d_stage1(units[idx])
        if idx >= 1:
            d_stage2(units[idx - 1])

    zsr = Ring(kb, "zs", 2, [128, 512], BF16)
    yz = kb.sb("yz", [128, 512], F32)
    junk = kb.sb("junkD", [128, 512], F32)
    yoD = kb.sb("yoD", [128, 512], BF16)
    ssq = kb.sb("ssq", [128, 1], F32)
    ydr = Ring(kb, "ydT", 2, [128, 4, 128], BF16)
    for c in range(NT):
        zs, zst = zsr.next()
        kb.dma("sp", out=zs[:, :], in_=ZSTM[c * 128:(c + 1) * 128, :], writes=[zst])
        kb.op("pool", "tensor_tensor", reads=[("xtm", c), "rp"], writes=["tmpDb"], out=tmpb[:, :].rearrange("p (h e) -> p h e", h=8), in0=xtm[:, c, :].rearrange("p (h e) -> p h e", h=8),
              in1=dsk.unsqueeze(2).to_broadcast([128, 8, 64]), op=ALU.mult)
        kb.op("pool", "tensor_tensor", reads=["tmpDb", ("ytm", c)], writes=[("ytm", c)], out=ytm[:, c, :], in0=ytm[:, c, :], in1=tmpb[:, :], op=ALU.add)
        kb.op("dve", "tensor_tensor", reads=[("ytm", c), zst], writes=["yz"], out=yz[:, :], in0=ytm[:, c, :], in1=zs[:, :], op=ALU.mult)
        kb.op("act", "activation", reads=["yz"], writes=["junkD", "ssq"], out=junk[:, :], in_=yz[:, :], func=AF.Square, accum_out=ssq[:, 0:1])
        kb.op("dve", "tensor_scalar", reads=["ssq"], writes=["ssq"], out=ssq[:, :], in0=ssq[:, :], scalar1=1.0 / 512, scalar2=EPS, op0=ALU.mult, op1=ALU.add)
        kb.op("act", "activation", reads=["ssq"], writes=["ssq"], out=ssq[:, :], in_=ssq[:, :], func=AF.Sqrt)
        kb.op("dve", "reciprocal", reads=["ssq"], writes=["ssq"], out=ssq[:, :], in_=ssq[:, :])
        kb.op("dve", "scalar_tensor_tensor", reads=["yz", "ssq", "rp"], writes=["yoD"], out=yoD[:, :], in0=yz[:, :], scalar=ssq[:, 0:1], in1=gnd, op0=ALU.mult, op1=ALU.mult)
        pt, ptt = pTr.next()
        for j in range(4):
            kb.op("pe", "transpose", reads=["yoD", "cstb"], writes=[ptt], out=pt[:, j * 128:(j + 1) * 128], in_=yoD[:, j * 128:(j + 1) * 128], identity=ident_b)
        yd, ydt = ydr.next()
        kb.op("act", "activation", reads=[ptt], writes=[ydt], out=yd[:, :, :], in_=pt[:, :].rearrange("p (a b) -> p a b", a=4), func=AF.Copy)
        kb.dma("sp", out=YBR[3, :, :, c * 128:(c + 1) * 128], in_=yd[:, :, :], reads=[ydt], writes=[("YBR3", c)])
    kb.phase_end()
    if finish_check("D%d" % l):
        return

    kb.phase_begin()
    cp = kb.sb("cpM", [128, 256], F32)
    kb.dma("sp", out=cp[:, :], in_=din["colp"][l], writes=["cp"])
    hfb = kb.sb("hfbM", [128, 16, 1024], BF16)
    ybm = kb.sb("ybm", [128, 4, 4, 1024], BF16)
    mg = kb.sb("mgM", [128, 16, 1024], BF16)
    wgr = Ring(kb, "wg", 6, [128, 16, 128], BF16)
    wbr = Ring(kb, "wb", 6, [128, 4, 128], BF16)
    gsr = Ring(kb, "gs", 3, [128, 512], BF16)
    maccr = Ring(kb, "macc", 2, [128, 512], F32)
    tmr = Ring(kb, "tmpM", 3, [128, 512], F32)
    pgr = Ring(kb, "pg", 4, [128, 512], F32, psum=True)
    pbr = Ring(kb, "pbm", 4, [128, 512], F32, psum=True)
    for sbk in range(2):
        sbs = slice(sbk * 1024, (sbk + 1) * 1024)
        kb.dma("sp", out=hfb[:, :, :], in_=HFM[:, :, sbs], writes=["hfb"])
        for i in range(4):
            kb.dma("sp", out=ybm[:, i, :, :], in_=YBR[i, :, :, sbs], writes=[("ybm", i)])
        for fc in range(16):
            macs = [maccr.next() for _ in range(2)]
            for i in range(4):
                wg, wgt = wgr.next()
                wb, wbt = wbr.next()
                kb.dma("pool", out=wg[:, :, :], in_=din["w_gate_t"][l, i, fc], writes=[wgt])
                kb.dma("pool", out=wb[:, :, :], in_=din["w_br_t"][l, i, fc], writes=[wbt])
                for hf in range(2):
                    hs = slice(hf * 512, (hf + 1) * 512)
                    pg, pgt = pgr.next()
                    pb, pbt = pbr.next()
                    for kc in range(16):
                        kb.mm(pg[:, :], wg[:, kc, :], hfb[:, kc, hs], kc == 0, kc == 15, reads=[wgt, "hfb"], writes=[pgt])
                    for kc in range(4):
                        kb.mm(pb[:, :], wb[:, kc, :], ybm[:, i, kc, hs], kc == 0, kc == 3, reads=[wbt, ("ybm", i)], writes=[pbt])
                    gs, gst = gsr.next()
                    kb.op("act", "activation", reads=[pgt, "cp"], writes=[gst], out=gs[:, :], in_=pg[:, :], func=AF.Sigmoid, bias=cp[:, i * 16 + fc:i * 16 + fc + 1], scale=1.0)
                    mac, mact = macs[hf]
                    if i == 0:
                        kb.op("dve", "tensor_tensor", reads=[gst, pbt], writes=[mact], out=mac[:, :], in0=pb[:, :], in1=gs[:, :], op=ALU.mult)
                    else:
                        tm, tmt = tmr.next()
                        kb.op("dve", "tensor_tensor", reads=[gst, pbt], writes=[tmt], out=tm[:, :], in0=pb[:, :], in1=gs[:, :], op=ALU.mult)
                        if i < 3:
                            kb.op("dve", "tensor_tensor", reads=[tmt, mact], writes=[mact], out=mac[:, :], in0=mac[:, :], in1=tm[:, :], op=ALU.add)
                        else:
                            kb.op("dve", "tensor_tensor", reads=[tmt, mact], writes=[("mg", fc)], out=mg[:, fc, hs], in0=mac[:, :], in1=tm[:, :], op=ALU.add)
        kb.dma("sp", out=MG[:, :, sbs], in_=mg[:, :, :], reads=[("mg", fc) for fc in range(16)], writes=[("MG", sbk)])
    kb.phase_end()
    if finish_check("M1%d" % l):
        return

    UD = SC["UD"]
    kb.phase_begin()
    mga = kb.sb("mga", [128, 16, S], BF16)
    for tb in range(4):
        kb.dma("sp", out=mga[:, :, tb * 512:(tb + 1) * 512], in_=MG[:, :, tb * 512:(tb + 1) * 512], writes=[("mga", tb)])
    wor = Ring(kb, "wo", 2, [128, 16, 512], BF16)
    hsr = Ring(kb, "hsl", 3, [128, 512], F32)
    usr = Ring(kb, "usl", 3, [128, 512], F32)
    por = Ring(kb, "po", 4, [128, 512], F32, psum=True)
    for nb in range(4):
        nbs = slice(nb * 512, (nb + 1) * 512)
        wo, wot = wor.next()
        kb.dma("pool", out=wo[:, :, :], in_=din["w_out_t"][l, nb], writes=[wot])
        for t in range(NT):
            hs_, hst = hsr.next()
            kb.dma("sp", out=hs_[:, :], in_=HTM[t * 128:(t + 1) * 128, nbs], writes=[hst])
            po, pot = por.next()
            for kc in range(16):
                kb.mm(po[:, :], mga[:, kc, t * 128:(t + 1) * 128], wo[:, kc, :], kc == 0, kc == 15, reads=[wot, ("mga", t // 4)], writes=[pot])
            us, ust = usr.next()
            kb.op("dve", "scalar_tensor_tensor", reads=[hst, pot], writes=[ust], out=us[:, :], in0=hs_[:, :], scalar=ALPHA, in1=po[:, :], op0=ALU.mult, op1=ALU.add)
            kb.dma("sp", out=UD[t * 128:(t + 1) * 128, nbs], in_=us[:, :], reads=[ust], writes=[("UD", t, nb)])
    kb.phase_end()
    if finish_check("M2%d" % l):
        return

    kb.phase_begin()
    ln_alloc()
    g1 = kb.sb("g1", [128, 2048], F32)
    b1 = kb.sb("b1", [128, 2048], F32)
    kb.dma("sp", out=g1[:, :], in_=din["rowp"][l, 0:1, 0:2048].partition_broadcast(128), writes=["gb1"])
    kb.dma("sp", out=b1[:, :], in_=din["rowp"][l, 0:1, 2048:4096].partition_broadcast(128), writes=["gb1"])
    wrt = kb.sb("wrt", [128, 16, 36], F32)
    kb.dma("sp", out=wrt[:, :, :], in_=din["w_r_t"][l], writes=["wrt"])
    brt = kb.sb("brt", [128, 36], F32)
    kb.dma("sp", out=brt[:, :], in_=din["rowp"][l, 0:1, 8704:8740].partition_broadcast(128), writes=["brt"])
    ur = Ring(kb, "uM", 4, [128, 2048], F32)
    h1r = Ring(kb, "h1M", 2, [128, 2048], F32)
    hbr = Ring(kb, "hbM", 2, [128, 2048], BF16)
    h1fm = kb.sb("h1fm", [128, 16, 128], F32)
    lgr = Ring(kb, "lgs", 2, [128, 36], F32)
    ptfr = Ring(kb, "ptf", 3, [128, 512], F32, psum=True)
    plg = kb.ps("plg", [128, 512], F32)
    loadedM = {}
    statM = {}
    for i in range(-2, NT):
        tl = i + 2
        if tl < NT:
            u, ut = ur.next()
            kb.dma("sp", out=u[:, :], in_=UD[tl * 128:(tl + 1) * 128, :], writes=[ut])
            loadedM[tl] = (u, ut)
        ta = i + 1
        if 0 <= ta < NT:
            u, ut = loadedM[ta]
            statM[ta] = kb.ln_stats(u[:, :], ut)
        if i >= 0:
            t = i
            u, ut = loadedM.pop(i)
            utg = statM.pop(i)
            h1, h1t = h1r.next()
            ln_apply(u[:, :], ut, h1[:, :], h1t, g1[:, :], b1[:, :], "gb1", tg=utg)
            kb.dma("sp", out=HTM[t * 128:(t + 1) * 128, :], in_=h1[:, :], reads=[h1t], writes=[("HTM", t)])
            hb, hbt = hbr.next()
            kb.op("act", "activation", reads=[h1t], writes=[hbt], out=hb[:, :], in_=h1[:, :], func=AF.Copy)
            kb.dma("sp", out=H1B[t * 128:(t + 1) * 128, :], in_=hb[:, :], reads=[hbt], writes=[("H1B", t)])
            for g in range(4):
                pt, ptt = ptfr.next()
                for j in range(4):
                    c = g * 4 + j
                    kb.op("pe", "transpose", reads=[h1t, "cstf"], writes=[ptt], out=pt[:, j * 128:(j + 1) * 128], in_=h1[:, c * 128:(c + 1) * 128], identity=ident_f)
                if g % 2 == 0:
                    kb.op("dve", "tensor_copy", reads=[ptt], writes=[("h1fm", g)], out=h1fm[:, g * 4:(g + 1) * 4, :], in_=pt[:, :].rearrange("p (a b) -> p a b", a=4))
                else:
                    kb.op("act", "activation", reads=[ptt], writes=[("h1fm", g)], out=h1fm[:, g * 4:(g + 1) * 4, :], in_=pt[:, :].rearrange("p (a b) -> p a b", a=4), func=AF.Copy)
            for kc in range(16):
                kb.mm(plg[:, 0:36], h1fm[:, kc, :], wrt[:, kc, :], kc == 0, kc == 15, reads=[("h1fm", kc // 4), "wrt"], writes=["plg"])
            lg_, lgt = lgr.next()
            kb.op("dve", "tensor_tensor", reads=["plg", "brt"], writes=[lgt], out=lg_[:, :], in0=plg[:, 0:36], in1=brt[:, :], op=ALU.add)
            kb.dma("sp", out=LGT[t * 128:(t + 1) * 128, :], in_=lg_[:, :], reads=[lgt], writes=[("LGT", t)])

    kb.phase_end()
    if finish_check("M3%d" % l):
        return

    SLD, WTD = SC["SLD"], SC["WTD"]
    BIG = 1.0e9
    kb.phase_begin()
    lg = kb.sb("lgE", [128, 16, 36], F32)
    kb.dma("sp", out=lg[:, :, :], in_=LGT.rearrange("(t p) n -> p t n", p=128), writes=["lg"])
    sbase = kb.sb("sbase", [128, 32], F32)
    kb.dma("sp", out=sbase[:, :], in_=din["rowc"][0:1, 0:32].partition_broadcast(128), writes=["sbase"])
    def t2(name, n):
        return kb.sb(name, [128, 16, n], F32)
    gmax = kb.sb("gmax", [128, 16], F32)
    gsum = kb.sb("gsum", [128, 16], F32)
    gw = kb.sb("gw", [128, 16], F32)
    gd, goh, pen = t2("gd", 4), t2("goh", 4), t2("pen", 4)
    elm, oh1, elm2, oh2, tmpP, tmpQ = [t2(n, 32) for n in ("elm", "oh1", "elm2", "oh2", "tmpP", "tmpQ")]
    m1 = kb.sb("m1", [128, 16], F32)
    m2_ = kb.sb("m2E", [128, 16], F32)
    w1 = kb.sb("w1", [128, 16], F32)
    wts = kb.sb("wts", [128, 16, 2], F32)
    s12 = kb.sb("s12", [128, 16, 2], F32)
    sli = kb.sb("sli", [128, 16, 2], I32)
    Cb = kb.sb("Cb", [128, 16, 32], BF16)
    gl = lg[:, :, 0:4]
    el = lg[:, :, 4:36]
    def bc(ap2, n):
        return ap2.unsqueeze(2).to_broadcast([128, 16, n])
    kb.op("dve", "tensor_reduce", reads=["lg"], writes=["gmax"], out=gmax[:, :], in_=gl, axis=AX.X, op=ALU.max)
    kb.op("dve", "tensor_tensor", reads=["lg", "gmax"], writes=["gd"], out=gd[:, :, :], in0=gl, in1=bc(gmax[:, :], 4), op=ALU.subtract)
    kb.op("act", "activation", reads=["gd"], writes=["gd"], out=gd[:, :, :], in_=gd[:, :, :], func=AF.Exp)
    kb.op("dve", "tensor_reduce", reads=["gd"], writes=["gsum"], out=gsum[:, :], in_=gd[:, :, :], axis=AX.X, op=ALU.add)
    kb.op("dve", "reciprocal", reads=["gsum"], writes=["gw"], out=gw[:, :], in_=gsum[:, :])
    kb.op("dve", "tensor_tensor", reads=["lg", "gmax"], writes=["goh"], out=goh[:, :, :], in0=gl, in1=bc(gmax[:, :], 4), op=ALU.is_equal)
    kb.op("dve", "tensor_scalar", reads=["goh"], writes=["pen"], out=pen[:, :, :], in0=goh[:, :, :], scalar1=BIG, scalar2=-BIG, op0=ALU.mult, op1=ALU.add)
    kb.op("dve", "tensor_tensor", reads=["lg", "pen"], writes=["elm"], out=elm[:, :, :].rearrange("p t (g e) -> p t g e", g=4),
          in0=el.rearrange("p t (g e) -> p t g e", g=4), in1=pen[:, :, :].unsqueeze(3).to_broadcast([128, 16, 4, 8]), op=ALU.add)
    kb.op("dve", "tensor_reduce", reads=["elm"], writes=["m1"], out=m1[:, :], in_=elm[:, :, :], axis=AX.X, op=ALU.max)
    kb.op("dve", "tensor_tensor", reads=["elm", "m1"], writes=["oh1"], out=oh1[:, :, :], in0=elm[:, :, :], in1=bc(m1[:, :], 32), op=ALU.is_equal)
    kb.op("dve", "scalar_tensor_tensor", reads=["oh1", "elm"], writes=["elm2"], out=elm2[:, :, :], in0=oh1[:, :, :], scalar=-BIG, in1=elm[:, :, :], op0=ALU.mult, op1=ALU.add)
    kb.op("dve", "tensor_reduce", reads=["elm2"], writes=["m2"], out=m2_[:, :], in_=elm2[:, :, :], axis=AX.X, op=ALU.max)
    kb.op("dve", "tensor_tensor", reads=["elm2", "m2"], writes=["oh2"], out=oh2[:, :, :], in0=elm2[:, :, :], in1=bc(m2_[:, :], 32), op=ALU.is_equal)
    kb.op("dve", "tensor_tensor", reads=["m1", "m2"], writes=["w1"], out=w1[:, :], in0=m1[:, :], in1=m2_[:, :], op=ALU.subtract)
    kb.op("act", "activation", reads=["w1"], writes=["w1"], out=w1[:, :], in_=w1[:, :], func=AF.Sigmoid)
    kb.op("dve", "tensor_tensor", reads=["w1", "gw"], writes=["wts0"], out=wts[:, :, 0], in0=w1[:, :], in1=gw[:, :], op=ALU.mult)
    kb.op("dve", "tensor_tensor", reads=["wts0", "gw"], writes=["wts1"], out=wts[:, :, 1], in0=gw[:, :], in1=wts[:, :, 0], op=ALU.subtract)
    kb.op("dve", "tensor_tensor", reads=["oh1", "oh2"], writes=["Cb"], out=Cb[:, :, :], in0=oh1[:, :, :], in1=oh2[:, :, :], op=ALU.add)
    ppre = kb.ps("ppre", [128, 512], F32)
    strict_b = cstb[:, 4, :]
    for t in range(NT):
        for tp in range(t):
            kb.mm(ppre[:, t * 32:(t + 1) * 32], ones_b, Cb[:, tp, :], tp == 0, False, reads=["Cb", "cstb"], writes=["ppre"], last=False)
        kb.mm(ppre[:, t * 32:(t + 1) * 32], strict_b, Cb[:, t, :], t == 0, True, reads=["Cb", "cstb"], writes=["ppre"], last=(t == NT - 1))
    kb.op("dve", "tensor_tensor", reads=["ppre", "sbase"], writes=["tmpP"], out=tmpP[:, :, :], in0=ppre[:, :].rearrange("p (t e) -> p t e", t=16),
          in1=sbase[:, :].unsqueeze(1).to_broadcast([128, 16, 32]), op=ALU.add)
    kb.op("dve", "tensor_tensor", reads=["tmpP", "oh1"], writes=["tmpQ"], out=tmpQ[:, :, :], in0=tmpP[:, :, :], in1=oh1[:, :, :], op=ALU.mult)
    kb.op("dve", "tensor_reduce", reads=["tmpQ"], writes=["s120"], out=s12[:, :, 0], in_=tmpQ[:, :, :], axis=AX.X, op=ALU.add)
    kb.op("dve", "tensor_tensor", reads=["tmpP", "oh2", "s120"], writes=["tmpQ"], out=tmpQ[:, :, :], in0=tmpP[:, :, :], in1=oh2[:, :, :], op=ALU.mult)
    kb.op("dve", "tensor_reduce", reads=["tmpQ"], writes=["s121"], out=s12[:, :, 1], in_=tmpQ[:, :, :], axis=AX.X, op=ALU.add)
    kb.op("dve", "tensor_copy", reads=["s120", "s121"], writes=["sli"], out=sli[:, :, :], in_=s12[:, :, :])
    kb.dma("sp", out=SLD.rearrange("(t p) n -> p t n", p=128), in_=sli[:, :, :], reads=["sli"], writes=["SLD"])
    kb.dma("sp", out=WTD.rearrange("(t p) n -> p t n", p=128), in_=wts[:, :, :], reads=["wts0", "wts1"], writes=["WTD"])
    hbr = Ring(kb, "h1bE", 3, [128, 2048], BF16)
    for t in range(NT):
        hb, hbt = hbr.next()
        kb.dma("sp", out=hb[:, :], in_=H1B[t * 128:(t + 1) * 128, :], writes=[hbt])
        for k2 in range(2):
            kb.dma("pool", out=XROWS, in_=hb[:, :], reads=[hbt, "sli"], writes=[("XR", t, k2)], name="indirect_dma_start",
                   out_offset=bass.IndirectOffsetOnAxis(ap=sli[:, t, k2:k2 + 1], axis=0), in_offset=None)
    kb.phase_end()
    if finish_check("E1%d" % l):
        return

    kb.phase_begin()
    moe_experts(kb, l, din, XROWS, YROWS, ident_b, NEXP)
    kb.phase_end()
    if finish_check("E3%d" % l):
        return

    kb.phase_begin()
    ln_alloc()
    g2 = kb.sb("g2", [128, 2048], F32)
    b2 = kb.sb("b2", [128, 2048], F32)
    kb.dma("sp", out=g2[:, :], in_=din["rowp"][l, 0:1, 4096:6144].partition_broadcast(128), writes=["gb2"])
    kb.dma("sp", out=b2[:, :], in_=din["rowp"][l, 0:1, 6144:8192].partition_broadcast(128), writes=["gb2"])
    sli = kb.sb("sliF", [128, 16, 2], I32)
    wts = kb.sb("wtsF", [128, 16, 2], F32)
    kb.dma("sp", out=sli[:, :, :], in_=SLD.rearrange("(t p) n -> p t n", p=128), writes=["sli"])
    kb.dma("sp", out=wts[:, :, :], in_=WTD.rearrange("(t p) n -> p t n", p=128), writes=["wts"])
    gr1 = Ring(kb, "gy1", 4, [128, 2048], F32)
    gr2 = Ring(kb, "gy2", 4, [128, 2048], F32)
    hTr = Ring(kb, "hTE", 4, [128, 2048], F32)
    h2r = Ring(kb, "h2E", 2, [128, 2048], F32)
    rings = dict(hb=Ring(kb, "hbE4", 2, [128, 2048], BF16), fm=Ring(kb, "fmE4", 2, [128, 16, 128], BF16),
                 pt=Ring(kb, "ptE4", 4, [128, 512], BF16, psum=True))
    loadedE = {}
    statE = {}
    for i in range(-2, NT):
        tl = i + 2
        if tl < NT:
            t = tl
            ga, gat = gr1.next()
            gb_, gbt = gr2.next()
            kb.dma("pool", out=ga[:, :], in_=YROWS, reads=["sli"], writes=[gat], name="indirect_dma_start", out_offset=None,
                   in_offset=bass.IndirectOffsetOnAxis(ap=sli[:, t, 0:1], axis=0))
            kb.dma("pool", out=gb_[:, :], in_=YROWS, reads=["sli"], writes=[gbt], name="indirect_dma_start", out_offset=None,
                   in_offset=bass.IndirectOffsetOnAxis(ap=sli[:, t, 1:2], axis=0))
            hT, hTt = hTr.next()
            kb.dma("sp", out=hT[:, :], in_=HTM[t * 128:(t + 1) * 128, :], writes=[hTt])
            loadedE[tl] = (ga, gat, gb_, gbt, hT, hTt)
        ta = i + 1
        if 0 <= ta < NT:
            t = ta
            ga, gat, gb_, gbt, hT, hTt = loadedE[ta]
            kb.op("act", "activation", reads=[hTt], writes=[hTt], out=hT[:, :], in_=hT[:, :], func=AF.Copy, scale=ALPHA)
            kb.op("dve", "scalar_tensor_tensor", reads=[gat, hTt, "wts"], writes=[hTt], out=hT[:, :], in0=ga[:, :], scalar=wts[:, t, 0:1], in1=hT[:, :], op0=ALU.mult, op1=ALU.add)
            kb.op("dve", "scalar_tensor_tensor", reads=[gbt, hTt, "wts"], writes=[hTt], out=hT[:, :], in0=gb_[:, :], scalar=wts[:, t, 1:2], in1=hT[:, :], op0=ALU.mult, op1=ALU.add)
            statE[ta] = kb.ln_stats(hT[:, :], hTt)
        if i >= 0:
            ga, gat, gb_, gbt, hT, hTt = loadedE.pop(i)
            h2, h2t = h2r.next()
            ln_apply(hT[:, :], hTt, h2[:, :], h2t, g2[:, :], b2[:, :], "gb2", tg=statE.pop(i))
            emit_h_tile(i, h2[:, :], h2t, rings, l == NL - 1)
    kb.phase_end()
    if finish_check("E4%d" % l):
        return


_CACHE = {}


def kernel(**inputs):
    inp = {k: np.asarray(v) for k, v in inputs.items()}
    lay = _host_layout(inp)
    if "nc" not in _CACHE:
        _CACHE["nc"] = build()[0]
    nc = _CACHE["nc"]
    x = inp["x"].astype(np.float32)
    in_maps = []
    for b in range(x.shape[0]):
        m = dict(lay)
        m["x"] = np.ascontiguousarray(x[b])
        in_maps.append(m)
    res = run_bass_kernel_spmd(nc, in_maps, core_ids=list(range(x.shape[0])))
    out = np.stack([np.asarray(r["out"], dtype=np.float32) for r in res.results], axis=0)
    return out
```

```python
import math
from contextlib import ExitStack

import numpy as np
import ml_dtypes
import concourse.bass as bass
import concourse.mybir as mybir
from concourse.bass_utils import run_bass_kernel_spmd

F32 = mybir.dt.float32
BF16 = mybir.dt.bfloat16
I32 = mybir.dt.int32
AF = mybir.ActivationFunctionType
ALU = mybir.AluOpType
AX = mybir.AxisListType

S = 2048
D = 2048
NT = 16
NL = 2
ALPHA = 4.0 ** 0.25
EPS = 1e-5
R_E = 1535
RVLEN = 3072
CAP = 256
NEXP = 32
ENGS = ("pe", "act", "dve", "pool", "sp")


class KB:
    def __init__(self, nc, n_dma_sems=10):
        self.nc = nc
        self.ops = {e: [] for e in ENGS}
        self.sem = {}
        self.cnt = {e: 0 for e in ENGS}
        self.seen = {e: {} for e in ENGS}
        self.lastw = {}
        self.readers = {}
        self.n_dma_sems = n_dma_sems
        self.dma_sems = {}
        self.dma_rr = {e: 0 for e in ENGS}
        self.pstack = None
        self.out_events = []
        self.n_ins = 0

    def open(self, stack):
        nc = self.nc
        for e in ENGS:
            self.sem[e] = stack.enter_context(nc.semaphore("s_" + e))
        for q in ("sp", "act", "pool"):
            lst = []
            for i in range(6 if q == "pool" else self.n_dma_sems):
                key = "d_%s_%d" % (q, i)
                self.sem[key] = stack.enter_context(nc.semaphore(key))
                lst.append([key, 0])
            self.dma_sems[q] = lst
        self.gstack = stack

    def _uniq(self, name):
        self.uid = getattr(self, "uid", 0) + 1
        return "%s_u%d" % (name, self.uid)

    def gsb(self, name, shape, dt):
        return self.gstack.enter_context(self.nc.sbuf_tensor(self._uniq(name), list(shape), dt))

    def sb(self, name, shape, dt):
        return self.pstack.enter_context(self.nc.sbuf_tensor(self._uniq(name), list(shape), dt))

    def ps(self, name, shape, dt=F32):
        return self.pstack.enter_context(self.nc.psum_tensor(self._uniq(name), list(shape), dt))

    def _deps(self, reads, writes):
        evs = []
        for t in reads:
            ev = self.lastw.get(t)
            if ev is not None:
                evs.append(ev)
        for t in writes:
            ev = self.lastw.get(t)
            if ev is not None:
                evs.append(ev)
            evs.extend(self.readers.get(t, ()))
        return evs

    def _waits(self, eng, evs):
        seen = self.seen[eng]
        need = {}
        for (k, v) in evs:
            if seen.get(k, 0) >= v:
                continue
            if need.get(k, 0) < v:
                need[k] = v
        for k, v in need.items():
            seen[k] = v
        return list(need.items())

    def _commit(self, ev, reads, writes):
        for t in writes:
            self.lastw[t] = ev
            self.readers[t] = []
        for t in reads:
            if t in writes:
                continue
            self.readers.setdefault(t, []).append(ev)

    def op(self, eng, name, reads=(), writes=(), **kw):
        evs = self._deps(reads, writes)
        waits = self._waits(eng, evs)
        self.cnt[eng] += 1
        ev = (eng, self.cnt[eng])
        self.ops[eng].append((waits, name, kw, (eng, 1)))
        self._commit(ev, reads, writes)
        return ev

    def op_noinc(self, eng, name, reads=(), writes=(), **kw):
        evs = self._deps(reads, writes)
        waits = self._waits(eng, evs)
        self.ops[eng].append((waits, name, kw, None))

    def mm(self, out, lhsT, rhs, start, stop, reads=(), writes=(), last=None):
        if last is None:
            last = stop
        f = self.op if last else self.op_noinc
        return f("pe", "matmul", reads=reads, writes=writes, out=out, lhsT=lhsT, rhs=rhs, start=start, stop=stop)

    def dma(self, q, out, in_, reads=(), writes=(), is_out=False, name="dma_start", **kw):
        evs = self._deps(reads, writes)
        lst = self.dma_sems[q]
        i = self.dma_rr[q] % len(lst)
        self.dma_rr[q] += 1
        key, c = lst[i]
        if c > 0:
            evs.append((key, c))
        waits = self._waits(q, evs)
        lst[i][1] = c + 16
        ev = (key, c + 16)
        kw = dict(kw)
        kw["out"] = out
        kw["in_"] = in_
        self.ops[q].append((waits, name, kw, (key, 16)))
        self._commit(ev, reads, writes)
        if is_out:
            self.out_events.append(ev)
        return ev

    def barrier(self):
        evs = []
        for q, lst in self.dma_sems.items():
            for key, c in lst:
                if c > 0:
                    evs.append((key, c))
        for e in ENGS:
            if e != "sp" and self.cnt[e] > 0:
                evs.append((e, self.cnt[e]))
        waits = self._waits("sp", evs)
        self.cnt["sp"] += 1
        self.ops["sp"].append((waits, "sem_inc", dict(sem=self.sem["sp"], val=1), None))
        mark = ("sp", self.cnt["sp"])
        for e in ENGS:
            if e == "sp":
                continue
            w = self._waits(e, [mark])
            self.ops[e].append((w, None, None, None))
            for (k, v) in evs:
                if self.seen[e].get(k, 0) < v:
                    self.seen[e][k] = v
        self.lastw = {}
        self.readers = {}

    def emit(self):
        nc = self.nc
        sem = self.sem
        ops = self.ops
        with nc.named_scope("ph%02d" % getattr(self, "phase_no", 0)), nc.Block() as block:
            def run(engname):
                def body(e):
                    for waits, name, kw, inc in ops[engname]:
                        for k, v in waits:
                            e.wait_ge(sem[k], v)
                        if name is None:
                            continue
                        ins = getattr(e, name)(**kw)
                        self.n_ins += 1
                        if inc is not None:
                            ins.then_inc(sem[inc[0]], inc[1])
                return body
            block.tensor(run("pe"))
            block.scalar(run("act"))
            block.vector(run("dve"))
            block.gpsimd(run("pool"))
            block.sync(run("sp"))
        self.ops = {e: [] for e in ENGS}

    def phase_begin(self):
        self.phase_no = getattr(self, "phase_no", -1) + 1
        self.pstack = ExitStack()
        self.pstack.__enter__()

    def phase_end(self):
        self.barrier()
        self.emit()
        self.pstack.close()
        self.pstack = None


class Ring:
    def __init__(self, kb, name, n, shape, dt, psum=False):
        self.tiles = []
        self.names = []
        for i in range(n):
            nm = "%s%d" % (name, i)
            t = kb.ps(nm, shape, dt) if psum else kb.sb(nm, shape, dt)
            self.tiles.append(t)
            self.names.append(nm)
        self.i = 0

    @classmethod
    def wrap(cls, tiles, names):
        r = cls.__new__(cls)
        r.tiles = list(tiles)
        r.names = list(names)
        r.i = 0
        return r

    def next(self):
        j = self.i % len(self.tiles)
        self.i += 1
        return self.tiles[j], self.names[j]


def _t5_bucket(rel):
    half = 16
    max_exact = 8
    n = np.abs(rel)
    large = max_exact + (np.log(np.maximum(n, 1) / max_exact) / np.log(1024 / max_exact) * (half - max_exact)).astype(np.int32)
    large = np.minimum(large, half - 1)
    return (rel > 0).astype(np.int32) * half + np.where(n < max_exact, n, large)


def _host_consts():
    c = {}
    i = np.arange(RVLEN)
    r = R_E - i
    a = np.abs(r)
    mult = (a <= 64).astype(np.float32) + ((r % 4 == 0) & (a <= 256)).astype(np.float32) + ((r % 16 == 0) & (a <= 1024)).astype(np.float32)
    bk = _t5_bucket(r)
    mohr = np.zeros((32, RVLEN), np.float32)
    mohr[bk, i] = mult
    c["mohr"] = mohr
    p = np.arange(128)[:, None]
    f = np.arange(128)[None, :]
    cst = np.zeros((128, 6, 128), np.float32)
    cst[:, 0] = (p == f)
    cst[:, 1] = (p + f == 127)
    cst[:, 2] = (p <= f)
    cst[:, 3] = (p >= f)
    cst[:, 4] = (p < f)
    cst[:, 5] = 1.0
    c["cst"] = cst
    inv_freq = (np.float32(10000.0) ** (-np.arange(0, 32, 2, dtype=np.float32) / np.float32(32))).astype(np.float32)
    ang = (np.arange(S, dtype=np.float32)[:, None] * inv_freq[None]).astype(np.float32)
    cos = np.cos(ang).astype(np.float32).T
    sin = np.sin(ang).astype(np.float32).T
    rope = np.zeros((2, 96, S), np.float32)
    rope[0, 64:80] = cos
    rope[0, 80:96] = cos
    rope[1, 64:80] = -sin
    rope[1, 80:96] = sin
    c["rope"] = rope
    m = np.ones((16, S), np.float32)
    m[:, ::128] = 0.0
    c["scanmask"] = m
    rowc = np.zeros((1, 64), np.float32)
    rowc[0, 0:32] = np.arange(32) * CAP
    c["rowc"] = rowc
    return c


def _host_layout(inp):
    f = np.float32
    o = {}
    w_in = inp["w_in"]
    cuts = np.cumsum((512, 512, 512, 512, 256, 32, 1024, 512, 512, 256, 256, 16))
    st = np.concatenate([[0], cuts[:-1]])
    seg = {n: (int(a), int(b)) for n, a, b in zip(("qa", "ka", "va", "cq", "ckv", "kr", "glu", "z", "xs", "bm", "cm", "dt"), st, cuts)}
    cols = []
    def rng(n, a=0, b=None):
        s0, s1 = seg[n]
        b = (s1 - s0) if b is None else b
        return list(range(s0 + a, s0 + b))
    cols += rng("qa") + rng("ka") + rng("cq") + rng("ckv")
    kr0 = seg["kr"][0]
    cols += rng("kr") + [kr0 + (r + 16) % 32 for r in range(32)] + [-1] * 64
    cols += rng("glu", 0, 512) + rng("glu", 512, 1024) + rng("xs") + rng("bm") + rng("cm")
    cols = np.array(cols)
    assert cols.size == 31 * 128
    w_in_t = np.zeros((NL, 31, 128, 16, 128), f)
    w_dt_tm = np.zeros((NL, 128, 16, 16), f)
    w_in_tm = np.zeros((NL, 2, 128, 16, 512), f)
    for l in range(NL):
        wc = np.where(cols[None, :] >= 0, w_in[l][:, np.maximum(cols, 0)], 0.0).astype(f)
        w_in_t[l] = wc.reshape(16, 128, 31, 128).transpose(2, 1, 0, 3)
        w_dt_tm[l] = w_in[l][:, seg["dt"][0]:seg["dt"][1]].reshape(16, 128, 16).transpose(1, 0, 2)
        for b, n in enumerate(("va", "z")):
            s0, s1 = seg[n]
            w_in_tm[l, b] = w_in[l][:, s0:s1].reshape(16, 128, 512).transpose(1, 0, 2)
    o["w_in_t"] = w_in_t
    o["w_in_tm"] = w_in_tm
    o["w_dt_tm"] = w_dt_tm
    w_uq = inp["w_uq"]
    pc = np.array([96 * h + 64 + (r + 16) % 32 for h in range(8) for r in range(32)])
    w_uq_t = np.concatenate([w_uq, w_uq[:, :, pc]], axis=2)
    o["w_uq_t"] = np.ascontiguousarray(w_uq_t.reshape(NL, 4, 128, 1024).transpose(0, 2, 1, 3))
    w_ukv = inp["w_ukv"]
    kc_ = np.array([128 * h + j for h in range(8) for j in range(64)])
    vc_ = kc_ + 64
    w_ukv_r = np.concatenate([w_ukv[:, :, kc_], w_ukv[:, :, vc_]], axis=2)
    o["w_ukv_t"] = np.ascontiguousarray(w_ukv_r.reshape(NL, 2, 128, 1024).transpose(0, 2, 1, 3))
    o["w_gate_t"] = np.ascontiguousarray(inp["w_gate"].reshape(NL, 4, 16, 128, 16, 128).transpose(0, 1, 4, 3, 2, 5))
    o["w_br_t"] = np.ascontiguousarray(inp["w_br"].reshape(NL, 4, 4, 128, 16, 128).transpose(0, 1, 4, 3, 2, 5))
    o["w_out_t"] = np.ascontiguousarray(inp["w_out"].reshape(NL, 16, 128, 4, 512).transpose(0, 3, 2, 1, 4))
    w_r = np.concatenate([inp["w_rg"], inp["w_re"]], axis=2)
    o["w_r_t"] = np.ascontiguousarray(w_r.reshape(NL, 16, 128, 36).transpose(0, 2, 1, 3))
    o["w_e_gate"] = inp["w_e_gate"]
    o["w_e_up"] = inp["w_e_up"]
    o["w_e_down"] = inp["w_e_down"]
    colp = np.zeros((NL, 128, 256), f)
    for l in range(NL):
        c0 = 0
        colp[l, :, 0:64] = inp["b_gate"][l].reshape(4, 16, 128).transpose(2, 0, 1).reshape(128, 64)
        colp[l, :, 64:68] = inp["g_cq"][l].reshape(4, 128).T
        colp[l, :, 68:70] = inp["g_ckv"][l].reshape(2, 128).T
        colp[l, :, 70:74] = inp["b_dw_c"][l].reshape(4, 128).T
        colp[l, :, 74:78] = inp["ln_c_g"][l].reshape(4, 128).T
        colp[l, :, 78:82] = inp["ln_c_b"][l].reshape(4, 128).T
        colp[l, :, 82:206] = inp["w_dw_c"][l].reshape(31, 4, 128).transpose(2, 1, 0).reshape(128, 124)
        colp[l, :, 206:246] = inp["w_conv_d"][l].reshape(5, 8, 128).transpose(2, 1, 0).reshape(128, 40)
        colp[l, :, 246:254] = inp["b_conv_d"][l].reshape(8, 128).T
    o["colp"] = colp
    rowp = np.zeros((NL, 1, 8832), f)
    for l in range(NL):
        rowp[l, 0, 0:2048] = inp["ln1_g"][l]
        rowp[l, 0, 2048:4096] = inp["ln1_b"][l]
        rowp[l, 0, 4096:6144] = inp["ln2_g"][l]
        rowp[l, 0, 6144:8192] = inp["ln2_b"][l]
        rowp[l, 0, 8192:8704] = inp["g_norm_d"][l]
        rowp[l, 0, 8704:8708] = inp["b_rg"][l]
        rowp[l, 0, 8708:8740] = inp["b_re"][l]
        rowp[l, 0, 8740:8748] = inp["d_skip"][l]
        rowp[l, 0, 8748:8756] = inp["dt_bias_f"][l]
        rowp[l, 0, 8756:8764] = inp["dt_bias_b"][l]
        rowp[l, 0, 8764:8772] = inp["a_log_f"][l]
        rowp[l, 0, 8772:8780] = inp["a_log_b"][l]
    o["rowp"] = rowp
    o["rowg"] = np.concatenate([inp["ln_in_g"], inp["ln_in_b"]]).reshape(1, 4096).astype(f)
    o["rel_bias"] = inp["rel_bias"].astype(f)
    o.update(_host_consts())
    return o


IN_SHAPES = {
    "x": ([S, D], F32),
    "w_in_t": ([NL, 31, 128, 16, 128], F32),
    "w_dt_tm": ([NL, 128, 16, 16], F32),
    "w_in_tm": ([NL, 2, 128, 16, 512], F32),
    "w_uq_t": ([NL, 128, 4, 1024], F32),
    "w_ukv_t": ([NL, 128, 2, 1024], F32),
    "w_gate_t": ([NL, 4, 16, 128, 16, 128], F32),
    "w_br_t": ([NL, 4, 16, 128, 4, 128], F32),
    "w_out_t": ([NL, 4, 128, 16, 512], F32),
    "w_r_t": ([NL, 128, 16, 36], F32),
    "w_e_gate": ([NL, 32, 2048, 512], F32),
    "w_e_up": ([NL, 32, 2048, 512], F32),
    "w_e_down": ([NL, 32, 512, 2048], F32),
    "colp": ([NL, 128, 256], F32),
    "rowp": ([NL, 1, 8832], F32),
    "rowg": ([1, 4096], F32),
    "rel_bias": ([32, 8], F32),
    "mohr": ([32, RVLEN], F32),
    "cst": ([128, 6, 128], F32),
    "rope": ([2, 96, S], F32),
    "scanmask": ([16, S], F32),
    "rowc": ([1, 64], F32),
}


CH = dict(qa=0, ka=4, cq=8, ckv=12, krx=14, glua=15, glug=19, xs=23, bm=27, cm=29)


def build(debug=False, stop_after=None, small_moe=False):
    nc = bass.Bass("TRN2", target_bir_lowering=False)
    scr = "ExternalOutput" if debug else "Internal"
    din = {}
    hnd = {}
    for n, (shp, dt) in IN_SHAPES.items():
        if small_moe and n.startswith("w_e_"):
            shp = [shp[0], 1] + list(shp[2:])
        hnd[n] = nc.dram_tensor(n, shp, dt, kind="ExternalInput")
        din[n] = hnd[n].ap()
    out_d = nc.dram_tensor("out", [S, D], F32, kind="ExternalOutput").ap()

    def scratch(name, shape, dt):
        h = nc.dram_tensor(name, shape, dt, kind=scr)
        hnd[name] = h
        return h.ap()

    HTM = scratch("HTM", [S, D], F32)
    HFM = scratch("HFM", [128, 16, S], BF16)
    PROJ = scratch("PROJ", [31, 128, S], BF16)
    DTR = scratch("DTR", [S, 16], F32)
    MG = scratch("MG", [128, 16, S], BF16)
    UD = scratch("UD", [S, D], F32)
    SLD = scratch("SLD", [S, 2], I32)
    WTD = scratch("WTD", [S, 2], F32)
    VATM = scratch("VATM", [S, 512], BF16)
    ZSTM = scratch("ZSTM", [S, 512], BF16)
    YBR = scratch("YBR", [4, 128, 4, S], BF16)
    RV = scratch("RV", [8, RVLEN], F32)
    H1B = scratch("H1B", [S, D], BF16)
    LGT = scratch("LGT", [S, 36], F32)
    XROWS = scratch("XROWS", [NEXP * CAP, D], BF16)
    YROWS = scratch("YROWS", [NEXP * CAP, D], F32)

    kb = KB(nc)
    done = [False]

    def finish_check(name):
        if stop_after == name:
            done[0] = True
        return done[0]

    with ExitStack() as gstack:
        kb.open(gstack)
        cstf = kb.gsb("cstf", [128, 6, 128], F32)
        cstb = kb.gsb("cstb", [128, 6, 128], BF16)
        ident_f = cstf[:, 0, :]
        ident_b = cstb[:, 0, :]
        ones_f = cstf[:, 5, :]
        ones_b = cstb[:, 5, :]

        def ln_stats(src, tag):
            tg = "ab"[kb._ln_i % 2]
            kb._ln_i += 1
            st = kb._ln_st
            for q in range(4):
                kb.op("dve", "bn_stats", reads=[tag], writes=[tg + "lnst%d" % q], out=st[tg + "stats"][:, q, :], in_=src[:, q * 512:(q + 1) * 512])
            kb.op("dve", "bn_aggr", reads=[tg + "lnst%d" % q for q in range(4)], writes=[tg + "lnmv"], out=st[tg + "mv"][:, :], in_=st[tg + "stats"][:, :, :])
            kb.op("dve", "tensor_scalar_add", reads=[tg + "lnmv"], writes=[tg + "lnve"], out=st[tg + "ve"][:, :], in0=st[tg + "mv"][:, 1:2], scalar1=EPS)
            kb.op("pool", "tensor_tensor", reads=[tg + "lnve", "lnmhalf"], writes=[tg + "rs"], out=st[tg + "rs"][:, :], in0=st[tg + "ve"][:, :], in1=st["mhalf"][:, :], op=ALU.pow)
            return tg

        def ln_alloc():
            st = {}
            for tg in ("a", "b"):
                st[tg + "stats"] = kb.sb("ln_stats" + tg, [128, 4, 6], F32)
                st[tg + "mv"] = kb.sb("ln_mv" + tg, [128, 2], F32)
                st[tg + "ve"] = kb.sb("ln_ve" + tg, [128, 1], F32)
                st[tg + "rs"] = kb.sb("ln_rs" + tg, [128, 1], F32)
            st["mhalf"] = kb.sb("ln_mhalf", [128, 1], F32)
            kb.op("pool", "memset", writes=["lnmhalf"], ap=st["mhalf"][:, :], constant=-0.5)
            kb._ln_st = st
            kb._ln_i = 0

        def ln_apply(src, stag, dst, dtag, gbc, bbc, gtag, tg=None):
            if tg is None:
                tg = ln_stats(src, stag)
            st = kb._ln_st
            kb.op("dve", "scalar_tensor_tensor", reads=[stag, tg + "lnmv", gtag], writes=[dtag], out=dst, in0=src, scalar=st[tg + "mv"][:, 0:1], in1=gbc,
                  op0=ALU.subtract, op1=ALU.mult)
            kb.op("dve", "scalar_tensor_tensor", reads=[dtag, tg + "rs", gtag], writes=[dtag], out=dst, in0=dst, scalar=st[tg + "rs"][:, 0:1], in1=bbc,
                  op0=ALU.mult, op1=ALU.add)

        kb.ln_stats = ln_stats

        def emit_h_tile(t, hT, htag, rings, write_out):
            if write_out:
                kb.dma("sp", out=out_d[t * 128:(t + 1) * 128, :], in_=hT, reads=[htag], is_out=True)
                return
            kb.dma("sp", out=HTM[t * 128:(t + 1) * 128, :], in_=hT, reads=[htag], writes=[("HTM", t)])
            hb, hbt = rings["hb"].next()
            kb.op("act", "activation", reads=[htag], writes=[hbt], out=hb[:, :], in_=hT, func=AF.Copy)
            fm, fmt = rings["fm"].next()
            for g in range(4):
                pt, ptt = rings["pt"].next()
                for j in range(4):
                    c = g * 4 + j
                    kb.op("pe", "transpose", reads=[hbt], writes=[ptt], out=pt[:, j * 128:(j + 1) * 128], in_=hb[:, c * 128:(c + 1) * 128], identity=ident_b)
                eng = "dve" if g % 2 == 0 else "act"
                if eng == "dve":
                    kb.op("dve", "tensor_copy", reads=[ptt], writes=[fmt], out=fm[:, g * 4:(g + 1) * 4, :], in_=pt[:, :].rearrange("p (a b) -> p a b", a=4))
                else:
                    kb.op("act", "activation", reads=[ptt], writes=[fmt], out=fm[:, g * 4:(g + 1) * 4, :], in_=pt[:, :].rearrange("p (a b) -> p a b", a=4), func=AF.Copy)
            kb.dma("sp", out=HFM[:, :, t * 128:(t + 1) * 128], in_=fm[:, :, :], reads=[fmt], writes=[("HFM", t)])

        kb.phase_begin()
        kb.dma("sp", out=cstf[:, :, :], in_=din["cst"], writes=["cstf"])
        kb.op("dve", "tensor_copy", reads=["cstf"], writes=["cstb"], out=cstb[:, :, :], in_=cstf[:, :, :])
        rb = kb.sb("rb", [32, 8], F32)
        eb = kb.sb("eb", [32, 8], F32)
        mo = kb.sb("mo", [32, RVLEN], F32)
        rv = kb.sb("rv", [8, RVLEN], F32)
        kb.dma("sp", out=rb[:, :], in_=din["rel_bias"], writes=["rb"])
        kb.dma("sp", out=mo[:, :], in_=din["mohr"], writes=["mo"])
        kb.op("act", "activation", reads=["rb"], writes=["eb"], out=eb[:, :], in_=rb[:, :], func=AF.Exp)
        pr = Ring(kb, "ipr", 2, [128, 512], F32, psum=True)
        for j in range(RVLEN // 512):
            p, ptg = pr.next()
            kb.mm(p[0:8, :], eb[:, :], mo[:, j * 512:(j + 1) * 512], True, True, reads=["eb", "mo"], writes=[ptg])
            kb.op("dve", "tensor_copy", reads=[ptg], writes=["rv"], out=rv[:, j * 512:(j + 1) * 512], in_=p[0:8, :])
        kb.dma("sp", out=RV, in_=rv[:, :], reads=["rv"], writes=["RV"])
        zt = kb.sb("zt", [128, 2048], BF16)
        kb.op("pool", "memset", writes=["zt"], ap=zt[:, :], constant=0.0)
        for i in range(NEXP * CAP // 128):
            kb.dma("sp", out=XROWS[i * 128:(i + 1) * 128, :], in_=zt[:, :], reads=["zt"], writes=[("XR0", i)])
        kb.phase_end()

        kb.phase_begin()
        ln_alloc()
        gbc = kb.sb("gbc", [128, 2048], F32)
        bbc = kb.sb("bbc", [128, 2048], F32)
        kb.dma("sp", out=gbc[:, :], in_=din["rowg"][0:1, 0:2048].partition_broadcast(128), writes=["gb"])
        kb.dma("sp", out=bbc[:, :], in_=din["rowg"][0:1, 2048:4096].partition_broadcast(128), writes=["gb"])
        xr = Ring(kb, "xin", 4, [128, 2048], F32)
        hr = Ring(kb, "hout", 2, [128, 2048], F32)
        rings = dict(hb=Ring(kb, "hb", 2, [128, 2048], BF16), fm=Ring(kb, "fm", 2, [128, 16, 128], BF16),
                     pt=Ring(kb, "ptb", 4, [128, 512], BF16, psum=True))
        loaded = {}
        stat = {}
        for i in range(-2, NT):
            tl = i + 2
            if tl < NT:
                xt, xtag = xr.next()
                kb.dma("sp", out=xt[:, :], in_=din["x"][tl * 128:(tl + 1) * 128, :], writes=[xtag])
                loaded[tl] = (xt, xtag)
            ta = i + 1
            if 0 <= ta < NT:
                xt, xtag = loaded[ta]
                stat[ta] = ln_stats(xt[:, :], xtag)
            if i >= 0:
                pxt, pxtag = loaded.pop(i)
                ht, htag = hr.next()
                ln_apply(pxt[:, :], pxtag, ht[:, :], htag, gbc[:, :], bbc[:, :], "gb", tg=stat.pop(i))
                emit_h_tile(i, ht[:, :], htag, rings, False)
        kb.phase_end()
        if finish_check("ln_in"):
            return nc, kb

        for l in range(NL):
            build_layer(nc, kb, din, hnd, out_d, l, finish_check, dict(
                HTM=HTM, HFM=HFM, PROJ=PROJ, DTR=DTR, VATM=VATM, ZSTM=ZSTM, YBR=YBR, RV=RV, MG=MG, UD=UD, SLD=SLD, WTD=WTD, H1B=H1B, LGT=LGT, XROWS=XROWS, YROWS=YROWS),
                dict(cstf=cstf, cstb=cstb), ln_alloc, ln_apply, emit_h_tile)
            if done[0]:
                return nc, kb
    return nc, kb


def attention(kb, nheads, kT_ap, qT_ap, v_ap, kcs_for, e_info, scale, yst, prefix, reads_k, reads_q, reads_v, pre_head=None, srr=None, per_unit=None, warm=None, nwarm=2):
    if srr is None:
        srr = Ring(kb, prefix + "S", 4, [128, 512], F32, psum=True)
    accr = Ring(kb, prefix + "acc", 3, [128, 512], F32, psum=True)
    ptr = Ring(kb, prefix + "pt", 4, [128, 512], BF16)
    pmr = Ring(kb, prefix + "pm", 4, [128, 512], BF16) if e_info is not None else None
    rden = kb.sb(prefix + "rden", [128, 512], F32)
    units = []
    for h in range(nheads):
        for qb in range(4):
            kcs = kcs_for(qb)
            for i, kc in enumerate(kcs):
                units.append((h, qb, kc, i == 0, i == len(kcs) - 1))
    state = {}
    cur_acc = {}
    seen_heads = set()
    wstate = [True]

    def keep_warm(n):
        if warm is None:
            return
        dps, dtok, dl, dr = warm
        for _ in range(n):
            kb.op_noinc("pe", "matmul", reads=[], writes=([dtok] if wstate[0] else []), out=dps[:, 0:256], lhsT=dl, rhs=dr, start=True, stop=True)
            wstate[0] = False

    def stage1(u):
        h, qb, kc, first, last = u
        if pre_head is not None and h not in seen_heads:
            seen_heads.add(h)
            pre_head(h)
        sp_, spt = srr.next()
        kb.mm(sp_[:, :], kT_ap(h, kc), qT_ap(h, qb), True, True, reads=[reads_k(h), reads_q(h)], writes=[spt])
        keep_warm(nwarm)
        pt_, ptt = ptr.next()
        kb.op("act", "activation", reads=[spt], writes=[ptt], out=pt_[:, :], in_=sp_[:, :], func=AF.Exp, scale=scale)
        if e_info is not None:
            eap, etok = e_info(h, qb, kc)
            pm_, pmt = pmr.next()
            kb.op("dve", "tensor_tensor", reads=[ptt, etok], writes=[pmt], out=pm_[:, :], in0=pt_[:, :], in1=eap, op=ALU.mult)
            state[u] = (pm_, pmt)
        else:
            state[u] = (pt_, ptt)

    def stage2(u):
        h, qb, kc, first, last = u
        pm_, pmt = state.pop(u)
        if first:
            cur_acc[0] = accr.next()
        acc, acct = cur_acc[0]
        kb.mm(acc[:, :], v_ap(h, kc), pm_[:, :], first, last, reads=[reads_v(h), pmt], writes=[acct])
        keep_warm(nwarm)
        if last:
            po = (h % 2) * 64
            do = 64 - po
            kb.op("dve", "reciprocal", reads=[acct], writes=[prefix + "rden"], out=rden[do:do + 64, :], in_=acc[do:do + 64, :])
            kb.op("dve", "tensor_tensor", reads=[acct, prefix + "rden"], writes=[("yst", h, qb)],
                  out=yst[po:po + 64, h // 2, qb * 512:(qb + 1) * 512], in0=acc[po:po + 64, :], in1=rden[do:do + 64, :], op=ALU.mult)

    LAG = 3
    for idx in range(len(units) + LAG):
        if idx < len(units):
            stage1(units[idx])
            if per_unit is not None:
                per_unit(idx)
        if idx - LAG >= 0:
            stage2(units[idx - LAG])


def build_vaug(kb, vaug, vsrc, reads, wtok):
    kb.op("pool", "memset", writes=[wtok + "ones"], ap=vaug[:, :, :, :], constant=1.0)
    v5 = vsrc.rearrange("p k (hp two e) -> p k hp two e", two=2, e=64)
    a5 = vaug.rearrange("p k (hp two) e -> p k hp two e", two=2)
    kb.op("dve", "tensor_copy", reads=reads + [wtok + "ones"], writes=[wtok + "e"], out=a5[:, :, :, 0, 0:64], in_=v5[:, :, :, 0, :])
    kb.op("pool", "tensor_copy", reads=reads + [wtok + "ones"], writes=[wtok + "o"], out=a5[:, :, :, 1, 64:128], in_=v5[:, :, :, 1, :])
    kb.op("pool", "engine_nop", reads=[wtok + "e", wtok + "o"], writes=[wtok])


def moe_experts(kb, l, din, XROWS, YROWS, ident_b, nexp):
    wgr = Ring(kb, "weg", 2, [128, 16, 512], BF16)
    wur = Ring(kb, "weu", 2, [128, 16, 512], BF16)
    wdr = Ring(kb, "wed", 2, [128, 4, 2048], BF16)
    xrr = Ring(kb, "xrE", 4, [128, 2048], BF16)
    xTr = Ring(kb, "xTE", 2, [128, 16, 256], BF16)
    aTr = Ring(kb, "aTE", 2, [128, 4, 256], BF16)
    sgr = Ring(kb, "sgE", 2, [128, 256], F32)
    yrr = Ring(kb, "yrE", 2, [128, 2048], F32)
    ptxr = Ring(kb, "ptx", 2, [128, 512], BF16, psum=True)
    pgur = Ring(kb, "pgu", 3, [128, 512], F32, psum=True)
    pyr = Ring(kb, "pyE", 3, [128, 512], F32, psum=True)
    ke = 0
    for e in range(nexp):
        wg, wgt = wgr.next()
        wu, wut = wur.next()
        wd, wdt = wdr.next()
        kb.dma("pool", out=wg[:, :, :], in_=din["w_e_gate"][l, e].rearrange("(kc p) n -> p kc n", p=128), writes=[wgt])
        kb.dma("pool", out=wu[:, :, :], in_=din["w_e_up"][l, e].rearrange("(kc p) n -> p kc n", p=128), writes=[wut])
        for nb in range(4):
            kb.dma("pool", out=wd[:, :, nb * 512:(nb + 1) * 512], in_=din["w_e_down"][l, e].rearrange("(kc p) n -> p kc n", p=128)[:, :, nb * 512:(nb + 1) * 512], writes=[wdt])
        xT, xTt = xTr.next()
        for s2 in range(2):
            xr, xrt = xrr.next()
            r0 = e * CAP + s2 * 128
            kb.dma("sp", out=xr[:, :], in_=XROWS[r0:r0 + 128, :], writes=[xrt])
            for g in range(4):
                pt, ptt = ptxr.next()
                for j in range(4):
                    c = g * 4 + j
                    kb.op("pe", "transpose", reads=[xrt, "cstb"], writes=[ptt], out=pt[:, j * 128:(j + 1) * 128], in_=xr[:, c * 128:(c + 1) * 128], identity=ident_b)
                kb.op("dve", "tensor_copy", reads=[ptt], writes=[(xTt, s2, g)], out=xT[:, g * 4:(g + 1) * 4, s2 * 128:(s2 + 1) * 128], in_=pt[:, :].rearrange("p (a b) -> p a b", a=4))
        xdeps = [(xTt, s2, g) for s2 in range(2) for g in range(4)]
        aT, aTt = aTr.next()
        for fcn in range(4):
            bank, bkt = pgur.next()
            for kc in range(16):
                kb.mm(bank[:, 0:256], wg[:, kc, fcn * 128:(fcn + 1) * 128], xT[:, kc, :], kc == 0, kc == 15, reads=[wgt] + xdeps, writes=[bkt])
            for kc in range(16):
                kb.mm(bank[:, 256:512], wu[:, kc, fcn * 128:(fcn + 1) * 128], xT[:, kc, :], kc == 0, kc == 15, reads=[wut] + xdeps, writes=[bkt])
            sg, sgt = sgr.next()
            kb.op("act", "activation", reads=[bkt], writes=[sgt], out=sg[:, :], in_=bank[:, 0:256], func=AF.Silu)
            kb.op("dve", "tensor_tensor", reads=[sgt, bkt], writes=[(aTt, fcn)], out=aT[:, fcn, :], in0=bank[:, 256:512], in1=sg[:, :], op=ALU.mult)
        for s2 in range(2):
            yr, yrt = yrr.next()
            for nb in range(4):
                py, pyt = pyr.next()
                for kc in range(4):
                    kb.mm(py[:, :], aT[:, kc, s2 * 128:(s2 + 1) * 128], wd[:, kc, nb * 512:(nb + 1) * 512], kc == 0, kc == 3, reads=[wdt] + [(aTt, f) for f in range(4)], writes=[pyt])
                kb.op("dve", "tensor_copy", reads=[pyt], writes=[(yrt, nb)], out=yr[:, nb * 512:(nb + 1) * 512], in_=py[:, :])
            r0 = e * CAP + s2 * 128
            kb.dma("sp", out=YROWS[r0:r0 + 128, :], in_=yr[:, :], reads=[(yrt, nb) for nb in range(4)], writes=[("YR", e, s2)])


def build_layer(nc, kb, din, hnd, out_d, l, finish_check, SC, CST, ln_alloc, ln_apply, emit_h_tile):
    HTM, HFM, PROJ, DTR, VATM, ZSTM, YBR, RV, MG, H1B, LGT, XROWS, YROWS = [SC[k] for k in
        ("HTM", "HFM", "PROJ", "DTR", "VATM", "ZSTM", "YBR", "RV", "MG", "H1B", "LGT", "XROWS", "YROWS")]
    cstf, cstb = CST["cstf"], CST["cstb"]
    ident_b = cstb[:, 0, :]
    ident_f = cstf[:, 0, :]
    J_b = cstb[:, 1, :]
    U_f = cstf[:, 2, :]
    Lo_f = cstf[:, 3, :]
    ones_b = cstb[:, 5, :]
    ones_f = cstf[:, 5, :]

    kb.phase_begin()
    hfm = kb.sb("hfm", [128, 16, S], BF16)
    for tb in range(4):
        kb.dma("sp", out=hfm[:, :, tb * 512:(tb + 1) * 512], in_=HFM[:, :, tb * 512:(tb + 1) * 512], writes=[("hfm", tb)])
    wr = Ring(kb, "wpin", 4, [128, 16, 128], BF16)
    pr = Ring(kb, "pp", 6, [128, 512], F32, psum=True)
    sr = Ring(kb, "pstage", 3, [128, S], BF16)
    k = 0
    for c in range(31):
        w, wt = wr.next()
        kb.dma("pool", out=w[:, :, :], in_=din["w_in_t"][l, c], writes=[wt])
        stg, stt = sr.next()
        for tb in range(4):
            p, pt = pr.next()
            for kc in range(16):
                kb.mm(p[:, :], w[:, kc, :], hfm[:, kc, tb * 512:(tb + 1) * 512], kc == 0, kc == 15, reads=[wt, ("hfm", tb)], writes=[pt])
            k += 1
            if k % 2 == 0:
                kb.op("act", "activation", reads=[pt], writes=[(stt, tb)], out=stg[:, tb * 512:(tb + 1) * 512], in_=p[:, :], func=AF.Copy)
            else:
                kb.op("dve", "tensor_copy", reads=[pt], writes=[(stt, tb)], out=stg[:, tb * 512:(tb + 1) * 512], in_=p[:, :])
        kb.dma("sp", out=PROJ[c], in_=stg[:, :], reads=[(stt, tb) for tb in range(4)], writes=[("PROJ", c)])
    wtm = Ring(kb, "wtm", 2, [128, 16, 512], BF16)
    tmst = kb.sb("tmstage", [128, 16, 512], BF16)
    for b in range(2):
        w, wt = wtm.next()
        kb.dma("pool", out=w[:, :, :], in_=din["w_in_tm"][l, b], writes=[wt])
        for t in range(NT):
            p, pt = pr.next()
            for kc in range(16):
                kb.mm(p[:, :], hfm[:, kc, t * 128:(t + 1) * 128], w[:, kc, :], kc == 0, kc == 15, reads=[wt, ("hfm", t // 4)], writes=[pt])
            if b == 1:
                kb.op("act", "activation", reads=[pt], writes=[("tmst", t)], out=tmst[:, t, :], in_=p[:, :], func=AF.Silu)
            elif t % 2 == 0:
                kb.op("act", "activation", reads=[pt], writes=[("tmst", t)], out=tmst[:, t, :], in_=p[:, :], func=AF.Copy)
            else:
                kb.op("dve", "tensor_copy", reads=[pt], writes=[("tmst", t)], out=tmst[:, t, :], in_=p[:, :])
        dst = VATM if b == 0 else ZSTM
        kb.dma("sp", out=dst.rearrange("(t p) n -> p t n", p=128), in_=tmst[:, :, :], reads=[("tmst", t) for t in range(NT)], writes=["TMD%d" % b])
    wdt = kb.sb("wdt", [128, 16, 16], BF16)
    dtst = kb.sb("dtst", [128, 16, 16], F32)
    kb.dma("pool", out=wdt[:, :, :], in_=din["w_dt_tm"][l], writes=["wdt"])
    for t in range(NT):
        p, pt = pr.next()
        for kc in range(16):
            kb.mm(p[:, 0:16], hfm[:, kc, t * 128:(t + 1) * 128], wdt[:, kc, :], kc == 0, kc == 15, reads=["wdt", ("hfm", t // 4)], writes=[pt])
        kb.op("dve", "tensor_copy", reads=[pt], writes=[("dtst", t)], out=dtst[:, t, :], in_=p[:, 0:16])
    kb.dma("sp", out=DTR.rearrange("(t p) n -> p t n", p=128), in_=dtst[:, :, :], reads=[("dtst", t) for t in range(NT)], writes=["DTR"])
    kb.phase_end()
    if finish_check("P%d" % l):
        return

    kb.phase_begin()
    qT = kb.sb("qT", [128, 4, S], BF16)
    kTz = kb.sb("kTz", [128, 8, S], BF16)
    vtm = kb.sb("vtm", [128, 16, 512], BF16)
    yst = kb.sb("yst", [128, 4, S], BF16)
    for c in range(4):
        kb.op("pool", "memset", writes=[("kTzz", c)], ap=kTz[64:128, 2 * c, :], constant=0.0)
        kb.op("dve", "memset", writes=[("kTzz", c)], ap=kTz[0:64, 2 * c + 1, :], constant=0.0)
        kb.dma("sp", out=qT[:, c, :], in_=PROJ[CH["qa"] + c], writes=[("qT", c)])
        kb.dma("sp", out=kTz[0:64, 2 * c, :], in_=PROJ[CH["ka"] + c, 0:64, :], writes=[("kT", c)])
        kb.dma("sp", out=kTz[64:128, 2 * c + 1, :], in_=PROJ[CH["ka"] + c, 64:128, :], writes=[("kT", c)])
        kb.op("pool", "engine_nop", reads=[("kTzz", c), ("kT", c)], writes=[("kTall", c)])
    kb.dma("sp", out=vtm[:, :, :], in_=VATM.rearrange("(t p) n -> p t n", p=128), writes=["vtm"])
    vaug = kb.sb("vaugA", [128, 16, 8, 128], BF16)
    build_vaug(kb, vaug[:, :, :, :], vtm[:, :, :], ["vtm"], "vaug")
    DEL = [d for d in range(-1536, 1921, 128) if -1151 <= d <= 1535]
    Et = kb.sb("Et", [128, 2, len(DEL), 512], BF16)
    hkr = Ring(kb, "hk", 6, [128, 512], BF16)
    pj = kb.ps("pj", [128, 512], F32)

    def E_dma(h, di):
        hk, hkt = hkr.next()
        base = 1408 - DEL[di]
        kb.dma("pool", out=hk[:, :], in_=bass.AP(hnd["RV"], h * RVLEN + base, [[1, 128], [1, 512]]), writes=[hkt])
        return (h, di, hk, hkt)

    def E_mm(h, di, hk, hkt):
        kb.mm(pj[:, :], J_b, hk[:, :], True, True, reads=[hkt, "cstb"], writes=["pj"])
        if di % 3 == 0:
            kb.op("dve", "tensor_copy", reads=["pj"], writes=[("E", h % 2, di)], out=Et[:, h % 2, di, :], in_=pj[:, :])
        else:
            kb.op("act", "activation", reads=["pj"], writes=[("E", h % 2, di)], out=Et[:, h % 2, di, :], in_=pj[:, :], func=AF.Copy)

    pendingE = []
    inflight = []

    def pre_head(h):
        if h == 0:
            q = []
            for di in range(len(DEL)):
                q.append(E_dma(0, di))
                if len(q) > 2:
                    E_mm(*q.pop(0))
            while q:
                E_mm(*q.pop(0))
        while inflight:
            E_mm(*inflight.pop(0))
        while pendingE:
            E_mm(*E_dma(*pendingE.pop(0)))
        if h + 1 < 8:
            pendingE.extend((h + 1, di) for di in range(len(DEL)))

    def per_unit(i):
        if i % 2 == 1:
            if len(inflight) >= 4 or (inflight and not pendingE):
                E_mm(*inflight.pop(0))
            if pendingE:
                inflight.append(E_dma(*pendingE.pop(0)))

    def e_info(h, qb, kc):
        di = DEL.index(128 * kc - 512 * qb)
        return Et[:, h % 2, di, :], ("E", h % 2, di)

    attention(kb, 8,
              kT_ap=lambda h, kc: kTz[:, h, kc * 128:(kc + 1) * 128],
              qT_ap=lambda h, qb: qT[:, h // 2, qb * 512:(qb + 1) * 512],
              v_ap=lambda h, kc: vaug[:, kc, h, :],
              kcs_for=lambda qb: [kc for kc in range(16) if -1151 <= 128 * kc - 512 * qb <= 1535],
              e_info=e_info, scale=0.125, yst=yst, prefix="A",
              reads_k=lambda h: ("kTall", h // 2), reads_q=lambda h: ("qT", h // 2), reads_v=lambda h: "vaug", pre_head=pre_head, per_unit=per_unit)
    kb.dma("sp", out=YBR[0], in_=yst[:, :, :], reads=[("yst", h, qb) for h in range(8) for qb in range(4)], writes=[("YBR", 0)])
    kb.phase_end()
    if finish_check("A%d" % l):
        return

    kb.phase_begin()
    cp = kb.sb("cp", [128, 256], F32)
    kb.dma("sp", out=cp[:, :], in_=din["colp"][l], writes=["cp"])
    wuq = kb.sb("wuq", [128, 4, 1024], BF16)
    wukv = kb.sb("wukv", [128, 2, 1024], BF16)
    kb.dma("pool", out=wuq[:, :, :], in_=din["w_uq_t"][l], writes=["wuq"])
    kb.dma("pool", out=wukv[:, :, :], in_=din["w_ukv_t"][l], writes=["wukv"])
    ccr = Ring(kb, "cc", 2, [96, 512], F32)
    ssr = Ring(kb, "ss", 2, [96, 512], F32)
    krAr = Ring(kb, "krA", 2, [96, 512], BF16)
    krBr = Ring(kb, "krB", 2, [96, 512], BF16)
    QT = kb.sb("QT", [96, 8, S], BF16)
    KT = kb.sb("KT", [96, 8, S], BF16)
    vaug = kb.sb("vaugB", [128, 16, 8, 128], BF16)
    vtm = kb.sb("vtmB", [128, 16, 512], BF16)
    yst = kb.sb("ystB", [128, 4, S], BF16)
    cqr = Ring(kb, "cqb", 2, [128, 4, 512], BF16)
    ckr = Ring(kb, "ckb", 2, [128, 2, 512], BF16)
    sqb = kb.sb("sqb", [128, 4, 512], BF16)
    cqn = kb.sb("cqn", [128, 4, 512], BF16)
    ckvn = kb.sb("ckvn", [128, 2, 512], BF16)
    rt = kb.sb("rt", [128, 512], F32)
    t1 = kb.sb("t1", [96, 512], F32)
    t2 = kb.sb("t2", [96, 512], F32)
    krope = kb.sb("krope", [96, 512], BF16)
    pss = kb.ps("pss", [128, 512], F32)
    srrB = Ring(kb, "BS", 4, [128, 512], F32, psum=True)
    pqr = Ring.wrap(srrB.tiles[0:2], srrB.names[0:2])
    pq2r = Ring.wrap(srrB.tiles[2:3], srrB.names[2:3])

    def rms_block(src, stag, nch, gcol, dst, dtag, inv_n):
        kb.op("dve", "tensor_tensor", reads=[stag], writes=["sqb"], out=sqb[:, 0:nch, :], in0=src[:, 0:nch, :], in1=src[:, 0:nch, :], op=ALU.mult)
        for c in range(nch):
            kb.mm(pss[:, :], ones_b, sqb[:, c, :], c == 0, c == nch - 1, reads=["sqb", "cstb"], writes=["pss"])
        kb.op("dve", "tensor_scalar", reads=["pss"], writes=["rt"], out=rt[:, :], in0=pss[:, :], scalar1=inv_n, scalar2=EPS, op0=ALU.mult, op1=ALU.add)
        kb.op("act", "activation", reads=["rt"], writes=["rt"], out=rt[:, :], in_=rt[:, :], func=AF.Sqrt)
        kb.op("dve", "reciprocal", reads=["rt"], writes=["rt"], out=rt[:, :], in_=rt[:, :])
        for c in range(nch):
            kb.op("dve", "scalar_tensor_tensor", reads=[stag, "rt", "cp"], writes=[dtag], out=dst[:, c, :], in0=src[:, c, :],
                  scalar=cp[:, gcol + c:gcol + c + 1], in1=rt[:, :], op0=ALU.mult, op1=ALU.mult)

    for tb in range(4):
        tbs = slice(tb * 512, (tb + 1) * 512)
        cqb, cqt = cqr.next()
        ckb, ckt = ckr.next()
        cc, cct = ccr.next()
        ss, sst = ssr.next()
        krA, krAt = krAr.next()
        krB, krBt = krBr.next()
        kb.dma("sp", out=cc[64:96, :], in_=din["rope"][0, 64:96, tbs], writes=[cct])
        kb.dma("sp", out=ss[64:96, :], in_=din["rope"][1, 64:96, tbs], writes=[sst])
        kb.dma("sp", out=krA[64:96, :], in_=PROJ[CH["krx"], 0:32, tbs], writes=[krAt])
        kb.dma("sp", out=krB[64:96, :], in_=PROJ[CH["krx"], 32:64, tbs], writes=[krBt])
        for c in range(4):
            kb.dma("sp", out=cqb[:, c, :], in_=PROJ[CH["cq"] + c, :, tbs], writes=[cqt])
        for c in range(2):
            kb.dma("sp", out=ckb[:, c, :], in_=PROJ[CH["ckv"] + c, :, tbs], writes=[ckt])
        rms_block(cqb, cqt, 4, 64, cqn, "cqn", 1.0 / 512)
        for h in range(8):
            pq, pqt = pqr.next()
            pq2, pq2t = pq2r.next()
            for kc in range(4):
                kb.mm(pq[0:96, :], wuq[:, kc, 96 * h:96 * h + 96], cqn[:, kc, :], kc == 0, kc == 3, reads=["wuq", "cqn"], writes=[pqt])
            for kc in range(4):
                kb.mm(pq2[64:96, :], wuq[:, kc, 768 + 32 * h:768 + 32 * h + 32], cqn[:, kc, :], kc == 0, kc == 3, reads=["wuq", "cqn"], writes=[pq2t])
            kb.op("act", "activation", reads=[pqt], writes=[("QT", h, tb)], out=QT[0:64, h, tbs], in_=pq[0:64, :], func=AF.Copy)
            kb.op("dve", "tensor_tensor", reads=[pqt, cct], writes=["t1"], out=t1[64:96, :], in0=pq[64:96, :], in1=cc[64:96, :], op=ALU.mult)
            kb.op("dve", "tensor_tensor", reads=[pq2t, sst], writes=["t2"], out=t2[64:96, :], in0=pq2[64:96, :], in1=ss[64:96, :], op=ALU.mult)
            kb.op("pool", "tensor_tensor", reads=["t1", "t2"], writes=[("QT", h, tb)], out=QT[64:96, h, tbs], in0=t1[64:96, :], in1=t2[64:96, :], op=ALU.add)
        rms_block(ckb, ckt, 2, 68, ckvn, "ckvn", 1.0 / 256)
        for h in range(8):
            pq, pqt = pqr.next()
            for kc in range(2):
                kb.mm(pq[0:64, :], wukv[:, kc, 64 * h:64 * h + 64], ckvn[:, kc, :], kc == 0, kc == 1, reads=["wukv", "ckvn"], writes=[pqt])
            if h % 2 == 0:
                kb.op("act", "activation", reads=[pqt], writes=[("KT", h, tb)], out=KT[0:64, h, tbs], in_=pq[0:64, :], func=AF.Copy)
            else:
                kb.op("dve", "tensor_copy", reads=[pqt], writes=[("KT", h, tb)], out=KT[0:64, h, tbs], in_=pq[0:64, :])
        kb.op("dve", "tensor_tensor", reads=[krAt, cct], writes=["t1"], out=t1[64:96, :], in0=krA[64:96, :], in1=cc[64:96, :], op=ALU.mult)
        kb.op("dve", "tensor_tensor", reads=[krBt, sst], writes=["t2"], out=t2[64:96, :], in0=krB[64:96, :], in1=ss[64:96, :], op=ALU.mult)
        kb.op("pool", "tensor_tensor", reads=["t1", "t2"], writes=["krope"], out=krope[64:96, :], in0=t1[64:96, :], in1=t2[64:96, :], op=ALU.add)
        for h in range(8):
            kb.op("pool", "tensor_copy", reads=["krope"], writes=[("KT", h, tb)], out=KT[64:96, h, tbs], in_=krope[64:96, :])
        for tt in range(4):
            pq, pqt = pqr.next()
            for kc in range(2):
                kb.mm(pq[:, :], ckvn[:, kc, tt * 128:(tt + 1) * 128], wukv[:, kc, 512:1024], kc == 0, kc == 1, reads=["wukv", "ckvn"], writes=[pqt])
            if tt % 2 == 0:
                kb.op("act", "activation", reads=[pqt], writes=[("vtm", tb, tt)], out=vtm[:, tb * 4 + tt, :], in_=pq[:, :], func=AF.Copy)
            else:
                kb.op("dve", "tensor_copy", reads=[pqt], writes=[("vtm", tb, tt)], out=vtm[:, tb * 4 + tt, :], in_=pq[:, :])

    build_vaug(kb, vaug[:, :, :, :], vtm[:, :, :], [("vtm", tb, tt) for tb in range(4) for tt in range(4)], "vaugB")
    attention(kb, 8,
              kT_ap=lambda h, kc: KT[0:96, h, kc * 128:(kc + 1) * 128],
              qT_ap=lambda h, qb: QT[0:96, h, qb * 512:(qb + 1) * 512],
              v_ap=lambda h, kc: vaug[:, kc, h, :],
              kcs_for=lambda qb: list(range(16)),
              e_info=None, scale=96.0 ** -0.5, yst=yst, prefix="B",
              reads_k=lambda h: ("KTall", h), reads_q=lambda h: ("QTall", h), reads_v=lambda h: "vtmall",
              pre_head=lambda h: [kb.op("pool", "engine_nop", reads=[("KT", h, tb) for tb in range(4)] + [("QT", h, tb) for tb in range(4)] + ["vaugB"],
                                        writes=[("KTall", h), ("QTall", h), "vtmall"])], srr=srrB)
    kb.dma("sp", out=YBR[1], in_=yst[:, :, :], reads=[("yst", h, qb) for h in range(8) for qb in range(4)], writes=[("YBR", 1)])
    kb.phase_end()
    if finish_check("B%d" % l):
        return

    kb.phase_begin()
    cp = kb.sb("cpC", [128, 256], F32)
    kb.dma("sp", out=cp[:, :], in_=din["colp"][l], writes=["cp"])
    hg = kb.sb("hg", [128, 4, S + 30], BF16)
    kb.op("pool", "memset", writes=["hgpad"], ap=hg[:, :, 0:15], constant=0.0)
    kb.op("pool", "memset", writes=["hgpad"], ap=hg[:, :, S + 15:S + 30], constant=0.0)
    gar = Ring(kb, "ga", 2, [128, S], BF16)
    ggr = Ring(kb, "gg", 2, [128, S], BF16)
    for c in range(4):
        ga, gat = gar.next()
        gg, ggt = ggr.next()
        kb.dma("sp", out=ga[:, :], in_=PROJ[CH["glua"] + c], writes=[gat])
        kb.dma("sp", out=gg[:, :], in_=PROJ[CH["glug"] + c], writes=[ggt])
        kb.op("act", "activation", reads=[ggt], writes=[ggt], out=gg[:, :], in_=gg[:, :], func=AF.Sigmoid)
        kb.op("dve", "tensor_tensor", reads=[gat, ggt, "hgpad"], writes=[("hg", c)], out=hg[:, c, 15:15 + S], in0=ga[:, :], in1=gg[:, :], op=ALU.mult)
    dg = kb.sb("dg", [128, 4, 31, 128], BF16)
    for c in range(4):
        for j in range(31):
            eng = "dve" if (c * 31 + j) % 2 == 0 else "pool"
            kb.op(eng, "tensor_scalar", reads=["cp", "cstb"], writes=[("dg", c)], out=dg[:, c, j, :], in0=ident_b,
                  scalar1=cp[:, 82 + c * 31 + j:83 + c * 31 + j], scalar2=None, op0=ALU.mult)
    yc = kb.sb("yc", [128, 4, 512], F32)
    ysq = kb.sb("ysq", [128, 4, 512], F32)
    mt = kb.sb("mt", [128, 512], F32)
    m2 = kb.sb("m2", [128, 512], F32)
    vt = kb.sb("vt", [128, 512], F32)
    tmr = Ring(kb, "tmpC", 2, [128, 512], F32)
    yo = kb.sb("yoC", [128, 4, S], BF16)
    pcr = Ring(kb, "pc", 3, [128, 512], F32, psum=True)
    psm = kb.ps("psm", [128, 512], F32)
    psq = kb.ps("psq", [128, 512], F32)
    for tb in range(4):
        tbs = slice(tb * 512, (tb + 1) * 512)
        for c in range(4):
            pc, pct = pcr.next()
            for j in range(31):
                kb.mm(pc[:, :], dg[:, c, j, :], hg[:, c, tb * 512 + j:tb * 512 + j + 512], j == 0, j == 30, reads=[("dg", c), ("hg", c)], writes=[pct])
            kb.op("dve", "tensor_scalar", reads=[pct, "cp"], writes=[("yc", c)], out=yc[:, c, :], in0=pc[:, :], scalar1=cp[:, 70 + c:71 + c], scalar2=None, op0=ALU.add)
            kb.op("act", "activation", reads=[("yc", c)], writes=[("ysq", c)], out=ysq[:, c, :], in_=yc[:, c, :], func=AF.Square)
        for c in range(4):
            kb.mm(psm[:, :], ones_f, yc[:, c, :], c == 0, c == 3, reads=[("yc", c), "cstf"], writes=["psm"])
        for c in range(4):
            kb.mm(psq[:, :], ones_f, ysq[:, c, :], c == 0, c == 3, reads=[("ysq", c), "cstf"], writes=["psq"])
        kb.op("dve", "tensor_scalar", reads=["psm"], writes=["mt"], out=mt[:, :], in0=psm[:, :], scalar1=1.0 / 512, scalar2=None, op0=ALU.mult)
        kb.op("dve", "tensor_tensor", reads=["mt"], writes=["m2"], out=m2[:, :], in0=mt[:, :], in1=mt[:, :], op=ALU.mult)
        kb.op("dve", "scalar_tensor_tensor", reads=["psq", "m2"], writes=["vt"], out=vt[:, :], in0=psq[:, :], scalar=1.0 / 512, in1=m2[:, :], op0=ALU.mult, op1=ALU.subtract)
        kb.op("dve", "tensor_scalar", reads=["vt"], writes=["vt"], out=vt[:, :], in0=vt[:, :], scalar1=EPS, scalar2=None, op0=ALU.add)
        kb.op("act", "activation", reads=["vt"], writes=["vt"], out=vt[:, :], in_=vt[:, :], func=AF.Sqrt)
        kb.op("dve", "reciprocal", reads=["vt"], writes=["vt"], out=vt[:, :], in_=vt[:, :])
        for c in range(4):
            tm, tmt = tmr.next()
            kb.op("dve", "tensor_tensor", reads=[("yc", c), "mt"], writes=[tmt], out=tm[:, :], in0=yc[:, c, :], in1=mt[:, :], op=ALU.subtract)
            kb.op("pool", "tensor_tensor", reads=[tmt, "vt"], writes=[tmt], out=tm[:, :], in0=tm[:, :], in1=vt[:, :], op=ALU.mult)
            kb.op("act", "activation", reads=[tmt, "cp"], writes=[("yoC", c, tb)], out=yo[:, c, tbs], in_=tm[:, :], func=AF.Silu,
                  scale=cp[:, 74 + c:75 + c], bias=cp[:, 78 + c:79 + c])
    kb.dma("sp", out=YBR[2], in_=yo[:, :, :], reads=[("yoC", c, tb) for c in range(4) for tb in range(4)], writes=[("YBR", 2)])
    kb.phase_end()
    if finish_check("C%d" % l):
        return

    kb.phase_begin()
    cp = kb.sb("cpD", [128, 256], F32)
    kb.dma("sp", out=cp[:, :], in_=din["colp"][l], writes=["cp"])
    rp = kb.sb("rpD", [128, 640], F32)
    kb.dma("sp", out=rp[:, :], in_=din["rowp"][l, 0:1, 8192:8832].partition_broadcast(128), writes=["rp"])
    gnd = rp[:, 0:512]
    dsk = rp[:, 548:556]
    pbig = kb.ps("pbig", [128, 1024], F32)
    pA = kb.ps("pA", [128, 512], F32)
    pB = kb.ps("pB", [128, 512], F32)
    pC = kb.ps("pC", [128, 512], F32)
    pD = kb.ps("pD", [128, 512], F32)
    pTr = Ring(kb, "pT", 2, [128, 512], BF16, psum=True)
    convr = Ring.wrap([pA, pB, pC], ["pA", "pB", "pC"])
    dgd = kb.sb("dgd", [128, 8, 5, 128], BF16)
    for c in range(8):
        for j in range(5):
            eng = "dve" if (c * 5 + j) % 2 == 0 else "pool"
            kb.op(eng, "tensor_scalar", reads=["cp", "cstb"], writes=[("dgd", c)], out=dgd[:, c, j, :], in0=ident_b,
                  scalar1=cp[:, 206 + c * 5 + j:207 + c * 5 + j], scalar2=None, op0=ALU.mult)
    xbc = kb.sb("xbc", [128, 8, S], BF16)
    xinr = Ring(kb, "xinD", 2, [128, S + 4], BF16)
    for i in range(2):
        kb.op("pool", "memset", writes=[xinr.names[i] + "pad"], ap=xinr.tiles[i][:, 0:2], constant=0.0)
        kb.op("pool", "memset", writes=[xinr.names[i] + "pad"], ap=xinr.tiles[i][:, S + 2:S + 4], constant=0.0)
    for c in range(8):
        xi, xit = xinr.next()
        kb.dma("sp", out=xi[:, 2:2 + S], in_=PROJ[CH["xs"] + c], reads=[xit + "pad"], writes=[xit])
        for tb in range(4):
            pc, pct = convr.next()
            for j in range(5):
                kb.mm(pc[:, :], dgd[:, c, j, :], xi[:, tb * 512 + j:tb * 512 + j + 512], j == 0, j == 4, reads=[("dgd", c), xit, xit + "pad"], writes=[pct])
            kb.op("act", "activation", reads=[pct, "cp"], writes=[("xbc", c)], out=xbc[:, c, tb * 512:(tb + 1) * 512], in_=pc[:, :], func=AF.Silu,
                  bias=cp[:, 246 + c:247 + c], scale=1.0)
    xtm = kb.sb("xtm", [128, 16, 512], BF16)
    btm = kb.sb("btm", [128, 16, 256], BF16)
    for t in range(NT):
        pt, ptt = pTr.next()
        for c in range(4):
            kb.op("pe", "transpose", reads=[("xbc", c), "cstb"], writes=[ptt], out=pt[:, c * 128:(c + 1) * 128], in_=xbc[:, c, t * 128:(t + 1) * 128], identity=ident_b)
        kb.op("dve", "tensor_copy", reads=[ptt], writes=[("xtm", t)], out=xtm[:, t, :], in_=pt[:, :])
        pt, ptt = pTr.next()
        for g in range(2):
            kb.op("pe", "transpose", reads=[("xbc", 4 + g), "cstb"], writes=[ptt], out=pt[:, g * 128:(g + 1) * 128], in_=xbc[:, 4 + g, t * 128:(t + 1) * 128], identity=ident_b)
        kb.op("dve", "tensor_copy", reads=[ptt], writes=[("btm", t)], out=btm[:, t, :], in_=pt[:, 0:256])
    def small(name):
        return kb.sb(name, [128, 16, 16], F32)
    dtr, dtv, av, Ecs, Tot, negE, wend, expE, dec = [small(n) for n in ("dtr", "dtv", "av", "Ecs", "Tot", "negE", "wend", "expE", "dec")]
    negA = kb.sb("negA", [128, 16], F32)
    kb.dma("sp", out=dtr[:, :, :], in_=DTR.rearrange("(t p) n -> p t n", p=128), writes=["dtr"])
    kb.op("dve", "tensor_tensor", reads=["dtr", "rp"], writes=["dtv"], out=dtv[:, :, :], in0=dtr[:, :, :],
          in1=rp[:, 556:572].unsqueeze(1).to_broadcast([128, 16, 16]), op=ALU.add)
    kb.op("act", "activation", reads=["dtv"], writes=["dtv"], out=dtv[:, :, :], in_=dtv[:, :, :], func=AF.Exp)
    kb.op("act", "activation", reads=["dtv"], writes=["dtv"], out=dtv[:, :, :], in_=dtv[:, :, :], func=AF.Ln, bias=1.0, scale=1.0)
    kb.op("act", "activation", reads=["rp"], writes=["negA"], out=negA[:, :], in_=rp[:, 572:588], func=AF.Exp)
    kb.op("dve", "tensor_scalar", reads=["negA"], writes=["negA"], out=negA[:, :], in0=negA[:, :], scalar1=-1.0, scalar2=None, op0=ALU.mult)
    kb.op("dve", "tensor_tensor", reads=["dtv", "negA"], writes=["av"], out=av[:, :, :], in0=dtv[:, :, :],
          in1=negA[:, :].unsqueeze(1).to_broadcast([128, 16, 16]), op=ALU.mult)
    for t in range(NT):
        kb.mm(pD[:, t * 16:t * 16 + 8], U_f, av[:, t, 0:8], True, True, reads=["av", "cstf"], writes=["pD"], last=False)
        kb.mm(pD[:, t * 16 + 8:t * 16 + 16], Lo_f, av[:, t, 8:16], True, True, reads=["av", "cstf"], writes=["pD"], last=False)
        kb.mm(pD[:, 256 + t * 16:256 + t * 16 + 16], ones_f, av[:, t, :], True, True, reads=["av", "cstf"], writes=["pD"], last=(t == NT - 1))
    kb.op("dve", "tensor_copy", reads=["pD"], writes=["Ecs"], out=Ecs[:, :, :], in_=pD[:, 0:256].rearrange("p (a b) -> p a b", b=16))
    kb.op("dve", "tensor_copy", reads=["pD"], writes=["Tot"], out=Tot[:, :, :], in_=pD[:, 256:512].rearrange("p (a b) -> p a b", b=16))
    kb.op("dve", "tensor_scalar", reads=["Ecs"], writes=["negE"], out=negE[:, :, :], in0=Ecs[:, :, :], scalar1=-1.0, scalar2=None, op0=ALU.mult)
    kb.op("dve", "tensor_tensor", reads=["Tot", "Ecs"], writes=["wend"], out=wend[:, :, :], in0=Tot[:, :, :], in1=Ecs[:, :, :], op=ALU.subtract)
    kb.op("act", "activation", reads=["wend"], writes=["wend"], out=wend[:, :, :], in_=wend[:, :, :], func=AF.Exp)
    kb.op("dve", "tensor_tensor", reads=["wend", "dtv"], writes=["wend"], out=wend[:, :, :], in0=wend[:, :, :], in1=dtv[:, :, :], op=ALU.mult)
    kb.op("act", "activation", reads=["Ecs"], writes=["expE"], out=expE[:, :, :], in_=Ecs[:, :, :], func=AF.Exp)
    kb.op("act", "activation", reads=["Tot"], writes=["dec"], out=dec[:, :, :], in_=Tot[:, :, :], func=AF.Exp)

    ytm = kb.sb("ytm", [128, 16, 512], F32)
    cbb = kb.sb("cbb", [128, 16, 2, 128], BF16)
    cbf = kb.sb("cbf", [128, 2, 128], BF16)
    Dt = kb.sb("Dt", [128, 8, 128], F32)
    ex = kb.sb("ex", [128, 8, 128], BF16)
    Mtr = Ring(kb, "Mt", 2, [128, 8, 128], BF16)
    xdr = Ring(kb, "xdt", 2, [128, 8, 64], BF16)
    xwr = Ring(kb, "xw", 2, [128, 8, 64], BF16)
    tmpa = kb.sb("tmpDa", [128, 512], F32)
    tmpb = kb.sb("tmpDb", [128, 512], F32)
    Hs = [kb.sb("Hs%d" % d, [128, 512], F32) for d in range(2)]
    Hb = [kb.sb("Hb%d" % d, [128, 512], BF16) for d in range(2)]
    for d in range(2):
        kb.op("pool", "memset", writes=["Hs%d" % d], ap=Hs[d][:, :], constant=0.0)
        kb.op("pool", "memset", writes=["Hb%d" % d], ap=Hb[d][:, :], constant=0.0)
    units = [(0, c) for c in range(16)] + [(1, c) for c in range(15, -1, -1)]
    st = {}

    def d_stage1(u):
        d, c = u
        cs = slice(c * 128, (c + 1) * 128)
        if d == 0:
            for g in range(2):
                kb.mm(pA[:, g * 128:(g + 1) * 128], xbc[:, 4 + g, cs], xbc[:, 6 + g, cs], True, True, reads=[("xbc", 4 + g), ("xbc", 6 + g)], writes=["pA"], last=(g == 1))
            kb.op("dve", "tensor_tensor", reads=["pA", "cstf"], writes=["cbf"], out=cbf[:, :, :], in0=pA[:, 0:256].rearrange("p (g i) -> p g i", g=2),
                  in1=U_f.unsqueeze(1).to_broadcast([128, 2, 128]), op=ALU.mult)
            kb.op("dve", "tensor_tensor", reads=["pA", "cstf"], writes=[("cbb", c)], out=cbb[:, c, :, :], in0=pA[:, 0:256].rearrange("p (g i) -> p g i", g=2),
                  in1=Lo_f.unsqueeze(1).to_broadcast([128, 2, 128]), op=ALU.mult)
        tri = U_f if d == 0 else Lo_f
        for h in range(8):
            hd = d * 8 + h
            kb.mm(pbig[:, h * 128:(h + 1) * 128], av[:, c, hd:hd + 1].to_broadcast([128, 128]), tri, True, True, reads=["av", "cstf"], writes=["pbig"], last=(h == 7))
        kb.op("dve", "tensor_tensor", reads=["pbig", "negE"], writes=["Dt"], out=Dt[:, :, :], in0=pbig[:, :].rearrange("p (h i) -> p h i", h=8),
              in1=negE[:, c, d * 8:d * 8 + 8].unsqueeze(2).to_broadcast([128, 8, 128]), op=ALU.add)
        kb.op("act", "activation", reads=["Dt"], writes=["ex"], out=ex[:, :, :], in_=Dt[:, :, :], func=AF.Exp)
        Mt, Mtt = Mtr.next()
        for g in range(2):
            cbx = cbf[:, g:g + 1, :] if d == 0 else cbb[:, c, g:g + 1, :]
            kb.op("dve", "scalar_tensor_tensor", reads=["ex", "cbf" if d == 0 else ("cbb", c)], writes=[Mtt], out=Mt[:, 4 * g:4 * g + 4, :], in0=ex[:, 4 * g:4 * g + 4, :],
                  scalar=1.0, in1=cbx.to_broadcast([128, 4, 128]), op0=ALU.min, op1=ALU.mult)
        xd, xdt_ = xdr.next()
        xw, xwt = xwr.next()
        x3 = xtm[:, c, :].rearrange("p (h e) -> p h e", h=8)
        kb.op("pool", "tensor_tensor", reads=[("xtm", c), "dtv"], writes=[xdt_], out=xd[:, :, :], in0=x3,
              in1=dtv[:, c, d * 8:d * 8 + 8].unsqueeze(2).to_broadcast([128, 8, 64]), op=ALU.mult)
        kb.op("pool", "tensor_tensor", reads=[("xtm", c), "wend"], writes=[xwt], out=xw[:, :, :], in0=x3,
              in1=wend[:, c, d * 8:d * 8 + 8].unsqueeze(2).to_broadcast([128, 8, 64]), op=ALU.mult)
        st[u] = (Mt, Mtt, xd, xdt_, xw, xwt)

    def d_stage2(u):
        d, c = u
        cs = slice(c * 128, (c + 1) * 128)
        Mt, Mtt, xd, xdt_, xw, xwt = st.pop(u)
        for h in range(8):
            kb.mm(pB[:, h * 64:(h + 1) * 64], Mt[:, h, :], xd[:, h, :], True, True, reads=[Mtt, xdt_], writes=["pB"], last=(h == 7))
        for g in range(2):
            kb.mm(pD[:, g * 256:(g + 1) * 256], xbc[:, 6 + g, cs], Hb[d][:, g * 256:(g + 1) * 256], True, True, reads=[("xbc", 6 + g), "Hb%d" % d], writes=["pD"], last=(g == 1))
        for g in range(2):
            kb.mm(pC[:, g * 256:(g + 1) * 256], btm[:, c, g * 128:(g + 1) * 128], xw[:, 4 * g:4 * g + 4, :].rearrange("p h e -> p (h e)"), True, True,
                  reads=[("btm", c), xwt], writes=["pC"], last=(g == 1))
        kb.op("dve", "tensor_tensor", reads=["pD", "expE"], writes=["tmpDa"], out=tmpa[:, :].rearrange("p (h e) -> p h e", h=8), in0=pD[:, :].rearrange("p (h e) -> p h e", h=8),
              in1=expE[:, c, d * 8:d * 8 + 8].unsqueeze(2).to_broadcast([128, 8, 64]), op=ALU.mult)
        if d == 0:
            kb.op("dve", "tensor_tensor", reads=["pB", "tmpDa"], writes=[("ytm", c)], out=ytm[:, c, :], in0=pB[:, :], in1=tmpa[:, :], op=ALU.add)
        else:
            kb.op("dve", "tensor_tensor", reads=["pB", "tmpDa"], writes=["tmpDb"], out=tmpb[:, :], in0=pB[:, :], in1=tmpa[:, :], op=ALU.add)
            kb.op("pool", "tensor_tensor", reads=["tmpDb", ("ytm", c)], writes=[("ytm", c)], out=ytm[:, c, :], in0=ytm[:, c, :], in1=tmpb[:, :], op=ALU.add)
        hn = "Hs%d" % d
        kb.op("dve", "tensor_tensor", reads=[hn, "dec"], writes=[hn], out=Hs[d][:, :].rearrange("p (h e) -> p h e", h=8), in0=Hs[d][:, :].rearrange("p (h e) -> p h e", h=8),
              in1=dec[:, c, d * 8:d * 8 + 8].unsqueeze(2).to_broadcast([128, 8, 64]), op=ALU.mult)
        kb.op("dve", "tensor_tensor", reads=[hn, "pC"], writes=[hn], out=Hs[d][:, :], in0=pC[:, :], in1=Hs[d][:, :], op=ALU.add)
        kb.op("act", "activation", reads=[hn], writes=["Hb%d" % d], out=Hb[d][:, :], in_=Hs[d][:, :], func=AF.Copy)

    for idx in range(len(units) + 1):
        if idx < len(units):
            d_stage1(units[idx])
        if idx >= 1:
            d_stage2(units[idx - 1])

    zsr = Ring(kb, "zs", 2, [128, 512], BF16)
    yz = kb.sb("yz", [128, 512], F32)
    junk = kb.sb("junkD", [128, 512], F32)
    yoD = kb.sb("yoD", [128, 512], BF16)
    ssq = kb.sb("ssq", [128, 1], F32)
    mhalfD = kb.sb("mhalfD", [128, 1], F32)
    kb.op("pool", "memset", writes=["mhalfD"], ap=mhalfD[:, :], constant=-0.5)
    ydr = Ring(kb, "ydT", 2, [128, 4, 128], BF16)
    for c in range(NT):
        zs, zst = zsr.next()
        kb.dma("sp", out=zs[:, :], in_=ZSTM[c * 128:(c + 1) * 128, :], writes=[zst])
        kb.op("pool", "tensor_tensor", reads=[("xtm", c), "rp"], writes=["tmpDb"], out=tmpb[:, :].rearrange("p (h e) -> p h e", h=8), in0=xtm[:, c, :].rearrange("p (h e) -> p h e", h=8),
              in1=dsk.unsqueeze(2).to_broadcast([128, 8, 64]), op=ALU.mult)
        kb.op("pool", "tensor_tensor", reads=["tmpDb", ("ytm", c)], writes=[("ytm", c)], out=ytm[:, c, :], in0=ytm[:, c, :], in1=tmpb[:, :], op=ALU.add)
        kb.op("dve", "tensor_tensor", reads=[("ytm", c), zst], writes=["yz"], out=yz[:, :], in0=ytm[:, c, :], in1=zs[:, :], op=ALU.mult)
        kb.op("act", "activation", reads=["yz"], writes=["junkD", "ssq"], out=junk[:, :], in_=yz[:, :], func=AF.Square, accum_out=ssq[:, 0:1])
        kb.op("dve", "tensor_scalar", reads=["ssq"], writes=["ssq"], out=ssq[:, :], in0=ssq[:, :], scalar1=1.0 / 512, scalar2=EPS, op0=ALU.mult, op1=ALU.add)
        kb.op("pool", "tensor_tensor", reads=["ssq", "mhalfD"], writes=["ssq"], out=ssq[:, :], in0=ssq[:, :], in1=mhalfD[:, :], op=ALU.pow)
        kb.op("dve", "scalar_tensor_tensor", reads=["yz", "ssq", "rp"], writes=["yoD"], out=yoD[:, :], in0=yz[:, :], scalar=ssq[:, 0:1], in1=gnd, op0=ALU.mult, op1=ALU.mult)
        pt, ptt = pTr.next()
        for j in range(4):
            kb.op("pe", "transpose", reads=["yoD", "cstb"], writes=[ptt], out=pt[:, j * 128:(j + 1) * 128], in_=yoD[:, j * 128:(j + 1) * 128], identity=ident_b)
        yd, ydt = ydr.next()
        kb.op("dve", "tensor_copy", reads=[ptt], writes=[ydt], out=yd[:, :, :], in_=pt[:, :].rearrange("p (a b) -> p a b", a=4))
        kb.dma("sp", out=YBR[3, :, :, c * 128:(c + 1) * 128], in_=yd[:, :, :], reads=[ydt], writes=[("YBR3", c)])
    kb.phase_end()
    if finish_check("D%d" % l):
        return

    kb.phase_begin()
    cp = kb.sb("cpM", [128, 256], F32)
    kb.dma("sp", out=cp[:, :], in_=din["colp"][l], writes=["cp"])
    hfb = kb.sb("hfbM", [128, 16, 1024], BF16)
    ybm = kb.sb("ybm", [128, 4, 4, 1024], BF16)
    mg = kb.sb("mgM", [128, 16, 1024], BF16)
    wgr = Ring(kb, "wg", 6, [128, 16, 128], BF16)
    wbr = Ring(kb, "wb", 6, [128, 4, 128], BF16)
    gsr = Ring(kb, "gs", 3, [128, 512], BF16)
    maccr = Ring(kb, "macc", 2, [128, 512], F32)
    tmr = Ring(kb, "tmpM", 3, [128, 512], F32)
    pgr = Ring(kb, "pg", 4, [128, 512], F32, psum=True)
    pbr = Ring(kb, "pbm", 4, [128, 512], F32, psum=True)
    for sbk in range(2):
        sbs = slice(sbk * 1024, (sbk + 1) * 1024)
        kb.dma("sp", out=hfb[:, :, :], in_=HFM[:, :, sbs], writes=["hfb"])
        for i in range(4):
            kb.dma("sp", out=ybm[:, i, :, :], in_=YBR[i, :, :, sbs], writes=[("ybm", i)])
        for fc in range(16):
            macs = [maccr.next() for _ in range(2)]
            for i in range(4):
                wg, wgt = wgr.next()
                wb, wbt = wbr.next()
                kb.dma("pool", out=wg[:, :, :], in_=din["w_gate_t"][l, i, fc], writes=[wgt])
                kb.dma("pool", out=wb[:, :, :], in_=din["w_br_t"][l, i, fc], writes=[wbt])
                for hf in range(2):
                    hs = slice(hf * 512, (hf + 1) * 512)
                    pg, pgt = pgr.next()
                    pb, pbt = pbr.next()
                    for kc in range(16):
                        kb.mm(pg[:, :], wg[:, kc, :], hfb[:, kc, hs], kc == 0, kc == 15, reads=[wgt, "hfb"], writes=[pgt])
                    for kc in range(4):
                        kb.mm(pb[:, :], wb[:, kc, :], ybm[:, i, kc, hs], kc == 0, kc == 3, reads=[wbt, ("ybm", i)], writes=[pbt])
                    gs, gst = gsr.next()
                    kb.op("act", "activation", reads=[pgt, "cp"], writes=[gst], out=gs[:, :], in_=pg[:, :], func=AF.Sigmoid, bias=cp[:, i * 16 + fc:i * 16 + fc + 1], scale=1.0)
                    mac, mact = macs[hf]
                    if i == 0:
                        kb.op("dve", "tensor_tensor", reads=[gst, pbt], writes=[mact], out=mac[:, :], in0=pb[:, :], in1=gs[:, :], op=ALU.mult)
                    else:
                        tm, tmt = tmr.next()
                        kb.op("dve", "tensor_tensor", reads=[gst, pbt], writes=[tmt], out=tm[:, :], in0=pb[:, :], in1=gs[:, :], op=ALU.mult)
                        if i < 3:
                            kb.op("dve", "tensor_tensor", reads=[tmt, mact], writes=[mact], out=mac[:, :], in0=mac[:, :], in1=tm[:, :], op=ALU.add)
                        else:
                            kb.op("dve", "tensor_tensor", reads=[tmt, mact], writes=[("mg", fc)], out=mg[:, fc, hs], in0=mac[:, :], in1=tm[:, :], op=ALU.add)
        kb.dma("sp", out=MG[:, :, sbs], in_=mg[:, :, :], reads=[("mg", fc) for fc in range(16)], writes=[("MG", sbk)])
    kb.phase_end()
    if finish_check("M1%d" % l):
        return

    UD = SC["UD"]
    kb.phase_begin()
    mga = kb.sb("mga", [128, 16, S], BF16)
    for tb in range(4):
        kb.dma("sp", out=mga[:, :, tb * 512:(tb + 1) * 512], in_=MG[:, :, tb * 512:(tb + 1) * 512], writes=[("mga", tb)])
    wor = Ring(kb, "wo", 2, [128, 16, 512], BF16)
    hsr = Ring(kb, "hsl", 3, [128, 512], F32)
    usr = Ring(kb, "usl", 3, [128, 512], F32)
    por = Ring(kb, "po", 4, [128, 512], F32, psum=True)
    for nb in range(4):
        nbs = slice(nb * 512, (nb + 1) * 512)
        wo, wot = wor.next()
        kb.dma("pool", out=wo[:, :, :], in_=din["w_out_t"][l, nb], writes=[wot])
        for t in range(NT):
            hs_, hst = hsr.next()
            kb.dma("sp", out=hs_[:, :], in_=HTM[t * 128:(t + 1) * 128, nbs], writes=[hst])
            po, pot = por.next()
            for kc in range(16):
                kb.mm(po[:, :], mga[:, kc, t * 128:(t + 1) * 128], wo[:, kc, :], kc == 0, kc == 15, reads=[wot, ("mga", t // 4)], writes=[pot])
            us, ust = usr.next()
            kb.op("dve", "scalar_tensor_tensor", reads=[hst, pot], writes=[ust], out=us[:, :], in0=hs_[:, :], scalar=ALPHA, in1=po[:, :], op0=ALU.mult, op1=ALU.add)
            kb.dma("sp", out=UD[t * 128:(t + 1) * 128, nbs], in_=us[:, :], reads=[ust], writes=[("UD", t, nb)])
    kb.phase_end()
    if finish_check("M2%d" % l):
        return

    kb.phase_begin()
    ln_alloc()
    g1 = kb.sb("g1", [128, 2048], F32)
    b1 = kb.sb("b1", [128, 2048], F32)
    kb.dma("sp", out=g1[:, :], in_=din["rowp"][l, 0:1, 0:2048].partition_broadcast(128), writes=["gb1"])
    kb.dma("sp", out=b1[:, :], in_=din["rowp"][l, 0:1, 2048:4096].partition_broadcast(128), writes=["gb1"])
    wrt = kb.sb("wrt", [128, 16, 36], F32)
    kb.dma("sp", out=wrt[:, :, :], in_=din["w_r_t"][l], writes=["wrt"])
    brt = kb.sb("brt", [128, 36], F32)
    kb.dma("sp", out=brt[:, :], in_=din["rowp"][l, 0:1, 8704:8740].partition_broadcast(128), writes=["brt"])
    ur = Ring(kb, "uM", 4, [128, 2048], F32)
    h1r = Ring(kb, "h1M", 2, [128, 2048], F32)
    hbr = Ring(kb, "hbM", 2, [128, 2048], BF16)
    h1fm = kb.sb("h1fm", [128, 16, 128], F32)
    lgr = Ring(kb, "lgs", 2, [128, 36], F32)
    ptfr = Ring(kb, "ptf", 3, [128, 512], F32, psum=True)
    plg = kb.ps("plg", [128, 512], F32)
    loadedM = {}
    statM = {}
    for i in range(-2, NT):
        tl = i + 2
        if tl < NT:
            u, ut = ur.next()
            kb.dma("sp", out=u[:, :], in_=UD[tl * 128:(tl + 1) * 128, :], writes=[ut])
            loadedM[tl] = (u, ut)
        ta = i + 1
        if 0 <= ta < NT:
            u, ut = loadedM[ta]
            statM[ta] = kb.ln_stats(u[:, :], ut)
        if i >= 0:
            t = i
            u, ut = loadedM.pop(i)
            utg = statM.pop(i)
            h1, h1t = h1r.next()
            ln_apply(u[:, :], ut, h1[:, :], h1t, g1[:, :], b1[:, :], "gb1", tg=utg)
            kb.dma("sp", out=HTM[t * 128:(t + 1) * 128, :], in_=h1[:, :], reads=[h1t], writes=[("HTM", t)])
            hb, hbt = hbr.next()
            kb.op("act", "activation", reads=[h1t], writes=[hbt], out=hb[:, :], in_=h1[:, :], func=AF.Copy)
            kb.dma("sp", out=H1B[t * 128:(t + 1) * 128, :], in_=hb[:, :], reads=[hbt], writes=[("H1B", t)])
            for g in range(4):
                pt, ptt = ptfr.next()
                for j in range(4):
                    c = g * 4 + j
                    kb.op("pe", "transpose", reads=[h1t, "cstf"], writes=[ptt], out=pt[:, j * 128:(j + 1) * 128], in_=h1[:, c * 128:(c + 1) * 128], identity=ident_f)
                if g % 2 == 0:
                    kb.op("dve", "tensor_copy", reads=[ptt], writes=[("h1fm", g)], out=h1fm[:, g * 4:(g + 1) * 4, :], in_=pt[:, :].rearrange("p (a b) -> p a b", a=4))
                else:
                    kb.op("act", "activation", reads=[ptt], writes=[("h1fm", g)], out=h1fm[:, g * 4:(g + 1) * 4, :], in_=pt[:, :].rearrange("p (a b) -> p a b", a=4), func=AF.Copy)
            for kc in range(16):
                kb.mm(plg[:, 0:36], h1fm[:, kc, :], wrt[:, kc, :], kc == 0, kc == 15, reads=[("h1fm", kc // 4), "wrt"], writes=["plg"])
            lg_, lgt = lgr.next()
            kb.op("dve", "tensor_tensor", reads=["plg", "brt"], writes=[lgt], out=lg_[:, :], in0=plg[:, 0:36], in1=brt[:, :], op=ALU.add)
            kb.dma("sp", out=LGT[t * 128:(t + 1) * 128, :], in_=lg_[:, :], reads=[lgt], writes=[("LGT", t)])

    kb.phase_end()
    if finish_check("M3%d" % l):
        return

    SLD, WTD = SC["SLD"], SC["WTD"]
    BIG = 1.0e9
    kb.phase_begin()
    lg = kb.sb("lgE", [128, 16, 36], F32)
    kb.dma("sp", out=lg[:, :, :], in_=LGT.rearrange("(t p) n -> p t n", p=128), writes=["lg"])
    sbase = kb.sb("sbase", [128, 32], F32)
    kb.dma("sp", out=sbase[:, :], in_=din["rowc"][0:1, 0:32].partition_broadcast(128), writes=["sbase"])
    def t2(name, n):
        return kb.sb(name, [128, 16, n], F32)
    gmax = kb.sb("gmax", [128, 16], F32)
    gsum = kb.sb("gsum", [128, 16], F32)
    gw = kb.sb("gw", [128, 16], F32)
    gd, goh, pen = t2("gd", 4), t2("goh", 4), t2("pen", 4)
    elm, oh1, elm2, oh2, tmpP, tmpQ = [t2(n, 32) for n in ("elm", "oh1", "elm2", "oh2", "tmpP", "tmpQ")]
    m1 = kb.sb("m1", [128, 16], F32)
    m2_ = kb.sb("m2E", [128, 16], F32)
    w1 = kb.sb("w1", [128, 16], F32)
    wts = kb.sb("wts", [128, 16, 2], F32)
    s12 = kb.sb("s12", [128, 16, 2], F32)
    sli = kb.sb("sli", [128, 16, 2], I32)
    Cb = kb.sb("Cb", [128, 16, 32], BF16)
    gl = lg[:, :, 0:4]
    el = lg[:, :, 4:36]
    def bc(ap2, n):
        return ap2.unsqueeze(2).to_broadcast([128, 16, n])
    kb.op("dve", "tensor_reduce", reads=["lg"], writes=["gmax"], out=gmax[:, :], in_=gl, axis=AX.X, op=ALU.max)
    kb.op("dve", "tensor_tensor", reads=["lg", "gmax"], writes=["gd"], out=gd[:, :, :], in0=gl, in1=bc(gmax[:, :], 4), op=ALU.subtract)
    kb.op("act", "activation", reads=["gd"], writes=["gd"], out=gd[:, :, :], in_=gd[:, :, :], func=AF.Exp)
    kb.op("dve", "tensor_reduce", reads=["gd"], writes=["gsum"], out=gsum[:, :], in_=gd[:, :, :], axis=AX.X, op=ALU.add)
    kb.op("dve", "reciprocal", reads=["gsum"], writes=["gw"], out=gw[:, :], in_=gsum[:, :])
    kb.op("dve", "tensor_tensor", reads=["lg", "gmax"], writes=["goh"], out=goh[:, :, :], in0=gl, in1=bc(gmax[:, :], 4), op=ALU.is_equal)
    kb.op("dve", "tensor_scalar", reads=["goh"], writes=["pen"], out=pen[:, :, :], in0=goh[:, :, :], scalar1=BIG, scalar2=-BIG, op0=ALU.mult, op1=ALU.add)
    kb.op("dve", "tensor_tensor", reads=["lg", "pen"], writes=["elm"], out=elm[:, :, :].rearrange("p t (g e) -> p t g e", g=4),
          in0=el.rearrange("p t (g e) -> p t g e", g=4), in1=pen[:, :, :].unsqueeze(3).to_broadcast([128, 16, 4, 8]), op=ALU.add)
    kb.op("dve", "tensor_reduce", reads=["elm"], writes=["m1"], out=m1[:, :], in_=elm[:, :, :], axis=AX.X, op=ALU.max)
    kb.op("dve", "tensor_tensor", reads=["elm", "m1"], writes=["oh1"], out=oh1[:, :, :], in0=elm[:, :, :], in1=bc(m1[:, :], 32), op=ALU.is_equal)
    kb.op("dve", "scalar_tensor_tensor", reads=["oh1", "elm"], writes=["elm2"], out=elm2[:, :, :], in0=oh1[:, :, :], scalar=-BIG, in1=elm[:, :, :], op0=ALU.mult, op1=ALU.add)
    kb.op("dve", "tensor_reduce", reads=["elm2"], writes=["m2"], out=m2_[:, :], in_=elm2[:, :, :], axis=AX.X, op=ALU.max)
    kb.op("dve", "tensor_tensor", reads=["elm2", "m2"], writes=["oh2"], out=oh2[:, :, :], in0=elm2[:, :, :], in1=bc(m2_[:, :], 32), op=ALU.is_equal)
    kb.op("dve", "tensor_tensor", reads=["m1", "m2"], writes=["w1"], out=w1[:, :], in0=m1[:, :], in1=m2_[:, :], op=ALU.subtract)
    kb.op("act", "activation", reads=["w1"], writes=["w1"], out=w1[:, :], in_=w1[:, :], func=AF.Sigmoid)
    kb.op("dve", "tensor_tensor", reads=["w1", "gw"], writes=["wts0"], out=wts[:, :, 0], in0=w1[:, :], in1=gw[:, :], op=ALU.mult)
    kb.op("dve", "tensor_tensor", reads=["wts0", "gw"], writes=["wts1"], out=wts[:, :, 1], in0=gw[:, :], in1=wts[:, :, 0], op=ALU.subtract)
    kb.op("dve", "tensor_tensor", reads=["oh1", "oh2"], writes=["Cb"], out=Cb[:, :, :], in0=oh1[:, :, :], in1=oh2[:, :, :], op=ALU.add)
    ppre = kb.ps("ppre", [128, 512], F32)
    strict_b = cstb[:, 4, :]
    for t in range(NT):
        for tp in range(t):
            kb.mm(ppre[:, t * 32:(t + 1) * 32], ones_b, Cb[:, tp, :], tp == 0, False, reads=["Cb", "cstb"], writes=["ppre"], last=False)
        kb.mm(ppre[:, t * 32:(t + 1) * 32], strict_b, Cb[:, t, :], t == 0, True, reads=["Cb", "cstb"], writes=["ppre"], last=(t == NT - 1))
    kb.op("dve", "tensor_tensor", reads=["ppre", "sbase"], writes=["tmpP"], out=tmpP[:, :, :], in0=ppre[:, :].rearrange("p (t e) -> p t e", t=16),
          in1=sbase[:, :].unsqueeze(1).to_broadcast([128, 16, 32]), op=ALU.add)
    kb.op("dve", "tensor_tensor", reads=["tmpP", "oh1"], writes=["tmpQ"], out=tmpQ[:, :, :], in0=tmpP[:, :, :], in1=oh1[:, :, :], op=ALU.mult)
    kb.op("dve", "tensor_reduce", reads=["tmpQ"], writes=["s120"], out=s12[:, :, 0], in_=tmpQ[:, :, :], axis=AX.X, op=ALU.add)
    kb.op("dve", "tensor_tensor", reads=["tmpP", "oh2", "s120"], writes=["tmpQ"], out=tmpQ[:, :, :], in0=tmpP[:, :, :], in1=oh2[:, :, :], op=ALU.mult)
    kb.op("dve", "tensor_reduce", reads=["tmpQ"], writes=["s121"], out=s12[:, :, 1], in_=tmpQ[:, :, :], axis=AX.X, op=ALU.add)
    kb.op("dve", "tensor_copy", reads=["s120", "s121"], writes=["sli"], out=sli[:, :, :], in_=s12[:, :, :])
    kb.dma("sp", out=SLD.rearrange("(t p) n -> p t n", p=128), in_=sli[:, :, :], reads=["sli"], writes=["SLD"])
    kb.dma("sp", out=WTD.rearrange("(t p) n -> p t n", p=128), in_=wts[:, :, :], reads=["wts0", "wts1"], writes=["WTD"])
    hbr = Ring(kb, "h1bE", 3, [128, 2048], BF16)
    for t in range(NT):
        hb, hbt = hbr.next()
        kb.dma("sp", out=hb[:, :], in_=H1B[t * 128:(t + 1) * 128, :], writes=[hbt])
        for k2 in range(2):
            kb.dma("pool", out=XROWS, in_=hb[:, :], reads=[hbt, "sli"], writes=[("XR", t, k2)], name="indirect_dma_start",
                   out_offset=bass.IndirectOffsetOnAxis(ap=sli[:, t, k2:k2 + 1], axis=0), in_offset=None)
    kb.phase_end()
    if finish_check("E1%d" % l):
        return

    kb.phase_begin()
    moe_experts(kb, l, din, XROWS, YROWS, ident_b, NEXP)
    kb.phase_end()
    if finish_check("E3%d" % l):
        return

    kb.phase_begin()
    ln_alloc()
    g2 = kb.sb("g2", [128, 2048], F32)
    b2 = kb.sb("b2", [128, 2048], F32)
    kb.dma("sp", out=g2[:, :], in_=din["rowp"][l, 0:1, 4096:6144].partition_broadcast(128), writes=["gb2"])
    kb.dma("sp", out=b2[:, :], in_=din["rowp"][l, 0:1, 6144:8192].partition_broadcast(128), writes=["gb2"])
    sli = kb.sb("sliF", [128, 16, 2], I32)
    wts = kb.sb("wtsF", [128, 16, 2], F32)
    kb.dma("sp", out=sli[:, :, :], in_=SLD.rearrange("(t p) n -> p t n", p=128), writes=["sli"])
    kb.dma("sp", out=wts[:, :, :], in_=WTD.rearrange("(t p) n -> p t n", p=128), writes=["wts"])
    gr1 = Ring(kb, "gy1", 4, [128, 2048], F32)
    gr2 = Ring(kb, "gy2", 4, [128, 2048], F32)
    hTr = Ring(kb, "hTE", 4, [128, 2048], F32)
    h2r = Ring(kb, "h2E", 2, [128, 2048], F32)
    rings = dict(hb=Ring(kb, "hbE4", 2, [128, 2048], BF16), fm=Ring(kb, "fmE4", 2, [128, 16, 128], BF16),
                 pt=Ring(kb, "ptE4", 4, [128, 512], BF16, psum=True))
    loadedE = {}
    statE = {}
    for i in range(-2, NT):
        tl = i + 2
        if tl < NT:
            t = tl
            ga, gat = gr1.next()
            gb_, gbt = gr2.next()
            kb.dma("pool", out=ga[:, :], in_=YROWS, reads=["sli"], writes=[gat], name="indirect_dma_start", out_offset=None,
                   in_offset=bass.IndirectOffsetOnAxis(ap=sli[:, t, 0:1], axis=0))
            kb.dma("pool", out=gb_[:, :], in_=YROWS, reads=["sli"], writes=[gbt], name="indirect_dma_start", out_offset=None,
                   in_offset=bass.IndirectOffsetOnAxis(ap=sli[:, t, 1:2], axis=0))
            hT, hTt = hTr.next()
            kb.dma("sp", out=hT[:, :], in_=HTM[t * 128:(t + 1) * 128, :], writes=[hTt])
            loadedE[tl] = (ga, gat, gb_, gbt, hT, hTt)
        ta = i + 1
        if 0 <= ta < NT:
            t = ta
            ga, gat, gb_, gbt, hT, hTt = loadedE[ta]
            kb.op("act", "activation", reads=[hTt], writes=[hTt], out=hT[:, :], in_=hT[:, :], func=AF.Copy, scale=ALPHA)
            kb.op("dve", "scalar_tensor_tensor", reads=[gat, hTt, "wts"], writes=[hTt], out=hT[:, :], in0=ga[:, :], scalar=wts[:, t, 0:1], in1=hT[:, :], op0=ALU.mult, op1=ALU.add)
            kb.op("dve", "scalar_tensor_tensor", reads=[gbt, hTt, "wts"], writes=[hTt], out=hT[:, :], in0=gb_[:, :], scalar=wts[:, t, 1:2], in1=hT[:, :], op0=ALU.mult, op1=ALU.add)
            statE[ta] = kb.ln_stats(hT[:, :], hTt)
        if i >= 0:
            ga, gat, gb_, gbt, hT, hTt = loadedE.pop(i)
            h2, h2t = h2r.next()
            ln_apply(hT[:, :], hTt, h2[:, :], h2t, g2[:, :], b2[:, :], "gb2", tg=statE.pop(i))
            emit_h_tile(i, h2[:, :], h2t, rings, l == NL - 1)
    kb.phase_end()
    if finish_check("E4%d" % l):
        return


_CACHE = {}


def kernel(**inputs):
    inp = {k: np.asarray(v) for k, v in inputs.items()}
    lay = _host_layout(inp)
    if "nc" not in _CACHE:
        _CACHE["nc"] = build()[0]
    nc = _CACHE["nc"]
    x = inp["x"].astype(np.float32)
    in_maps = []
    for b in range(x.shape[0]):
        m = dict(lay)
        m["x"] = np.ascontiguousarray(x[b])
        in_maps.append(m)
    res = run_bass_kernel_spmd(nc, in_maps, core_ids=list(range(x.shape[0])))
    out = np.stack([np.asarray(r["out"], dtype=np.float32) for r in res.results], axis=0)
    return out
```

```python
import math
from contextlib import ExitStack

import numpy as np
import ml_dtypes
import concourse.bass as bass
import concourse.mybir as mybir
from concourse.bass_utils import run_bass_kernel_spmd

F32 = mybir.dt.float32
BF16 = mybir.dt.bfloat16
I32 = mybir.dt.int32
AF = mybir.ActivationFunctionType
ALU = mybir.AluOpType
AX = mybir.AxisListType

S = 2048
D = 2048
NT = 16
NL = 2
ALPHA = 4.0 ** 0.25
EPS = 1e-5
R_E = 1535
RVLEN = 3072
CAP = 256
NEXP = 32
ENGS = ("pe", "act", "dve", "pool", "sp")


class KB:
    def __init__(self, nc, n_dma_sems=10):
        self.nc = nc
        self.ops = {e: [] for e in ENGS}
        self.sem = {}
        self.cnt = {e: 0 for e in ENGS}
        self.seen = {e: {} for e in ENGS}
        self.lastw = {}
        self.readers = {}
        self.n_dma_sems = n_dma_sems
        self.dma_sems = {}
        self.dma_rr = {e: 0 for e in ENGS}
        self.pstack = None
        self.out_events = []
        self.n_ins = 0

    def open(self, stack):
        nc = self.nc
        for e in ENGS:
            self.sem[e] = stack.enter_context(nc.semaphore("s_" + e))
        for q in ("sp", "act", "pool"):
            lst = []
            for i in range(6 if q == "pool" else self.n_dma_sems):
                key = "d_%s_%d" % (q, i)
                self.sem[key] = stack.enter_context(nc.semaphore(key))
                lst.append([key, 0])
            self.dma_sems[q] = lst
        self.gstack = stack

    def _uniq(self, name):
        self.uid = getattr(self, "uid", 0) + 1
        return "%s_u%d" % (name, self.uid)

    def gsb(self, name, shape, dt):
        return self.gstack.enter_context(self.nc.sbuf_tensor(self._uniq(name), list(shape), dt))

    def sb(self, name, shape, dt):
        return self.pstack.enter_context(self.nc.sbuf_tensor(self._uniq(name), list(shape), dt))

    def ps(self, name, shape, dt=F32):
        return self.pstack.enter_context(self.nc.psum_tensor(self._uniq(name), list(shape), dt))

    def _deps(self, reads, writes):
        evs = []
        for t in reads:
            ev = self.lastw.get(t)
            if ev is not None:
                evs.append(ev)
        for t in writes:
            ev = self.lastw.get(t)
            if ev is not None:
                evs.append(ev)
            evs.extend(self.readers.get(t, ()))
        return evs

    def _waits(self, eng, evs):
        seen = self.seen[eng]
        need = {}
        for (k, v) in evs:
            if seen.get(k, 0) >= v:
                continue
            if need.get(k, 0) < v:
                need[k] = v
        for k, v in need.items():
            seen[k] = v
        return list(need.items())

    def _commit(self, ev, reads, writes):
        for t in writes:
            self.lastw[t] = ev
            self.readers[t] = []
        for t in reads:
            if t in writes:
                continue
            self.readers.setdefault(t, []).append(ev)

    def op(self, eng, name, reads=(), writes=(), **kw):
        evs = self._deps(reads, writes)
        waits = self._waits(eng, evs)
        self.cnt[eng] += 1
        ev = (eng, self.cnt[eng])
        self.ops[eng].append((waits, name, kw, (eng, 1)))
        self._commit(ev, reads, writes)
        return ev

    def op_noinc(self, eng, name, reads=(), writes=(), **kw):
        evs = self._deps(reads, writes)
        waits = self._waits(eng, evs)
        self.ops[eng].append((waits, name, kw, None))

    def mm(self, out, lhsT, rhs, start, stop, reads=(), writes=(), last=None):
        if last is None:
            last = stop
        f = self.op if last else self.op_noinc
        return f("pe", "matmul", reads=reads, writes=writes, out=out, lhsT=lhsT, rhs=rhs, start=start, stop=stop)

    def dma(self, q, out, in_, reads=(), writes=(), is_out=False, name="dma_start", **kw):
        evs = self._deps(reads, writes)
        lst = self.dma_sems[q]
        i = self.dma_rr[q] % len(lst)
        self.dma_rr[q] += 1
        key, c = lst[i]
        if c > 0:
            evs.append((key, c))
        waits = self._waits(q, evs)
        lst[i][1] = c + 16
        ev = (key, c + 16)
        kw = dict(kw)
        kw["out"] = out
        kw["in_"] = in_
        self.ops[q].append((waits, name, kw, (key, 16)))
        self._commit(ev, reads, writes)
        if is_out:
            self.out_events.append(ev)
        return ev

    def barrier(self):
        evs = []
        for q, lst in self.dma_sems.items():
            for key, c in lst:
                if c > 0:
                    evs.append((key, c))
        for e in ENGS:
            if e != "sp" and self.cnt[e] > 0:
                evs.append((e, self.cnt[e]))
        waits = self._waits("sp", evs)
        self.cnt["sp"] += 1
        self.ops["sp"].append((waits, "sem_inc", dict(sem=self.sem["sp"], val=1), None))
        mark = ("sp", self.cnt["sp"])
        for e in ENGS:
            if e == "sp":
                continue
            w = self._waits(e, [mark])
            self.ops[e].append((w, None, None, None))
            for (k, v) in evs:
                if self.seen[e].get(k, 0) < v:
                    self.seen[e][k] = v
        self.lastw = {}
        self.readers = {}

    def emit(self):
        nc = self.nc
        sem = self.sem
        ops = self.ops
        with nc.named_scope("ph%02d" % getattr(self, "phase_no", 0)), nc.Block() as block:
            def run(engname):
                def body(e):
                    for waits, name, kw, inc in ops[engname]:
                        for k, v in waits:
                            e.wait_ge(sem[k], v)
                        if name is None:
                            continue
                        ins = getattr(e, name)(**kw)
                        self.n_ins += 1
                        if inc is not None:
                            ins.then_inc(sem[inc[0]], inc[1])
                return body
            block.tensor(run("pe"))
            block.scalar(run("act"))
            block.vector(run("dve"))
            block.gpsimd(run("pool"))
            block.sync(run("sp"))
        self.ops = {e: [] for e in ENGS}

    def phase_begin(self):
        self.phase_no = getattr(self, "phase_no", -1) + 1
        self.pstack = ExitStack()
        self.pstack.__enter__()

    def phase_end(self):
        self.barrier()
        self.emit()
        self.pstack.close()
        self.pstack = None


class Ring:
    def __init__(self, kb, name, n, shape, dt, psum=False):
        self.tiles = []
        self.names = []
        for i in range(n):
            nm = "%s%d" % (name, i)
            t = kb.ps(nm, shape, dt) if psum else kb.sb(nm, shape, dt)
            self.tiles.append(t)
            self.names.append(nm)
        self.i = 0

    @classmethod
    def wrap(cls, tiles, names):
        r = cls.__new__(cls)
        r.tiles = list(tiles)
        r.names = list(names)
        r.i = 0
        return r

    def next(self):
        j = self.i % len(self.tiles)
        self.i += 1
        return self.tiles[j], self.names[j]


def _t5_bucket(rel):
    half = 16
    max_exact = 8
    n = np.abs(rel)
    large = max_exact + (np.log(np.maximum(n, 1) / max_exact) / np.log(1024 / max_exact) * (half - max_exact)).astype(np.int32)
    large = np.minimum(large, half - 1)
    return (rel > 0).astype(np.int32) * half + np.where(n < max_exact, n, large)


def _host_consts():
    c = {}
    i = np.arange(RVLEN)
    r = R_E - i
    a = np.abs(r)
    mult = (a <= 64).astype(np.float32) + ((r % 4 == 0) & (a <= 256)).astype(np.float32) + ((r % 16 == 0) & (a <= 1024)).astype(np.float32)
    bk = _t5_bucket(r)
    mohr = np.zeros((32, RVLEN), np.float32)
    mohr[bk, i] = mult
    c["mohr"] = mohr
    p = np.arange(128)[:, None]
    f = np.arange(128)[None, :]
    cst = np.zeros((128, 6, 128), np.float32)
    cst[:, 0] = (p == f)
    cst[:, 1] = (p + f == 127)
    cst[:, 2] = (p <= f)
    cst[:, 3] = (p >= f)
    cst[:, 4] = (p < f)
    cst[:, 5] = 1.0
    c["cst"] = cst
    inv_freq = (np.float32(10000.0) ** (-np.arange(0, 32, 2, dtype=np.float32) / np.float32(32))).astype(np.float32)
    ang = (np.arange(S, dtype=np.float32)[:, None] * inv_freq[None]).astype(np.float32)
    cos = np.cos(ang).astype(np.float32).T
    sin = np.sin(ang).astype(np.float32).T
    rope = np.zeros((2, 96, S), np.float32)
    rope[0, 64:80] = cos
    rope[0, 80:96] = cos
    rope[1, 64:80] = -sin
    rope[1, 80:96] = sin
    c["rope"] = rope
    m = np.ones((16, S), np.float32)
    m[:, ::128] = 0.0
    c["scanmask"] = m
    rowc = np.zeros((1, 64), np.float32)
    rowc[0, 0:32] = np.arange(32) * CAP
    c["rowc"] = rowc
    return c


def _host_layout(inp):
    f = np.float32
    o = {}
    w_in = inp["w_in"]
    cuts = np.cumsum((512, 512, 512, 512, 256, 32, 1024, 512, 512, 256, 256, 16))
    st = np.concatenate([[0], cuts[:-1]])
    seg = {n: (int(a), int(b)) for n, a, b in zip(("qa", "ka", "va", "cq", "ckv", "kr", "glu", "z", "xs", "bm", "cm", "dt"), st, cuts)}
    cols = []
    def rng(n, a=0, b=None):
        s0, s1 = seg[n]
        b = (s1 - s0) if b is None else b
        return list(range(s0 + a, s0 + b))
    cols += rng("qa") + rng("ka") + rng("cq") + rng("ckv")
    kr0 = seg["kr"][0]
    cols += rng("kr") + [kr0 + (r + 16) % 32 for r in range(32)] + [-1] * 64
    cols += rng("glu", 0, 512) + rng("glu", 512, 1024) + rng("xs") + rng("bm") + rng("cm")
    cols = np.array(cols)
    assert cols.size == 31 * 128
    w_in_t = np.zeros((NL, 31, 128, 16, 128), f)
    w_dt_tm = np.zeros((NL, 128, 16, 16), f)
    w_in_tm = np.zeros((NL, 2, 128, 16, 512), f)
    for l in range(NL):
        wc = np.where(cols[None, :] >= 0, w_in[l][:, np.maximum(cols, 0)], 0.0).astype(f)
        w_in_t[l] = wc.reshape(16, 128, 31, 128).transpose(2, 1, 0, 3)
        w_dt_tm[l] = w_in[l][:, seg["dt"][0]:seg["dt"][1]].reshape(16, 128, 16).transpose(1, 0, 2)
        for b, n in enumerate(("va", "z")):
            s0, s1 = seg[n]
            w_in_tm[l, b] = w_in[l][:, s0:s1].reshape(16, 128, 512).transpose(1, 0, 2)
    o["w_in_t"] = w_in_t
    o["w_in_tm"] = w_in_tm
    o["w_dt_tm"] = w_dt_tm
    w_uq = inp["w_uq"]
    pc = np.array([96 * h + 64 + (r + 16) % 32 for h in range(8) for r in range(32)])
    w_uq_t = np.concatenate([w_uq, w_uq[:, :, pc]], axis=2)
    o["w_uq_t"] = np.ascontiguousarray(w_uq_t.reshape(NL, 4, 128, 1024).transpose(0, 2, 1, 3))
    w_ukv = inp["w_ukv"]
    kc_ = np.array([128 * h + j for h in range(8) for j in range(64)])
    vc_ = kc_ + 64
    w_ukv_r = np.concatenate([w_ukv[:, :, kc_], w_ukv[:, :, vc_]], axis=2)
    o["w_ukv_t"] = np.ascontiguousarray(w_ukv_r.reshape(NL, 2, 128, 1024).transpose(0, 2, 1, 3))
    o["w_gate_t"] = np.ascontiguousarray(inp["w_gate"].reshape(NL, 4, 16, 128, 16, 128).transpose(0, 1, 4, 3, 2, 5))
    o["w_br_t"] = np.ascontiguousarray(inp["w_br"].reshape(NL, 4, 4, 128, 16, 128).transpose(0, 1, 4, 3, 2, 5))
    o["w_out_t"] = np.ascontiguousarray(inp["w_out"].reshape(NL, 16, 128, 4, 512).transpose(0, 3, 2, 1, 4))
    w_r = np.concatenate([inp["w_rg"], inp["w_re"]], axis=2)
    o["w_r_t"] = np.ascontiguousarray(w_r.reshape(NL, 16, 128, 36).transpose(0, 2, 1, 3))
    o["w_e_gate"] = inp["w_e_gate"]
    o["w_e_up"] = inp["w_e_up"]
    o["w_e_down"] = inp["w_e_down"]
    colp = np.zeros((NL, 128, 256), f)
    for l in range(NL):
        c0 = 0
        colp[l, :, 0:64] = inp["b_gate"][l].reshape(4, 16, 128).transpose(2, 0, 1).reshape(128, 64)
        colp[l, :, 64:68] = inp["g_cq"][l].reshape(4, 128).T
        colp[l, :, 68:70] = inp["g_ckv"][l].reshape(2, 128).T
        colp[l, :, 70:74] = inp["b_dw_c"][l].reshape(4, 128).T
        colp[l, :, 74:78] = inp["ln_c_g"][l].reshape(4, 128).T
        colp[l, :, 78:82] = inp["ln_c_b"][l].reshape(4, 128).T
        colp[l, :, 82:206] = inp["w_dw_c"][l].reshape(31, 4, 128).transpose(2, 1, 0).reshape(128, 124)
        colp[l, :, 206:246] = inp["w_conv_d"][l].reshape(5, 8, 128).transpose(2, 1, 0).reshape(128, 40)
        colp[l, :, 246:254] = inp["b_conv_d"][l].reshape(8, 128).T
    o["colp"] = colp
    rowp = np.zeros((NL, 1, 8832), f)
    for l in range(NL):
        rowp[l, 0, 0:2048] = inp["ln1_g"][l]
        rowp[l, 0, 2048:4096] = inp["ln1_b"][l]
        rowp[l, 0, 4096:6144] = inp["ln2_g"][l]
        rowp[l, 0, 6144:8192] = inp["ln2_b"][l]
        rowp[l, 0, 8192:8704] = inp["g_norm_d"][l]
        rowp[l, 0, 8704:8708] = inp["b_rg"][l]
        rowp[l, 0, 8708:8740] = inp["b_re"][l]
        rowp[l, 0, 8740:8748] = inp["d_skip"][l]
        rowp[l, 0, 8748:8756] = inp["dt_bias_f"][l]
        rowp[l, 0, 8756:8764] = inp["dt_bias_b"][l]
        rowp[l, 0, 8764:8772] = inp["a_log_f"][l]
        rowp[l, 0, 8772:8780] = inp["a_log_b"][l]
    o["rowp"] = rowp
    o["rowg"] = np.concatenate([inp["ln_in_g"], inp["ln_in_b"]]).reshape(1, 4096).astype(f)
    o["rel_bias"] = inp["rel_bias"].astype(f)
    o.update(_host_consts())
    return o


IN_SHAPES = {
    "x": ([S, D], F32),
    "w_in_t": ([NL, 31, 128, 16, 128], F32),
    "w_dt_tm": ([NL, 128, 16, 16], F32),
    "w_in_tm": ([NL, 2, 128, 16, 512], F32),
    "w_uq_t": ([NL, 128, 4, 1024], F32),
    "w_ukv_t": ([NL, 128, 2, 1024], F32),
    "w_gate_t": ([NL, 4, 16, 128, 16, 128], F32),
    "w_br_t": ([NL, 4, 16, 128, 4, 128], F32),
    "w_out_t": ([NL, 4, 128, 16, 512], F32),
    "w_r_t": ([NL, 128, 16, 36], F32),
    "w_e_gate": ([NL, 32, 2048, 512], F32),
    "w_e_up": ([NL, 32, 2048, 512], F32),
    "w_e_down": ([NL, 32, 512, 2048], F32),
    "colp": ([NL, 128, 256], F32),
    "rowp": ([NL, 1, 8832], F32),
    "rowg": ([1, 4096], F32),
    "rel_bias": ([32, 8], F32),
    "mohr": ([32, RVLEN], F32),
    "cst": ([128, 6, 128], F32),
    "rope": ([2, 96, S], F32),
    "scanmask": ([16, S], F32),
    "rowc": ([1, 64], F32),
}


CH = dict(qa=0, ka=4, cq=8, ckv=12, krx=14, glua=15, glug=19, xs=23, bm=27, cm=29)


def build(debug=False, stop_after=None, small_moe=False):
    nc = bass.Bass("TRN2", target_bir_lowering=False)
    scr = "ExternalOutput" if debug else "Internal"
    din = {}
    hnd = {}
    for n, (shp, dt) in IN_SHAPES.items():
        if small_moe and n.startswith("w_e_"):
            shp = [shp[0], 1] + list(shp[2:])
        hnd[n] = nc.dram_tensor(n, shp, dt, kind="ExternalInput")
        din[n] = hnd[n].ap()
    out_d = nc.dram_tensor("out", [S, D], F32, kind="ExternalOutput").ap()

    def scratch(name, shape, dt):
        h = nc.dram_tensor(name, shape, dt, kind=scr)
        hnd[name] = h
        return h.ap()

    HTM = scratch("HTM", [S, D], F32)
    HFM = scratch("HFM", [128, 16, S], BF16)
    PROJ = scratch("PROJ", [31, 128, S], BF16)
    DTR = scratch("DTR", [S, 16], F32)
    MG = scratch("MG", [128, 16, S], BF16)
    UD = scratch("UD", [S, D], F32)
    SLD = scratch("SLD", [S, 2], I32)
    WTD = scratch("WTD", [S, 2], F32)
    VATM = scratch("VATM", [S, 512], BF16)
    ZSTM = scratch("ZSTM", [S, 512], BF16)
    YBR = scratch("YBR", [4, 128, 4, S], BF16)
    RV = scratch("RV", [8, RVLEN], F32)
    H1B = scratch("H1B", [S, D], BF16)
    LGT = scratch("LGT", [S, 36], F32)
    XROWS = scratch("XROWS", [NEXP * CAP, D], BF16)
    YROWS = scratch("YROWS", [NEXP * CAP, D], F32)

    kb = KB(nc)
    done = [False]

    def finish_check(name):
        if stop_after == name:
            done[0] = True
        return done[0]

    with ExitStack() as gstack:
        kb.open(gstack)
        cstf = kb.gsb("cstf", [128, 6, 128], F32)
        cstb = kb.gsb("cstb", [128, 6, 128], BF16)
        ident_f = cstf[:, 0, :]
        ident_b = cstb[:, 0, :]
        ones_f = cstf[:, 5, :]
        ones_b = cstb[:, 5, :]

        def ln_stats(src, tag):
            tg = "ab"[kb._ln_i % 2]
            kb._ln_i += 1
            st = kb._ln_st
            for q in range(4):
                kb.op("dve", "bn_stats", reads=[tag], writes=[tg + "lnst%d" % q], out=st[tg + "stats"][:, q, :], in_=src[:, q * 512:(q + 1) * 512])
            kb.op("dve", "bn_aggr", reads=[tg + "lnst%d" % q for q in range(4)], writes=[tg + "lnmv"], out=st[tg + "mv"][:, :], in_=st[tg + "stats"][:, :, :])
            kb.op("dve", "tensor_scalar_add", reads=[tg + "lnmv"], writes=[tg + "lnve"], out=st[tg + "ve"][:, :], in0=st[tg + "mv"][:, 1:2], scalar1=EPS)
            kb.op("pool", "tensor_tensor", reads=[tg + "lnve", "lnmhalf"], writes=[tg + "rs"], out=st[tg + "rs"][:, :], in0=st[tg + "ve"][:, :], in1=st["mhalf"][:, :], op=ALU.pow)
            return tg

        def ln_alloc():
            st = {}
            for tg in ("a", "b"):
                st[tg + "stats"] = kb.sb("ln_stats" + tg, [128, 4, 6], F32)
                st[tg + "mv"] = kb.sb("ln_mv" + tg, [128, 2], F32)
                st[tg + "ve"] = kb.sb("ln_ve" + tg, [128, 1], F32)
                st[tg + "rs"] = kb.sb("ln_rs" + tg, [128, 1], F32)
            st["mhalf"] = kb.sb("ln_mhalf", [128, 1], F32)
            kb.op("pool", "memset", writes=["lnmhalf"], ap=st["mhalf"][:, :], constant=-0.5)
            kb._ln_st = st
            kb._ln_i = 0

        def ln_apply(src, stag, dst, dtag, gbc, bbc, gtag, tg=None):
            if tg is None:
                tg = ln_stats(src, stag)
            st = kb._ln_st
            kb.op("dve", "scalar_tensor_tensor", reads=[stag, tg + "lnmv", gtag], writes=[dtag], out=dst, in0=src, scalar=st[tg + "mv"][:, 0:1], in1=gbc,
                  op0=ALU.subtract, op1=ALU.mult)
            kb.op("dve", "scalar_tensor_tensor", reads=[dtag, tg + "rs", gtag], writes=[dtag], out=dst, in0=dst, scalar=st[tg + "rs"][:, 0:1], in1=bbc,
                  op0=ALU.mult, op1=ALU.add)

        kb.ln_stats = ln_stats

        def emit_h_tile(t, hT, htag, rings, write_out):
            if write_out:
                kb.dma("sp", out=out_d[t * 128:(t + 1) * 128, :], in_=hT, reads=[htag], is_out=True)
                return
            kb.dma("sp", out=HTM[t * 128:(t + 1) * 128, :], in_=hT, reads=[htag], writes=[("HTM", t)])
            hb, hbt = rings["hb"].next()
            kb.op("act", "activation", reads=[htag], writes=[hbt], out=hb[:, :], in_=hT, func=AF.Copy)
            fm, fmt = rings["fm"].next()
            for g in range(4):
                pt, ptt = rings["pt"].next()
                for j in range(4):
                    c = g * 4 + j
                    kb.op("pe", "transpose", reads=[hbt], writes=[ptt], out=pt[:, j * 128:(j + 1) * 128], in_=hb[:, c * 128:(c + 1) * 128], identity=ident_b)
                eng = "dve" if g % 2 == 0 else "act"
                if eng == "dve":
                    kb.op("dve", "tensor_copy", reads=[ptt], writes=[fmt], out=fm[:, g * 4:(g + 1) * 4, :], in_=pt[:, :].rearrange("p (a b) -> p a b", a=4))
                else:
                    kb.op("act", "activation", reads=[ptt], writes=[fmt], out=fm[:, g * 4:(g + 1) * 4, :], in_=pt[:, :].rearrange("p (a b) -> p a b", a=4), func=AF.Copy)
            kb.dma("sp", out=HFM[:, :, t * 128:(t + 1) * 128], in_=fm[:, :, :], reads=[fmt], writes=[("HFM", t)])

        kb.phase_begin()
        kb.dma("sp", out=cstf[:, :, :], in_=din["cst"], writes=["cstf"])
        kb.op("dve", "tensor_copy", reads=["cstf"], writes=["cstb"], out=cstb[:, :, :], in_=cstf[:, :, :])
        rb = kb.sb("rb", [32, 8], F32)
        eb = kb.sb("eb", [32, 8], F32)
        mo = kb.sb("mo", [32, RVLEN], F32)
        rv = kb.sb("rv", [8, RVLEN], F32)
        kb.dma("sp", out=rb[:, :], in_=din["rel_bias"], writes=["rb"])
        kb.dma("sp", out=mo[:, :], in_=din["mohr"], writes=["mo"])
        kb.op("act", "activation", reads=["rb"], writes=["eb"], out=eb[:, :], in_=rb[:, :], func=AF.Exp)
        pr = Ring(kb, "ipr", 2, [128, 512], F32, psum=True)
        for j in range(RVLEN // 512):
            p, ptg = pr.next()
            kb.mm(p[0:8, :], eb[:, :], mo[:, j * 512:(j + 1) * 512], True, True, reads=["eb", "mo"], writes=[ptg])
            kb.op("dve", "tensor_copy", reads=[ptg], writes=["rv"], out=rv[:, j * 512:(j + 1) * 512], in_=p[0:8, :])
        kb.dma("sp", out=RV, in_=rv[:, :], reads=["rv"], writes=["RV"])
        zt = kb.sb("zt", [128, 2048], BF16)
        kb.op("pool", "memset", writes=["zt"], ap=zt[:, :], constant=0.0)
        for i in range(NEXP * CAP // 128):
            kb.dma("sp", out=XROWS[i * 128:(i + 1) * 128, :], in_=zt[:, :], reads=["zt"], writes=[("XR0", i)])
        kb.phase_end()

        kb.phase_begin()
        ln_alloc()
        gbc = kb.sb("gbc", [128, 2048], F32)
        bbc = kb.sb("bbc", [128, 2048], F32)
        kb.dma("sp", out=gbc[:, :], in_=din["rowg"][0:1, 0:2048].partition_broadcast(128), writes=["gb"])
        kb.dma("sp", out=bbc[:, :], in_=din["rowg"][0:1, 2048:4096].partition_broadcast(128), writes=["gb"])
        xr = Ring(kb, "xin", 4, [128, 2048], F32)
        hr = Ring(kb, "hout", 2, [128, 2048], F32)
        rings = dict(hb=Ring(kb, "hb", 2, [128, 2048], BF16), fm=Ring(kb, "fm", 2, [128, 16, 128], BF16),
                     pt=Ring(kb, "ptb", 4, [128, 512], BF16, psum=True))
        loaded = {}
        stat = {}
        for i in range(-2, NT):
            tl = i + 2
            if tl < NT:
                xt, xtag = xr.next()
                kb.dma("sp", out=xt[:, :], in_=din["x"][tl * 128:(tl + 1) * 128, :], writes=[xtag])
                loaded[tl] = (xt, xtag)
            ta = i + 1
            if 0 <= ta < NT:
                xt, xtag = loaded[ta]
                stat[ta] = ln_stats(xt[:, :], xtag)
            if i >= 0:
                pxt, pxtag = loaded.pop(i)
                ht, htag = hr.next()
                ln_apply(pxt[:, :], pxtag, ht[:, :], htag, gbc[:, :], bbc[:, :], "gb", tg=stat.pop(i))
                emit_h_tile(i, ht[:, :], htag, rings, False)
        kb.phase_end()
        if finish_check("ln_in"):
            return nc, kb

        for l in range(NL):
            build_layer(nc, kb, din, hnd, out_d, l, finish_check, dict(
                HTM=HTM, HFM=HFM, PROJ=PROJ, DTR=DTR, VATM=VATM, ZSTM=ZSTM, YBR=YBR, RV=RV, MG=MG, UD=UD, SLD=SLD, WTD=WTD, H1B=H1B, LGT=LGT, XROWS=XROWS, YROWS=YROWS),
                dict(cstf=cstf, cstb=cstb), ln_alloc, ln_apply, emit_h_tile)
            if done[0]:
                return nc, kb
    return nc, kb


def attention(kb, nheads, kT_ap, qT_ap, v_ap, kcs_for, e_info, scale, yst, prefix, reads_k, reads_q, reads_v, pre_head=None, srr=None, per_unit=None, warm=None, nwarm=2):
    if srr is None:
        srr = Ring(kb, prefix + "S", 4, [128, 512], F32, psum=True)
    accr = Ring(kb, prefix + "acc", 3, [128, 512], F32, psum=True)
    ptr = Ring(kb, prefix + "pt", 4, [128, 512], BF16)
    pmr = Ring(kb, prefix + "pm", 4, [128, 512], BF16) if e_info is not None else None
    rden = kb.sb(prefix + "rden", [128, 512], F32)
    units = []
    for h in range(nheads):
        for qb in range(4):
            kcs = kcs_for(qb)
            for i, kc in enumerate(kcs):
                units.append((h, qb, kc, i == 0, i == len(kcs) - 1))
    state = {}
    cur_acc = {}
    seen_heads = set()
    wstate = [True]

    def keep_warm(n):
        if warm is None:
            return
        dps, dtok, dl, dr = warm
        for _ in range(n):
            kb.op_noinc("pe", "matmul", reads=[], writes=([dtok] if wstate[0] else []), out=dps[:, 0:256], lhsT=dl, rhs=dr, start=True, stop=True)
            wstate[0] = False

    def stage1(u):
        h, qb, kc, first, last = u
        if pre_head is not None and h not in seen_heads:
            seen_heads.add(h)
            pre_head(h)
        sp_, spt = srr.next()
        kb.mm(sp_[:, :], kT_ap(h, kc), qT_ap(h, qb), True, True, reads=[reads_k(h), reads_q(h)], writes=[spt])
        keep_warm(nwarm)
        pt_, ptt = ptr.next()
        kb.op("act", "activation", reads=[spt], writes=[ptt], out=pt_[:, :], in_=sp_[:, :], func=AF.Exp, scale=scale)
        if e_info is not None:
            eap, etok = e_info(h, qb, kc)
            pm_, pmt = pmr.next()
            kb.op("dve", "tensor_tensor", reads=[ptt, etok], writes=[pmt], out=pm_[:, :], in0=pt_[:, :], in1=eap, op=ALU.mult)
            state[u] = (pm_, pmt)
        else:
            state[u] = (pt_, ptt)

    def stage2(u):
        h, qb, kc, first, last = u
        pm_, pmt = state.pop(u)
        if first:
            cur_acc[0] = accr.next()
        acc, acct = cur_acc[0]
        kb.mm(acc[:, :], v_ap(h, kc), pm_[:, :], first, last, reads=[reads_v(h), pmt], writes=[acct])
        keep_warm(nwarm)
        if last:
            po = (h % 2) * 64
            do = 64 - po
            kb.op("dve", "reciprocal", reads=[acct], writes=[prefix + "rden"], out=rden[do:do + 64, :], in_=acc[do:do + 64, :])
            kb.op("dve", "tensor_tensor", reads=[acct, prefix + "rden"], writes=[("yst", h, qb)],
                  out=yst[po:po + 64, h // 2, qb * 512:(qb + 1) * 512], in0=acc[po:po + 64, :], in1=rden[do:do + 64, :], op=ALU.mult)

    LAG = 3
    for idx in range(len(units) + LAG):
        if idx < len(units):
            stage1(units[idx])
            if per_unit is not None:
                per_unit(idx)
        if idx - LAG >= 0:
            stage2(units[idx - LAG])


def build_vaug(kb, vaug, vsrc, reads, wtok):
    kb.op("pool", "memset", writes=[wtok + "ones"], ap=vaug[:, :, :, :], constant=1.0)
    v5 = vsrc.rearrange("p k (hp two e) -> p k hp two e", two=2, e=64)
    a5 = vaug.rearrange("p k (hp two) e -> p k hp two e", two=2)
    kb.op("dve", "tensor_copy", reads=reads + [wtok + "ones"], writes=[wtok + "e"], out=a5[:, :, :, 0, 0:64], in_=v5[:, :, :, 0, :])
    kb.op("pool", "tensor_copy", reads=reads + [wtok + "ones"], writes=[wtok + "o"], out=a5[:, :, :, 1, 64:128], in_=v5[:, :, :, 1, :])
    kb.op("pool", "engine_nop", reads=[wtok + "e", wtok + "o"], writes=[wtok])


def moe_experts(kb, l, din, XROWS, YROWS, ident_b, nexp):
    wgr = Ring(kb, "weg", 2, [128, 16, 512], BF16)
    wur = Ring(kb, "weu", 2, [128, 16, 512], BF16)
    wdr = Ring(kb, "wed", 2, [128, 4, 2048], BF16)
    xrr = Ring(kb, "xrE", 4, [128, 2048], BF16)
    xTr = Ring(kb, "xTE", 2, [128, 16, 256], BF16)
    aTr = Ring(kb, "aTE", 2, [128, 4, 256], BF16)
    sgr = Ring(kb, "sgE", 2, [128, 256], F32)
    yrr = Ring(kb, "yrE", 2, [128, 2048], F32)
    ptxr = Ring(kb, "ptx", 2, [128, 512], BF16, psum=True)
    pgur = Ring(kb, "pgu", 3, [128, 512], F32, psum=True)
    pyr = Ring(kb, "pyE", 3, [128, 512], F32, psum=True)
    ke = 0
    for e in range(nexp):
        wg, wgt = wgr.next()
        wu, wut = wur.next()
        wd, wdt = wdr.next()
        kb.dma("pool", out=wg[:, :, :], in_=din["w_e_gate"][l, e].rearrange("(kc p) n -> p kc n", p=128), writes=[wgt])
        kb.dma("pool", out=wu[:, :, :], in_=din["w_e_up"][l, e].rearrange("(kc p) n -> p kc n", p=128), writes=[wut])
        for nb in range(4):
            kb.dma("pool", out=wd[:, :, nb * 512:(nb + 1) * 512], in_=din["w_e_down"][l, e].rearrange("(kc p) n -> p kc n", p=128)[:, :, nb * 512:(nb + 1) * 512], writes=[wdt])
        xT, xTt = xTr.next()
        for s2 in range(2):
            xr, xrt = xrr.next()
            r0 = e * CAP + s2 * 128
            kb.dma("sp", out=xr[:, :], in_=XROWS[r0:r0 + 128, :], writes=[xrt])
            for g in range(4):
                pt, ptt = ptxr.next()
                for j in range(4):
                    c = g * 4 + j
                    kb.op("pe", "transpose", reads=[xrt, "cstb"], writes=[ptt], out=pt[:, j * 128:(j + 1) * 128], in_=xr[:, c * 128:(c + 1) * 128], identity=ident_b)
                kb.op("dve", "tensor_copy", reads=[ptt], writes=[(xTt, s2, g)], out=xT[:, g * 4:(g + 1) * 4, s2 * 128:(s2 + 1) * 128], in_=pt[:, :].rearrange("p (a b) -> p a b", a=4))
        xdeps = [(xTt, s2, g) for s2 in range(2) for g in range(4)]
        aT, aTt = aTr.next()
        for fcn in range(4):
            bank, bkt = pgur.next()
            for kc in range(16):
                kb.mm(bank[:, 0:256], wg[:, kc, fcn * 128:(fcn + 1) * 128], xT[:, kc, :], kc == 0, kc == 15, reads=[wgt] + xdeps, writes=[bkt])
            for kc in range(16):
                kb.mm(bank[:, 256:512], wu[:, kc, fcn * 128:(fcn + 1) * 128], xT[:, kc, :], kc == 0, kc == 15, reads=[wut] + xdeps, writes=[bkt])
            sg, sgt = sgr.next()
            kb.op("act", "activation", reads=[bkt], writes=[sgt], out=sg[:, :], in_=bank[:, 0:256], func=AF.Silu)
            kb.op("dve", "tensor_tensor", reads=[sgt, bkt], writes=[(aTt, fcn)], out=aT[:, fcn, :], in0=bank[:, 256:512], in1=sg[:, :], op=ALU.mult)
        for s2 in range(2):
            yr, yrt = yrr.next()
            for nb in range(4):
                py, pyt = pyr.next()
                for kc in range(4):
                    kb.mm(py[:, :], aT[:, kc, s2 * 128:(s2 + 1) * 128], wd[:, kc, nb * 512:(nb + 1) * 512], kc == 0, kc == 3, reads=[wdt] + [(aTt, f) for f in range(4)], writes=[pyt])
                kb.op("dve", "tensor_copy", reads=[pyt], writes=[(yrt, nb)], out=yr[:, nb * 512:(nb + 1) * 512], in_=py[:, :])
            r0 = e * CAP + s2 * 128
            kb.dma("sp", out=YROWS[r0:r0 + 128, :], in_=yr[:, :], reads=[(yrt, nb) for nb in range(4)], writes=[("YR", e, s2)])


def build_layer(nc, kb, din, hnd, out_d, l, finish_check, SC, CST, ln_alloc, ln_apply, emit_h_tile):
    HTM, HFM, PROJ, DTR, VATM, ZSTM, YBR, RV, MG, H1B, LGT, XROWS, YROWS = [SC[k] for k in
        ("HTM", "HFM", "PROJ", "DTR", "VATM", "ZSTM", "YBR", "RV", "MG", "H1B", "LGT", "XROWS", "YROWS")]
    cstf, cstb = CST["cstf"], CST["cstb"]
    ident_b = cstb[:, 0, :]
    ident_f = cstf[:, 0, :]
    J_b = cstb[:, 1, :]
    U_f = cstf[:, 2, :]
    Lo_f = cstf[:, 3, :]
    ones_b = cstb[:, 5, :]
    ones_f = cstf[:, 5, :]

    kb.phase_begin()
    hfm = kb.sb("hfm", [128, 16, S], BF16)
    for tb in range(4):
        kb.dma("sp", out=hfm[:, :, tb * 512:(tb + 1) * 512], in_=HFM[:, :, tb * 512:(tb + 1) * 512], writes=[("hfm", tb)])
    wr = Ring(kb, "wpin", 4, [128, 16, 128], BF16)
    pr = Ring(kb, "pp", 6, [128, 512], F32, psum=True)
    sr = Ring(kb, "pstage", 3, [128, S], BF16)
    k = 0
    for c in range(31):
        w, wt = wr.next()
        kb.dma("pool", out=w[:, :, :], in_=din["w_in_t"][l, c], writes=[wt])
        stg, stt = sr.next()
        for tb in range(4):
            p, pt = pr.next()
            for kc in range(16):
                kb.mm(p[:, :], w[:, kc, :], hfm[:, kc, tb * 512:(tb + 1) * 512], kc == 0, kc == 15, reads=[wt, ("hfm", tb)], writes=[pt])
            k += 1
            if k % 2 == 0:
                kb.op("act", "activation", reads=[pt], writes=[(stt, tb)], out=stg[:, tb * 512:(tb + 1) * 512], in_=p[:, :], func=AF.Copy)
            else:
                kb.op("dve", "tensor_copy", reads=[pt], writes=[(stt, tb)], out=stg[:, tb * 512:(tb + 1) * 512], in_=p[:, :])
        kb.dma("sp", out=PROJ[c], in_=stg[:, :], reads=[(stt, tb) for tb in range(4)], writes=[("PROJ", c)])
    wtm = Ring(kb, "wtm", 2, [128, 16, 512], BF16)
    tmst = kb.sb("tmstage", [128, 16, 512], BF16)
    for b in range(2):
        w, wt = wtm.next()
        kb.dma("pool", out=w[:, :, :], in_=din["w_in_tm"][l, b], writes=[wt])
        for t in range(NT):
            p, pt = pr.next()
            for kc in range(16):
                kb.mm(p[:, :], hfm[:, kc, t * 128:(t + 1) * 128], w[:, kc, :], kc == 0, kc == 15, reads=[wt, ("hfm", t // 4)], writes=[pt])
            if b == 1:
                kb.op("act", "activation", reads=[pt], writes=[("tmst", t)], out=tmst[:, t, :], in_=p[:, :], func=AF.Silu)
            elif t % 2 == 0:
                kb.op("act", "activation", reads=[pt], writes=[("tmst", t)], out=tmst[:, t, :], in_=p[:, :], func=AF.Copy)
            else:
                kb.op("dve", "tensor_copy", reads=[pt], writes=[("tmst", t)], out=tmst[:, t, :], in_=p[:, :])
        dst = VATM if b == 0 else ZSTM
        kb.dma("sp", out=dst.rearrange("(t p) n -> p t n", p=128), in_=tmst[:, :, :], reads=[("tmst", t) for t in range(NT)], writes=["TMD%d" % b])
    wdt = kb.sb("wdt", [128, 16, 16], BF16)
    dtst = kb.sb("dtst", [128, 16, 16], F32)
    kb.dma("pool", out=wdt[:, :, :], in_=din["w_dt_tm"][l], writes=["wdt"])
    for t in range(NT):
        p, pt = pr.next()
        for kc in range(16):
            kb.mm(p[:, 0:16], hfm[:, kc, t * 128:(t + 1) * 128], wdt[:, kc, :], kc == 0, kc == 15, reads=["wdt", ("hfm", t // 4)], writes=[pt])
        kb.op("dve", "tensor_copy", reads=[pt], writes=[("dtst", t)], out=dtst[:, t, :], in_=p[:, 0:16])
    kb.dma("sp", out=DTR.rearrange("(t p) n -> p t n", p=128), in_=dtst[:, :, :], reads=[("dtst", t) for t in range(NT)], writes=["DTR"])
    kb.phase_end()
    if finish_check("P%d" % l):
        return

    kb.phase_begin()
    qT = kb.sb("qT", [128, 4, S], BF16)
    kTz = kb.sb("kTz", [128, 8, S], BF16)
    vtm = kb.sb("vtm", [128, 16, 512], BF16)
    yst = kb.sb("yst", [128, 4, S], BF16)
    for c in range(4):
        kb.op("pool", "memset", writes=[("kTzz", c)], ap=kTz[64:128, 2 * c, :], constant=0.0)
        kb.op("dve", "memset", writes=[("kTzz", c)], ap=kTz[0:64, 2 * c + 1, :], constant=0.0)
        kb.dma("sp", out=qT[:, c, :], in_=PROJ[CH["qa"] + c], writes=[("qT", c)])
        kb.dma("sp", out=kTz[0:64, 2 * c, :], in_=PROJ[CH["ka"] + c, 0:64, :], writes=[("kT", c)])
        kb.dma("sp", out=kTz[64:128, 2 * c + 1, :], in_=PROJ[CH["ka"] + c, 64:128, :], writes=[("kT", c)])
        kb.op("pool", "engine_nop", reads=[("kTzz", c), ("kT", c)], writes=[("kTall", c)])
    kb.dma("sp", out=vtm[:, :, :], in_=VATM.rearrange("(t p) n -> p t n", p=128), writes=["vtm"])
    vaug = kb.sb("vaugA", [128, 16, 8, 128], BF16)
    build_vaug(kb, vaug[:, :, :, :], vtm[:, :, :], ["vtm"], "vaug")
    DEL = [d for d in range(-1536, 1921, 128) if -1151 <= d <= 1535]
    Et = kb.sb("Et", [128, 2, len(DEL), 512], BF16)
    hkr = Ring(kb, "hk", 6, [128, 512], BF16)
    pj = kb.ps("pj", [128, 512], F32)

    def E_dma(h, di):
        hk, hkt = hkr.next()
        base = 1408 - DEL[di]
        kb.dma("pool", out=hk[:, :], in_=bass.AP(hnd["RV"], h * RVLEN + base, [[1, 128], [1, 512]]), writes=[hkt])
        return (h, di, hk, hkt)

    def E_mm(h, di, hk, hkt):
        kb.mm(pj[:, :], J_b, hk[:, :], True, True, reads=[hkt, "cstb"], writes=["pj"])
        if di % 3 == 0:
            kb.op("dve", "tensor_copy", reads=["pj"], writes=[("E", h % 2, di)], out=Et[:, h % 2, di, :], in_=pj[:, :])
        else:
            kb.op("act", "activation", reads=["pj"], writes=[("E", h % 2, di)], out=Et[:, h % 2, di, :], in_=pj[:, :], func=AF.Copy)

    pendingE = []
    inflight = []

    def pre_head(h):
        if h == 0:
            q = []
            for di in range(len(DEL)):
                q.append(E_dma(0, di))
                if len(q) > 2:
                    E_mm(*q.pop(0))
            while q:
                E_mm(*q.pop(0))
        while inflight:
            E_mm(*inflight.pop(0))
        while pendingE:
            E_mm(*E_dma(*pendingE.pop(0)))
        if h + 1 < 8:
            pendingE.extend((h + 1, di) for di in range(len(DEL)))

    def per_unit(i):
        if i % 2 == 1:
            if len(inflight) >= 4 or (inflight and not pendingE):
                E_mm(*inflight.pop(0))
            if pendingE:
                inflight.append(E_dma(*pendingE.pop(0)))

    def e_info(h, qb, kc):
        di = DEL.index(128 * kc - 512 * qb)
        return Et[:, h % 2, di, :], ("E", h % 2, di)

    attention(kb, 8,
              kT_ap=lambda h, kc: kTz[:, h, kc * 128:(kc + 1) * 128],
              qT_ap=lambda h, qb: qT[:, h // 2, qb * 512:(qb + 1) * 512],
              v_ap=lambda h, kc: vaug[:, kc, h, :],
              kcs_for=lambda qb: [kc for kc in range(16) if -1151 <= 128 * kc - 512 * qb <= 1535],
              e_info=e_info, scale=0.125, yst=yst, prefix="A",
              reads_k=lambda h: ("kTall", h // 2), reads_q=lambda h: ("qT", h // 2), reads_v=lambda h: "vaug", pre_head=pre_head, per_unit=per_unit)
    kb.dma("sp", out=YBR[0], in_=yst[:, :, :], reads=[("yst", h, qb) for h in range(8) for qb in range(4)], writes=[("YBR", 0)])
    kb.phase_end()
    if finish_check("A%d" % l):
        return

    kb.phase_begin()
    cp = kb.sb("cp", [128, 256], F32)
    kb.dma("sp", out=cp[:, :], in_=din["colp"][l], writes=["cp"])
    wuq = kb.sb("wuq", [128, 4, 1024], BF16)
    wukv = kb.sb("wukv", [128, 2, 1024], BF16)
    kb.dma("pool", out=wuq[:, :, :], in_=din["w_uq_t"][l], writes=["wuq"])
    kb.dma("pool", out=wukv[:, :, :], in_=din["w_ukv_t"][l], writes=["wukv"])
    ccr = Ring(kb, "cc", 2, [96, 512], F32)
    ssr = Ring(kb, "ss", 2, [96, 512], F32)
    krAr = Ring(kb, "krA", 2, [96, 512], BF16)
    krBr = Ring(kb, "krB", 2, [96, 512], BF16)
    QT = kb.sb("QT", [96, 8, S], BF16)
    KT = kb.sb("KT", [96, 8, S], BF16)
    vaug = kb.sb("vaugB", [128, 16, 8, 128], BF16)
    vtm = kb.sb("vtmB", [128, 16, 512], BF16)
    yst = kb.sb("ystB", [128, 4, S], BF16)
    cqr = Ring(kb, "cqb", 2, [128, 4, 512], BF16)
    ckr = Ring(kb, "ckb", 2, [128, 2, 512], BF16)
    sqb = kb.sb("sqb", [128, 4, 512], BF16)
    cqn = kb.sb("cqn", [128, 4, 512], BF16)
    ckvn = kb.sb("ckvn", [128, 2, 512], BF16)
    rt = kb.sb("rt", [128, 512], F32)
    t1 = kb.sb("t1", [96, 512], F32)
    t2 = kb.sb("t2", [96, 512], F32)
    krope = kb.sb("krope", [96, 512], BF16)
    pss = kb.ps("pss", [128, 512], F32)
    srrB = Ring(kb, "BS", 4, [128, 512], F32, psum=True)
    pqr = Ring.wrap(srrB.tiles[0:2], srrB.names[0:2])
    pq2r = Ring.wrap(srrB.tiles[2:3], srrB.names[2:3])

    def rms_block(src, stag, nch, gcol, dst, dtag, inv_n):
        kb.op("dve", "tensor_tensor", reads=[stag], writes=["sqb"], out=sqb[:, 0:nch, :], in0=src[:, 0:nch, :], in1=src[:, 0:nch, :], op=ALU.mult)
        for c in range(nch):
            kb.mm(pss[:, :], ones_b, sqb[:, c, :], c == 0, c == nch - 1, reads=["sqb", "cstb"], writes=["pss"])
        kb.op("dve", "tensor_scalar", reads=["pss"], writes=["rt"], out=rt[:, :], in0=pss[:, :], scalar1=inv_n, scalar2=EPS, op0=ALU.mult, op1=ALU.add)
        kb.op("act", "activation", reads=["rt"], writes=["rt"], out=rt[:, :], in_=rt[:, :], func=AF.Sqrt)
        kb.op("dve", "reciprocal", reads=["rt"], writes=["rt"], out=rt[:, :], in_=rt[:, :])
        for c in range(nch):
            kb.op("dve", "scalar_tensor_tensor", reads=[stag, "rt", "cp"], writes=[dtag], out=dst[:, c, :], in0=src[:, c, :],
                  scalar=cp[:, gcol + c:gcol + c + 1], in1=rt[:, :], op0=ALU.mult, op1=ALU.mult)

    for tb in range(4):
        tbs = slice(tb * 512, (tb + 1) * 512)
        cqb, cqt = cqr.next()
        ckb, ckt = ckr.next()
        cc, cct = ccr.next()
        ss, sst = ssr.next()
        krA, krAt = krAr.next()
        krB, krBt = krBr.next()
        kb.dma("sp", out=cc[64:96, :], in_=din["rope"][0, 64:96, tbs], writes=[cct])
        kb.dma("sp", out=ss[64:96, :], in_=din["rope"][1, 64:96, tbs], writes=[sst])
        kb.dma("sp", out=krA[64:96, :], in_=PROJ[CH["krx"], 0:32, tbs], writes=[krAt])
        kb.dma("sp", out=krB[64:96, :], in_=PROJ[CH["krx"], 32:64, tbs], writes=[krBt])
        for c in range(4):
            kb.dma("sp", out=cqb[:, c, :], in_=PROJ[CH["cq"] + c, :, tbs], writes=[cqt])
        for c in range(2):
            kb.dma("sp", out=ckb[:, c, :], in_=PROJ[CH["ckv"] + c, :, tbs], writes=[ckt])
        rms_block(cqb, cqt, 4, 64, cqn, "cqn", 1.0 / 512)
        for h in range(8):
            pq, pqt = pqr.next()
            pq2, pq2t = pq2r.next()
            for kc in range(4):
                kb.mm(pq[0:96, :], wuq[:, kc, 96 * h:96 * h + 96], cqn[:, kc, :], kc == 0, kc == 3, reads=["wuq", "cqn"], writes=[pqt])
            for kc in range(4):
                kb.mm(pq2[64:96, :], wuq[:, kc, 768 + 32 * h:768 + 32 * h + 32], cqn[:, kc, :], kc == 0, kc == 3, reads=["wuq", "cqn"], writes=[pq2t])
            kb.op("act", "activation", reads=[pqt], writes=[("QT", h, tb)], out=QT[0:64, h, tbs], in_=pq[0:64, :], func=AF.Copy)
            kb.op("dve", "tensor_tensor", reads=[pqt, cct], writes=["t1"], out=t1[64:96, :], in0=pq[64:96, :], in1=cc[64:96, :], op=ALU.mult)
            kb.op("dve", "tensor_tensor", reads=[pq2t, sst], writes=["t2"], out=t2[64:96, :], in0=pq2[64:96, :], in1=ss[64:96, :], op=ALU.mult)
            kb.op("pool", "tensor_tensor", reads=["t1", "t2"], writes=[("QT", h, tb)], out=QT[64:96, h, tbs], in0=t1[64:96, :], in1=t2[64:96, :], op=ALU.add)
        rms_block(ckb, ckt, 2, 68, ckvn, "ckvn", 1.0 / 256)
        for h in range(8):
            pq, pqt = pqr.next()
            for kc in range(2):
                kb.mm(pq[0:64, :], wukv[:, kc, 64 * h:64 * h + 64], ckvn[:, kc, :], kc == 0, kc == 1, reads=["wukv", "ckvn"], writes=[pqt])
            if h % 2 == 0:
                kb.op("act", "activation", reads=[pqt], writes=[("KT", h, tb)], out=KT[0:64, h, tbs], in_=pq[0:64, :], func=AF.Copy)
            else:
                kb.op("dve", "tensor_copy", reads=[pqt], writes=[("KT", h, tb)], out=KT[0:64, h, tbs], in_=pq[0:64, :])
        kb.op("dve", "tensor_tensor", reads=[krAt, cct], writes=["t1"], out=t1[64:96, :], in0=krA[64:96, :], in1=cc[64:96, :], op=ALU.mult)
        kb.op("dve", "tensor_tensor", reads=[krBt, sst], writes=["t2"], out=t2[64:96, :], in0=krB[64:96, :], in1=ss[64:96, :], op=ALU.mult)
        kb.op("pool", "tensor_tensor", reads=["t1", "t2"], writes=["krope"], out=krope[64:96, :], in0=t1[64:96, :], in1=t2[64:96, :], op=ALU.add)
        for h in range(8):
            kb.op("pool", "tensor_copy", reads=["krope"], writes=[("KT", h, tb)], out=KT[64:96, h, tbs], in_=krope[64:96, :])
        for tt in range(4):
            pq, pqt = pqr.next()
            for kc in range(2):
                kb.mm(pq[:, :], ckvn[:, kc, tt * 128:(tt + 1) * 128], wukv[:, kc, 512:1024], kc == 0, kc == 1, reads=["wukv", "ckvn"], writes=[pqt])
            if tt % 2 == 0:
                kb.op("act", "activation", reads=[pqt], writes=[("vtm", tb, tt)], out=vtm[:, tb * 4 + tt, :], in_=pq[:, :], func=AF.Copy)
            else:
                kb.op("dve", "tensor_copy", reads=[pqt], writes=[("vtm", tb, tt)], out=vtm[:, tb * 4 + tt, :], in_=pq[:, :])

    build_vaug(kb, vaug[:, :, :, :], vtm[:, :, :], [("vtm", tb, tt) for tb in range(4) for tt in range(4)], "vaugB")
    attention(kb, 8,
              kT_ap=lambda h, kc: KT[0:96, h, kc * 128:(kc + 1) * 128],
              qT_ap=lambda h, qb: QT[0:96, h, qb * 512:(qb + 1) * 512],
              v_ap=lambda h, kc: vaug[:, kc, h, :],
              kcs_for=lambda qb: list(range(16)),
              e_info=None, scale=96.0 ** -0.5, yst=yst, prefix="B",
              reads_k=lambda h: ("KTall", h), reads_q=lambda h: ("QTall", h), reads_v=lambda h: "vtmall",
              pre_head=lambda h: [kb.op("pool", "engine_nop", reads=[("KT", h, tb) for tb in range(4)] + [("QT", h, tb) for tb in range(4)] + ["vaugB"],
                                        writes=[("KTall", h), ("QTall", h), "vtmall"])], srr=srrB)
    kb.dma("sp", out=YBR[1], in_=yst[:, :, :], reads=[("yst", h, qb) for h in range(8) for qb in range(4)], writes=[("YBR", 1)])
    kb.phase_end()
    if finish_check("B%d" % l):
        return

    kb.phase_begin()
    cp = kb.sb("cpC", [128, 256], F32)
    kb.dma("sp", out=cp[:, :], in_=din["colp"][l], writes=["cp"])
    hg = kb.sb("hg", [128, 4, S + 30], BF16)
    kb.op("pool", "memset", writes=["hgpad"], ap=hg[:, :, 0:15], constant=0.0)
    kb.op("pool", "memset", writes=["hgpad"], ap=hg[:, :, S + 15:S + 30], constant=0.0)
    gar = Ring(kb, "ga", 2, [128, S], BF16)
    ggr = Ring(kb, "gg", 2, [128, S], BF16)
    for c in range(4):
        ga, gat = gar.next()
        gg, ggt = ggr.next()
        kb.dma("sp", out=ga[:, :], in_=PROJ[CH["glua"] + c], writes=[gat])
        kb.dma("sp", out=gg[:, :], in_=PROJ[CH["glug"] + c], writes=[ggt])
        kb.op("act", "activation", reads=[ggt], writes=[ggt], out=gg[:, :], in_=gg[:, :], func=AF.Sigmoid)
        kb.op("dve", "tensor_tensor", reads=[gat, ggt, "hgpad"], writes=[("hg", c)], out=hg[:, c, 15:15 + S], in0=ga[:, :], in1=gg[:, :], op=ALU.mult)
    dg = kb.sb("dg", [128, 4, 31, 128], BF16)
    for c in range(4):
        for j in range(31):
            eng = "dve" if (c * 31 + j) % 2 == 0 else "pool"
            kb.op(eng, "tensor_scalar", reads=["cp", "cstb"], writes=[("dg", c)], out=dg[:, c, j, :], in0=ident_b,
                  scalar1=cp[:, 82 + c * 31 + j:83 + c * 31 + j], scalar2=None, op0=ALU.mult)
    yc = kb.sb("yc", [128, 4, 512], F32)
    ysq = kb.sb("ysq", [128, 4, 512], F32)
    mt = kb.sb("mt", [128, 512], F32)
    m2 = kb.sb("m2", [128, 512], F32)
    vt = kb.sb("vt", [128, 512], F32)
    tmr = Ring(kb, "tmpC", 2, [128, 512], F32)
    yo = kb.sb("yoC", [128, 4, S], BF16)
    pcr = Ring(kb, "pc", 3, [128, 512], F32, psum=True)
    psm = kb.ps("psm", [128, 512], F32)
    psq = kb.ps("psq", [128, 512], F32)
    for tb in range(4):
        tbs = slice(tb * 512, (tb + 1) * 512)
        for c in range(4):
            pc, pct = pcr.next()
            for j in range(31):
                kb.mm(pc[:, :], dg[:, c, j, :], hg[:, c, tb * 512 + j:tb * 512 + j + 512], j == 0, j == 30, reads=[("dg", c), ("hg", c)], writes=[pct])
            kb.op("dve", "tensor_scalar", reads=[pct, "cp"], writes=[("yc", c)], out=yc[:, c, :], in0=pc[:, :], scalar1=cp[:, 70 + c:71 + c], scalar2=None, op0=ALU.add)
            kb.op("act", "activation", reads=[("yc", c)], writes=[("ysq", c)], out=ysq[:, c, :], in_=yc[:, c, :], func=AF.Square)
        for c in range(4):
            kb.mm(psm[:, :], ones_f, yc[:, c, :], c == 0, c == 3, reads=[("yc", c), "cstf"], writes=["psm"])
        for c in range(4):
            kb.mm(psq[:, :], ones_f, ysq[:, c, :], c == 0, c == 3, reads=[("ysq", c), "cstf"], writes=["psq"])
        kb.op("dve", "tensor_scalar", reads=["psm"], writes=["mt"], out=mt[:, :], in0=psm[:, :], scalar1=1.0 / 512, scalar2=None, op0=ALU.mult)
        kb.op("dve", "tensor_tensor", reads=["mt"], writes=["m2"], out=m2[:, :], in0=mt[:, :], in1=mt[:, :], op=ALU.mult)
        kb.op("dve", "scalar_tensor_tensor", reads=["psq", "m2"], writes=["vt"], out=vt[:, :], in0=psq[:, :], scalar=1.0 / 512, in1=m2[:, :], op0=ALU.mult, op1=ALU.subtract)
        kb.op("dve", "tensor_scalar", reads=["vt"], writes=["vt"], out=vt[:, :], in0=vt[:, :], scalar1=EPS, scalar2=None, op0=ALU.add)
        kb.op("act", "activation", reads=["vt"], writes=["vt"], out=vt[:, :], in_=vt[:, :], func=AF.Sqrt)
        kb.op("dve", "reciprocal", reads=["vt"], writes=["vt"], out=vt[:, :], in_=vt[:, :])
        for c in range(4):
            tm, tmt = tmr.next()
            kb.op("dve", "tensor_tensor", reads=[("yc", c), "mt"], writes=[tmt], out=tm[:, :], in0=yc[:, c, :], in1=mt[:, :], op=ALU.subtract)
            kb.op("pool", "tensor_tensor", reads=[tmt, "vt"], writes=[tmt], out=tm[:, :], in0=tm[:, :], in1=vt[:, :], op=ALU.mult)
            kb.op("act", "activation", reads=[tmt, "cp"], writes=[("yoC", c, tb)], out=yo[:, c, tbs], in_=tm[:, :], func=AF.Silu,
                  scale=cp[:, 74 + c:75 + c], bias=cp[:, 78 + c:79 + c])
    kb.dma("sp", out=YBR[2], in_=yo[:, :, :], reads=[("yoC", c, tb) for c in range(4) for tb in range(4)], writes=[("YBR", 2)])
    kb.phase_end()
    if finish_check("C%d" % l):
        return

    kb.phase_begin()
    cp = kb.sb("cpD", [128, 256], F32)
    kb.dma("sp", out=cp[:, :], in_=din["colp"][l], writes=["cp"])
    rp = kb.sb("rpD", [128, 640], F32)
    kb.dma("sp", out=rp[:, :], in_=din["rowp"][l, 0:1, 8192:8832].partition_broadcast(128), writes=["rp"])
    gnd = rp[:, 0:512]
    dsk = rp[:, 548:556]
    pbig = kb.ps("pbig", [128, 1024], F32)
    pA = kb.ps("pA", [128, 512], F32)
    pB = kb.ps("pB", [128, 512], F32)
    pC = kb.ps("pC", [128, 512], F32)
    pD = kb.ps("pD", [128, 512], F32)
    pTr = Ring(kb, "pT", 2, [128, 512], BF16, psum=True)
    convr = Ring.wrap([pA, pB, pC], ["pA", "pB", "pC"])
    dgd = kb.sb("dgd", [128, 8, 5, 128], BF16)
    for c in range(8):
        for j in range(5):
            eng = "dve" if (c * 5 + j) % 2 == 0 else "pool"
            kb.op(eng, "tensor_scalar", reads=["cp", "cstb"], writes=[("dgd", c)], out=dgd[:, c, j, :], in0=ident_b,
                  scalar1=cp[:, 206 + c * 5 + j:207 + c * 5 + j], scalar2=None, op0=ALU.mult)
    xbc = kb.sb("xbc", [128, 8, S], BF16)
    xinr = Ring(kb, "xinD", 2, [128, S + 4], BF16)
    for i in range(2):
        kb.op("pool", "memset", writes=[xinr.names[i] + "pad"], ap=xinr.tiles[i][:, 0:2], constant=0.0)
        kb.op("pool", "memset", writes=[xinr.names[i] + "pad"], ap=xinr.tiles[i][:, S + 2:S + 4], constant=0.0)
    for c in range(8):
        xi, xit = xinr.next()
        kb.dma("sp", out=xi[:, 2:2 + S], in_=PROJ[CH["xs"] + c], reads=[xit + "pad"], writes=[xit])
        for tb in range(4):
            pc, pct = convr.next()
            for j in range(5):
                kb.mm(pc[:, :], dgd[:, c, j, :], xi[:, tb * 512 + j:tb * 512 + j + 512], j == 0, j == 4, reads=[("dgd", c), xit, xit + "pad"], writes=[pct])
            kb.op("act", "activation", reads=[pct, "cp"], writes=[("xbc", c)], out=xbc[:, c, tb * 512:(tb + 1) * 512], in_=pc[:, :], func=AF.Silu,
                  bias=cp[:, 246 + c:247 + c], scale=1.0)
    xtm = kb.sb("xtm", [128, 16, 512], BF16)
    btm = kb.sb("btm", [128, 16, 256], BF16)
    for t in range(NT):
        pt, ptt = pTr.next()
        for c in range(4):
            kb.op("pe", "transpose", reads=[("xbc", c), "cstb"], writes=[ptt], out=pt[:, c * 128:(c + 1) * 128], in_=xbc[:, c, t * 128:(t + 1) * 128], identity=ident_b)
        kb.op("dve", "tensor_copy", reads=[ptt], writes=[("xtm", t)], out=xtm[:, t, :], in_=pt[:, :])
        pt, ptt = pTr.next()
        for g in range(2):
            kb.op("pe", "transpose", reads=[("xbc", 4 + g), "cstb"], writes=[ptt], out=pt[:, g * 128:(g + 1) * 128], in_=xbc[:, 4 + g, t * 128:(t + 1) * 128], identity=ident_b)
        kb.op("dve", "tensor_copy", reads=[ptt], writes=[("btm", t)], out=btm[:, t, :], in_=pt[:, 0:256])
    def small(name):
        return kb.sb(name, [128, 16, 16], F32)
    dtr, dtv, av, Ecs, Tot, negE, wend, expE, dec = [small(n) for n in ("dtr", "dtv", "av", "Ecs", "Tot", "negE", "wend", "expE", "dec")]
    negA = kb.sb("negA", [128, 16], F32)
    kb.dma("sp", out=dtr[:, :, :], in_=DTR.rearrange("(t p) n -> p t n", p=128), writes=["dtr"])
    kb.op("dve", "tensor_tensor", reads=["dtr", "rp"], writes=["dtv"], out=dtv[:, :, :], in0=dtr[:, :, :],
          in1=rp[:, 556:572].unsqueeze(1).to_broadcast([128, 16, 16]), op=ALU.add)
    kb.op("act", "activation", reads=["dtv"], writes=["dtv"], out=dtv[:, :, :], in_=dtv[:, :, :], func=AF.Exp)
    kb.op("act", "activation", reads=["dtv"], writes=["dtv"], out=dtv[:, :, :], in_=dtv[:, :, :], func=AF.Ln, bias=1.0, scale=1.0)
    kb.op("act", "activation", reads=["rp"], writes=["negA"], out=negA[:, :], in_=rp[:, 572:588], func=AF.Exp)
    kb.op("dve", "tensor_scalar", reads=["negA"], writes=["negA"], out=negA[:, :], in0=negA[:, :], scalar1=-1.0, scalar2=None, op0=ALU.mult)
    kb.op("dve", "tensor_tensor", reads=["dtv", "negA"], writes=["av"], out=av[:, :, :], in0=dtv[:, :, :],
          in1=negA[:, :].unsqueeze(1).to_broadcast([128, 16, 16]), op=ALU.mult)
    for t in range(NT):
        kb.mm(pD[:, t * 16:t * 16 + 8], U_f, av[:, t, 0:8], True, True, reads=["av", "cstf"], writes=["pD"], last=False)
        kb.mm(pD[:, t * 16 + 8:t * 16 + 16], Lo_f, av[:, t, 8:16], True, True, reads=["av", "cstf"], writes=["pD"], last=False)
        kb.mm(pD[:, 256 + t * 16:256 + t * 16 + 16], ones_f, av[:, t, :], True, True, reads=["av", "cstf"], writes=["pD"], last=(t == NT - 1))
    kb.op("dve", "tensor_copy", reads=["pD"], writes=["Ecs"], out=Ecs[:, :, :], in_=pD[:, 0:256].rearrange("p (a b) -> p a b", b=16))
    kb.op("dve", "tensor_copy", reads=["pD"], writes=["Tot"], out=Tot[:, :, :], in_=pD[:, 256:512].rearrange("p (a b) -> p a b", b=16))
    kb.op("dve", "tensor_scalar", reads=["Ecs"], writes=["negE"], out=negE[:, :, :], in0=Ecs[:, :, :], scalar1=-1.0, scalar2=None, op0=ALU.mult)
    kb.op("dve", "tensor_tensor", reads=["Tot", "Ecs"], writes=["wend"], out=wend[:, :, :], in0=Tot[:, :, :], in1=Ecs[:, :, :], op=ALU.subtract)
    kb.op("act", "activation", reads=["wend"], writes=["wend"], out=wend[:, :, :], in_=wend[:, :, :], func=AF.Exp)
    kb.op("dve", "tensor_tensor", reads=["wend", "dtv"], writes=["wend"], out=wend[:, :, :], in0=wend[:, :, :], in1=dtv[:, :, :], op=ALU.mult)
    kb.op("act", "activation", reads=["Ecs"], writes=["expE"], out=expE[:, :, :], in_=Ecs[:, :, :], func=AF.Exp)
    kb.op("act", "activation", reads=["Tot"], writes=["dec"], out=dec[:, :, :], in_=Tot[:, :, :], func=AF.Exp)

    ytm = kb.sb("ytm", [128, 16, 512], F32)
    cbb = kb.sb("cbb", [128, 16, 2, 128], BF16)
    cbf = kb.sb("cbf", [128, 2, 128], BF16)
    Dt = kb.sb("Dt", [128, 8, 128], F32)
    ex = kb.sb("ex", [128, 8, 128], BF16)
    Mtr = Ring(kb, "Mt", 2, [128, 8, 128], BF16)
    xdr = Ring(kb, "xdt", 2, [128, 8, 64], BF16)
    xwr = Ring(kb, "xw", 2, [128, 8, 64], BF16)
    tmpa = kb.sb("tmpDa", [128, 512], F32)
    tmpb = kb.sb("tmpDb", [128, 512], F32)
    Hs = [kb.sb("Hs%d" % d, [128, 512], F32) for d in range(2)]
    Hb = [kb.sb("Hb%d" % d, [128, 512], BF16) for d in range(2)]
    for d in range(2):
        kb.op("pool", "memset", writes=["Hs%d" % d], ap=Hs[d][:, :], constant=0.0)
        kb.op("pool", "memset", writes=["Hb%d" % d], ap=Hb[d][:, :], constant=0.0)
    units = [(0, c) for c in range(16)] + [(1, c) for c in range(15, -1, -1)]
    st = {}

    def d_stage1(u):
        d, c = u
        cs = slice(c * 128, (c + 1) * 128)
        if d == 0:
            for g in range(2):
                kb.mm(pA[:, g * 128:(g + 1) * 128], xbc[:, 4 + g, cs], xbc[:, 6 + g, cs], True, True, reads=[("xbc", 4 + g), ("xbc", 6 + g)], writes=["pA"], last=(g == 1))
            kb.op("dve", "tensor_tensor", reads=["pA", "cstf"], writes=["cbf"], out=cbf[:, :, :], in0=pA[:, 0:256].rearrange("p (g i) -> p g i", g=2),
                  in1=U_f.unsqueeze(1).to_broadcast([128, 2, 128]), op=ALU.mult)
            kb.op("dve", "tensor_tensor", reads=["pA", "cstf"], writes=[("cbb", c)], out=cbb[:, c, :, :], in0=pA[:, 0:256].rearrange("p (g i) -> p g i", g=2),
                  in1=Lo_f.unsqueeze(1).to_broadcast([128, 2, 128]), op=ALU.mult)
        tri = U_f if d == 0 else Lo_f
        for h in range(8):
            hd = d * 8 + h
            kb.mm(pbig[:, h * 128:(h + 1) * 128], av[:, c, hd:hd + 1].to_broadcast([128, 128]), tri, True, True, reads=["av", "cstf"], writes=["pbig"], last=(h == 7))
        kb.op("dve", "tensor_tensor", reads=["pbig", "negE"], writes=["Dt"], out=Dt[:, :, :], in0=pbig[:, :].rearrange("p (h i) -> p h i", h=8),
              in1=negE[:, c, d * 8:d * 8 + 8].unsqueeze(2).to_broadcast([128, 8, 128]), op=ALU.add)
        kb.op("dve", "tensor_scalar_min", reads=["Dt"], writes=["Dt"], out=Dt[:, :, :], in0=Dt[:, :, :], scalar1=0.0)
        kb.op("act", "activation", reads=["Dt"], writes=["ex"], out=ex[:, :, :], in_=Dt[:, :, :], func=AF.Exp)
        Mt, Mtt = Mtr.next()
        for g in range(2):
            cbx = cbf[:, g:g + 1, :] if d == 0 else cbb[:, c, g:g + 1, :]
            kb.op("dve", "scalar_tensor_tensor", reads=["ex", "cbf" if d == 0 else ("cbb", c)], writes=[Mtt], out=Mt[:, 4 * g:4 * g + 4, :], in0=ex[:, 4 * g:4 * g + 4, :],
                  scalar=1.0, in1=cbx.to_broadcast([128, 4, 128]), op0=ALU.min, op1=ALU.mult)
        xd, xdt_ = xdr.next()
        xw, xwt = xwr.next()
        x3 = xtm[:, c, :].rearrange("p (h e) -> p h e", h=8)
        kb.op("pool", "tensor_tensor", reads=[("xtm", c), "dtv"], writes=[xdt_], out=xd[:, :, :], in0=x3,
              in1=dtv[:, c, d * 8:d * 8 + 8].unsqueeze(2).to_broadcast([128, 8, 64]), op=ALU.mult)
        kb.op("pool", "tensor_tensor", reads=[("xtm", c), "wend"], writes=[xwt], out=xw[:, :, :], in0=x3,
              in1=wend[:, c, d * 8:d * 8 + 8].unsqueeze(2).to_broadcast([128, 8, 64]), op=ALU.mult)
        st[u] = (Mt, Mtt, xd, xdt_, xw, xwt)

    def d_stage2(u):
        d, c = u
        cs = slice(c * 128, (c + 1) * 128)
        Mt, Mtt, xd, xdt_, xw, xwt = st.pop(u)
        for h in range(8):
            kb.mm(pB[:, h * 64:(h + 1) * 64], Mt[:, h, :], xd[:, h, :], True, True, reads=[Mtt, xdt_], writes=["pB"], last=(h == 7))
        for g in range(2):
            kb.mm(pD[:, g * 256:(g + 1) * 256], xbc[:, 6 + g, cs], Hb[d][:, g * 256:(g + 1) * 256], True, True, reads=[("xbc", 6 + g), "Hb%d" % d], writes=["pD"], last=(g == 1))
        for g in range(2):
            kb.mm(pC[:, g * 256:(g + 1) * 256], btm[:, c, g * 128:(g + 1) * 128], xw[:, 4 * g:4 * g + 4, :].rearrange("p h e -> p (h e)"), True, True,
                  reads=[("btm", c), xwt], writes=["pC"], last=(g == 1))
        kb.op("dve", "tensor_tensor", reads=["pD", "expE"], writes=["tmpDa"], out=tmpa[:, :].rearrange("p (h e) -> p h e", h=8), in0=pD[:, :].rearrange("p (h e) -> p h e", h=8),
              in1=expE[:, c, d * 8:d * 8 + 8].unsqueeze(2).to_broadcast([128, 8, 64]), op=ALU.mult)
        if d == 0:
            kb.op("dve", "tensor_tensor", reads=["pB", "tmpDa"], writes=[("ytm", c)], out=ytm[:, c, :], in0=pB[:, :], in1=tmpa[:, :], op=ALU.add)
        else:
            kb.op("dve", "tensor_tensor", reads=["pB", "tmpDa"], writes=["tmpDb"], out=tmpb[:, :], in0=pB[:, :], in1=tmpa[:, :], op=ALU.add)
            kb.op("pool", "tensor_tensor", reads=["tmpDb", ("ytm", c)], writes=[("ytm", c)], out=ytm[:, c, :], in0=ytm[:, c, :], in1=tmpb[:, :], op=ALU.add)
        hn = "Hs%d" % d
        kb.op("dve", "tensor_tensor", reads=[hn, "dec"], writes=[hn], out=Hs[d][:, :].rearrange("p (h e) -> p h e", h=8), in0=Hs[d][:, :].rearrange("p (h e) -> p h e", h=8),
              in1=dec[:, c, d * 8:d * 8 + 8].unsqueeze(2).to_broadcast([128, 8, 64]), op=ALU.mult)
        kb.op("dve", "tensor_tensor", reads=[hn, "pC"], writes=[hn], out=Hs[d][:, :], in0=pC[:, :], in1=Hs[d][:, :], op=ALU.add)
        kb.op("act", "activation", reads=[hn], writes=["Hb%d" % d], out=Hb[d][:, :], in_=Hs[d][:, :], func=AF.Copy)

    for idx in range(len(units) + 1):
        if idx < len(units):
            d_stage1(units[idx])
        if idx >= 1:
            d_stage2(units[idx - 1])

    zsr = Ring(kb, "zs", 2, [128, 512], BF16)
    yz = kb.sb("yz", [128, 512], F32)
    junk = kb.sb("junkD", [128, 512], F32)
    yoD = kb.sb("yoD", [128, 512], BF16)
    ssq = kb.sb("ssq", [128, 1], F32)
    mhalfD = kb.sb("mhalfD", [128, 1], F32)
    kb.op("pool", "memset", writes=["mhalfD"], ap=mhalfD[:, :], constant=-0.5)
    ydr = Ring(kb, "ydT", 2, [128, 4, 128], BF16)
    for c in range(NT):
        zs, zst = zsr.next()
        kb.dma("sp", out=zs[:, :], in_=ZSTM[c * 128:(c + 1) * 128, :], writes=[zst])
        kb.op("pool", "tensor_tensor", reads=[("xtm", c), "rp"], writes=["tmpDb"], out=tmpb[:, :].rearrange("p (h e) -> p h e", h=8), in0=xtm[:, c, :].rearrange("p (h e) -> p h e", h=8),
              in1=dsk.unsqueeze(2).to_broadcast([128, 8, 64]), op=ALU.mult)
        kb.op("pool", "tensor_tensor", reads=["tmpDb", ("ytm", c)], writes=[("ytm", c)], out=ytm[:, c, :], in0=ytm[:, c, :], in1=tmpb[:, :], op=ALU.add)
        kb.op("dve", "tensor_tensor", reads=[("ytm", c), zst], writes=["yz"], out=yz[:, :], in0=ytm[:, c, :], in1=zs[:, :], op=ALU.mult)
        kb.op("act", "activation", reads=["yz"], writes=["junkD", "ssq"], out=junk[:, :], in_=yz[:, :], func=AF.Square, accum_out=ssq[:, 0:1])
        kb.op("dve", "tensor_scalar", reads=["ssq"], writes=["ssq"], out=ssq[:, :], in0=ssq[:, :], scalar1=1.0 / 512, scalar2=EPS, op0=ALU.mult, op1=ALU.add)
        kb.op("pool", "tensor_tensor", reads=["ssq", "mhalfD"], writes=["ssq"], out=ssq[:, :], in0=ssq[:, :], in1=mhalfD[:, :], op=ALU.pow)
        kb.op("dve", "scalar_tensor_tensor", reads=["yz", "ssq", "rp"], writes=["yoD"], out=yoD[:, :], in0=yz[:, :], scalar=ssq[:, 0:1], in1=gnd, op0=ALU.mult, op1=ALU.mult)
        pt, ptt = pTr.next()
        for j in range(4):
            kb.op("pe", "transpose", reads=["yoD", "cstb"], writes=[ptt], out=pt[:, j * 128:(j + 1) * 128], in_=yoD[:, j * 128:(j + 1) * 128], identity=ident_b)
        yd, ydt = ydr.next()
        kb.op("dve", "tensor_copy", reads=[ptt], writes=[ydt], out=yd[:, :, :], in_=pt[:, :].rearrange("p (a b) -> p a b", a=4))
        kb.dma("sp", out=YBR[3, :, :, c * 128:(c + 1) * 128], in_=yd[:, :, :], reads=[ydt], writes=[("YBR3", c)])
    kb.phase_end()
    if finish_check("D%d" % l):
        return

    kb.phase_begin()
    cp = kb.sb("cpM", [128, 256], F32)
    kb.dma("sp", out=cp[:, :], in_=din["colp"][l], writes=["cp"])
    hfb = kb.sb("hfbM", [128, 16, 1024], BF16)
    ybm = kb.sb("ybm", [128, 4, 4, 1024], BF16)
    mg = kb.sb("mgM", [128, 16, 1024], BF16)
    wgr = Ring(kb, "wg", 6, [128, 16, 128], BF16)
    wbr = Ring(kb, "wb", 6, [128, 4, 128], BF16)
    gsr = Ring(kb, "gs", 3, [128, 512], BF16)
    maccr = Ring(kb, "macc", 2, [128, 512], F32)
    tmr = Ring(kb, "tmpM", 3, [128, 512], F32)
    pgr = Ring(kb, "pg", 4, [128, 512], F32, psum=True)
    pbr = Ring(kb, "pbm", 4, [128, 512], F32, psum=True)
    for sbk in range(2):
        sbs = slice(sbk * 1024, (sbk + 1) * 1024)
        kb.dma("sp", out=hfb[:, :, :], in_=HFM[:, :, sbs], writes=["hfb"])
        for i in range(4):
            kb.dma("sp", out=ybm[:, i, :, :], in_=YBR[i, :, :, sbs], writes=[("ybm", i)])
        for fc in range(16):
            macs = [maccr.next() for _ in range(2)]
            for i in range(4):
                wg, wgt = wgr.next()
                wb, wbt = wbr.next()
                kb.dma("pool", out=wg[:, :, :], in_=din["w_gate_t"][l, i, fc], writes=[wgt])
                kb.dma("pool", out=wb[:, :, :], in_=din["w_br_t"][l, i, fc], writes=[wbt])
                for hf in range(2):
                    hs = slice(hf * 512, (hf + 1) * 512)
                    pg, pgt = pgr.next()
                    pb, pbt = pbr.next()
                    for kc in range(16):
                        kb.mm(pg[:, :], wg[:, kc, :], hfb[:, kc, hs], kc == 0, kc == 15, reads=[wgt, "hfb"], writes=[pgt])
                    for kc in range(4):
                        kb.mm(pb[:, :], wb[:, kc, :], ybm[:, i, kc, hs], kc == 0, kc == 3, reads=[wbt, ("ybm", i)], writes=[pbt])
                    gs, gst = gsr.next()
                    kb.op("act", "activation", reads=[pgt, "cp"], writes=[gst], out=gs[:, :], in_=pg[:, :], func=AF.Sigmoid, bias=cp[:, i * 16 + fc:i * 16 + fc + 1], scale=1.0)
                    mac, mact = macs[hf]
                    if i == 0:
                        kb.op("dve", "tensor_tensor", reads=[gst, pbt], writes=[mact], out=mac[:, :], in0=pb[:, :], in1=gs[:, :], op=ALU.mult)
                    else:
                        tm, tmt = tmr.next()
                        kb.op("dve", "tensor_tensor", reads=[gst, pbt], writes=[tmt], out=tm[:, :], in0=pb[:, :], in1=gs[:, :], op=ALU.mult)
                        if i < 3:
                            kb.op("dve", "tensor_tensor", reads=[tmt, mact], writes=[mact], out=mac[:, :], in0=mac[:, :], in1=tm[:, :], op=ALU.add)
                        else:
                            kb.op("dve", "tensor_tensor", reads=[tmt, mact], writes=[("mg", fc)], out=mg[:, fc, hs], in0=mac[:, :], in1=tm[:, :], op=ALU.add)
        kb.dma("sp", out=MG[:, :, sbs], in_=mg[:, :, :], reads=[("mg", fc) for fc in range(16)], writes=[("MG", sbk)])
    kb.phase_end()
    if finish_check("M1%d" % l):
        return

    UD = SC["UD"]
    kb.phase_begin()
    mga = kb.sb("mga", [128, 16, S], BF16)
    for tb in range(4):
        kb.dma("sp", out=mga[:, :, tb * 512:(tb + 1) * 512], in_=MG[:, :, tb * 512:(tb + 1) * 512], writes=[("mga", tb)])
    wor = Ring(kb, "wo", 2, [128, 16, 512], BF16)
    hsr = Ring(kb, "hsl", 3, [128, 512], F32)
    usr = Ring(kb, "usl", 3, [128, 512], F32)
    por = Ring(kb, "po", 4, [128, 512], F32, psum=True)
    for nb in range(4):
        nbs = slice(nb * 512, (nb + 1) * 512)
        wo, wot = wor.next()
        kb.dma("pool", out=wo[:, :, :], in_=din["w_out_t"][l, nb], writes=[wot])
        for t in range(NT):
            hs_, hst = hsr.next()
            kb.dma("sp", out=hs_[:, :], in_=HTM[t * 128:(t + 1) * 128, nbs], writes=[hst])
            po, pot = por.next()
            for kc in range(16):
                kb.mm(po[:, :], mga[:, kc, t * 128:(t + 1) * 128], wo[:, kc, :], kc == 0, kc == 15, reads=[wot, ("mga", t // 4)], writes=[pot])
            us, ust = usr.next()
            kb.op("dve", "scalar_tensor_tensor", reads=[hst, pot], writes=[ust], out=us[:, :], in0=hs_[:, :], scalar=ALPHA, in1=po[:, :], op0=ALU.mult, op1=ALU.add)
            kb.dma("act", out=UD[t * 128:(t + 1) * 128, nbs], in_=us[:, :], reads=[ust], writes=[("UD", t, nb)])
    kb.phase_end()
    if finish_check("M2%d" % l):
        return

    kb.phase_begin()
    ln_alloc()
    g1 = kb.sb("g1", [128, 2048], F32)
    b1 = kb.sb("b1", [128, 2048], F32)
    kb.dma("sp", out=g1[:, :], in_=din["rowp"][l, 0:1, 0:2048].partition_broadcast(128), writes=["gb1"])
    kb.dma("sp", out=b1[:, :], in_=din["rowp"][l, 0:1, 2048:4096].partition_broadcast(128), writes=["gb1"])
    wrt = kb.sb("wrt", [128, 16, 36], F32)
    kb.dma("sp", out=wrt[:, :, :], in_=din["w_r_t"][l], writes=["wrt"])
    brt = kb.sb("brt", [128, 36], F32)
    kb.dma("sp", out=brt[:, :], in_=din["rowp"][l, 0:1, 8704:8740].partition_broadcast(128), writes=["brt"])
    ur = Ring(kb, "uM", 4, [128, 2048], F32)
    h1r = Ring(kb, "h1M", 2, [128, 2048], F32)
    hbr = Ring(kb, "hbM", 2, [128, 2048], BF16)
    h1fm = kb.sb("h1fm", [128, 16, 128], F32)
    lgr = Ring(kb, "lgs", 2, [128, 36], F32)
    ptfr = Ring(kb, "ptf", 3, [128, 512], F32, psum=True)
    plg = kb.ps("plg", [128, 512], F32)
    loadedM = {}
    statM = {}
    for i in range(-2, NT):
        tl = i + 2
        if tl < NT:
            u, ut = ur.next()
            kb.dma("sp", out=u[:, :], in_=UD[tl * 128:(tl + 1) * 128, :], writes=[ut])
            loadedM[tl] = (u, ut)
        ta = i + 1
        if 0 <= ta < NT:
            u, ut = loadedM[ta]
            statM[ta] = kb.ln_stats(u[:, :], ut)
        if i >= 0:
            t = i
            u, ut = loadedM.pop(i)
            utg = statM.pop(i)
            h1, h1t = h1r.next()
            ln_apply(u[:, :], ut, h1[:, :], h1t, g1[:, :], b1[:, :], "gb1", tg=utg)
            kb.dma("act", out=HTM[t * 128:(t + 1) * 128, :], in_=h1[:, :], reads=[h1t], writes=[("HTM", t)])
            hb, hbt = hbr.next()
            kb.op("act", "activation", reads=[h1t], writes=[hbt], out=hb[:, :], in_=h1[:, :], func=AF.Copy)
            kb.dma("act", out=H1B[t * 128:(t + 1) * 128, :], in_=hb[:, :], reads=[hbt], writes=[("H1B", t)])
            for g in range(4):
                pt, ptt = ptfr.next()
                for j in range(4):
                    c = g * 4 + j
                    kb.op("pe", "transpose", reads=[h1t, "cstf"], writes=[ptt], out=pt[:, j * 128:(j + 1) * 128], in_=h1[:, c * 128:(c + 1) * 128], identity=ident_f)
                if g % 2 == 0:
                    kb.op("dve", "tensor_copy", reads=[ptt], writes=[("h1fm", g)], out=h1fm[:, g * 4:(g + 1) * 4, :], in_=pt[:, :].rearrange("p (a b) -> p a b", a=4))
                else:
                    kb.op("act", "activation", reads=[ptt], writes=[("h1fm", g)], out=h1fm[:, g * 4:(g + 1) * 4, :], in_=pt[:, :].rearrange("p (a b) -> p a b", a=4), func=AF.Copy)
            for kc in range(16):
                kb.mm(plg[:, 0:36], h1fm[:, kc, :], wrt[:, kc, :], kc == 0, kc == 15, reads=[("h1fm", kc // 4), "wrt"], writes=["plg"])
            lg_, lgt = lgr.next()
            kb.op("dve", "tensor_tensor", reads=["plg", "brt"], writes=[lgt], out=lg_[:, :], in0=plg[:, 0:36], in1=brt[:, :], op=ALU.add)
            kb.dma("act", out=LGT[t * 128:(t + 1) * 128, :], in_=lg_[:, :], reads=[lgt], writes=[("LGT", t)])

    kb.phase_end()
    if finish_check("M3%d" % l):
        return

    SLD, WTD = SC["SLD"], SC["WTD"]
    BIG = 1.0e9
    kb.phase_begin()
    lg = kb.sb("lgE", [128, 16, 36], F32)
    kb.dma("sp", out=lg[:, :, :], in_=LGT.rearrange("(t p) n -> p t n", p=128), writes=["lg"])
    sbase = kb.sb("sbase", [128, 32], F32)
    kb.dma("sp", out=sbase[:, :], in_=din["rowc"][0:1, 0:32].partition_broadcast(128), writes=["sbase"])
    def t2(name, n):
        return kb.sb(name, [128, 16, n], F32)
    gmax = kb.sb("gmax", [128, 16], F32)
    gsum = kb.sb("gsum", [128, 16], F32)
    gw = kb.sb("gw", [128, 16], F32)
    gd, goh, pen = t2("gd", 4), t2("goh", 4), t2("pen", 4)
    elm, oh1, elm2, oh2, tmpP, tmpQ = [t2(n, 32) for n in ("elm", "oh1", "elm2", "oh2", "tmpP", "tmpQ")]
    m1 = kb.sb("m1", [128, 16], F32)
    m2_ = kb.sb("m2E", [128, 16], F32)
    w1 = kb.sb("w1", [128, 16], F32)
    wts = kb.sb("wts", [128, 16, 2], F32)
    s12 = kb.sb("s12", [128, 16, 2], F32)
    sli = kb.sb("sli", [128, 16, 2], I32)
    Cb = kb.sb("Cb", [128, 16, 32], BF16)
    gl = lg[:, :, 0:4]
    el = lg[:, :, 4:36]
    def bc(ap2, n):
        return ap2.unsqueeze(2).to_broadcast([128, 16, n])
    kb.op("dve", "tensor_reduce", reads=["lg"], writes=["gmax"], out=gmax[:, :], in_=gl, axis=AX.X, op=ALU.max)
    kb.op("dve", "tensor_tensor", reads=["lg", "gmax"], writes=["gd"], out=gd[:, :, :], in0=gl, in1=bc(gmax[:, :], 4), op=ALU.subtract)
    kb.op("act", "activation", reads=["gd"], writes=["gd"], out=gd[:, :, :], in_=gd[:, :, :], func=AF.Exp)
    kb.op("dve", "tensor_reduce", reads=["gd"], writes=["gsum"], out=gsum[:, :], in_=gd[:, :, :], axis=AX.X, op=ALU.add)
    kb.op("dve", "reciprocal", reads=["gsum"], writes=["gw"], out=gw[:, :], in_=gsum[:, :])
    kb.op("dve", "tensor_tensor", reads=["lg", "gmax"], writes=["goh"], out=goh[:, :, :], in0=gl, in1=bc(gmax[:, :], 4), op=ALU.is_equal)
    kb.op("dve", "tensor_scalar", reads=["goh"], writes=["pen"], out=pen[:, :, :], in0=goh[:, :, :], scalar1=BIG, scalar2=-BIG, op0=ALU.mult, op1=ALU.add)
    kb.op("dve", "tensor_tensor", reads=["lg", "pen"], writes=["elm"], out=elm[:, :, :].rearrange("p t (g e) -> p t g e", g=4),
          in0=el.rearrange("p t (g e) -> p t g e", g=4), in1=pen[:, :, :].unsqueeze(3).to_broadcast([128, 16, 4, 8]), op=ALU.add)
    kb.op("dve", "tensor_reduce", reads=["elm"], writes=["m1"], out=m1[:, :], in_=elm[:, :, :], axis=AX.X, op=ALU.max)
    kb.op("dve", "tensor_tensor", reads=["elm", "m1"], writes=["oh1"], out=oh1[:, :, :], in0=elm[:, :, :], in1=bc(m1[:, :], 32), op=ALU.is_equal)
    kb.op("dve", "scalar_tensor_tensor", reads=["oh1", "elm"], writes=["elm2"], out=elm2[:, :, :], in0=oh1[:, :, :], scalar=-BIG, in1=elm[:, :, :], op0=ALU.mult, op1=ALU.add)
    kb.op("dve", "tensor_reduce", reads=["elm2"], writes=["m2"], out=m2_[:, :], in_=elm2[:, :, :], axis=AX.X, op=ALU.max)
    kb.op("dve", "tensor_tensor", reads=["elm2", "m2"], writes=["oh2"], out=oh2[:, :, :], in0=elm2[:, :, :], in1=bc(m2_[:, :], 32), op=ALU.is_equal)
    kb.op("dve", "tensor_tensor", reads=["m1", "m2"], writes=["w1"], out=w1[:, :], in0=m1[:, :], in1=m2_[:, :], op=ALU.subtract)
    kb.op("act", "activation", reads=["w1"], writes=["w1"], out=w1[:, :], in_=w1[:, :], func=AF.Sigmoid)
    kb.op("dve", "tensor_tensor", reads=["w1", "gw"], writes=["wts0"], out=wts[:, :, 0], in0=w1[:, :], in1=gw[:, :], op=ALU.mult)
    kb.op("dve", "tensor_tensor", reads=["wts0", "gw"], writes=["wts1"], out=wts[:, :, 1], in0=gw[:, :], in1=wts[:, :, 0], op=ALU.subtract)
    kb.op("dve", "tensor_tensor", reads=["oh1", "oh2"], writes=["Cb"], out=Cb[:, :, :], in0=oh1[:, :, :], in1=oh2[:, :, :], op=ALU.add)
    ppre = kb.ps("ppre", [128, 512], F32)
    strict_b = cstb[:, 4, :]
    for t in range(NT):
        for tp in range(t):
            kb.mm(ppre[:, t * 32:(t + 1) * 32], ones_b, Cb[:, tp, :], tp == 0, False, reads=["Cb", "cstb"], writes=["ppre"], last=False)
        kb.mm(ppre[:, t * 32:(t + 1) * 32], strict_b, Cb[:, t, :], t == 0, True, reads=["Cb", "cstb"], writes=["ppre"], last=(t == NT - 1))
    kb.op("dve", "tensor_tensor", reads=["ppre", "sbase"], writes=["tmpP"], out=tmpP[:, :, :], in0=ppre[:, :].rearrange("p (t e) -> p t e", t=16),
          in1=sbase[:, :].unsqueeze(1).to_broadcast([128, 16, 32]), op=ALU.add)
    kb.op("dve", "tensor_tensor", reads=["tmpP", "oh1"], writes=["tmpQ"], out=tmpQ[:, :, :], in0=tmpP[:, :, :], in1=oh1[:, :, :], op=ALU.mult)
    kb.op("dve", "tensor_reduce", reads=["tmpQ"], writes=["s120"], out=s12[:, :, 0], in_=tmpQ[:, :, :], axis=AX.X, op=ALU.add)
    kb.op("dve", "tensor_tensor", reads=["tmpP", "oh2", "s120"], writes=["tmpQ"], out=tmpQ[:, :, :], in0=tmpP[:, :, :], in1=oh2[:, :, :], op=ALU.mult)
    kb.op("dve", "tensor_reduce", reads=["tmpQ"], writes=["s121"], out=s12[:, :, 1], in_=tmpQ[:, :, :], axis=AX.X, op=ALU.add)
    kb.op("dve", "tensor_copy", reads=["s120", "s121"], writes=["sli"], out=sli[:, :, :], in_=s12[:, :, :])
    kb.dma("sp", out=SLD.rearrange("(t p) n -> p t n", p=128), in_=sli[:, :, :], reads=["sli"], writes=["SLD"])
    kb.dma("sp", out=WTD.rearrange("(t p) n -> p t n", p=128), in_=wts[:, :, :], reads=["wts0", "wts1"], writes=["WTD"])
    hbr = Ring(kb, "h1bE", 3, [128, 2048], BF16)
    for t in range(NT):
        hb, hbt = hbr.next()
        kb.dma("sp", out=hb[:, :], in_=H1B[t * 128:(t + 1) * 128, :], writes=[hbt])
        for k2 in range(2):
            kb.dma("pool", out=XROWS, in_=hb[:, :], reads=[hbt, "sli"], writes=[("XR", t, k2)], name="indirect_dma_start",
                   out_offset=bass.IndirectOffsetOnAxis(ap=sli[:, t, k2:k2 + 1], axis=0), in_offset=None)
    kb.phase_end()
    if finish_check("E1%d" % l):
        return

    kb.phase_begin()
    moe_experts(kb, l, din, XROWS, YROWS, ident_b, NEXP)
    kb.phase_end()
    if finish_check("E3%d" % l):
        return

    kb.phase_begin()
    ln_alloc()
    g2 = kb.sb("g2", [128, 2048], F32)
    b2 = kb.sb("b2", [128, 2048], F32)
    kb.dma("sp", out=g2[:, :], in_=din["rowp"][l, 0:1, 4096:6144].partition_broadcast(128), writes=["gb2"])
    kb.dma("sp", out=b2[:, :], in_=din["rowp"][l, 0:1, 6144:8192].partition_broadcast(128), writes=["gb2"])
    sli = kb.sb("sliF", [128, 16, 2], I32)
    wts = kb.sb("wtsF", [128, 16, 2], F32)
    kb.dma("sp", out=sli[:, :, :], in_=SLD.rearrange("(t p) n -> p t n", p=128), writes=["sli"])
    kb.dma("sp", out=wts[:, :, :], in_=WTD.rearrange("(t p) n -> p t n", p=128), writes=["wts"])
    gr1 = Ring(kb, "gy1", 4, [128, 2048], F32)
    gr2 = Ring(kb, "gy2", 4, [128, 2048], F32)
    hTr = Ring(kb, "hTE", 4, [128, 2048], F32)
    h2r = Ring(kb, "h2E", 2, [128, 2048], F32)
    rings = dict(hb=Ring(kb, "hbE4", 2, [128, 2048], BF16), fm=Ring(kb, "fmE4", 2, [128, 16, 128], BF16),
                 pt=Ring(kb, "ptE4", 4, [128, 512], BF16, psum=True))
    loadedE = {}
    statE = {}
    for i in range(-2, NT):
        tl = i + 2
        if tl < NT:
            t = tl
            ga, gat = gr1.next()
            gb_, gbt = gr2.next()
            kb.dma("pool", out=ga[:, :], in_=YROWS, reads=["sli"], writes=[gat], name="indirect_dma_start", out_offset=None,
                   in_offset=bass.IndirectOffsetOnAxis(ap=sli[:, t, 0:1], axis=0))
            kb.dma("pool", out=gb_[:, :], in_=YROWS, reads=["sli"], writes=[gbt], name="indirect_dma_start", out_offset=None,
                   in_offset=bass.IndirectOffsetOnAxis(ap=sli[:, t, 1:2], axis=0))
            hT, hTt = hTr.next()
            kb.dma("sp", out=hT[:, :], in_=HTM[t * 128:(t + 1) * 128, :], writes=[hTt])
            loadedE[tl] = (ga, gat, gb_, gbt, hT, hTt)
        ta = i + 1
        if 0 <= ta < NT:
            t = ta
            ga, gat, gb_, gbt, hT, hTt = loadedE[ta]
            kb.op("act", "activation", reads=[hTt], writes=[hTt], out=hT[:, :], in_=hT[:, :], func=AF.Copy, scale=ALPHA)
            kb.op("dve", "scalar_tensor_tensor", reads=[gat, hTt, "wts"], writes=[hTt], out=hT[:, :], in0=ga[:, :], scalar=wts[:, t, 0:1], in1=hT[:, :], op0=ALU.mult, op1=ALU.add)
            kb.op("dve", "scalar_tensor_tensor", reads=[gbt, hTt, "wts"], writes=[hTt], out=hT[:, :], in0=gb_[:, :], scalar=wts[:, t, 1:2], in1=hT[:, :], op0=ALU.mult, op1=ALU.add)
            statE[ta] = kb.ln_stats(hT[:, :], hTt)
        if i >= 0:
            ga, gat, gb_, gbt, hT, hTt = loadedE.pop(i)
            h2, h2t = h2r.next()
            ln_apply(hT[:, :], hTt, h2[:, :], h2t, g2[:, :], b2[:, :], "gb2", tg=statE.pop(i))
            emit_h_tile(i, h2[:, :], h2t, rings, l == NL - 1)
    kb.phase_end()
    if finish_check("E4%d" % l):
        return


_CACHE = {}


def kernel(**inputs):
    inp = {k: np.asarray(v) for k, v in inputs.items()}
    lay = _host_layout(inp)
    if "nc" not in _CACHE:
        _CACHE["nc"] = build()[0]
    nc = _CACHE["nc"]
    x = inp["x"].astype(np.float32)
    in_maps = []
    for b in range(x.shape[0]):
        m = dict(lay)
        m["x"] = np.ascontiguousarray(x[b])
        in_maps.append(m)
    res = run_bass_kernel_spmd(nc, in_maps, core_ids=list(range(x.shape[0])))
    out = np.stack([np.asarray(r["out"], dtype=np.float32) for r in res.results], axis=0)
    return out
```
